# Optimizing a Trainium2 kernel written in Bass

```python
import jax, jax.numpy as jnp
from jax import lax
import numpy as np

D_MODEL = 1024
BATCH = 8
SEQ = 2048
DEPTH = 1

GRID_W = 64
CTX_LEN = 256
ATTN_WIDTH = 512
N_HEADS = 8
N_KV_HEADS = 2
HEAD_DIM = ATTN_WIDTH // N_HEADS
KV_REP = N_HEADS // N_KV_HEADS
KV_WIDTH = N_KV_HEADS * HEAD_DIM
CONV_WIDTH = D_MODEL - ATTN_WIDTH
CONV_HEADS = 8
CONV_K = 3
MIX_WIDTH = ATTN_WIDTH + CONV_WIDTH
IN_COLS = ATTN_WIDTH + 2 * KV_WIDTH + 3 * CONV_WIDTH
ROPE_THETA = 10000.0
ROPE_AXIS_DIM = HEAD_DIM // 2
ROPE_FREQS = ROPE_AXIS_DIM // 2
Q_BLOCK = 128
N_GROUPS = 4
EXPERTS_PER_GROUP = 8
N_EXPERTS = N_GROUPS * EXPERTS_PER_GROUP
TOP_K = 2
D_EXPERT = 768
MOE_BLOCK = 128
N_MOD = 6
EPS = 1e-6

kernel_name = "hybrid_gqa_shortconv_hmoe_dit_block"


def rms_norm(x, g):
    xf = x.astype(jnp.float32)
    y = xf * lax.rsqrt(jnp.mean(xf * xf, axis=-1, keepdims=True) + EPS)
    return (y * g.astype(jnp.float32)).astype(x.dtype)


def head_rms(x, g, n_heads):
    xs = x.reshape(*x.shape[:-1], n_heads, -1)
    return rms_norm(xs, g.reshape(n_heads, -1)).reshape(x.shape)


def modulation(cond, w_mod, b_mod):
    mod = jax.nn.silu(cond) @ w_mod + b_mod
    return jnp.split(mod[..., None, :], N_MOD, axis=-1)


def axial_rope_tables(n_tokens, dtype):
    rows = n_tokens // GRID_W
    row_idx = jnp.repeat(jnp.arange(rows, dtype=jnp.float32), GRID_W)
    col_idx = jnp.tile(jnp.arange(GRID_W, dtype=jnp.float32), rows)
    inv_freq = ROPE_THETA ** (-jnp.arange(0, ROPE_AXIS_DIM, 2, dtype=jnp.float32) / ROPE_AXIS_DIM)
    ang = jnp.stack([row_idx[:, None] * inv_freq, col_idx[:, None] * inv_freq], axis=1)
    return jnp.cos(ang).astype(dtype), jnp.sin(ang).astype(dtype)


def apply_axial_rope(x, cos, sin):
    xs = x.reshape(*x.shape[:-1], 2, 2, ROPE_FREQS)
    x1, x2 = xs[..., 0, :], xs[..., 1, :]
    c, s = cos[:, None], sin[:, None]
    return jnp.stack([x1 * c - x2 * s, x1 * s + x2 * c], axis=-2).reshape(x.shape)


def in_proj(h, w_in, q_g, k_g):
    z = h @ w_in
    b, s = h.shape[:2]
    cuts = [ATTN_WIDTH, ATTN_WIDTH + KV_WIDTH, ATTN_WIDTH + 2 * KV_WIDTH,
            ATTN_WIDTH + 2 * KV_WIDTH + CONV_WIDTH, ATTN_WIDTH + 2 * KV_WIDTH + 2 * CONV_WIDTH]
    q, k, v, gb, gc, u = jnp.split(z, cuts, axis=-1)
    q = rms_norm(q.reshape(b, s, N_HEADS, HEAD_DIM), q_g)
    k = rms_norm(k.reshape(b, s, N_KV_HEADS, HEAD_DIM), k_g)
    v = v.reshape(b, s, N_KV_HEADS, HEAD_DIM)
    return q, k, v, gb, gc, u


def attend(q, k, v):
    s = jnp.einsum('bqgrd,bkgd->bgrqk', q, k, preferred_element_type=jnp.float32) * (HEAD_DIM ** -0.5)
    p = jax.nn.softmax(s, axis=-1).astype(v.dtype)
    return jnp.einsum('bgrqk,bkgd->bqgrd', p, v)


def latent_attention(q, k_all, v_all):
    b, s = q.shape[:2]
    nb = s // Q_BLOCK
    qb = q.reshape(b, nb, Q_BLOCK, N_KV_HEADS, KV_REP, HEAD_DIM).transpose(1, 0, 2, 3, 4, 5)
    ob = lax.map(lambda qi: attend(qi, k_all, v_all), qb)
    return ob.transpose(1, 0, 2, 3, 4, 5).reshape(b, s, ATTN_WIDTH)


def short_conv(gb, gc, u, conv_w):
    v = gc * u
    vp = jnp.pad(v, ((0, 0), (1, 1), (0, 0)))
    y = conv_w[0] * vp[:, :-2] + conv_w[1] * vp[:, 1:-1] + conv_w[2] * vp[:, 2:]
    return gb * y


def merge_out(a, cv, attn_g, conv_g, w_out):
    return jnp.concatenate([head_rms(a, attn_g, N_HEADS), head_rms(cv, conv_g, CONV_HEADS)], axis=-1) @ w_out


def hier_moe(h, w_group, w_router, w_gate, w_up, w_down):
    d = h.shape[-1]
    ht = h.reshape(-1, d)
    n = ht.shape[0]
    g_prob = jax.nn.softmax((ht @ w_group).astype(jnp.float32), axis=-1)
    g_sel = jnp.argmax(g_prob, axis=-1)
    g_w = jnp.take_along_axis(g_prob, g_sel[:, None], axis=1)[:, 0]
    e_logits = (ht @ w_router).astype(jnp.float32).reshape(n, N_GROUPS, EXPERTS_PER_GROUP)
    e_logits = jnp.take_along_axis(e_logits, g_sel[:, None, None], axis=1)[:, 0]
    e_prob = jax.nn.softmax(e_logits, axis=-1)
    top_p, top_i = lax.top_k(e_prob, TOP_K)
    top_p = top_p / jnp.sum(top_p, axis=-1, keepdims=True)
    wts = (g_w[:, None] * top_p).reshape(-1)
    eid = (g_sel[:, None] * EXPERTS_PER_GROUP + top_i).reshape(-1).astype(jnp.int32)
    tok = jnp.repeat(jnp.arange(n, dtype=jnp.int32), TOP_K)
    m = eid.shape[0]
    order = jnp.argsort(eid)
    s_e, s_tok, s_w = eid[order], tok[order], wts[order]
    counts = jnp.bincount(eid, length=N_EXPERTS).astype(jnp.int32)
    starts = jnp.cumsum(counts) - counts
    padded = ((counts + MOE_BLOCK - 1) // MOE_BLOCK) * MOE_BLOCK
    pstarts = jnp.cumsum(padded) - padded
    pends = pstarts + padded
    dest = pstarts[s_e] + (jnp.arange(m, dtype=jnp.int32) - starts[s_e])
    p_rows = m + N_EXPERTS * MOE_BLOCK
    n_blk = p_rows // MOE_BLOCK
    xbuf = jnp.zeros((p_rows, d), h.dtype).at[dest].set(ht[s_tok])
    blk_start = jnp.arange(n_blk, dtype=jnp.int32) * MOE_BLOCK
    blk_e = jnp.minimum(jnp.searchsorted(pends, blk_start, side='right'), N_EXPERTS - 1)

    def expert_block(args):
        xb, e = args
        return (jax.nn.silu(xb @ w_gate[e]) * (xb @ w_up[e])) @ w_down[e]

    ybuf = lax.map(expert_block, (xbuf.reshape(n_blk, MOE_BLOCK, d), blk_e)).reshape(p_rows, d)
    y = ybuf[dest] * s_w[:, None].astype(h.dtype)
    return jax.ops.segment_sum(y, s_tok, num_segments=n).reshape(h.shape)


def trunk_layer(x, ctx, c, c_ctx, cos, sin, w_mod, b_mod, norm1_g, w_in, q_norm_g, k_norm_g,
                conv_w, attn_out_g, conv_out_g, w_out, norm2_g, w_group, w_router, w_gate, w_up,
                w_down, update_ctx):
    sh1, sc1, gt1, sh2, sc2, gt2 = modulation(c, w_mod, b_mod)
    csh1, csc1, cgt1, csh2, csc2, cgt2 = modulation(c_ctx, w_mod, b_mod)
    hx = rms_norm(x, norm1_g) * (1.0 + sc1) + sh1
    hc = rms_norm(ctx, norm1_g) * (1.0 + csc1) + csh1
    qx, kx, vx, bx, cx, ux = in_proj(hx, w_in, q_norm_g, k_norm_g)
    qc, kc, vc, bc, cc, uc = in_proj(hc, w_in, q_norm_g, k_norm_g)
    qx = apply_axial_rope(qx, cos, sin)
    kx = apply_axial_rope(kx, cos, sin)
    k_all = jnp.concatenate([kc, kx], axis=1)
    v_all = jnp.concatenate([vc, vx], axis=1)
    ax = latent_attention(qx, k_all, v_all)
    convx = short_conv(bx, cx, ux, conv_w)
    x_new = x + gt1 * merge_out(ax, convx, attn_out_g, conv_out_g, w_out)
    hx2 = rms_norm(x_new, norm2_g) * (1.0 + sc2) + sh2
    x_new = x_new + gt2 * hier_moe(hx2, w_group, w_router, w_gate, w_up, w_down)
    if update_ctx:
        b = ctx.shape[0]
        ac = attend(qc.reshape(b, -1, N_KV_HEADS, KV_REP, HEAD_DIM), kc, vc).reshape(b, -1, ATTN_WIDTH)
        convc = short_conv(bc, cc, uc, conv_w)
        ctx = ctx + cgt1 * merge_out(ac, convc, attn_out_g, conv_out_g, w_out)
        hc2 = rms_norm(ctx, norm2_g) * (1.0 + csc2) + csh2
        ctx = ctx + cgt2 * hier_moe(hc2, w_group, w_router, w_gate, w_up, w_down)
    return x_new, ctx


def setup_inputs(seed: int = 0) -> dict:
    key = jax.random.key(seed)
    ks = jax.random.split(key, 21)
    f32 = jnp.float32
    L, D = DEPTH, D_MODEL

    def nrm(k, shape, scale):
        return jax.random.normal(k, shape, f32) * scale

    return {
        "x": nrm(ks[0], (BATCH, SEQ, D), 1.0),
        "c": nrm(ks[1], (BATCH, D), 1.0),
        "ctx": nrm(ks[2], (BATCH, CTX_LEN, D), 1.0),
        "c_ctx": nrm(ks[3], (D,), 1.0),
        "w_mod": nrm(ks[4], (L, D, N_MOD * D), 0.5 * D ** -0.5),
        "b_mod": nrm(ks[5], (L, N_MOD * D), 0.02),
        "norm1_g": 1.0 + nrm(ks[6], (L, D), 0.02),
        "w_in": nrm(ks[7], (L, D, IN_COLS), D ** -0.5),
        "q_norm_g": 1.0 + nrm(ks[8], (L, HEAD_DIM), 0.02),
        "k_norm_g": 1.0 + nrm(ks[9], (L, HEAD_DIM), 0.02),
        "conv_w": nrm(ks[10], (L, CONV_K, CONV_WIDTH), CONV_K ** -0.5),
        "attn_out_g": 1.0 + nrm(ks[11], (L, ATTN_WIDTH), 0.02),
        "conv_out_g": 1.0 + nrm(ks[12], (L, CONV_WIDTH), 0.02),
        "w_out": nrm(ks[13], (L, MIX_WIDTH, D), MIX_WIDTH ** -0.5),
        "norm2_g": 1.0 + nrm(ks[14], (L, D), 0.02),
        "w_group": nrm(ks[15], (L, D, N_GROUPS), D ** -0.5),
        "w_router": nrm(ks[16], (L, D, N_EXPERTS), D ** -0.5),
        "w_gate": nrm(ks[17], (L, N_EXPERTS, D, D_EXPERT), D ** -0.5),
        "w_up": nrm(ks[18], (L, N_EXPERTS, D, D_EXPERT), D ** -0.5),
        "w_down": nrm(ks[19], (L, N_EXPERTS, D_EXPERT, D), D_EXPERT ** -0.5),
        "final_g": 1.0 + nrm(ks[20], (D,), 0.02),
    }


def reference(x, c, ctx, c_ctx, w_mod, b_mod, norm1_g, w_in, q_norm_g, k_norm_g, conv_w,
              attn_out_g, conv_out_g, w_out, norm2_g, w_group, w_router, w_gate, w_up, w_down,
              final_g):
    cos, sin = axial_rope_tables(x.shape[1], x.dtype)
    for i in range(DEPTH):
        x, ctx = trunk_layer(x, ctx, c, c_ctx, cos, sin, w_mod[i], b_mod[i], norm1_g[i], w_in[i],
                             q_norm_g[i], k_norm_g[i], conv_w[i], attn_out_g[i], conv_out_g[i],
                             w_out[i], norm2_g[i], w_group[i], w_router[i], w_gate[i], w_up[i],
                             w_down[i], update_ctx=(i < DEPTH - 1))
    return rms_norm(x, final_g)
```

```python
import os
import numpy as np
from contextlib import ExitStack
import concourse.bass as bass
import concourse.mybir as mybir
from concourse.bass_utils import run_bass_kernel_spmd

F32 = mybir.dt.float32
BF16 = mybir.dt.bfloat16
I32 = mybir.dt.int32
AF = mybir.ActivationFunctionType
ALU = mybir.AluOpType
AX = mybir.AxisListType

D = 1024
SEQ = 2048
CTX = 256
NTOK = SEQ + CTX
NT = NTOK // 128
NL = SEQ // 128
EPS = 1e-6
NE = 32
DE = 768
N_DSEM = 40
CAP = 512
NJ = CAP // 128
_LVL = int(os.environ.get('KLVL', '9'))
_KEV = os.environ.get('KEV', 'act')


class Reg:
    __slots__ = ("w", "r", "name")

    def __init__(self, name=""):
        self.w = None
        self.r = {}
        self.name = name


class Sem:
    def __init__(self, handle):
        self.handle = handle
        self.count = 0


class EngW:
    def __init__(self, name, eng, sem):
        self.name = name
        self.eng = eng
        self.sem = sem
        self.waited = {}


class KB:
    def __init__(self, nc, es):
        self.nc = nc
        self._es = es
        self.pslots = []
        self.regs = []
        mk = lambda n: Sem(es.enter_context(nc.semaphore(n)))
        self.E = {
            "pe": EngW("pe", nc.tensor, mk("s_pe")),
            "act": EngW("act", nc.scalar, mk("s_act")),
            "dve": EngW("dve", nc.vector, mk("s_dve")),
            "pool": EngW("pool", nc.gpsimd, mk("s_pool")),
            "sp": EngW("sp", nc.sync, mk("s_sp")),
        }
        self.dsems = [mk(f"s_d{i}") for i in range(N_DSEM)]
        self.dnext = 0
        self.psems = [mk(f"s_q{i}") for i in range(24)]
        self.pnext = 0

    def reg(self, name=""):
        r = Reg(name)
        self.regs.append(r)
        return r

    def regs_n(self, n, name=""):
        return [self.reg(f"{name}{i}") for i in range(n)]

    def wait(self, ew, tok):
        sem, val = tok
        if ew.waited.get(id(sem), 0) >= val:
            return
        ew.eng.wait_ge(sem.handle, val)
        ew.waited[id(sem)] = val

    def _deps(self, ew, reads, writes):
        for r in reads:
            if r.w is not None:
                if ew.name == "pe" and r.w[0] is ew.sem:
                    continue
                self.wait(ew, r.w)
        for w in writes:
            toks = list(w.r.values())
            if w.w is not None:
                toks.append(w.w)
            for t in toks:
                if ew.name == "pe" and t[0] is ew.sem:
                    continue
                self.wait(ew, t)

    def _record(self, tok, reads, writes):
        for r in reads:
            r.r[id(tok[0])] = tok
        for w in writes:
            w.w = tok
            w.r = {}

    def op(self, en, fn, reads=(), writes=()):
        ew = self.E[en]
        self._deps(ew, reads, writes)
        ins = fn(ew.eng)
        ew.sem.count += 1
        ins.then_inc(ew.sem.handle, 1)
        tok = (ew.sem, ew.sem.count)
        self._record(tok, reads, writes)
        return tok

    def dma(self, qn, out, in_, reads=(), writes=(), fn=None):
        ew = self.E[qn]
        self._deps(ew, reads, writes)
        d = self.dsems[self.dnext % N_DSEM]
        self.dnext += 1
        if d.count:
            self.wait(ew, (d, d.count))
        if fn is None:
            ins = ew.eng.dma_start(out=out, in_=in_)
        else:
            ins = fn(ew.eng)
        d.count += 16
        ins.then_inc(d.handle, 16)
        tok = (d, d.count)
        self._record(tok, reads, writes)
        return tok

    def pslot(self, name):
        return None

    def pdma(self, slot, out, in_, reads=(), writes=(), fn=None):
        ew = self.E["pool"]
        self._deps(ew, reads, writes)
        d = self.psems[self.pnext % len(self.psems)]
        self.pnext += 1
        if d.count:
            self.wait(ew, (d, d.count))
        ins = ew.eng.dma_start(out=out, in_=in_) if fn is None else fn(ew.eng)
        d.count += 16
        ins.then_inc(d.handle, 16)
        tok = (d, d.count)
        self._record(tok, reads, writes)
        return tok

    def barrier(self):
        for ew in self.E.values():
            for other in self.E.values():
                if other.sem.count and not (other is ew and ew.name in ("pe", "sp")):
                    self.wait(ew, (other.sem, other.sem.count))
            for d in self.dsems:
                if d.count:
                    self.wait(ew, (d, d.count))
            for d in self.psems:
                if d.count:
                    self.wait(ew, (d, d.count))
        for r in self.regs:
            r.w = None
            r.r = {}


def build_program(debug=False, stop_after=None):
    nc = bass.Bass("TRN2", target_bir_lowering=False)

    def din(name, shape, dt=F32):
        return nc.dram_tensor(name, list(shape), dt, kind="ExternalInput").ap()

    xin = din("xin", [NTOK, D])
    cT_d = din("cT", [128, 16])
    w_mod_d = din("w_mod", [D, 6 * D])
    bm_d = din("bm_b", [128, 6 * D])
    g1_d = din("g1_b", [128, D])
    g2_d = din("g2_b", [128, D])
    fg_d = din("fg_b", [128, D])
    w_in_d = din("w_in", [D, 2304])
    qg_d = din("qg_b", [128, 64])
    kg_d = din("kg_b", [128, 64])
    cw_d = din("cwT", [128, 12])
    aog_d = din("aogT", [64, 8])
    cog_d = din("cogT", [128, 4])
    w_out_d = din("w_out", [D, D])
    wr_d = din("w_r", [D, 36])
    wg_d = din("w_gate", [NE, D, DE])
    wu_d = din("w_up", [NE, D, DE])
    wd_d = din("w_down", [NE, DE, D])
    identf_d = din("ident_f", [128, 128])
    cos_d = din("cosT", [128, NL, 32])
    sin_d = din("sinT", [128, NL, 32])
    bd64_d = din("bd64", [128, 128])
    c65_d = din("c65", [65, 64])
    e0_d = din("e0", [128, 1])
    ustr_d = din("ustr", [128, 128])
    eC_d = din("eC", [128, NE])

    y_d = nc.dram_tensor("y", [SEQ, D], F32, kind="ExternalOutput").ap()
    xnew_d = nc.dram_tensor("xnew_scr", [SEQ, D], F32,
                            kind="ExternalOutput" if debug else "Internal").ap()
    xbuf_d = nc.dram_tensor("xbuf_scr", [NE * CAP + 1, D], BF16, kind="Internal").ap()
    ybuf_d = nc.dram_tensor("ybuf_scr", [NE * CAP + 1, D], F32, kind="Internal").ap()
    hx2_d = nc.dram_tensor("hx2_scr", [SEQ, D], BF16, kind="Internal").ap()
    dbg = {}

    def dbg_out(name, shape, dt=F32):
        if debug:
            dbg[name] = nc.dram_tensor(name, list(shape), dt, kind="ExternalOutput").ap()
        return dbg.get(name)

    d_smod = dbg_out("d_smod", [128, 32])
    d_modb = dbg_out("d_modb", [128, 6 * D])
    d_QT = dbg_out("d_QT", [128, 4, SEQ], BF16)
    d_KT = dbg_out("d_KT", [128, 2, NTOK], BF16)
    d_V = dbg_out("d_V", [128, NT, 2, 65], BF16)
    d_convn = dbg_out("d_convn", [128, 4, SEQ], BF16)
    d_attn = dbg_out("d_attn", [64, 8, SEQ], BF16)
    d_wgt = dbg_out("d_wgt", [128, NL, NE])
    d_acc = dbg_out("d_acc", [128, NL, D])

    with ExitStack() as es:
        K = KB(nc, es)
        op, dma = K.op, K.dma

        def sb(stk, name, shape, dt):
            return stk.enter_context(nc.sbuf_tensor("sb_" + name, list(shape), dt))

        PSUM = es.enter_context(nc.psum_tensor("psum", [128, 8 * 512], F32))
        PS = [PSUM[:, i * 512:(i + 1) * 512] for i in range(8)]
        PSR = K.regs_n(8, "ps")

        identf = sb(es, "identf", [128, 128], F32)
        identb = sb(es, "identb", [128, 128], BF16)
        fgb = sb(es, "fgb", [128, D], F32)
        gt2b = sb(es, "gt2b", [128, D], F32)
        wgt = sb(es, "wgt", [128, NL, NE], F32)
        dest = sb(es, "dest", [128, NL, 2], I32)
        wsel = sb(es, "wsel", [128, NL, 2], F32)
        R_dest = K.regs_n(NL, "dest")
        R_const = K.reg("const")
        R_gt2 = K.reg("gt2")
        R_wgt = K.regs_n(NL, "wgt")

        dma("sp", identf[:], identf_d, writes=[R_const])
        dma("sp", fgb[:], fg_d, writes=[R_const])
        op("dve", lambda e: e.tensor_copy(identb[:], identf[:]), reads=[R_const], writes=[R_const])

        with ExitStack() as sAB:
            modb = sb(sAB, "modb", [128, 6 * D], F32)
            smod = sb(sAB, "smod", [128, 32], F32)
            cosT = sb(sAB, "cosT", [128, NL, 32], F32)
            sinT = sb(sAB, "sinT", [128, NL, 32], F32)
            qgb = sb(sAB, "qgb", [128, 64], F32)
            kgb = sb(sAB, "kgb", [128, 64], F32)
            cw = sb(sAB, "cw", [128, 12], F32)
            bd64 = sb(sAB, "bd64", [128, 128], F32)
            c65 = sb(sAB, "c65", [65, 64], F32)
            e0 = sb(sAB, "e0", [128, 1], F32)
            wr = sb(sAB, "wr", [128, 8, 36], F32)
            R_modb = K.regs_n(6, "modb")
            R_smod = K.reg("smod")
            for t_, d_ in ((cosT, cos_d), (sinT, sin_d), (qgb, qg_d), (kgb, kg_d), (cw, cw_d),
                           (bd64, bd64_d), (c65, c65_d), (e0, e0_d)):
                dma("sp", t_[:], d_, writes=[R_const])
            dma("sp", wr[:], wr_d.rearrange("(k p) n -> p k n", p=128), writes=[R_const])

            with ExitStack() as s0:
                cTt = sb(s0, "cTt", [128, 16], F32)
                scT = sb(s0, "scT", [128, 16], F32)
                lhs_c = sb(s0, "lhs_c", [128, 8, 128], BF16)
                lhs_x = sb(s0, "lhs_x", [128, 8, 128], BF16)
                bm = sb(s0, "bm", [128, 6 * D], F32)
                g1b = sb(s0, "g1b", [128, D], F32)
                g2b = sb(s0, "g2b", [128, D], F32)
                modx = sb(s0, "modx", [128, 2 * D], F32)
                wm = [sb(s0, f"wm{i}", [128, 8, 512], BF16) for i in range(2)]
                R_c = K.reg("c")
                R_bm = K.reg("bm")
                R_g = K.reg("g12")
                R_modx = K.regs_n(2, "modx")
                R_wm = K.regs_n(2, "wm")
                P_wm = [K.pslot(f"wm{i}") for i in range(2)]
                dma("sp", cTt[:], cT_d, writes=[R_c])
                dma("sp", bm[:], bm_d, writes=[R_bm])
                dma("sp", g1b[:], g1_d, writes=[R_g])
                dma("sp", g2b[:], g2_d, writes=[R_g])
                op("act", lambda e: e.activation(scT[:], cTt[:], AF.Silu), reads=[R_c], writes=[R_c])
                op("dve", lambda e: e.tensor_copy(
                    lhs_c[:], scT[:, 0:8].unsqueeze(2).to_broadcast([128, 8, 128])), reads=[R_c], writes=[R_c])
                op("dve", lambda e: e.tensor_copy(
                    lhs_x[:], scT[:, 8:16].unsqueeze(2).to_broadcast([128, 8, 128])), reads=[R_c], writes=[R_c])
                wmod_v = w_mod_d.rearrange("(k p) n -> p k n", p=128)
                for blk in range(12):
                    b = blk % 2
                    cs = slice(blk * 512, (blk + 1) * 512)
                    K.pdma(P_wm[b], wm[b][:], wmod_v[:, :, cs], writes=[R_wm[b]])
                    for k in range(8):
                        op("pe", lambda e, k=k: e.matmul(PS[b], lhs_c[:, k, :], wm[b][:, k, :],
                                                       start=(k == 0), stop=(k == 7)),
                           reads=[R_c, R_wm[b]], writes=[PSR[b]])
                    op("dve", lambda e: e.tensor_tensor(modb[:, cs], PS[b], bm[:, cs], ALU.add),
                       reads=[PSR[b], R_bm], writes=[R_modb[blk // 2]])
                    if blk < 4:
                        for k in range(8):
                            op("pe", lambda e, k=k: e.matmul(PS[2 + b], lhs_x[:, k, :], wm[b][:, k, :],
                                                           start=(k == 0), stop=(k == 7)),
                               reads=[R_c, R_wm[b]], writes=[PSR[2 + b]])
                        op("dve", lambda e: e.tensor_tensor(modx[:, cs], PS[2 + b], bm[:, cs], ALU.add),
                           reads=[PSR[2 + b], R_bm], writes=[R_modx[blk // 2]])
                op("dve", lambda e: e.scalar_tensor_tensor(modb[:, D:2 * D], modb[:, D:2 * D], 1.0, g1b[:],
                                                          ALU.add, ALU.mult),
                   reads=[R_modb[1], R_g], writes=[R_modb[1]])
                op("dve", lambda e: e.scalar_tensor_tensor(modx[:, D:2 * D], modx[:, D:2 * D], 1.0, g1b[:],
                                                          ALU.add, ALU.mult),
                   reads=[R_modx[1], R_g], writes=[R_modx[1]])
                op("dve", lambda e: e.scalar_tensor_tensor(modb[:, 4 * D:5 * D], modb[:, 4 * D:5 * D], 1.0, g2b[:],
                                                          ALU.add, ALU.mult),
                   reads=[R_modb[4], R_g], writes=[R_modb[4]])
                op("act", lambda e: e.activation(gt2b[:], modb[:, 5 * D:6 * D], AF.Copy),
                   reads=[R_modb[5]], writes=[R_gt2])
                srcs = [(modb, D, R_modb[1]), (modb, 0, R_modb[0]), (modx, D, R_modx[1]), (modx, 0, R_modx[0])]
                for vi, (tt, off, rr) in enumerate(srcs):
                    for k in range(8):
                        c0 = vi * 8 + k
                        op("pe", lambda e, tt=tt, off=off, k=k, c0=c0: e.matmul(
                            PS[4][:, c0:c0 + 1], tt[:, off + k * 128: off + (k + 1) * 128], e0[:, 0:1],
                            start=True, stop=True), reads=[rr, R_const], writes=[PSR[4]])
                op("act", lambda e: e.activation(smod[:], PS[4][:, 0:32], AF.Copy), reads=[PSR[4]], writes=[R_smod])
                if debug:
                    dma("sp", d_smod, smod[:], reads=[R_smod])
                    dma("sp", d_modb, modb[:], reads=R_modb)
                K.barrier()
                if stop_after == "0":
                    return nc, list(dbg.keys())

            with ExitStack() as sB:
                QT = sb(sB, "QT", [128, 4, SEQ], BF16)
                KT = sb(sB, "KT", [128, 2, 2, NTOK], BF16)
                VE = sb(sB, "VE", [128, NT, 2, 128], BF16)
                convn = sb(sB, "convn", [128, 4, SEQ], BF16)
                R_QT = K.regs_n(NL, "QT")
                R_KT = K.regs_n(NT, "KT")
                R_VE = K.regs_n(NT, "VE")
                R_convn = K.regs_n(4, "convn")
                R_ve1 = K.reg("ve1")
                if _LVL >= 0:
                    op("dve", lambda e: e.memset(VE[:], 0.0), writes=[R_ve1])
                    op("dve", lambda e: e.memset(VE[:, :, :, 64:65], 1.0), writes=[R_ve1])
                    op("dve", lambda e: e.memset(KT[:], 0.0), writes=[R_ve1])

                with ExitStack() as sA:
                    hT = sb(sA, "hT", [128, 8, NTOK], BF16)
                    R_hTd = K.regs_n(NT, "hTd")
                    R_hTa = K.regs_n(NT, "hTa")
                    with ExitStack() as sA1:
                        winq = sb(sA1, "winq", [128, 8, 768], BF16)
                        R_winq = K.reg("winq")
                        if _LVL >= 1:
                            K.pdma(K.pslot("winq"), winq[:], w_in_d.rearrange("(k p) n -> p k n", p=128)[:, :, 0:768],
                                   writes=[R_winq])
                        xt = [sb(sA1, f"xt{i}", [128, D], F32) for i in range(2)]
                        junk = sb(sA1, "junk", [128, D], BF16)
                        xn = [sb(sA1, f"xn{i}", [128, D], BF16) for i in range(2)]
                        st1 = sb(sA1, "st1", [128, 3, NT], F32)
                        zq = [sb(sA1, f"zq{i}", [128, 512], F32) for i in range(2)]
                        zkv = [sb(sA1, f"zkv{i}", [128, 256], F32) for i in range(2)]
                        sq = sb(sA1, "sq", [128, 512], F32)
                        qn = sb(sA1, "qn", [128, 512], F32)
                        kn = sb(sA1, "kn", [128, 128], F32)
                        knb = sb(sA1, "knb", [128, 128], BF16)
                        ta = sb(sA1, "ta", [128, 256], F32)
                        tb = sb(sA1, "tb", [128, 256], F32)
                        qst = sb(sA1, "qst", [128, 3, 16], F32)
                        qkb = [sb(sA1, f"qkb{i}", [128, 768], BF16) for i in range(2)]
                        R_xt = K.regs_n(2, "xt")
                        R_xn = K.regs_n(2, "xn")
                        R_st1 = K.regs_n(NT, "st1")
                        R_zq = K.regs_n(2, "zq")
                        R_zkv = K.regs_n(2, "zkv")
                        R_tmp = K.reg("tmpq")
                        R_qst = K.reg("qst")
                        R_qkb = K.regs_n(2, "qkb")
                        R_knb = K.reg("knb")
                        def a1_front(T):
                            b = T % 2
                            lat = T >= 2
                            t = T - 2
                            tsl = slice(T * 128, (T + 1) * 128)
                            dma("sp", xt[b][:], xin[tsl, :], writes=[R_xt[b]])
                            if _LVL < -4:
                                return
                            op("act", lambda e: e.activation(junk[:], xt[b][:], AF.Square,
                                                             accum_out=st1[:, 0, T:T + 1]),
                               reads=[R_xt[b]], writes=[R_st1[T]])
                            if _LVL < -3:
                                return
                            op("act", lambda e: e.activation(st1[:, 1, T:T + 1], st1[:, 0, T:T + 1], AF.Ln,
                                                             scale=1.0 / D, bias=EPS),
                               reads=[R_st1[T]], writes=[R_st1[T]])
                            op("act", lambda e: e.activation(st1[:, 2, T:T + 1], st1[:, 1, T:T + 1], AF.Exp,
                                                             scale=-0.5),
                               reads=[R_st1[T]], writes=[R_st1[T]])
                            op("act", lambda e: e.activation(xn[b][:], xt[b][:], AF.Copy, scale=st1[:, 2, T:T + 1]),
                               reads=[R_xt[b], R_st1[T]], writes=[R_xn[b]])
                            if _LVL < -2:
                                return
                            psb = PS[b].bitcast(BF16)
                            for k in range(8):
                                op("pe", lambda e, k=k: e.transpose(psb[:, k * 128:(k + 1) * 128],
                                                                    xn[b][:, k * 128:(k + 1) * 128], identb[:]),
                                   reads=[R_xn[b], R_const], writes=[PSR[b]])
                            if _LVL < -1:
                                return
                            so = 0 if lat else 16
                            for k in range(8):
                                if (b == 0 and _KEV != 'act') or _KEV == 'dve':
                                    op("dve", lambda e, k=k: e.tensor_scalar(
                                        hT[:, k, tsl], psb[:, k * 128:(k + 1) * 128],
                                        smod[:, so + k:so + k + 1], smod[:, so + 8 + k:so + 9 + k],
                                        ALU.mult, ALU.add), reads=[PSR[b], R_smod], writes=[R_hTd[T]])
                                else:
                                    op("act", lambda e, k=k: e.activation(
                                        hT[:, k, tsl], psb[:, k * 128:(k + 1) * 128], AF.Identity,
                                        bias=smod[:, so + 8 + k:so + 9 + k], scale=smod[:, so + k:so + k + 1]),
                                       reads=[PSR[b], R_smod], writes=[R_hTa[T]])
                            if _LVL < 1:
                                return
                            if lat:
                                for k in range(8):
                                    op("pe", lambda e, k=k: e.matmul(PS[2], hT[:, k, tsl], winq[:, k, 0:512],
                                                                   start=(k == 0), stop=(k == 7)),
                                       reads=[R_hTd[T], R_hTa[T], R_winq], writes=[PSR[2]])
                            for k in range(8):
                                op("pe", lambda e, k=k: e.matmul(PS[3][:, 0:256], hT[:, k, tsl], winq[:, k, 512:768],
                                                               start=(k == 0), stop=(k == 7)),
                                   reads=[R_hTd[T], R_hTa[T], R_winq], writes=[PSR[3]])

                        def a1_front_b(T):
                            b = T % 2
                            lat = T >= 2
                            t = T - 2
                            tsl = slice(T * 128, (T + 1) * 128)
                            if lat:
                                op("act", lambda e: e.activation(zq[b][:], PS[2], AF.Copy),
                                   reads=[PSR[2]], writes=[R_zq[b]])
                            op("act", lambda e: e.activation(zkv[b][:], PS[3][:, 0:256], AF.Copy),
                               reads=[PSR[3]], writes=[R_zkv[b]])
                            op("act", lambda e: e.activation(
                                VE[:, T, :, 0:64], zkv[b][:, 128:256].rearrange("p (g d) -> p g d", g=2), AF.Copy),
                               reads=[R_zkv[b], R_ve1], writes=[R_VE[T]])

                        def a1_back(T, part):
                            b = T % 2
                            lat = T >= 2
                            t = T - 2
                            tsl = slice(T * 128, (T + 1) * 128)
                            nh_list = ([("q", zq[b], 8, qn, qgb, R_zq[b])] if lat else []) + \
                                      [("k", zkv[b], 2, kn, kgb, R_zkv[b])]
                            for nm, src, nh, dst, gb_, rsrc in nh_list:
                                W = nh * 64
                                c0 = 0 if nm == "q" else 8
                                op("dve", lambda e: e.tensor_tensor(sq[:, 0:W], src[:, 0:W], src[:, 0:W], ALU.mult),
                                   reads=[rsrc], writes=[R_tmp])
                                op("dve", lambda e: e.tensor_reduce(
                                    qst[:, 0, c0:c0 + nh], sq[:, 0:W].rearrange("p (h d) -> p h d", h=nh),
                                    AX.X, ALU.add), reads=[R_tmp], writes=[R_qst])
                            lo = 0 if lat else 8
                            op("act", lambda e: e.activation(qst[:, 1, lo:10], qst[:, 0, lo:10],
                                                             AF.Ln, scale=1.0 / 64, bias=EPS),
                               reads=[R_qst], writes=[R_qst])
                            op("act", lambda e: e.activation(qst[:, 2, lo:10], qst[:, 1, lo:10],
                                                             AF.Exp, scale=-0.5),
                               reads=[R_qst], writes=[R_qst])

                        def a1_back_rest(T):
                            b = T % 2
                            lat = T >= 2
                            t = T - 2
                            tsl = slice(T * 128, (T + 1) * 128)
                            nh_list = ([("q", zq[b], 8, qn, qgb, R_zq[b])] if lat else []) + \
                                      [("k", zkv[b], 2, kn, kgb, R_zkv[b])]
                            for nm, src, nh, dst, gb_, rsrc in nh_list:
                                W = nh * 64
                                c0 = 0 if nm == "q" else 8
                                d3 = dst[:, 0:W].rearrange("p (h d) -> p h d", h=nh)
                                op("dve", lambda e: e.tensor_tensor(
                                    d3, src[:, 0:W].rearrange("p (h d) -> p h d", h=nh),
                                    qst[:, 2, c0:c0 + nh].unsqueeze(2).to_broadcast([128, nh, 64]), ALU.mult),
                                   reads=[rsrc, R_qst], writes=[R_tmp])
                                op("dve", lambda e: e.tensor_tensor(
                                    d3, d3, gb_[:, :].unsqueeze(1).to_broadcast([128, nh, 64]), ALU.mult),
                                   reads=[R_tmp, R_const], writes=[R_tmp])
                                if nm == "q":
                                    outb = qkb[b][:, 0:512]
                                    R_ob = R_qkb[b]
                                else:
                                    outb = knb[:, 0:128]
                                    R_ob = R_knb
                                if lat:
                                    d5 = dst[:, 0:W].rearrange("p (h a f d) -> p h a f d", h=nh, a=2, f=2)
                                    o5 = outb.rearrange("p (h a f d) -> p h a f d", h=nh, a=2, f=2)
                                    x1 = d5[:, :, :, 0, :]
                                    x2 = d5[:, :, :, 1, :]
                                    cb = cosT[:, t, :].rearrange("p (a d) -> p a d", a=2).unsqueeze(1) \
                                        .to_broadcast([128, nh, 2, 16])
                                    sb_ = sinT[:, t, :].rearrange("p (a d) -> p a d", a=2).unsqueeze(1) \
                                        .to_broadcast([128, nh, 2, 16])
                                    hw = nh * 32
                                    ta4 = ta[:, 0:hw].rearrange("p (h a d) -> p h a d", h=nh, a=2)
                                    tb4 = tb[:, 0:hw].rearrange("p (h a d) -> p h a d", h=nh, a=2)
                                    op("dve", lambda e: e.tensor_tensor(ta4, x1, cb, ALU.mult),
                                       reads=[R_tmp, R_const], writes=[R_tmp])
                                    op("dve", lambda e: e.tensor_tensor(tb4, x2, sb_, ALU.mult),
                                       reads=[R_tmp, R_const], writes=[R_tmp])
                                    op("dve", lambda e: e.tensor_tensor(o5[:, :, :, 0, :], ta4, tb4, ALU.subtract),
                                       reads=[R_tmp], writes=[R_ob])
                                    op("dve", lambda e: e.tensor_tensor(ta4, x1, sb_, ALU.mult),
                                       reads=[R_tmp, R_const], writes=[R_tmp])
                                    op("dve", lambda e: e.tensor_tensor(tb4, x2, cb, ALU.mult),
                                       reads=[R_tmp, R_const], writes=[R_tmp])
                                    op("dve", lambda e: e.tensor_tensor(o5[:, :, :, 1, :], ta4, tb4, ALU.add),
                                       reads=[R_tmp], writes=[R_ob])
                                else:
                                    op("dve", lambda e: e.tensor_copy(outb, dst[:, 0:W]),
                                       reads=[R_tmp], writes=[R_ob])
                            if _LVL < 4:
                                return
                            op("dve", lambda e: e.tensor_copy(
                                qkb[b][:, 512:768].rearrange("p (g r d) -> p g r d", g=2, r=2),
                                knb[:, 0:128].rearrange("p (g d) -> p g d", g=2).unsqueeze(2)
                                .to_broadcast([128, 2, 2, 64])), reads=[R_knb], writes=[R_qkb[b]])
                            if _LVL < 5:
                                return
                            ps4 = PS[4].bitcast(BF16)
                            ps5 = PS[5].bitcast(BF16)
                            if lat:
                                for j in range(4):
                                    op("pe", lambda e, j=j: e.transpose(ps4[:, j * 128:(j + 1) * 128],
                                                                        qkb[b][:, j * 128:(j + 1) * 128], identb[:]),
                                       reads=[R_qkb[b], R_const], writes=[PSR[4]])
                                op("dve", lambda e: e.tensor_copy(
                                    QT[:, :, t * 128:(t + 1) * 128],
                                    ps4[:, 0:512].rearrange("p (j n) -> p j n", j=4)),
                                   reads=[PSR[4]], writes=[R_QT[t]])
                            for j in range(2):
                                op("pe", lambda e, j=j: e.transpose(ps5[:, j * 128:(j + 1) * 128],
                                                                    qkb[b][:, 512 + j * 128:512 + (j + 1) * 128],
                                                                    identb[:]),
                                   reads=[R_qkb[b], R_const], writes=[PSR[5]])
                            for u in range(2):
                                op("dve", lambda e, u=u: e.tensor_copy(
                                    KT[64 * u:64 * u + 64, :, u, tsl],
                                    ps5[64 * u:64 * u + 64, 0:256].rearrange("p (j n) -> p j n", j=2)),
                                   reads=[PSR[5], R_ve1], writes=[R_KT[T]])

                        a1_front(0)
                        a1_front_b(0)
                        for T in range(NT):
                            if T + 1 < NT:
                                a1_front(T + 1)
                            a1_back(T, "stats")
                            if T + 1 < NT:
                                a1_front_b(T + 1)
                            a1_back_rest(T)
                        K.barrier()
                        if stop_after == "A1":
                            if debug:
                                dma("sp", d_QT, QT[:]); dma("sp", d_KT[0:64], KT[0:64, :, 0, :]); dma("sp", d_KT[64:128], KT[64:128, :, 1, :]); dma("sp", d_V, VE[:, :, :, 0:65])
                            K.barrier()
                            return nc, list(dbg.keys())
                    with ExitStack() as sA2:
                        wc = [sb(sA2, f"wc{i}", [128, 8, 384], BF16) for i in range(2)]
                        vT = [sb(sA2, f"vT{i}", [128, SEQ + 2], F32) for i in range(2)]
                        gbT = [sb(sA2, f"gbT{i}", [128, SEQ], F32) for i in range(2)]
                        gct = [sb(sA2, f"gct{i}", [128, 512], F32) for i in range(2)]
                        yb = sb(sA2, "yb", [128, 1024], F32)
                        ysq = sb(sA2, "ysq", [128, 1024], F32)
                        rs = sb(sA2, "rs", [128, 1024], F32)
                        R_wc = K.regs_n(2, "wc")
                        P_wc = [[K.pslot(f"wc{i}_{j}") for j in range(3)] for i in range(2)]
                        R_vT = K.regs_n(2, "vT")
                        R_gbT = K.regs_n(2, "gbT")
                        R_gct = K.regs_n(2, "gct")
                        R_y = K.reg("y")
                        R_ysq = K.reg("ysq")
                        R_rs = K.reg("rs")
                        winv = w_in_d.rearrange("(k p) n -> p k n", p=128)
                        for i in range(2):
                            op("dve", lambda e, i=i: e.memset(vT[i][:, 0:1], 0.0), writes=[R_vT[i]])
                            op("dve", lambda e, i=i: e.memset(vT[i][:, SEQ + 1:SEQ + 2], 0.0), writes=[R_vT[i]])
                        cnt2 = {'gi': 0, 'pi': 0}
                        def a2_mm(c4):
                            b = c4 % 2
                            for s3 in range(3):
                                c0 = 768 + s3 * 512 + c4 * 128
                                K.pdma(P_wc[b][s3], wc[b][:, :, s3 * 128:(s3 + 1) * 128], winv[:, :, c0:c0 + 128],
                                       writes=[R_wc[b]])
                            for tb_ in range(4):
                                tok = slice(256 + tb_ * 512, 256 + (tb_ + 1) * 512)
                                osl = slice(tb_ * 512, (tb_ + 1) * 512)
                                g_ = cnt2['gi'] % 2
                                cnt2['gi'] += 1
                                for s3 in range(3):
                                    pb = 5 + cnt2['pi'] % 3
                                    cnt2['pi'] += 1
                                    for k in range(8):
                                        op("pe", lambda e, k=k, s3=s3, pb=pb: e.matmul(
                                            PS[pb], wc[b][:, k, s3 * 128:(s3 + 1) * 128], hT[:, k, tok],
                                            start=(k == 0), stop=(k == 7)),
                                           reads=[R_wc[b]], writes=[PSR[pb]])
                                    if s3 == 0:
                                        op("act", lambda e, pb=pb: e.activation(gbT[b][:, osl], PS[pb], AF.Copy),
                                           reads=[PSR[pb]], writes=[R_gbT[b]])
                                    elif s3 == 1:
                                        op("act", lambda e, pb=pb: e.activation(gct[g_][:], PS[pb], AF.Copy),
                                           reads=[PSR[pb]], writes=[R_gct[g_]])
                                    else:
                                        op("dve", lambda e, pb=pb: e.tensor_tensor(
                                            vT[b][:, 1 + tb_ * 512:1 + (tb_ + 1) * 512], PS[pb], gct[g_][:], ALU.mult),
                                           reads=[PSR[pb], R_gct[g_]], writes=[R_vT[b]])

                        def a2_fin(c4):
                            b = c4 % 2
                            for hf in range(2):
                                o = hf * 1024
                                op("dve", lambda e: e.tensor_scalar(yb[:], vT[b][:, o:o + 1024],
                                                                    cw[:, c4 * 3:c4 * 3 + 1], None, ALU.mult),
                                   reads=[R_vT[b], R_const], writes=[R_y])
                                op("dve", lambda e: e.scalar_tensor_tensor(
                                    yb[:], vT[b][:, o + 1:o + 1025], cw[:, c4 * 3 + 1:c4 * 3 + 2], yb[:],
                                    ALU.mult, ALU.add), reads=[R_vT[b], R_const, R_y], writes=[R_y])
                                op("dve", lambda e: e.scalar_tensor_tensor(
                                    yb[:], vT[b][:, o + 2:o + 1026], cw[:, c4 * 3 + 2:c4 * 3 + 3], yb[:],
                                    ALU.mult, ALU.add), reads=[R_vT[b], R_const, R_y], writes=[R_y])
                                op("dve", lambda e: e.tensor_tensor(yb[:], yb[:], gbT[b][:, o:o + 1024], ALU.mult),
                                   reads=[R_y, R_gbT[b]], writes=[R_y])
                                op("act", lambda e: e.activation(ysq[:], yb[:], AF.Square),
                                   reads=[R_y], writes=[R_ysq])
                                for q2 in range(2):
                                    op("pe", lambda e, q2=q2: e.matmul(PS[q2], bd64[:], ysq[:, q2 * 512:(q2 + 1) * 512],
                                                                     start=True, stop=True),
                                       reads=[R_ysq, R_const], writes=[PSR[q2]])
                                    op("act", lambda e, q2=q2: e.activation(rs[:, q2 * 512:(q2 + 1) * 512], PS[q2],
                                                                            AF.Ln, bias=EPS),
                                       reads=[PSR[q2]], writes=[R_rs])
                                op("act", lambda e: e.activation(rs[:], rs[:], AF.Exp, scale=-0.5),
                                   reads=[R_rs], writes=[R_rs])
                                op("dve", lambda e: e.tensor_tensor(convn[:, c4, o:o + 1024], yb[:], rs[:], ALU.mult),
                                   reads=[R_y, R_rs], writes=[R_convn[c4]])

                        a2_mm(0)
                        for c4 in range(4):
                            if c4 + 1 < 4:
                                a2_mm(c4 + 1)
                            a2_fin(c4)
                        K.barrier()
                if debug:
                    dma("sp", d_QT, QT[:], reads=R_QT)
                    dma("sp", d_KT[0:64], KT[0:64, :, 0, :], reads=R_KT)
                    dma("sp", d_KT[64:128], KT[64:128, :, 1, :], reads=R_KT)
                    dma("sp", d_V, VE[:, :, :, 0:65], reads=R_VE)
                    dma("sp", d_convn, convn[:], reads=R_convn)
                if stop_after == "A2":
                    K.barrier()
                    return nc, list(dbg.keys())

                with ExitStack() as sB2:
                    attnT = sb(sB2, "attnT", [64, 8, SEQ], BF16)
                    R_attn = K.regs_n(8, "attn")
                    woa = sb(sB2, "woa", [64, 8, D], BF16)
                    woc = sb(sB2, "woc", [128, 4, D], BF16)
                    aog = sb(sB2, "aog", [64, 8], F32)
                    cog = sb(sB2, "cog", [128, 4], F32)
                    sW = ExitStack()
                    wst = sb(sW, "wst", [128, 4, D], F32)
                    R_wst = K.reg("wst")
                    R_wo = K.reg("wo")
                    dma("sp", aog[:], aog_d, writes=[R_wo])
                    dma("sp", cog[:], cog_d, writes=[R_wo])
                    for half in range(2):
                        dma("sp", wst[0:64, :, :],
                            w_out_d[half * 256:(half + 1) * 256, :].rearrange("(h d) n -> d h n", d=64),
                            writes=[R_wst])
                        op("dve", lambda e: e.tensor_tensor(
                            woa[:, half * 4:(half + 1) * 4, :], wst[0:64, :, :],
                            aog[:, half * 4:(half + 1) * 4].unsqueeze(2).to_broadcast([64, 4, D]), ALU.mult),
                           reads=[R_wst, R_wo], writes=[R_wo])
                    dma("sp", wst[:, :, :], w_out_d[512:1024, :].rearrange("(c p) n -> p c n", p=128),
                        writes=[R_wst])
                    op("dve", lambda e: e.tensor_tensor(
                        woc[:, :, :], wst[:, :, :], cog[:, :].unsqueeze(2).to_broadcast([128, 4, D]), ALU.mult),
                       reads=[R_wst, R_wo], writes=[R_wo])
                    K.barrier()
                    sW.close()
                    with ExitStack() as sBa:
                        PT = [sb(sBa, f"PT{i}", [128, 1024], BF16) for i in range(3)]
                        sqb = [sb(sBa, f"sqb{i}", [65, 512], F32) for i in range(2)]
                        lnb = [sb(sBa, f"lnb{i}", [64, 512], F32) for i in range(2)]
                        R_PT = K.regs_n(3, "PT")
                        R_sqb = K.regs_n(2, "sqb")
                        R_lnb = K.regs_n(2, "lnb")
                        zt = sb(sBa, "zt", [128, 8, D], BF16)
                        R_zt = K.reg("zt")
                        op("dve", lambda e: e.memset(zt[:], 0.0), writes=[R_zt])
                        for i in range(NE * CAP // 1024):
                            dma("sp", xbuf_d[i * 1024:(i + 1) * 1024, :].rearrange("(p j) d -> p j d", j=8), zt[:],
                                reads=[R_zt])
                        dma("sp", xbuf_d[NE * CAP:NE * CAP + 1, :], zt[0:1, 0, :], reads=[R_zt])
                        it = 0
                        pti = 0
                        spi = 0
                        for j in range(4):
                            g = j // 2
                            for qb in range(4):
                                qs = slice(qb * 512, (qb + 1) * 512)
                                obase = 4 + 2 * (it % 2)
                                it += 1
                                Oab = [PS[obase], PS[obase + 1]]

                                def s_step(kt, sp):
                                    for u in range(2):
                                        p0 = 64 * u
                                        bk = 2 * sp + u
                                        op("pe", lambda e: e.matmul(
                                            PS[bk], KT[:, g, u, kt * 128:(kt + 1) * 128],
                                            QT[:, j, qs], start=True, stop=True),
                                           writes=[PSR[2 * sp], PSR[2 * sp + 1]] if u == 0 else [PSR[bk]])
                                sp_of = {0: spi % 2}
                                spi += 1
                                s_step(0, sp_of[0])
                                for kt in range(NT):
                                    if kt + 1 < NT:
                                        sp_of[kt + 1] = spi % 2
                                        spi += 1
                                        s_step(kt + 1, sp_of[kt + 1])
                                    sp = sp_of[kt]
                                    pb_ = pti % 3
                                    pti += 1
                                    op("act", lambda e: e.activation(
                                        PT[pb_][:], PSUM[:, 2 * sp * 512:(2 * sp + 2) * 512], AF.Exp, scale=0.125),
                                       reads=[PSR[2 * sp], PSR[2 * sp + 1]], writes=[R_PT[pb_]])
                                    for u in range(2):
                                        op("pe", lambda e: e.matmul(Oab[u][:, :], VE[:, kt, g, :],
                                                                    PT[pb_][:, u * 512:(u + 1) * 512],
                                                                    start=(kt == 0), stop=(kt == NT - 1)),
                                           reads=[R_PT[pb_]], writes=[PSR[obase + u]])
                                sp = spi % 2
                                spi += 1
                                for u in range(2):
                                    op("act", lambda e: e.activation(sqb[u][:], Oab[u][0:65, :], AF.Square),
                                       reads=[PSR[obase + u]], writes=[R_sqb[u]])
                                    op("pe", lambda e: e.matmul(PS[2 * sp + u][0:64, :], c65[:, :], sqb[u][:],
                                                                start=True, stop=True),
                                       reads=[R_sqb[u], R_const], writes=[PSR[2 * sp + u]])
                                for u in range(2):
                                    op("act", lambda e: e.activation(lnb[u][:], PS[2 * sp + u][0:64, :], AF.Ln),
                                       reads=[PSR[2 * sp + u]], writes=[R_lnb[u]])
                                    op("act", lambda e: e.activation(lnb[u][:], lnb[u][:], AF.Exp, scale=-0.5),
                                       reads=[R_lnb[u]], writes=[R_lnb[u]])
                                    op("dve", lambda e: e.tensor_tensor(attnT[:, 2 * j + u, qs], Oab[u][0:64, :],
                                                                        lnb[u][:], ALU.mult),
                                       reads=[PSR[obase + u], R_lnb[u]], writes=[R_attn[2 * j + u]])
                        K.barrier()
                    if debug:
                        dma("sp", d_attn, attnT[:], reads=R_attn)
                    if stop_after == "B":
                        K.barrier()
                        return nc, list(dbg.keys())

                    if True:
                        with ExitStack() as sB3:
                            xr = [sb(sB3, "xr0", [128, D], F32)] * 2
                            xnw = [sb(sB3, "xnw0", [128, D], F32)] * 2
                            hx = [sb(sB3, f"hx{i}", [128, D], F32) for i in range(2)]
                            hxT = [sb(sB3, "hxT0", [128, 8, 128], F32)] * 2
                            st2 = sb(sB3, "st2", [128, 3, NL], F32)
                            lg = sb(sB3, "lg", [128, NL, 36], F32)
                            rt = sb(sB3, "rt", [128, 64], F32)
                            RB = sb(sB3, "RB", [128, 16, NL, 8], F32)
                            m8 = sb(sB3, "m8", [128, 8], F32)
                            ustr = sb(sB3, "ustr", [128, 128], BF16)
                            onesb = sb(sB3, "onesb", [128, 128], BF16)
                            ustf = sb(sB3, "ustf", [128, 128], F32)
                            eC = sb(sB3, "eC", [128, NE], F32)
                            maskb = sb(sB3, "maskb", [128, NL, NE], BF16)
                            mkk = sb(sB3, "mkk", [128, 2, NL, NE], F32)
                            rkk = sb(sB3, "rkk", [128, 2, NL, NE], F32)
                            t3 = sb(sB3, "t3", [128, NL, NE], F32)
                            dfa = sb(sB3, "dfa", [128, 3, 2, NL], F32)
                            hxb = [sb(sB3, f"hxb{i}", [128, D], BF16) for i in range(2)]
                            R_mask = K.regs_n(NL, "mask")
                            R_rk = K.reg("rk")
                            R_hxb = K.regs_n(2, "hxb")
                            R_xbuf = K.reg("xbuf")
                            dma("sp", ustf[:], ustr_d, writes=[R_wo])
                            dma("sp", eC[:], eC_d, writes=[R_wo])
                            op("dve", lambda e: e.tensor_copy(ustr[:], ustf[:]), reads=[R_wo], writes=[R_wo])
                            op("dve", lambda e: e.memset(onesb[:], 1.0), writes=[R_wo])
                            R_xr = [K.reg("xr")] * 2
                            R_xnw = [K.reg("xnw")] * 2
                            R_hx = K.regs_n(2, "hx")
                            R_hxT = [K.reg("hxT")] * 2
                            R_st2 = K.regs_n(NL, "st2")
                            R_lg = K.reg("lg")
                            R_rt = K.reg("rt")
                            def b2_front(t):
                                b = t % 2
                                tsl = slice(t * 128, (t + 1) * 128)
                                dma("sp", xr[b][:], xin[256 + t * 128:256 + (t + 1) * 128, :], writes=[R_xr[b]])
                                for hf in range(2):
                                    pb = hf + 6 * (t % 2)
                                    cs = slice(hf * 512, (hf + 1) * 512)
                                    for h in range(8):
                                        op("pe", lambda e, h=h: e.matmul(PS[pb], attnT[:, h, tsl], woa[:, h, cs],
                                                                       start=(h == 0), stop=False),
                                           reads=[R_wo], writes=[PSR[pb]])
                                    for c4 in range(4):
                                        op("pe", lambda e, c4=c4: e.matmul(PS[pb], convn[:, c4, tsl], woc[:, c4, cs],
                                                                         start=False, stop=(c4 == 3)),
                                           reads=[R_wo], writes=[PSR[pb]])
                                    op("dve", lambda e: e.tensor_tensor(xnw[b][:, cs], PS[pb], modb[:, 2 * D + hf * 512:
                                                                                                  2 * D + (hf + 1) * 512],
                                                                        ALU.mult),
                                       reads=[PSR[pb]], writes=[R_xnw[b]])
                                op("dve", lambda e: e.tensor_tensor(xnw[b][:], xnw[b][:], xr[b][:], ALU.add),
                                   reads=[R_xnw[b], R_xr[b]], writes=[R_xnw[b]])
                                dma("sp", xnew_d[tsl, :], xnw[b][:], reads=[R_xnw[b]])
                                op("act", lambda e: e.activation(hxb[b][:], xnw[b][:], AF.Square,
                                                                 accum_out=st2[:, 0, t:t + 1]),
                                   reads=[R_xnw[b]], writes=[R_st2[t], R_hxb[b]])
                                op("act", lambda e: e.activation(st2[:, 1, t:t + 1], st2[:, 0, t:t + 1], AF.Ln,
                                                                 scale=1.0 / D, bias=EPS),
                                   reads=[R_st2[t]], writes=[R_st2[t]])
                                op("act", lambda e: e.activation(st2[:, 2, t:t + 1], st2[:, 1, t:t + 1], AF.Exp,
                                                                 scale=-0.5),
                                   reads=[R_st2[t]], writes=[R_st2[t]])
                                op("dve", lambda e: e.scalar_tensor_tensor(
                                    hx[b][:], xnw[b][:], st2[:, 2, t:t + 1], modb[:, 4 * D:5 * D], ALU.mult, ALU.mult),
                                   reads=[R_xnw[b], R_st2[t]], writes=[R_hx[b]])
                                op("dve", lambda e: e.tensor_tensor(hx[b][:], hx[b][:], modb[:, 3 * D:4 * D], ALU.add),
                                   reads=[R_hx[b]], writes=[R_hx[b]])

                            def b2_back(t):
                                b = t % 2
                                tsl = slice(t * 128, (t + 1) * 128)
                                PP = PSUM[:, 2 * 512:4 * 512]
                                for k in range(8):
                                    op("pe", lambda e, k=k: e.transpose(PP[:, k * 128:(k + 1) * 128],
                                                                        hx[b][:, k * 128:(k + 1) * 128], identf[:]),
                                       reads=[R_hx[b], R_const], writes=[PSR[2], PSR[3]])
                                op("act", lambda e: e.activation(
                                    hxT[b][:], PP.rearrange("p (k n) -> p k n", k=8), AF.Copy),
                                   reads=[PSR[2], PSR[3]], writes=[R_hxT[b]])
                                op("act", lambda e: e.activation(hxb[b][:], hx[b][:], AF.Copy),
                                   reads=[R_hx[b]], writes=[R_hxb[b]])
                                for k in range(8):
                                    op("pe", lambda e, k=k: e.matmul(PS[4][:, 0:36], hxT[b][:, k, :], wr[:, k, :],
                                                                   start=(k == 0), stop=(k == 7)),
                                       reads=[R_hxT[b], R_const], writes=[PSR[4]])
                                op("act", lambda e: e.activation(lg[:, t, :], PS[4][:, 0:36], AF.Copy),
                                   reads=[PSR[4]], writes=[R_lg])
                                dma("sp", hx2_d[tsl, :], hxb[b][:], reads=[R_hxb[b]], writes=[R_xbuf])

                            b2_front(0)
                            for t in range(NL):
                                if t + 1 < NL:
                                    b2_front(t + 1)
                                b2_back(t)
                            def slab(i, w=8):
                                return RB[:, i, :, 0:w]

                            def col(i):
                                return RB[:, i, :, 0]

                            def bc(a2, w):
                                return a2.unsqueeze(2).to_broadcast([128, NL, w])
                            R_B = K.reg("RB")

                            def dv(fn, rd=(), wr_=()):
                                op("dve", fn, reads=[R_lg, R_B] + list(rd), writes=[R_B] + list(wr_))

                            def ac(fn):
                                op("act", fn, reads=[R_lg, R_B], writes=[R_B])
                            G4 = lg[:, :, 0:4]
                            dv(lambda e: e.tensor_reduce(col(0), G4, AX.X, ALU.max))
                            dv(lambda e: e.tensor_tensor(slab(1, 4), G4, bc(col(0), 4), ALU.is_equal))
                            dv(lambda e: e.tensor_tensor(slab(2, 4), G4, bc(col(0), 4), ALU.subtract))
                            ac(lambda e: e.activation(slab(2, 4), slab(2, 4), AF.Exp))
                            dv(lambda e: e.tensor_reduce(col(3), slab(2, 4), AX.X, ALU.add))
                            dv(lambda e: e.reciprocal(col(4), col(3)))
                            dv(lambda e: e.tensor_tensor(slab(5), lg[:, :, 4:12], bc(RB[:, 1, :, 0], 8), ALU.mult))
                            for gq in range(1, 4):
                                dv(lambda e, gq=gq: e.tensor_tensor(slab(6), lg[:, :, 4 + 8 * gq:12 + 8 * gq],
                                                                    bc(RB[:, 1, :, gq], 8), ALU.mult))
                                dv(lambda e: e.tensor_tensor(slab(5), slab(5), slab(6), ALU.add))
                            dv(lambda e: e.tensor_reduce(col(7), slab(5), AX.X, ALU.max))
                            dv(lambda e: e.tensor_tensor(slab(8), slab(5), bc(col(7), 8), ALU.is_equal))
                            dv(lambda e: e.scalar_tensor_tensor(slab(9), slab(8), -1.0e30, slab(5), ALU.mult, ALU.add))
                            dv(lambda e: e.tensor_reduce(col(10), slab(9), AX.X, ALU.max))
                            dv(lambda e: e.tensor_tensor(slab(11), slab(5), bc(col(10), 8), ALU.is_ge))
                            dv(lambda e: e.tensor_tensor(slab(12), slab(11), slab(8), ALU.subtract))
                            dv(lambda e: e.tensor_tensor(slab(13), slab(5), bc(col(7), 8), ALU.subtract))
                            ac(lambda e: e.activation(slab(13), slab(13), AF.Exp))
                            dv(lambda e: e.tensor_tensor(slab(13), slab(13), slab(11), ALU.mult))
                            dv(lambda e: e.tensor_reduce(col(14), slab(13), AX.X, ALU.add))
                            dv(lambda e: e.reciprocal(col(15), col(14)))
                            dv(lambda e: e.tensor_tensor(col(15), col(15), col(4), ALU.mult))
                            dv(lambda e: e.tensor_tensor(slab(13), slab(13), bc(col(15), 8), ALU.mult))
                            for gq in range(4):
                                ohg = bc(RB[:, 1, :, gq], 8)
                                dv(lambda e, gq=gq, ohg=ohg: e.tensor_tensor(wgt[:, :, gq * 8:(gq + 1) * 8], slab(13), ohg,
                                                                             ALU.mult), wr_=R_wgt)
                                dv(lambda e, gq=gq, ohg=ohg: e.tensor_tensor(mkk[:, 0, :, gq * 8:(gq + 1) * 8], slab(8), ohg,
                                                                             ALU.mult))
                                dv(lambda e, gq=gq, ohg=ohg: e.tensor_tensor(mkk[:, 1, :, gq * 8:(gq + 1) * 8], slab(12), ohg,
                                                                             ALU.mult))
                            dv(lambda e: e.tensor_tensor(maskb[:], mkk[:, 0], mkk[:, 1], ALU.add), wr_=[R_mask[0]])
                            for t in range(NL):
                                op("pe", lambda e, t=t: e.matmul(PS[5][:, t * NE:(t + 1) * NE], ustr[:], maskb[:, t, :],
                                                               start=True, stop=(t == 0)),
                                   reads=[R_mask[0], R_wo], writes=[PSR[5]])
                                for t2 in range(t):
                                    op("pe", lambda e, t=t, t2=t2: e.matmul(PS[5][:, t * NE:(t + 1) * NE], onesb[:],
                                                                           maskb[:, t2, :], start=False, stop=(t2 == t - 1)),
                                       reads=[R_mask[0], R_wo], writes=[PSR[5]])
                            op("act", lambda e: e.activation(rkk[:, 0].rearrange("p t e -> p (t e)"), PS[5], AF.Copy),
                               reads=[PSR[5]], writes=[R_rk])
                            op("dve", lambda e: e.tensor_tensor(rkk[:, 1], rkk[:, 0],
                                                                eC[:, :].unsqueeze(1).to_broadcast([128, NL, NE]), ALU.add),
                               reads=[R_rk, R_wo], writes=[R_rk])
                            for kk in range(2):
                                for src_i, di in ((1, 0), (0, 1)):
                                    op("dve", lambda e, kk=kk, src_i=src_i: e.tensor_tensor(
                                        t3[:], mkk[:, kk], rkk[:, src_i], ALU.mult), reads=[R_rk, R_B], writes=[R_B])
                                    op("dve", lambda e, kk=kk, di=di: e.tensor_reduce(
                                        dfa[:, di, kk, :], t3[:], AX.X, ALU.add), reads=[R_B], writes=[R_B])
                                op("dve", lambda e, kk=kk: e.tensor_tensor(t3[:], mkk[:, kk], wgt[:], ALU.mult),
                                   reads=[R_B] + R_wgt, writes=[R_B])
                                op("dve", lambda e, kk=kk: e.tensor_reduce(wsel[:, :, kk], t3[:], AX.X, ALU.add),
                                   reads=[R_B], writes=R_dest)
                            dv(lambda e: e.tensor_scalar(dfa[:, 2], dfa[:, 1], float(CAP), None, ALU.is_ge))
                            dv(lambda e: e.scalar_tensor_tensor(dfa[:, 0], dfa[:, 2], 1.0e6, dfa[:, 0], ALU.mult, ALU.add))
                            dv(lambda e: e.tensor_scalar(dfa[:, 0], dfa[:, 0], float(NE * CAP), None, ALU.min))
                            dv(lambda e: e.tensor_copy(dest[:].rearrange("p t k -> p k t"), dfa[:, 0]), wr_=R_dest)
                            for t in range(NL):
                                b = t % 2
                                dma("sp", hxb[b][:], hx2_d[t * 128:(t + 1) * 128, :], reads=[R_xbuf], writes=[R_hxb[b]])
                                for kk in range(2):
                                    K.pdma(None, None, None, reads=[R_hxb[b]] + R_dest, writes=[],
                                           fn=lambda g_, kk=kk, t=t: g_.indirect_dma_start(
                                               out=xbuf_d[:, :],
                                               out_offset=bass.IndirectOffsetOnAxis(ap=dest[:, t, kk:kk + 1], axis=0),
                                               in_=hxb[b][:, :], in_offset=None))
                            K.barrier()
                        if debug:
                            dma("sp", d_wgt, wgt[:], reads=R_wgt)
                        if stop_after == "B2":
                            K.barrier()
                            return nc, list(dbg.keys())

        with ExitStack() as sM:
            NWB = 3
            wgs = [sb(sM, f"wgs{i}", [128, 8, DE], BF16) for i in range(NWB)]
            wus = [sb(sM, f"wus{i}", [128, 8, DE], BF16) for i in range(NWB)]
            wds = [sb(sM, f"wds{i}", [128, 6, D], BF16) for i in range(NWB)]
            Xs = [sb(sM, f"Xs{i}", [128, NJ, D], BF16) for i in range(2)]
            XT = [sb(sM, f"XT{i}", [128, 8, CAP], BF16) for i in range(2)]
            HT = [sb(sM, f"HT{i}", [128, 6, CAP], BF16) for i in range(2)]
            sg = [sb(sM, f"sg{i}", [128, CAP], F32) for i in range(2)]
            Yst = [sb(sM, f"Yst{i}", [128, D], F32) for i in range(2)]
            R_wg = K.regs_n(NWB, "wg")
            R_wu = K.regs_n(NWB, "wu")
            R_wd = K.regs_n(NWB, "wd")
            R_Xs = K.regs_n(2, "Xs")
            R_XT = K.regs_n(2, "XT")
            R_HT = K.regs_n(2, "HT")
            R_sg = K.regs_n(2, "sg")
            R_Yst = K.regs_n(2, "Yst")
            cnt = {"si": 0, "yi": 0, "ti": 0}
            op("dve", lambda e: e.memset(Yst[0][0:1, :], 0.0), writes=[R_Yst[0]])
            dma("sp", ybuf_d[NE * CAP:NE * CAP + 1, :], Yst[0][0:1, :], reads=[R_Yst[0]])

            def load_w(ex):
                w3 = ex % NWB
                K.pdma(None, wgs[w3][:], wg_d[ex].rearrange("(k p) f -> p k f", p=128), writes=[R_wg[w3]])
                K.pdma(None, wus[w3][:], wu_d[ex].rearrange("(k p) f -> p k f", p=128), writes=[R_wu[w3]])
                K.pdma(None, wds[w3][:], wd_d[ex].rearrange("(c p) n -> p c n", p=128), writes=[R_wd[w3]])

            def load_x(ex):
                xb_ = ex % 2
                dma("sp", Xs[xb_][:], xbuf_d[ex * CAP:(ex + 1) * CAP, :].rearrange("(j p) d -> p j d", p=128),
                    writes=[R_Xs[xb_]])

            def transposes(ex):
                xb_ = ex % 2
                for j in range(NJ):
                    tbk = 6 + cnt["ti"] % 2
                    cnt["ti"] += 1
                    psb = PS[tbk].bitcast(BF16)
                    for k in range(8):
                        op("pe", lambda e, k=k: e.transpose(psb[:, k * 128:(k + 1) * 128],
                                                            Xs[xb_][:, j, k * 128:(k + 1) * 128], identb[:]),
                           reads=[R_Xs[xb_], R_const], writes=[PSR[tbk]])
                    if tbk == 6:
                        op("dve", lambda e: e.tensor_copy(XT[xb_][:, :, j * 128:(j + 1) * 128],
                                                          psb.rearrange("p (k n) -> p k n", k=8)),
                           reads=[PSR[tbk]], writes=[R_XT[xb_]])
                    else:
                        op("act", lambda e: e.activation(XT[xb_][:, :, j * 128:(j + 1) * 128],
                                                         psb.rearrange("p (k n) -> p k n", k=8), AF.Copy),
                           reads=[PSR[tbk]], writes=[R_XT[xb_]])

            load_w(0)
            load_w(1)
            load_x(0)
            transposes(0)
            for ex in range(NE):
                wb = ex % 2
                w3 = ex % NWB
                if ex + 2 < NE:
                    load_w(ex + 2)
                if ex + 1 < NE:
                    load_x(ex + 1)
                for fc in range(6):
                    fs = slice(fc * 128, (fc + 1) * 128)
                    gbk = fc % 2
                    ubk = 2 + fc % 2
                    for k in range(8):
                        op("pe", lambda e, k=k: e.matmul(PS[gbk][:, 0:CAP], wgs[w3][:, k, fs], XT[wb][:, k, :],
                                                       start=(k == 0), stop=(k == 7)),
                           reads=[R_wg[w3], R_XT[wb]], writes=[PSR[gbk]])
                    for k in range(8):
                        op("pe", lambda e, k=k: e.matmul(PS[ubk][:, 0:CAP], wus[w3][:, k, fs], XT[wb][:, k, :],
                                                       start=(k == 0), stop=(k == 7)),
                           reads=[R_wu[w3], R_XT[wb]], writes=[PSR[ubk]])
                    sb2 = cnt["si"] % 2
                    cnt["si"] += 1
                    op("act", lambda e: e.activation(sg[sb2][:], PS[gbk][:, 0:CAP], AF.Silu),
                       reads=[PSR[gbk]], writes=[R_sg[sb2]])
                    op("dve", lambda e: e.tensor_tensor(HT[wb][:, fc, :], PS[ubk][:, 0:CAP], sg[sb2][:], ALU.mult),
                       reads=[PSR[ubk], R_sg[sb2]], writes=[R_HT[wb]])
                if ex + 1 < NE:
                    transposes(ex + 1)
                for j in range(NJ):
                    ys = cnt["yi"] % 2
                    cnt["yi"] += 1
                    for hf in range(2):
                        yb_ = 4 + hf
                        cs = slice(hf * 512, (hf + 1) * 512)
                        for fc in range(6):
                            op("pe", lambda e, fc=fc: e.matmul(
                                PS[yb_], HT[wb][:, fc, j * 128:(j + 1) * 128], wds[w3][:, fc, cs],
                                start=(fc == 0), stop=(fc == 5)),
                               reads=[R_HT[wb], R_wd[w3]], writes=[PSR[yb_]])
                        op("dve", lambda e: e.tensor_copy(Yst[ys][:, cs], PS[yb_]),
                           reads=[PSR[yb_]], writes=[R_Yst[ys]])
                    dma("sp", ybuf_d[ex * CAP + j * 128:ex * CAP + (j + 1) * 128, :], Yst[ys][:],
                        reads=[R_Yst[ys]])
            K.barrier()
        with ExitStack() as sF:
            y12 = [[sb(sF, f"y{k}_{i}", [128, D], F32) for k in range(2)] for i in range(2)]
            xr2 = [sb(sF, f"xq{i}", [128, D], F32) for i in range(2)]
            ot = [sb(sF, f"ot{i}", [128, D], F32) for i in range(2)]
            junk3 = sb(sF, "junk3", [128, D], BF16)
            st3 = sb(sF, "st3", [128, 3, NL], F32)
            R_y12 = [K.regs_n(2, f"y12_{i}") for i in range(2)]
            R_xr2 = K.regs_n(2, "xr2")
            R_ot = K.regs_n(2, "ot")
            R_st3 = K.regs_n(NL, "st3")
            def fin_front(tg):
                b = tg % 2
                tsl = slice(tg * 128, (tg + 1) * 128)
                dma("sp", xr2[b][:], xnew_d[tsl, :], writes=[R_xr2[b]])
                for kk in range(2):
                    K.pdma(None, None, None, reads=[], writes=[R_y12[b][kk]],
                           fn=lambda g, kk=kk: g.indirect_dma_start(
                               out=y12[b][kk][:, :], out_offset=None, in_=ybuf_d[:, :],
                               in_offset=bass.IndirectOffsetOnAxis(ap=dest[:, tg, kk:kk + 1], axis=0)))
                op("dve", lambda e: e.tensor_scalar(ot[b][:], y12[b][0][:], wsel[:, tg, 0:1], None, ALU.mult),
                   reads=[R_y12[b][0]], writes=[R_ot[b]])
                op("dve", lambda e: e.scalar_tensor_tensor(ot[b][:], y12[b][1][:], wsel[:, tg, 1:2], ot[b][:],
                                                          ALU.mult, ALU.add),
                   reads=[R_y12[b][1], R_ot[b]], writes=[R_ot[b]])
                if debug:
                    dma("sp", d_acc[:, tg, :], ot[b][:], reads=[R_ot[b]])
                op("dve", lambda e: e.tensor_tensor(ot[b][:], ot[b][:], gt2b[:], ALU.mult),
                   reads=[R_ot[b], R_gt2], writes=[R_ot[b]])
                op("dve", lambda e: e.tensor_tensor(ot[b][:], ot[b][:], xr2[b][:], ALU.add),
                   reads=[R_ot[b], R_xr2[b]], writes=[R_ot[b]])

            def fin_back(tg):
                b = tg % 2
                tsl = slice(tg * 128, (tg + 1) * 128)
                op("act", lambda e: e.activation(junk3[:], ot[b][:], AF.Square, accum_out=st3[:, 0, tg:tg + 1]),
                   reads=[R_ot[b]], writes=[R_st3[tg]])
                op("act", lambda e: e.activation(st3[:, 1, tg:tg + 1], st3[:, 0, tg:tg + 1], AF.Ln,
                                                 scale=1.0 / D, bias=EPS),
                   reads=[R_st3[tg]], writes=[R_st3[tg]])
                op("act", lambda e: e.activation(st3[:, 2, tg:tg + 1], st3[:, 1, tg:tg + 1], AF.Exp, scale=-0.5),
                   reads=[R_st3[tg]], writes=[R_st3[tg]])
                op("dve", lambda e: e.scalar_tensor_tensor(
                    ot[b][:], ot[b][:], st3[:, 2, tg:tg + 1], fgb[:], ALU.mult, ALU.mult),
                   reads=[R_ot[b], R_st3[tg], R_const], writes=[R_ot[b]])
                dma("sp", y_d[tsl, :], ot[b][:], reads=[R_ot[b]])

            fin_front(0)
            for tg in range(NL):
                if tg + 1 < NL:
                    fin_front(tg + 1)
                fin_back(tg)
            K.barrier()
    return nc, list(dbg.keys())


def _host_consts():
    ident = np.eye(128, dtype=np.float32)
    rows = SEQ // 64
    row_idx = np.repeat(np.arange(rows, dtype=np.float32), 64)
    col_idx = np.tile(np.arange(64, dtype=np.float32), rows)
    inv_freq = (np.float32(10000.0) ** (-np.arange(0, 32, 2, dtype=np.float32) / np.float32(32))).astype(np.float32)
    ang = np.stack([row_idx[:, None] * inv_freq, col_idx[:, None] * inv_freq], axis=1).astype(np.float32)
    cos = np.cos(ang).astype(np.float32).reshape(SEQ, 32)
    sin = np.sin(ang).astype(np.float32).reshape(SEQ, 32)
    cosT = np.ascontiguousarray(cos.reshape(NL, 128, 32).transpose(1, 0, 2))
    sinT = np.ascontiguousarray(sin.reshape(NL, 128, 32).transpose(1, 0, 2))
    bd64 = np.zeros((128, 128), np.float32)
    bd64[:64, :64] = 1.0 / 64
    bd64[64:, 64:] = 1.0 / 64
    c65 = np.full((65, 64), 1.0 / 64, np.float32)
    c65[64, :] = EPS
    e0 = np.zeros((128, 1), np.float32)
    e0[0, 0] = 1.0
    ustr = np.triu(np.ones((128, 128), np.float32), k=1)
    eC = np.ascontiguousarray(np.broadcast_to((np.arange(NE, dtype=np.float32) * CAP)[None, :], (128, NE)))
    return dict(ident_f=ident, cosT=cosT, sinT=sinT, bd64=bd64, c65=c65, e0=e0, ustr=ustr, eC=eC)


def make_in_maps(inputs, cores=range(8)):
    f = lambda a: np.ascontiguousarray(np.asarray(a, dtype=np.float32))
    x = f(inputs["x"]); c = f(inputs["c"]); ctx = f(inputs["ctx"]); c_ctx = f(inputs["c_ctx"])
    consts = _host_consts()
    bc = lambda v: np.ascontiguousarray(np.broadcast_to(f(v).reshape(1, -1), (128, f(v).size)))
    shared = dict(
        w_mod=f(inputs["w_mod"])[0], bm_b=bc(inputs["b_mod"][0]), g1_b=bc(inputs["norm1_g"][0]),
        g2_b=bc(inputs["norm2_g"][0]), fg_b=bc(inputs["final_g"]), w_in=f(inputs["w_in"])[0],
        qg_b=bc(inputs["q_norm_g"][0]), kg_b=bc(inputs["k_norm_g"][0]),
        cwT=np.ascontiguousarray(f(inputs["conv_w"])[0].reshape(3, 4, 128).transpose(2, 1, 0).reshape(128, 12)),
        aogT=np.ascontiguousarray(f(inputs["attn_out_g"])[0].reshape(8, 64).T),
        cogT=np.ascontiguousarray(f(inputs["conv_out_g"])[0].reshape(4, 128).T),
        w_out=f(inputs["w_out"])[0],
        w_r=np.ascontiguousarray(np.concatenate([f(inputs["w_group"])[0], f(inputs["w_router"])[0]], axis=1)),
        w_gate=f(inputs["w_gate"])[0], w_up=f(inputs["w_up"])[0], w_down=f(inputs["w_down"])[0],
        **consts,
    )
    maps = []
    for b in cores:
        cT = np.concatenate([c[b].reshape(8, 128).T, c_ctx.reshape(8, 128).T], axis=1)
        m = dict(shared)
        m["xin"] = np.ascontiguousarray(np.concatenate([ctx[b], x[b]], axis=0))
        m["cT"] = np.ascontiguousarray(cT.astype(np.float32))
        maps.append(m)
    return maps


_CACHE = {}


def kernel(**inputs):
    if "nc" not in _CACHE:
        _CACHE["nc"] = build_program(debug=False)[0]
    nc = _CACHE["nc"]
    maps = make_in_maps(inputs)
    res = run_bass_kernel_spmd(nc, maps, core_ids=list(range(8)))
    out = np.stack([np.asarray(r["y"], dtype=np.float32) for r in res.results], axis=0)
    return out
```

```python
import os
import numpy as np
from contextlib import ExitStack
import concourse.bass as bass
import concourse.mybir as mybir
from concourse.bass_utils import run_bass_kernel_spmd

F32 = mybir.dt.float32
BF16 = mybir.dt.bfloat16
I32 = mybir.dt.int32
AF = mybir.ActivationFunctionType
ALU = mybir.AluOpType
AX = mybir.AxisListType

D = 1024
SEQ = 2048
CTX = 256
NTOK = SEQ + CTX
NT = NTOK // 128
NL = SEQ // 128
EPS = 1e-6
NE = 32
DE = 768
N_DSEM = 40
CAP = 512
NJ = CAP // 128
_LVL = int(os.environ.get('KLVL', '9'))
_KEV = os.environ.get('KEV', 'act')


class Reg:
    __slots__ = ("w", "r", "name")

    def __init__(self, name=""):
        self.w = None
        self.r = {}
        self.name = name


class Sem:
    def __init__(self, handle):
        self.handle = handle
        self.count = 0


class EngW:
    def __init__(self, name, eng, sem):
        self.name = name
        self.eng = eng
        self.sem = sem
        self.waited = {}


class KB:
    def __init__(self, nc, es):
        self.nc = nc
        self._es = es
        self.pslots = []
        self.regs = []
        mk = lambda n: Sem(es.enter_context(nc.semaphore(n)))
        self.E = {
            "pe": EngW("pe", nc.tensor, mk("s_pe")),
            "act": EngW("act", nc.scalar, mk("s_act")),
            "dve": EngW("dve", nc.vector, mk("s_dve")),
            "pool": EngW("pool", nc.gpsimd, mk("s_pool")),
            "sp": EngW("sp", nc.sync, mk("s_sp")),
        }
        self.dsems = [mk(f"s_d{i}") for i in range(N_DSEM)]
        self.dnext = 0
        self.psems = [mk(f"s_q{i}") for i in range(24)]
        self.pnext = 0

    def reg(self, name=""):
        r = Reg(name)
        self.regs.append(r)
        return r

    def regs_n(self, n, name=""):
        return [self.reg(f"{name}{i}") for i in range(n)]

    def wait(self, ew, tok):
        sem, val = tok
        if ew.waited.get(id(sem), 0) >= val:
            return
        ew.eng.wait_ge(sem.handle, val)
        ew.waited[id(sem)] = val

    def _deps(self, ew, reads, writes):
        for r in reads:
            if r.w is not None:
                if ew.name == "pe" and r.w[0] is ew.sem:
                    continue
                self.wait(ew, r.w)
        for w in writes:
            toks = list(w.r.values())
            if w.w is not None:
                toks.append(w.w)
            for t in toks:
                if ew.name == "pe" and t[0] is ew.sem:
                    continue
                self.wait(ew, t)

    def _record(self, tok, reads, writes):
        for r in reads:
            r.r[id(tok[0])] = tok
        for w in writes:
            w.w = tok
            w.r = {}

    def op(self, en, fn, reads=(), writes=()):
        ew = self.E[en]
        self._deps(ew, reads, writes)
        ins = fn(ew.eng)
        ew.sem.count += 1
        ins.then_inc(ew.sem.handle, 1)
        tok = (ew.sem, ew.sem.count)
        self._record(tok, reads, writes)
        return tok

    def dma(self, qn, out, in_, reads=(), writes=(), fn=None):
        ew = self.E[qn]
        self._deps(ew, reads, writes)
        d = self.dsems[self.dnext % N_DSEM]
        self.dnext += 1
        if d.count:
            self.wait(ew, (d, d.count))
        if fn is None:
            ins = ew.eng.dma_start(out=out, in_=in_)
        else:
            ins = fn(ew.eng)
        d.count += 16
        ins.then_inc(d.handle, 16)
        tok = (d, d.count)
        self._record(tok, reads, writes)
        return tok

    def pslot(self, name):
        return None

    def pdma(self, slot, out, in_, reads=(), writes=(), fn=None):
        ew = self.E["pool"]
        self._deps(ew, reads, writes)
        d = self.psems[self.pnext % len(self.psems)]
        self.pnext += 1
        if d.count:
            self.wait(ew, (d, d.count))
        ins = ew.eng.dma_start(out=out, in_=in_) if fn is None else fn(ew.eng)
        d.count += 16
        ins.then_inc(d.handle, 16)
        tok = (d, d.count)
        self._record(tok, reads, writes)
        return tok

    def barrier(self):
        for ew in self.E.values():
            for other in self.E.values():
                if other.sem.count and not (other is ew and ew.name in ("pe", "sp")):
                    self.wait(ew, (other.sem, other.sem.count))
            for d in self.dsems:
                if d.count:
                    self.wait(ew, (d, d.count))
            for d in self.psems:
                if d.count:
                    self.wait(ew, (d, d.count))
        for r in self.regs:
            r.w = None
            r.r = {}


def build_program(debug=False, stop_after=None):
    nc = bass.Bass("TRN2", target_bir_lowering=False)

    def din(name, shape, dt=F32):
        return nc.dram_tensor(name, list(shape), dt, kind="ExternalInput").ap()

    xin = din("xin", [NTOK, D])
    cT_d = din("cT", [128, 16])
    w_mod_d = din("w_mod", [D, 6 * D])
    bm_d = din("bm_b", [128, 6 * D])
    g1_d = din("g1_b", [128, D])
    g2_d = din("g2_b", [128, D])
    fg_d = din("fg_b", [128, D])
    w_in_d = din("w_in", [D, 2304])
    qg_d = din("qg_b", [128, 64])
    kg_d = din("kg_b", [128, 64])
    cw_d = din("cwT", [128, 12])
    gall_d = din("gallT", [128, 8])
    w_out_d = din("w_out", [D, D])
    wr_d = din("w_r", [D, 36])
    wg_d = din("w_gate", [NE, D, DE])
    wu_d = din("w_up", [NE, D, DE])
    wd_d = din("w_down", [NE, DE, D])
    identf_d = din("ident_f", [128, 128])
    cos_d = din("cosT", [128, NL, 32])
    sin_d = din("sinT", [128, NL, 32])
    bd64_d = din("bd64", [128, 128])
    c65_d = din("c65", [65, 64])
    e0_d = din("e0", [128, 1])
    ustr_d = din("ustr", [128, 128])
    eC_d = din("eC", [128, NE])

    y_d = nc.dram_tensor("y", [SEQ, D], F32, kind="ExternalOutput").ap()
    xnew_d = nc.dram_tensor("xnew_scr", [SEQ, D], F32,
                            kind="ExternalOutput" if debug else "Internal").ap()
    xbuf_d = nc.dram_tensor("xbuf_scr", [NE * CAP + 1, D], BF16, kind="Internal").ap()
    ybuf_d = nc.dram_tensor("ybuf_scr", [NE * CAP + 1, D], F32, kind="Internal").ap()
    hx2_d = nc.dram_tensor("hx2_scr", [SEQ, D], BF16, kind="Internal").ap()
    dbg = {}

    def dbg_out(name, shape, dt=F32):
        if debug:
            dbg[name] = nc.dram_tensor(name, list(shape), dt, kind="ExternalOutput").ap()
        return dbg.get(name)

    d_smod = dbg_out("d_smod", [128, 32])
    d_modb = dbg_out("d_modb", [128, 6 * D])
    d_QT = dbg_out("d_QT", [128, 4, SEQ], BF16)
    d_KT = dbg_out("d_KT", [128, 2, NTOK], BF16)
    d_V = dbg_out("d_V", [128, NT, 2, 65], BF16)
    d_convn = dbg_out("d_convn", [128, 4, SEQ], BF16)
    d_attn = dbg_out("d_attn", [64, 8, SEQ], BF16)
    d_wgt = dbg_out("d_wgt", [128, NL, NE])
    d_acc = dbg_out("d_acc", [128, NL, D])

    with ExitStack() as es:
        K = KB(nc, es)
        op, dma = K.op, K.dma

        def sb(stk, name, shape, dt):
            return stk.enter_context(nc.sbuf_tensor("sb_" + name, list(shape), dt))

        PSUM = es.enter_context(nc.psum_tensor("psum", [128, 8 * 512], F32))
        PS = [PSUM[:, i * 512:(i + 1) * 512] for i in range(8)]
        PSR = K.regs_n(8, "ps")

        identf = sb(es, "identf", [128, 128], F32)
        identb = sb(es, "identb", [128, 128], BF16)
        fgb = sb(es, "fgb", [128, D], F32)
        gt2b = sb(es, "gt2b", [128, D], F32)
        wgt = sb(es, "wgt", [128, NL, NE], F32)
        dest = sb(es, "dest", [128, NL, 2], I32)
        wsel = sb(es, "wsel", [128, NL, 2], F32)
        R_dest = K.regs_n(NL, "dest")
        R_const = K.reg("const")
        R_gt2 = K.reg("gt2")
        R_wgt = K.regs_n(NL, "wgt")

        dma("sp", identf[:], identf_d, writes=[R_const])
        dma("sp", fgb[:], fg_d, writes=[R_const])
        op("dve", lambda e: e.tensor_copy(identb[:], identf[:]), reads=[R_const], writes=[R_const])

        with ExitStack() as sAB:
            modb = sb(sAB, "modb", [128, 6 * D], F32)
            smod = sb(sAB, "smod", [128, 32], F32)
            cosT = sb(sAB, "cosT", [128, NL, 32], F32)
            sinT = sb(sAB, "sinT", [128, NL, 32], F32)
            qgb = sb(sAB, "qgb", [128, 64], F32)
            kgb = sb(sAB, "kgb", [128, 64], F32)
            cw = sb(sAB, "cw", [128, 12], F32)
            bd64 = sb(sAB, "bd64", [128, 128], F32)
            c65 = sb(sAB, "c65", [65, 64], F32)
            e0 = sb(sAB, "e0", [128, 1], F32)
            wr = sb(sAB, "wr", [128, 8, 36], F32)
            R_modb = K.regs_n(6, "modb")
            R_smod = K.reg("smod")
            for t_, d_ in ((cosT, cos_d), (sinT, sin_d), (qgb, qg_d), (kgb, kg_d), (cw, cw_d),
                           (bd64, bd64_d), (c65, c65_d), (e0, e0_d)):
                dma("sp", t_[:], d_, writes=[R_const])
            dma("sp", wr[:], wr_d.rearrange("(k p) n -> p k n", p=128), writes=[R_const])

            with ExitStack() as s0:
                cTt = sb(s0, "cTt", [128, 16], F32)
                scT = sb(s0, "scT", [128, 16], F32)
                lhs_c = sb(s0, "lhs_c", [128, 8, 128], BF16)
                lhs_x = sb(s0, "lhs_x", [128, 8, 128], BF16)
                bm = sb(s0, "bm", [128, 6 * D], F32)
                g1b = sb(s0, "g1b", [128, D], F32)
                g2b = sb(s0, "g2b", [128, D], F32)
                modx = sb(s0, "modx", [128, 2 * D], F32)
                wm = [sb(s0, f"wm{i}", [128, 8, 512], BF16) for i in range(2)]
                R_c = K.reg("c")
                R_bm = K.reg("bm")
                R_g = K.reg("g12")
                R_modx = K.regs_n(2, "modx")
                R_wm = K.regs_n(2, "wm")
                P_wm = [K.pslot(f"wm{i}") for i in range(2)]
                dma("sp", cTt[:], cT_d, writes=[R_c])
                dma("sp", bm[:], bm_d, writes=[R_bm])
                dma("sp", g1b[:], g1_d, writes=[R_g])
                dma("sp", g2b[:], g2_d, writes=[R_g])
                op("act", lambda e: e.activation(scT[:], cTt[:], AF.Silu), reads=[R_c], writes=[R_c])
                op("dve", lambda e: e.tensor_copy(
                    lhs_c[:], scT[:, 0:8].unsqueeze(2).to_broadcast([128, 8, 128])), reads=[R_c], writes=[R_c])
                op("dve", lambda e: e.tensor_copy(
                    lhs_x[:], scT[:, 8:16].unsqueeze(2).to_broadcast([128, 8, 128])), reads=[R_c], writes=[R_c])
                wmod_v = w_mod_d.rearrange("(k p) n -> p k n", p=128)
                for blk in range(12):
                    b = blk % 2
                    cs = slice(blk * 512, (blk + 1) * 512)
                    K.pdma(P_wm[b], wm[b][:], wmod_v[:, :, cs], writes=[R_wm[b]])
                    for k in range(8):
                        op("pe", lambda e, k=k: e.matmul(PS[b], lhs_c[:, k, :], wm[b][:, k, :],
                                                       start=(k == 0), stop=(k == 7)),
                           reads=[R_c, R_wm[b]], writes=[PSR[b]])
                    op("dve", lambda e: e.tensor_tensor(modb[:, cs], PS[b], bm[:, cs], ALU.add),
                       reads=[PSR[b], R_bm], writes=[R_modb[blk // 2]])
                    if blk < 4:
                        for k in range(8):
                            op("pe", lambda e, k=k: e.matmul(PS[2 + b], lhs_x[:, k, :], wm[b][:, k, :],
                                                           start=(k == 0), stop=(k == 7)),
                               reads=[R_c, R_wm[b]], writes=[PSR[2 + b]])
                        op("dve", lambda e: e.tensor_tensor(modx[:, cs], PS[2 + b], bm[:, cs], ALU.add),
                           reads=[PSR[2 + b], R_bm], writes=[R_modx[blk // 2]])
                op("dve", lambda e: e.scalar_tensor_tensor(modb[:, D:2 * D], modb[:, D:2 * D], 1.0, g1b[:],
                                                          ALU.add, ALU.mult),
                   reads=[R_modb[1], R_g], writes=[R_modb[1]])
                op("dve", lambda e: e.scalar_tensor_tensor(modx[:, D:2 * D], modx[:, D:2 * D], 1.0, g1b[:],
                                                          ALU.add, ALU.mult),
                   reads=[R_modx[1], R_g], writes=[R_modx[1]])
                op("dve", lambda e: e.scalar_tensor_tensor(modb[:, 4 * D:5 * D], modb[:, 4 * D:5 * D], 1.0, g2b[:],
                                                          ALU.add, ALU.mult),
                   reads=[R_modb[4], R_g], writes=[R_modb[4]])
                op("act", lambda e: e.activation(gt2b[:], modb[:, 5 * D:6 * D], AF.Copy),
                   reads=[R_modb[5]], writes=[R_gt2])
                srcs = [(modb, D, R_modb[1]), (modb, 0, R_modb[0]), (modx, D, R_modx[1]), (modx, 0, R_modx[0])]
                for vi, (tt, off, rr) in enumerate(srcs):
                    for k in range(8):
                        c0 = vi * 8 + k
                        op("pe", lambda e, tt=tt, off=off, k=k, c0=c0: e.matmul(
                            PS[4][:, c0:c0 + 1], tt[:, off + k * 128: off + (k + 1) * 128], e0[:, 0:1],
                            start=True, stop=True), reads=[rr, R_const], writes=[PSR[4]])
                op("act", lambda e: e.activation(smod[:], PS[4][:, 0:32], AF.Copy), reads=[PSR[4]], writes=[R_smod])
                if debug:
                    dma("sp", d_smod, smod[:], reads=[R_smod])
                    dma("sp", d_modb, modb[:], reads=R_modb)
                K.barrier()
                if stop_after == "0":
                    return nc, list(dbg.keys())

            with ExitStack() as sB:
                QT = sb(sB, "QT", [128, 4, SEQ], BF16)
                KT = sb(sB, "KT", [128, 2, 2, NTOK], BF16)
                VE = sb(sB, "VE", [128, NT, 2, 128], BF16)
                convn = sb(sB, "convn", [128, 4, SEQ], BF16)
                R_QT = K.regs_n(NL, "QT")
                R_KT = K.regs_n(NT, "KT")
                R_VE = K.regs_n(NT, "VE")
                R_convn = K.regs_n(4, "convn")
                R_ve1 = K.reg("ve1")
                if _LVL >= 0:
                    op("dve", lambda e: e.memset(VE[:], 0.0), writes=[R_ve1])
                    op("dve", lambda e: e.memset(VE[:, :, :, 64:65], 1.0), writes=[R_ve1])
                    op("dve", lambda e: e.memset(KT[:], 0.0), writes=[R_ve1])

                with ExitStack() as sA:
                    hT = sb(sA, "hT", [128, 8, NTOK], BF16)
                    R_hTd = K.regs_n(NT, "hTd")
                    R_hTa = K.regs_n(NT, "hTa")
                    with ExitStack() as sA1:
                        winq = sb(sA1, "winq", [128, 8, 768], BF16)
                        R_winq = K.reg("winq")
                        if _LVL >= 1:
                            K.pdma(K.pslot("winq"), winq[:], w_in_d.rearrange("(k p) n -> p k n", p=128)[:, :, 0:768],
                                   writes=[R_winq])
                        xt = [sb(sA1, f"xt{i}", [128, D], F32) for i in range(2)]
                        junk = sb(sA1, "junk", [128, D], BF16)
                        xn = [sb(sA1, f"xn{i}", [128, D], BF16) for i in range(2)]
                        st1 = sb(sA1, "st1", [128, 3, NT], F32)
                        zq = [sb(sA1, f"zq{i}", [128, 512], F32) for i in range(2)]
                        zkv = [sb(sA1, f"zkv{i}", [128, 256], F32) for i in range(2)]
                        sq = sb(sA1, "sq", [128, 512], F32)
                        qn = sb(sA1, "qn", [128, 512], F32)
                        kn = sb(sA1, "kn", [128, 128], F32)
                        knb = sb(sA1, "knb", [128, 128], BF16)
                        ta = sb(sA1, "ta", [128, 256], F32)
                        tb = sb(sA1, "tb", [128, 256], F32)
                        qst = sb(sA1, "qst", [128, 3, 16], F32)
                        qkb = [sb(sA1, f"qkb{i}", [128, 768], BF16) for i in range(2)]
                        R_xt = K.regs_n(2, "xt")
                        R_xn = K.regs_n(2, "xn")
                        R_st1 = K.regs_n(NT, "st1")
                        R_zq = K.regs_n(2, "zq")
                        R_zkv = K.regs_n(2, "zkv")
                        R_tmp = K.reg("tmpq")
                        R_qst = K.reg("qst")
                        R_qkb = K.regs_n(2, "qkb")
                        R_knb = K.reg("knb")
                        def a1_front(T):
                            b = T % 2
                            lat = T >= 2
                            t = T - 2
                            tsl = slice(T * 128, (T + 1) * 128)
                            dma("sp", xt[b][:], xin[tsl, :], writes=[R_xt[b]])
                            if _LVL < -4:
                                return
                            op("act", lambda e: e.activation(junk[:], xt[b][:], AF.Square,
                                                             accum_out=st1[:, 0, T:T + 1]),
                               reads=[R_xt[b]], writes=[R_st1[T]])
                            if _LVL < -3:
                                return
                            op("act", lambda e: e.activation(st1[:, 1, T:T + 1], st1[:, 0, T:T + 1], AF.Ln,
                                                             scale=1.0 / D, bias=EPS),
                               reads=[R_st1[T]], writes=[R_st1[T]])
                            op("act", lambda e: e.activation(st1[:, 2, T:T + 1], st1[:, 1, T:T + 1], AF.Exp,
                                                             scale=-0.5),
                               reads=[R_st1[T]], writes=[R_st1[T]])
                            op("act", lambda e: e.activation(xn[b][:], xt[b][:], AF.Copy, scale=st1[:, 2, T:T + 1]),
                               reads=[R_xt[b], R_st1[T]], writes=[R_xn[b]])
                            if _LVL < -2:
                                return
                            psb = PS[b].bitcast(BF16)
                            for k in range(8):
                                op("pe", lambda e, k=k: e.transpose(psb[:, k * 128:(k + 1) * 128],
                                                                    xn[b][:, k * 128:(k + 1) * 128], identb[:]),
                                   reads=[R_xn[b], R_const], writes=[PSR[b]])
                            if _LVL < -1:
                                return
                            so = 0 if lat else 16
                            for k in range(8):
                                if (b == 0 and _KEV != 'act') or _KEV == 'dve':
                                    op("dve", lambda e, k=k: e.tensor_scalar(
                                        hT[:, k, tsl], psb[:, k * 128:(k + 1) * 128],
                                        smod[:, so + k:so + k + 1], smod[:, so + 8 + k:so + 9 + k],
                                        ALU.mult, ALU.add), reads=[PSR[b], R_smod], writes=[R_hTd[T]])
                                else:
                                    op("act", lambda e, k=k: e.activation(
                                        hT[:, k, tsl], psb[:, k * 128:(k + 1) * 128], AF.Identity,
                                        bias=smod[:, so + 8 + k:so + 9 + k], scale=smod[:, so + k:so + k + 1]),
                                       reads=[PSR[b], R_smod], writes=[R_hTa[T]])
                            if _LVL < 1:
                                return
                            if lat:
                                for k in range(8):
                                    op("pe", lambda e, k=k: e.matmul(PS[2], hT[:, k, tsl], winq[:, k, 0:512],
                                                                   start=(k == 0), stop=(k == 7)),
                                       reads=[R_hTd[T], R_hTa[T], R_winq], writes=[PSR[2]])
                            for k in range(8):
                                op("pe", lambda e, k=k: e.matmul(PS[3][:, 0:256], hT[:, k, tsl], winq[:, k, 512:768],
                                                               start=(k == 0), stop=(k == 7)),
                                   reads=[R_hTd[T], R_hTa[T], R_winq], writes=[PSR[3]])

                        def a1_front_b(T):
                            b = T % 2
                            lat = T >= 2
                            t = T - 2
                            tsl = slice(T * 128, (T + 1) * 128)
                            if lat:
                                op("act", lambda e: e.activation(zq[b][:], PS[2], AF.Copy),
                                   reads=[PSR[2]], writes=[R_zq[b]])
                            op("act", lambda e: e.activation(zkv[b][:], PS[3][:, 0:256], AF.Copy),
                               reads=[PSR[3]], writes=[R_zkv[b]])
                            op("act", lambda e: e.activation(
                                VE[:, T, :, 0:64], zkv[b][:, 128:256].rearrange("p (g d) -> p g d", g=2), AF.Copy),
                               reads=[R_zkv[b], R_ve1], writes=[R_VE[T]])

                        def a1_back(T, part):
                            b = T % 2
                            lat = T >= 2
                            t = T - 2
                            tsl = slice(T * 128, (T + 1) * 128)
                            nh_list = ([("q", zq[b], 8, qn, qgb, R_zq[b])] if lat else []) + \
                                      [("k", zkv[b], 2, kn, kgb, R_zkv[b])]
                            for nm, src, nh, dst, gb_, rsrc in nh_list:
                                W = nh * 64
                                c0 = 0 if nm == "q" else 8
                                op("dve", lambda e: e.tensor_tensor(sq[:, 0:W], src[:, 0:W], src[:, 0:W], ALU.mult),
                                   reads=[rsrc], writes=[R_tmp])
                                op("dve", lambda e: e.tensor_reduce(
                                    qst[:, 0, c0:c0 + nh], sq[:, 0:W].rearrange("p (h d) -> p h d", h=nh),
                                    AX.X, ALU.add), reads=[R_tmp], writes=[R_qst])
                            lo = 0 if lat else 8
                            op("act", lambda e: e.activation(qst[:, 1, lo:10], qst[:, 0, lo:10],
                                                             AF.Ln, scale=1.0 / 64, bias=EPS),
                               reads=[R_qst], writes=[R_qst])
                            op("act", lambda e: e.activation(qst[:, 2, lo:10], qst[:, 1, lo:10],
                                                             AF.Exp, scale=-0.5),
                               reads=[R_qst], writes=[R_qst])

                        def a1_back_rest(T):
                            b = T % 2
                            lat = T >= 2
                            t = T - 2
                            tsl = slice(T * 128, (T + 1) * 128)
                            nh_list = ([("q", zq[b], 8, qn, qgb, R_zq[b])] if lat else []) + \
                                      [("k", zkv[b], 2, kn, kgb, R_zkv[b])]
                            for nm, src, nh, dst, gb_, rsrc in nh_list:
                                W = nh * 64
                                c0 = 0 if nm == "q" else 8
                                d3 = dst[:, 0:W].rearrange("p (h d) -> p h d", h=nh)
                                op("dve", lambda e: e.tensor_tensor(
                                    d3, src[:, 0:W].rearrange("p (h d) -> p h d", h=nh),
                                    qst[:, 2, c0:c0 + nh].unsqueeze(2).to_broadcast([128, nh, 64]), ALU.mult),
                                   reads=[rsrc, R_qst], writes=[R_tmp])
                                op("dve", lambda e: e.tensor_tensor(
                                    d3, d3, gb_[:, :].unsqueeze(1).to_broadcast([128, nh, 64]), ALU.mult),
                                   reads=[R_tmp, R_const], writes=[R_tmp])
                                if nm == "q":
                                    outb = qkb[b][:, 0:512]
                                    R_ob = R_qkb[b]
                                else:
                                    outb = knb[:, 0:128]
                                    R_ob = R_knb
                                if lat:
                                    d5 = dst[:, 0:W].rearrange("p (h a f d) -> p h a f d", h=nh, a=2, f=2)
                                    o5 = outb.rearrange("p (h a f d) -> p h a f d", h=nh, a=2, f=2)
                                    x1 = d5[:, :, :, 0, :]
                                    x2 = d5[:, :, :, 1, :]
                                    cb = cosT[:, t, :].rearrange("p (a d) -> p a d", a=2).unsqueeze(1) \
                                        .to_broadcast([128, nh, 2, 16])
                                    sb_ = sinT[:, t, :].rearrange("p (a d) -> p a d", a=2).unsqueeze(1) \
                                        .to_broadcast([128, nh, 2, 16])
                                    hw = nh * 32
                                    ta4 = ta[:, 0:hw].rearrange("p (h a d) -> p h a d", h=nh, a=2)
                                    tb4 = tb[:, 0:hw].rearrange("p (h a d) -> p h a d", h=nh, a=2)
                                    op("dve", lambda e: e.tensor_tensor(ta4, x1, cb, ALU.mult),
                                       reads=[R_tmp, R_const], writes=[R_tmp])
                                    op("dve", lambda e: e.tensor_tensor(tb4, x2, sb_, ALU.mult),
                                       reads=[R_tmp, R_const], writes=[R_tmp])
                                    op("dve", lambda e: e.tensor_tensor(o5[:, :, :, 0, :], ta4, tb4, ALU.subtract),
                                       reads=[R_tmp], writes=[R_ob])
                                    op("dve", lambda e: e.tensor_tensor(ta4, x1, sb_, ALU.mult),
                                       reads=[R_tmp, R_const], writes=[R_tmp])
                                    op("dve", lambda e: e.tensor_tensor(tb4, x2, cb, ALU.mult),
                                       reads=[R_tmp, R_const], writes=[R_tmp])
                                    op("dve", lambda e: e.tensor_tensor(o5[:, :, :, 1, :], ta4, tb4, ALU.add),
                                       reads=[R_tmp], writes=[R_ob])
                                else:
                                    op("dve", lambda e: e.tensor_copy(outb, dst[:, 0:W]),
                                       reads=[R_tmp], writes=[R_ob])
                            if _LVL < 4:
                                return
                            op("dve", lambda e: e.tensor_copy(
                                qkb[b][:, 512:768].rearrange("p (g r d) -> p g r d", g=2, r=2),
                                knb[:, 0:128].rearrange("p (g d) -> p g d", g=2).unsqueeze(2)
                                .to_broadcast([128, 2, 2, 64])), reads=[R_knb], writes=[R_qkb[b]])
                            if _LVL < 5:
                                return
                            ps4 = PS[4].bitcast(BF16)
                            ps5 = PS[5].bitcast(BF16)
                            if lat:
                                for j in range(4):
                                    op("pe", lambda e, j=j: e.transpose(ps4[:, j * 128:(j + 1) * 128],
                                                                        qkb[b][:, j * 128:(j + 1) * 128], identb[:]),
                                       reads=[R_qkb[b], R_const], writes=[PSR[4]])
                                op("dve", lambda e: e.tensor_copy(
                                    QT[:, :, t * 128:(t + 1) * 128],
                                    ps4[:, 0:512].rearrange("p (j n) -> p j n", j=4)),
                                   reads=[PSR[4]], writes=[R_QT[t]])
                            for j in range(2):
                                op("pe", lambda e, j=j: e.transpose(ps5[:, j * 128:(j + 1) * 128],
                                                                    qkb[b][:, 512 + j * 128:512 + (j + 1) * 128],
                                                                    identb[:]),
                                   reads=[R_qkb[b], R_const], writes=[PSR[5]])
                            for u in range(2):
                                op("dve", lambda e, u=u: e.tensor_copy(
                                    KT[64 * u:64 * u + 64, :, u, tsl],
                                    ps5[64 * u:64 * u + 64, 0:256].rearrange("p (j n) -> p j n", j=2)),
                                   reads=[PSR[5], R_ve1], writes=[R_KT[T]])

                        a1_front(0)
                        a1_front_b(0)
                        for T in range(NT):
                            if T + 1 < NT:
                                a1_front(T + 1)
                            a1_back(T, "stats")
                            if T + 1 < NT:
                                a1_front_b(T + 1)
                            a1_back_rest(T)
                        K.barrier()
                        if stop_after == "A1":
                            if debug:
                                dma("sp", d_QT, QT[:]); dma("sp", d_KT[0:64], KT[0:64, :, 0, :]); dma("sp", d_KT[64:128], KT[64:128, :, 1, :]); dma("sp", d_V, VE[:, :, :, 0:65])
                            K.barrier()
                            return nc, list(dbg.keys())
                    with ExitStack() as sA2:
                        wc = [sb(sA2, f"wc{i}", [128, 8, 384], BF16) for i in range(2)]
                        vT = [sb(sA2, f"vT{i}", [128, SEQ + 2], F32) for i in range(2)]
                        gbT = [sb(sA2, f"gbT{i}", [128, SEQ], F32) for i in range(2)]
                        gct = [sb(sA2, f"gct{i}", [128, 512], F32) for i in range(2)]
                        yb = sb(sA2, "yb", [128, 1024], F32)
                        ysq = sb(sA2, "ysq", [128, 1024], F32)
                        rs = sb(sA2, "rs", [128, 1024], F32)
                        R_wc = K.regs_n(2, "wc")
                        P_wc = [[K.pslot(f"wc{i}_{j}") for j in range(3)] for i in range(2)]
                        R_vT = K.regs_n(2, "vT")
                        R_gbT = K.regs_n(2, "gbT")
                        R_gct = K.regs_n(2, "gct")
                        R_y = K.reg("y")
                        R_ysq = K.reg("ysq")
                        R_rs = K.reg("rs")
                        winv = w_in_d.rearrange("(k p) n -> p k n", p=128)
                        for i in range(2):
                            op("dve", lambda e, i=i: e.memset(vT[i][:, 0:1], 0.0), writes=[R_vT[i]])
                            op("dve", lambda e, i=i: e.memset(vT[i][:, SEQ + 1:SEQ + 2], 0.0), writes=[R_vT[i]])
                        cnt2 = {'gi': 0, 'pi': 0}
                        def a2_mm(c4):
                            b = c4 % 2
                            for s3 in range(3):
                                c0 = 768 + s3 * 512 + c4 * 128
                                K.pdma(P_wc[b][s3], wc[b][:, :, s3 * 128:(s3 + 1) * 128], winv[:, :, c0:c0 + 128],
                                       writes=[R_wc[b]])
                            for tb_ in range(4):
                                tok = slice(256 + tb_ * 512, 256 + (tb_ + 1) * 512)
                                osl = slice(tb_ * 512, (tb_ + 1) * 512)
                                g_ = cnt2['gi'] % 2
                                cnt2['gi'] += 1
                                for s3 in range(3):
                                    pb = 5 + cnt2['pi'] % 3
                                    cnt2['pi'] += 1
                                    for k in range(8):
                                        op("pe", lambda e, k=k, s3=s3, pb=pb: e.matmul(
                                            PS[pb], wc[b][:, k, s3 * 128:(s3 + 1) * 128], hT[:, k, tok],
                                            start=(k == 0), stop=(k == 7)),
                                           reads=[R_wc[b]], writes=[PSR[pb]])
                                    if s3 == 0:
                                        op("act", lambda e, pb=pb: e.activation(gbT[b][:, osl], PS[pb], AF.Copy),
                                           reads=[PSR[pb]], writes=[R_gbT[b]])
                                    elif s3 == 1:
                                        op("act", lambda e, pb=pb: e.activation(gct[g_][:], PS[pb], AF.Copy),
                                           reads=[PSR[pb]], writes=[R_gct[g_]])
                                    else:
                                        op("dve", lambda e, pb=pb: e.tensor_tensor(
                                            vT[b][:, 1 + tb_ * 512:1 + (tb_ + 1) * 512], PS[pb], gct[g_][:], ALU.mult),
                                           reads=[PSR[pb], R_gct[g_]], writes=[R_vT[b]])

                        def a2_fin(c4):
                            b = c4 % 2
                            for hf in range(2):
                                o = hf * 1024
                                op("dve", lambda e: e.tensor_scalar(yb[:], vT[b][:, o:o + 1024],
                                                                    cw[:, c4 * 3:c4 * 3 + 1], None, ALU.mult),
                                   reads=[R_vT[b], R_const], writes=[R_y])
                                op("dve", lambda e: e.scalar_tensor_tensor(
                                    yb[:], vT[b][:, o + 1:o + 1025], cw[:, c4 * 3 + 1:c4 * 3 + 2], yb[:],
                                    ALU.mult, ALU.add), reads=[R_vT[b], R_const, R_y], writes=[R_y])
                                op("dve", lambda e: e.scalar_tensor_tensor(
                                    yb[:], vT[b][:, o + 2:o + 1026], cw[:, c4 * 3 + 2:c4 * 3 + 3], yb[:],
                                    ALU.mult, ALU.add), reads=[R_vT[b], R_const, R_y], writes=[R_y])
                                op("dve", lambda e: e.tensor_tensor(yb[:], yb[:], gbT[b][:, o:o + 1024], ALU.mult),
                                   reads=[R_y, R_gbT[b]], writes=[R_y])
                                op("act", lambda e: e.activation(ysq[:], yb[:], AF.Square),
                                   reads=[R_y], writes=[R_ysq])
                                for q2 in range(2):
                                    op("pe", lambda e, q2=q2: e.matmul(PS[q2], bd64[:], ysq[:, q2 * 512:(q2 + 1) * 512],
                                                                     start=True, stop=True),
                                       reads=[R_ysq, R_const], writes=[PSR[q2]])
                                    op("act", lambda e, q2=q2: e.activation(rs[:, q2 * 512:(q2 + 1) * 512], PS[q2],
                                                                            AF.Ln, bias=EPS),
                                       reads=[PSR[q2]], writes=[R_rs])
                                op("act", lambda e: e.activation(rs[:], rs[:], AF.Exp, scale=-0.5),
                                   reads=[R_rs], writes=[R_rs])
                                op("dve", lambda e: e.tensor_tensor(convn[:, c4, o:o + 1024], yb[:], rs[:], ALU.mult),
                                   reads=[R_y, R_rs], writes=[R_convn[c4]])

                        a2_mm(0)
                        for c4 in range(4):
                            if c4 + 1 < 4:
                                a2_mm(c4 + 1)
                            a2_fin(c4)
                        K.barrier()
                if debug:
                    dma("sp", d_QT, QT[:], reads=R_QT)
                    dma("sp", d_KT[0:64], KT[0:64, :, 0, :], reads=R_KT)
                    dma("sp", d_KT[64:128], KT[64:128, :, 1, :], reads=R_KT)
                    dma("sp", d_V, VE[:, :, :, 0:65], reads=R_VE)
                    dma("sp", d_convn, convn[:], reads=R_convn)
                if stop_after == "A2":
                    K.barrier()
                    return nc, list(dbg.keys())

                with ExitStack() as sB2:
                    attnT = sb(sB2, "attnT", [128, 4, SEQ], BF16)
                    R_attn = K.regs_n(8, "attn")
                    woall = sb(sB2, "woall", [128, 8, D], BF16)
                    gall = sb(sB2, "gall", [128, 8], F32)
                    R_wst = K.reg("wst")
                    R_wo = K.reg("wo")
                    with ExitStack() as sBa:
                        PT = [sb(sBa, f"PT{i}", [128, 1024], BF16) for i in range(3)]
                        sqb = [sb(sBa, f"sqb{i}", [65, 512], F32) for i in range(2)]
                        lnb = [sb(sBa, f"lnb{i}", [64, 512], F32) for i in range(2)]
                        R_PT = K.regs_n(3, "PT")
                        R_sqb = K.regs_n(2, "sqb")
                        R_lnb = K.regs_n(2, "lnb")
                        wst = sb(sBa, "wst", [128, 4, D], F32)
                        dma("sp", gall[:], gall_d, writes=[R_wo])
                        wov = w_out_d.rearrange("(c p) n -> p c n", p=128)
                        for half in range(2):
                            dma("sp", wst[:], wov[:, half * 4:(half + 1) * 4, :], writes=[R_wst])
                            op("dve", lambda e: e.tensor_tensor(
                                woall[:, half * 4:(half + 1) * 4, :], wst[:],
                                gall[:, half * 4:(half + 1) * 4].unsqueeze(2).to_broadcast([128, 4, D]), ALU.mult),
                               reads=[R_wst, R_wo], writes=[R_wo])
                        zt = sb(sBa, "zt", [128, 8, D], BF16)
                        R_zt = K.reg("zt")
                        op("dve", lambda e: e.memset(zt[:], 0.0), writes=[R_zt])
                        for i in range(NE * CAP // 1024):
                            dma("sp", xbuf_d[i * 1024:(i + 1) * 1024, :].rearrange("(p j) d -> p j d", j=8), zt[:],
                                reads=[R_zt])
                        dma("sp", xbuf_d[NE * CAP:NE * CAP + 1, :], zt[0:1, 0, :], reads=[R_zt])
                        it = 0
                        pti = 0
                        spi = 0
                        for j in range(4):
                            g = j // 2
                            for qb in range(4):
                                qs = slice(qb * 512, (qb + 1) * 512)
                                obase = 4 + 2 * (it % 2)
                                it += 1
                                Oab = [PS[obase], PS[obase + 1]]

                                def s_step(kt, sp):
                                    for u in range(2):
                                        p0 = 64 * u
                                        bk = 2 * sp + u
                                        op("pe", lambda e: e.matmul(
                                            PS[bk], KT[:, g, u, kt * 128:(kt + 1) * 128],
                                            QT[:, j, qs], start=True, stop=True),
                                           writes=[PSR[2 * sp], PSR[2 * sp + 1]] if u == 0 else [PSR[bk]])
                                sp_of = {0: spi % 2}
                                spi += 1
                                s_step(0, sp_of[0])
                                for kt in range(NT):
                                    if kt + 1 < NT:
                                        sp_of[kt + 1] = spi % 2
                                        spi += 1
                                        s_step(kt + 1, sp_of[kt + 1])
                                    sp = sp_of[kt]
                                    pb_ = pti % 3
                                    pti += 1
                                    op("act", lambda e: e.activation(
                                        PT[pb_][:], PSUM[:, 2 * sp * 512:(2 * sp + 2) * 512], AF.Exp, scale=0.125),
                                       reads=[PSR[2 * sp], PSR[2 * sp + 1]], writes=[R_PT[pb_]])
                                    for u in range(2):
                                        op("pe", lambda e: e.matmul(Oab[u][:, :], VE[:, kt, g, :],
                                                                    PT[pb_][:, u * 512:(u + 1) * 512],
                                                                    start=(kt == 0), stop=(kt == NT - 1)),
                                           reads=[R_PT[pb_]], writes=[PSR[obase + u]])
                                sp = spi % 2
                                spi += 1
                                for u in range(2):
                                    op("act", lambda e: e.activation(sqb[u][:], Oab[u][0:65, :], AF.Square),
                                       reads=[PSR[obase + u]], writes=[R_sqb[u]])
                                    op("pe", lambda e: e.matmul(PS[2 * sp + u][0:64, :], c65[:, :], sqb[u][:],
                                                                start=True, stop=True),
                                       reads=[R_sqb[u], R_const], writes=[PSR[2 * sp + u]])
                                for u in range(2):
                                    op("act", lambda e: e.activation(lnb[u][:], PS[2 * sp + u][0:64, :], AF.Ln),
                                       reads=[PSR[2 * sp + u]], writes=[R_lnb[u]])
                                    op("act", lambda e: e.activation(lnb[u][:], lnb[u][:], AF.Exp, scale=-0.5),
                                       reads=[R_lnb[u]], writes=[R_lnb[u]])
                                    op("dve", lambda e: e.tensor_tensor(attnT[64 * u:64 * u + 64, j, qs], Oab[u][0:64, :],
                                                                        lnb[u][:], ALU.mult),
                                       reads=[PSR[obase + u], R_lnb[u]], writes=[R_attn[2 * j + u]])
                        K.barrier()
                    if debug:
                        for u in range(2):
                            dma("sp", d_attn.rearrange("d (j u) s -> d u j s", u=2)[:, u], attnT[64 * u:64 * u + 64, :, :],
                                reads=R_attn)
                    if stop_after == "B":
                        K.barrier()
                        return nc, list(dbg.keys())

                    if True:
                        with ExitStack() as sB3:
                            xr = [sb(sB3, "xr0", [128, D], F32)] * 2
                            xnw = [sb(sB3, "xnw0", [128, D], F32)] * 2
                            hx = [sb(sB3, f"hx{i}", [128, D], F32) for i in range(2)]
                            hxT = [sb(sB3, "hxT0", [128, 8, 128], F32)] * 2
                            st2 = sb(sB3, "st2", [128, 3, NL], F32)
                            lg = sb(sB3, "lg", [128, NL, 36], F32)
                            rt = sb(sB3, "rt", [128, 64], F32)
                            RB = sb(sB3, "RB", [128, 16, NL, 8], F32)
                            m8 = sb(sB3, "m8", [128, 8], F32)
                            ustr = sb(sB3, "ustr", [128, 128], BF16)
                            onesb = sb(sB3, "onesb", [128, 128], BF16)
                            ustf = sb(sB3, "ustf", [128, 128], F32)
                            eC = sb(sB3, "eC", [128, NE], F32)
                            maskb = sb(sB3, "maskb", [128, NL, NE], BF16)
                            mkk = sb(sB3, "mkk", [128, 2, NL, NE], F32)
                            rkk = sb(sB3, "rkk", [128, 2, NL, NE], F32)
                            t3 = sb(sB3, "t3", [128, NL, NE], F32)
                            dfa = sb(sB3, "dfa", [128, 3, 2, NL], F32)
                            hxb = [sb(sB3, f"hxb{i}", [128, D], BF16) for i in range(2)]
                            R_mask = K.regs_n(NL, "mask")
                            R_rk = K.reg("rk")
                            R_hxb = K.regs_n(2, "hxb")
                            R_xbuf = K.reg("xbuf")
                            dma("sp", ustf[:], ustr_d, writes=[R_wo])
                            dma("sp", eC[:], eC_d, writes=[R_wo])
                            op("dve", lambda e: e.tensor_copy(ustr[:], ustf[:]), reads=[R_wo], writes=[R_wo])
                            op("dve", lambda e: e.memset(onesb[:], 1.0), writes=[R_wo])
                            R_xr = [K.reg("xr")] * 2
                            R_xnw = [K.reg("xnw")] * 2
                            R_hx = K.regs_n(2, "hx")
                            R_hxT = [K.reg("hxT")] * 2
                            R_st2 = K.regs_n(NL, "st2")
                            R_lg = K.reg("lg")
                            R_rt = K.reg("rt")
                            def b2_front(t):
                                b = t % 2
                                tsl = slice(t * 128, (t + 1) * 128)
                                dma("sp", xr[b][:], xin[256 + t * 128:256 + (t + 1) * 128, :], writes=[R_xr[b]])
                                for hf in range(2):
                                    pb = hf + 6 * (t % 2)
                                    cs = slice(hf * 512, (hf + 1) * 512)
                                    for c in range(4):
                                        op("pe", lambda e, c=c: e.matmul(PS[pb], attnT[:, c, tsl], woall[:, c, cs],
                                                                       start=(c == 0), stop=False),
                                           reads=[R_wo], writes=[PSR[pb]])
                                    for c4 in range(4):
                                        op("pe", lambda e, c4=c4: e.matmul(PS[pb], convn[:, c4, tsl], woall[:, 4 + c4, cs],
                                                                         start=False, stop=(c4 == 3)),
                                           reads=[R_wo], writes=[PSR[pb]])
                                    op("dve", lambda e: e.tensor_tensor(xnw[b][:, cs], PS[pb], modb[:, 2 * D + hf * 512:
                                                                                                  2 * D + (hf + 1) * 512],
                                                                        ALU.mult),
                                       reads=[PSR[pb]], writes=[R_xnw[b]])
                                op("dve", lambda e: e.tensor_tensor(xnw[b][:], xnw[b][:], xr[b][:], ALU.add),
                                   reads=[R_xnw[b], R_xr[b]], writes=[R_xnw[b]])
                                dma("sp", xnew_d[tsl, :], xnw[b][:], reads=[R_xnw[b]])
                                op("act", lambda e: e.activation(hxb[b][:], xnw[b][:], AF.Square,
                                                                 accum_out=st2[:, 0, t:t + 1]),
                                   reads=[R_xnw[b]], writes=[R_st2[t], R_hxb[b]])
                                op("act", lambda e: e.activation(st2[:, 1, t:t + 1], st2[:, 0, t:t + 1], AF.Ln,
                                                                 scale=1.0 / D, bias=EPS),
                                   reads=[R_st2[t]], writes=[R_st2[t]])
                                op("act", lambda e: e.activation(st2[:, 2, t:t + 1], st2[:, 1, t:t + 1], AF.Exp,
                                                                 scale=-0.5),
                                   reads=[R_st2[t]], writes=[R_st2[t]])
                                op("dve", lambda e: e.scalar_tensor_tensor(
                                    hx[b][:], xnw[b][:], st2[:, 2, t:t + 1], modb[:, 4 * D:5 * D], ALU.mult, ALU.mult),
                                   reads=[R_xnw[b], R_st2[t]], writes=[R_hx[b]])
                                op("dve", lambda e: e.tensor_tensor(hx[b][:], hx[b][:], modb[:, 3 * D:4 * D], ALU.add),
                                   reads=[R_hx[b]], writes=[R_hx[b]])

                            def b2_back(t):
                                b = t % 2
                                tsl = slice(t * 128, (t + 1) * 128)
                                PP = PSUM[:, 2 * 512:4 * 512]
                                for k in range(8):
                                    op("pe", lambda e, k=k: e.transpose(PP[:, k * 128:(k + 1) * 128],
                                                                        hx[b][:, k * 128:(k + 1) * 128], identf[:]),
                                       reads=[R_hx[b], R_const], writes=[PSR[2], PSR[3]])
                                op("act", lambda e: e.activation(
                                    hxT[b][:], PP.rearrange("p (k n) -> p k n", k=8), AF.Copy),
                                   reads=[PSR[2], PSR[3]], writes=[R_hxT[b]])
                                op("act", lambda e: e.activation(hxb[b][:], hx[b][:], AF.Copy),
                                   reads=[R_hx[b]], writes=[R_hxb[b]])
                                for k in range(8):
                                    op("pe", lambda e, k=k: e.matmul(PS[4][:, 0:36], hxT[b][:, k, :], wr[:, k, :],
                                                                   start=(k == 0), stop=(k == 7)),
                                       reads=[R_hxT[b], R_const], writes=[PSR[4]])
                                op("act", lambda e: e.activation(lg[:, t, :], PS[4][:, 0:36], AF.Copy),
                                   reads=[PSR[4]], writes=[R_lg])
                                dma("sp", hx2_d[tsl, :], hxb[b][:], reads=[R_hxb[b]], writes=[R_xbuf])

                            b2_front(0)
                            for t in range(NL):
                                if t + 1 < NL:
                                    b2_front(t + 1)
                                b2_back(t)
                            def slab(i, w=8):
                                return RB[:, i, :, 0:w]

                            def col(i):
                                return RB[:, i, :, 0]

                            def bc(a2, w):
                                return a2.unsqueeze(2).to_broadcast([128, NL, w])
                            R_B = K.reg("RB")

                            def dv(fn, rd=(), wr_=()):
                                op("dve", fn, reads=[R_lg, R_B] + list(rd), writes=[R_B] + list(wr_))

                            def ac(fn):
                                op("act", fn, reads=[R_lg, R_B], writes=[R_B])
                            G4 = lg[:, :, 0:4]
                            dv(lambda e: e.tensor_reduce(col(0), G4, AX.X, ALU.max))
                            dv(lambda e: e.tensor_tensor(slab(1, 4), G4, bc(col(0), 4), ALU.is_equal))
                            dv(lambda e: e.tensor_tensor(slab(2, 4), G4, bc(col(0), 4), ALU.subtract))
                            ac(lambda e: e.activation(slab(2, 4), slab(2, 4), AF.Exp))
                            dv(lambda e: e.tensor_reduce(col(3), slab(2, 4), AX.X, ALU.add))
                            dv(lambda e: e.reciprocal(col(4), col(3)))
                            dv(lambda e: e.tensor_tensor(slab(5), lg[:, :, 4:12], bc(RB[:, 1, :, 0], 8), ALU.mult))
                            for gq in range(1, 4):
                                dv(lambda e, gq=gq: e.tensor_tensor(slab(6), lg[:, :, 4 + 8 * gq:12 + 8 * gq],
                                                                    bc(RB[:, 1, :, gq], 8), ALU.mult))
                                dv(lambda e: e.tensor_tensor(slab(5), slab(5), slab(6), ALU.add))
                            dv(lambda e: e.tensor_reduce(col(7), slab(5), AX.X, ALU.max))
                            dv(lambda e: e.tensor_tensor(slab(8), slab(5), bc(col(7), 8), ALU.is_equal))
                            dv(lambda e: e.scalar_tensor_tensor(slab(9), slab(8), -1.0e30, slab(5), ALU.mult, ALU.add))
                            dv(lambda e: e.tensor_reduce(col(10), slab(9), AX.X, ALU.max))
                            dv(lambda e: e.tensor_tensor(slab(11), slab(5), bc(col(10), 8), ALU.is_ge))
                            dv(lambda e: e.tensor_tensor(slab(12), slab(11), slab(8), ALU.subtract))
                            dv(lambda e: e.tensor_tensor(slab(13), slab(5), bc(col(7), 8), ALU.subtract))
                            ac(lambda e: e.activation(slab(13), slab(13), AF.Exp))
                            dv(lambda e: e.tensor_tensor(slab(13), slab(13), slab(11), ALU.mult))
                            dv(lambda e: e.tensor_reduce(col(14), slab(13), AX.X, ALU.add))
                            dv(lambda e: e.reciprocal(col(15), col(14)))
                            dv(lambda e: e.tensor_tensor(col(15), col(15), col(4), ALU.mult))
                            dv(lambda e: e.tensor_tensor(slab(13), slab(13), bc(col(15), 8), ALU.mult))
                            for gq in range(4):
                                ohg = bc(RB[:, 1, :, gq], 8)
                                dv(lambda e, gq=gq, ohg=ohg: e.tensor_tensor(wgt[:, :, gq * 8:(gq + 1) * 8], slab(13), ohg,
                                                                             ALU.mult), wr_=R_wgt)
                                dv(lambda e, gq=gq, ohg=ohg: e.tensor_tensor(mkk[:, 0, :, gq * 8:(gq + 1) * 8], slab(8), ohg,
                                                                             ALU.mult))
                                dv(lambda e, gq=gq, ohg=ohg: e.tensor_tensor(mkk[:, 1, :, gq * 8:(gq + 1) * 8], slab(12), ohg,
                                                                             ALU.mult))
                            dv(lambda e: e.tensor_tensor(maskb[:], mkk[:, 0], mkk[:, 1], ALU.add), wr_=[R_mask[0]])
                            for t in range(NL):
                                op("pe", lambda e, t=t: e.matmul(PS[5][:, t * NE:(t + 1) * NE], ustr[:], maskb[:, t, :],
                                                               start=True, stop=(t == 0)),
                                   reads=[R_mask[0], R_wo], writes=[PSR[5]])
                                for t2 in range(t):
                                    op("pe", lambda e, t=t, t2=t2: e.matmul(PS[5][:, t * NE:(t + 1) * NE], onesb[:],
                                                                           maskb[:, t2, :], start=False, stop=(t2 == t - 1)),
                                       reads=[R_mask[0], R_wo], writes=[PSR[5]])
                            op("act", lambda e: e.activation(rkk[:, 0].rearrange("p t e -> p (t e)"), PS[5], AF.Copy),
                               reads=[PSR[5]], writes=[R_rk])
                            op("dve", lambda e: e.tensor_tensor(rkk[:, 1], rkk[:, 0],
                                                                eC[:, :].unsqueeze(1).to_broadcast([128, NL, NE]), ALU.add),
                               reads=[R_rk, R_wo], writes=[R_rk])
                            for kk in range(2):
                                for src_i, di in ((1, 0), (0, 1)):
                                    op("dve", lambda e, kk=kk, src_i=src_i: e.tensor_tensor(
                                        t3[:], mkk[:, kk], rkk[:, src_i], ALU.mult), reads=[R_rk, R_B], writes=[R_B])
                                    op("dve", lambda e, kk=kk, di=di: e.tensor_reduce(
                                        dfa[:, di, kk, :], t3[:], AX.X, ALU.add), reads=[R_B], writes=[R_B])
                                op("dve", lambda e, kk=kk: e.tensor_tensor(t3[:], mkk[:, kk], wgt[:], ALU.mult),
                                   reads=[R_B] + R_wgt, writes=[R_B])
                                op("dve", lambda e, kk=kk: e.tensor_reduce(wsel[:, :, kk], t3[:], AX.X, ALU.add),
                                   reads=[R_B], writes=R_dest)
                            dv(lambda e: e.tensor_scalar(dfa[:, 2], dfa[:, 1], float(CAP), None, ALU.is_ge))
                            dv(lambda e: e.scalar_tensor_tensor(dfa[:, 0], dfa[:, 2], 1.0e6, dfa[:, 0], ALU.mult, ALU.add))
                            dv(lambda e: e.tensor_scalar(dfa[:, 0], dfa[:, 0], float(NE * CAP), None, ALU.min))
                            dv(lambda e: e.tensor_copy(dest[:].rearrange("p t k -> p k t"), dfa[:, 0]), wr_=R_dest)
                            for t in range(NL):
                                b = t % 2
                                dma("sp", hxb[b][:], hx2_d[t * 128:(t + 1) * 128, :], reads=[R_xbuf], writes=[R_hxb[b]])
                                for kk in range(2):
                                    K.pdma(None, None, None, reads=[R_hxb[b]] + R_dest, writes=[],
                                           fn=lambda g_, kk=kk, t=t: g_.indirect_dma_start(
                                               out=xbuf_d[:, :],
                                               out_offset=bass.IndirectOffsetOnAxis(ap=dest[:, t, kk:kk + 1], axis=0),
                                               in_=hxb[b][:, :], in_offset=None))
                            K.barrier()
                        if debug:
                            dma("sp", d_wgt, wgt[:], reads=R_wgt)
                        if stop_after == "B2":
                            K.barrier()
                            return nc, list(dbg.keys())

        with ExitStack() as sM:
            NWB = 3
            wgs = [sb(sM, f"wgs{i}", [128, 8, DE], BF16) for i in range(NWB)]
            wus = [sb(sM, f"wus{i}", [128, 8, DE], BF16) for i in range(NWB)]
            wds = [sb(sM, f"wds{i}", [128, 6, D], BF16) for i in range(NWB)]
            Xs = [sb(sM, f"Xs{i}", [128, NJ, D], BF16) for i in range(2)]
            XT = [sb(sM, f"XT{i}", [128, 8, CAP], BF16) for i in range(2)]
            HT = [sb(sM, f"HT{i}", [128, 6, CAP], BF16) for i in range(2)]
            sg = [sb(sM, f"sg{i}", [128, CAP], F32) for i in range(2)]
            Yst = [sb(sM, f"Yst{i}", [128, D], F32) for i in range(2)]
            R_wg = K.regs_n(NWB, "wg")
            R_wu = K.regs_n(NWB, "wu")
            R_wd = K.regs_n(NWB, "wd")
            R_Xs = K.regs_n(2, "Xs")
            R_XT = K.regs_n(2, "XT")
            R_HT = K.regs_n(2, "HT")
            R_sg = K.regs_n(2, "sg")
            R_Yst = K.regs_n(2, "Yst")
            cnt = {"si": 0, "yi": 0, "ti": 0}
            op("dve", lambda e: e.memset(Yst[0][0:1, :], 0.0), writes=[R_Yst[0]])
            dma("sp", ybuf_d[NE * CAP:NE * CAP + 1, :], Yst[0][0:1, :], reads=[R_Yst[0]])

            def load_w(ex):
                w3 = ex % NWB
                K.pdma(None, wgs[w3][:], wg_d[ex].rearrange("(k p) f -> p k f", p=128), writes=[R_wg[w3]])
                K.pdma(None, wus[w3][:], wu_d[ex].rearrange("(k p) f -> p k f", p=128), writes=[R_wu[w3]])
                K.pdma(None, wds[w3][:], wd_d[ex].rearrange("(c p) n -> p c n", p=128), writes=[R_wd[w3]])

            def load_x(ex):
                xb_ = ex % 2
                dma("sp", Xs[xb_][:], xbuf_d[ex * CAP:(ex + 1) * CAP, :].rearrange("(j p) d -> p j d", p=128),
                    writes=[R_Xs[xb_]])

            def transposes(ex):
                xb_ = ex % 2
                for j in range(NJ):
                    tbk = 6 + cnt["ti"] % 2
                    cnt["ti"] += 1
                    psb = PS[tbk].bitcast(BF16)
                    for k in range(8):
                        op("pe", lambda e, k=k: e.transpose(psb[:, k * 128:(k + 1) * 128],
                                                            Xs[xb_][:, j, k * 128:(k + 1) * 128], identb[:]),
                           reads=[R_Xs[xb_], R_const], writes=[PSR[tbk]])
                    if tbk == 6:
                        op("dve", lambda e: e.tensor_copy(XT[xb_][:, :, j * 128:(j + 1) * 128],
                                                          psb.rearrange("p (k n) -> p k n", k=8)),
                           reads=[PSR[tbk]], writes=[R_XT[xb_]])
                    else:
                        op("act", lambda e: e.activation(XT[xb_][:, :, j * 128:(j + 1) * 128],
                                                         psb.rearrange("p (k n) -> p k n", k=8), AF.Copy),
                           reads=[PSR[tbk]], writes=[R_XT[xb_]])

            load_w(0)
            load_w(1)
            load_x(0)
            transposes(0)
            for ex in range(NE):
                wb = ex % 2
                w3 = ex % NWB
                if ex + 2 < NE:
                    load_w(ex + 2)
                if ex + 1 < NE:
                    load_x(ex + 1)
                for fc in range(6):
                    fs = slice(fc * 128, (fc + 1) * 128)
                    gbk = fc % 2
                    ubk = 2 + fc % 2
                    for k in range(8):
                        op("pe", lambda e, k=k: e.matmul(PS[gbk][:, 0:CAP], wgs[w3][:, k, fs], XT[wb][:, k, :],
                                                       start=(k == 0), stop=(k == 7)),
                           reads=[R_wg[w3], R_XT[wb]], writes=[PSR[gbk]])
                    for k in range(8):
                        op("pe", lambda e, k=k: e.matmul(PS[ubk][:, 0:CAP], wus[w3][:, k, fs], XT[wb][:, k, :],
                                                       start=(k == 0), stop=(k == 7)),
                           reads=[R_wu[w3], R_XT[wb]], writes=[PSR[ubk]])
                    sb2 = cnt["si"] % 2
                    cnt["si"] += 1
                    op("act", lambda e: e.activation(sg[sb2][:], PS[gbk][:, 0:CAP], AF.Silu),
                       reads=[PSR[gbk]], writes=[R_sg[sb2]])
                    op("dve", lambda e: e.tensor_tensor(HT[wb][:, fc, :], PS[ubk][:, 0:CAP], sg[sb2][:], ALU.mult),
                       reads=[PSR[ubk], R_sg[sb2]], writes=[R_HT[wb]])
                if ex + 1 < NE:
                    transposes(ex + 1)
                for j in range(NJ):
                    ys = cnt["yi"] % 2
                    cnt["yi"] += 1
                    for hf in range(2):
                        yb_ = 4 + hf
                        cs = slice(hf * 512, (hf + 1) * 512)
                        for fc in range(6):
                            op("pe", lambda e, fc=fc: e.matmul(
                                PS[yb_], HT[wb][:, fc, j * 128:(j + 1) * 128], wds[w3][:, fc, cs],
                                start=(fc == 0), stop=(fc == 5)),
                               reads=[R_HT[wb], R_wd[w3]], writes=[PSR[yb_]])
                        op("dve", lambda e: e.tensor_copy(Yst[ys][:, cs], PS[yb_]),
                           reads=[PSR[yb_]], writes=[R_Yst[ys]])
                    dma("sp", ybuf_d[ex * CAP + j * 128:ex * CAP + (j + 1) * 128, :], Yst[ys][:],
                        reads=[R_Yst[ys]])
            K.barrier()
        with ExitStack() as sF:
            y12 = [[sb(sF, f"y{k}_{i}", [128, D], F32) for k in range(2)] for i in range(4)]
            xr2 = [sb(sF, f"xq{i}", [128, D], F32) for i in range(2)]
            ot = [sb(sF, f"ot{i}", [128, D], F32) for i in range(2)]
            junk3 = sb(sF, "junk3", [128, D], BF16)
            st3 = sb(sF, "st3", [128, 3, NL], F32)
            R_y12 = [K.regs_n(2, f"y12_{i}") for i in range(4)]
            R_xr2 = K.regs_n(2, "xr2")
            R_ot = K.regs_n(2, "ot")
            R_st3 = K.regs_n(NL, "st3")
            def fin_front(tg):
                b = tg % 2
                tsl = slice(tg * 128, (tg + 1) * 128)
                dma("sp", xr2[b][:], xnew_d[tsl, :], writes=[R_xr2[b]])
                for kk in range(2):
                    K.pdma(None, None, None, reads=[], writes=[R_y12[tg % 4][kk]],
                           fn=lambda g, kk=kk: g.indirect_dma_start(
                               out=y12[tg % 4][kk][:, :], out_offset=None, in_=ybuf_d[:, :],
                               in_offset=bass.IndirectOffsetOnAxis(ap=dest[:, tg, kk:kk + 1], axis=0)))
                op("dve", lambda e: e.tensor_scalar(ot[b][:], y12[tg % 4][0][:], wsel[:, tg, 0:1], None, ALU.mult),
                   reads=[R_y12[tg % 4][0]], writes=[R_ot[b]])
                op("dve", lambda e: e.scalar_tensor_tensor(ot[b][:], y12[tg % 4][1][:], wsel[:, tg, 1:2], ot[b][:],
                                                          ALU.mult, ALU.add),
                   reads=[R_y12[tg % 4][1], R_ot[b]], writes=[R_ot[b]])
                if debug:
                    dma("sp", d_acc[:, tg, :], ot[b][:], reads=[R_ot[b]])
                op("dve", lambda e: e.tensor_tensor(ot[b][:], ot[b][:], gt2b[:], ALU.mult),
                   reads=[R_ot[b], R_gt2], writes=[R_ot[b]])
                op("dve", lambda e: e.tensor_tensor(ot[b][:], ot[b][:], xr2[b][:], ALU.add),
                   reads=[R_ot[b], R_xr2[b]], writes=[R_ot[b]])

            def fin_back(tg):
                b = tg % 2
                tsl = slice(tg * 128, (tg + 1) * 128)
                op("act", lambda e: e.activation(junk3[:], ot[b][:], AF.Square, accum_out=st3[:, 0, tg:tg + 1]),
                   reads=[R_ot[b]], writes=[R_st3[tg]])
                op("act", lambda e: e.activation(st3[:, 1, tg:tg + 1], st3[:, 0, tg:tg + 1], AF.Ln,
                                                 scale=1.0 / D, bias=EPS),
                   reads=[R_st3[tg]], writes=[R_st3[tg]])
                op("act", lambda e: e.activation(st3[:, 2, tg:tg + 1], st3[:, 1, tg:tg + 1], AF.Exp, scale=-0.5),
                   reads=[R_st3[tg]], writes=[R_st3[tg]])
                op("dve", lambda e: e.scalar_tensor_tensor(
                    ot[b][:], ot[b][:], st3[:, 2, tg:tg + 1], fgb[:], ALU.mult, ALU.mult),
                   reads=[R_ot[b], R_st3[tg], R_const], writes=[R_ot[b]])
                dma("sp", y_d[tsl, :], ot[b][:], reads=[R_ot[b]])

            fin_front(0)
            for tg in range(NL):
                if tg + 1 < NL:
                    fin_front(tg + 1)
                fin_back(tg)
            K.barrier()
    return nc, list(dbg.keys())


def _host_consts():
    ident = np.eye(128, dtype=np.float32)
    rows = SEQ // 64
    row_idx = np.repeat(np.arange(rows, dtype=np.float32), 64)
    col_idx = np.tile(np.arange(64, dtype=np.float32), rows)
    inv_freq = (np.float32(10000.0) ** (-np.arange(0, 32, 2, dtype=np.float32) / np.float32(32))).astype(np.float32)
    ang = np.stack([row_idx[:, None] * inv_freq, col_idx[:, None] * inv_freq], axis=1).astype(np.float32)
    cos = np.cos(ang).astype(np.float32).reshape(SEQ, 32)
    sin = np.sin(ang).astype(np.float32).reshape(SEQ, 32)
    cosT = np.ascontiguousarray(cos.reshape(NL, 128, 32).transpose(1, 0, 2))
    sinT = np.ascontiguousarray(sin.reshape(NL, 128, 32).transpose(1, 0, 2))
    bd64 = np.zeros((128, 128), np.float32)
    bd64[:64, :64] = 1.0 / 64
    bd64[64:, 64:] = 1.0 / 64
    c65 = np.full((65, 64), 1.0 / 64, np.float32)
    c65[64, :] = EPS
    e0 = np.zeros((128, 1), np.float32)
    e0[0, 0] = 1.0
    ustr = np.triu(np.ones((128, 128), np.float32), k=1)
    eC = np.ascontiguousarray(np.broadcast_to((np.arange(NE, dtype=np.float32) * CAP)[None, :], (128, NE)))
    return dict(ident_f=ident, cosT=cosT, sinT=sinT, bd64=bd64, c65=c65, e0=e0, ustr=ustr, eC=eC)


def make_in_maps(inputs, cores=range(8)):
    f = lambda a: np.ascontiguousarray(np.asarray(a, dtype=np.float32))
    x = f(inputs["x"]); c = f(inputs["c"]); ctx = f(inputs["ctx"]); c_ctx = f(inputs["c_ctx"])
    consts = _host_consts()
    bc = lambda v: np.ascontiguousarray(np.broadcast_to(f(v).reshape(1, -1), (128, f(v).size)))
    shared = dict(
        w_mod=f(inputs["w_mod"])[0], bm_b=bc(inputs["b_mod"][0]), g1_b=bc(inputs["norm1_g"][0]),
        g2_b=bc(inputs["norm2_g"][0]), fg_b=bc(inputs["final_g"]), w_in=f(inputs["w_in"])[0],
        qg_b=bc(inputs["q_norm_g"][0]), kg_b=bc(inputs["k_norm_g"][0]),
        cwT=np.ascontiguousarray(f(inputs["conv_w"])[0].reshape(3, 4, 128).transpose(2, 1, 0).reshape(128, 12)),
        gallT=np.ascontiguousarray(np.concatenate([f(inputs["attn_out_g"])[0], f(inputs["conv_out_g"])[0]])
                                   .reshape(8, 128).T),
        w_out=f(inputs["w_out"])[0],
        w_r=np.ascontiguousarray(np.concatenate([f(inputs["w_group"])[0], f(inputs["w_router"])[0]], axis=1)),
        w_gate=f(inputs["w_gate"])[0], w_up=f(inputs["w_up"])[0], w_down=f(inputs["w_down"])[0],
        **consts,
    )
    maps = []
    for b in cores:
        cT = np.concatenate([c[b].reshape(8, 128).T, c_ctx.reshape(8, 128).T], axis=1)
        m = dict(shared)
        m["xin"] = np.ascontiguousarray(np.concatenate([ctx[b], x[b]], axis=0))
        m["cT"] = np.ascontiguousarray(cT.astype(np.float32))
        maps.append(m)
    return maps


_CACHE = {}


def kernel(**inputs):
    if "nc" not in _CACHE:
        _CACHE["nc"] = build_program(debug=False)[0]
    nc = _CACHE["nc"]
    maps = make_in_maps(inputs)
    res = run_bass_kernel_spmd(nc, maps, core_ids=list(range(8)))
    out = np.stack([np.asarray(r["y"], dtype=np.float32) for r in res.results], axis=0)
    return out
```

```python
import os
import numpy as np
from contextlib import ExitStack
import concourse.bass as bass
import concourse.mybir as mybir
from concourse.bass_utils import run_bass_kernel_spmd

F32 = mybir.dt.float32
BF16 = mybir.dt.bfloat16
I32 = mybir.dt.int32
AF = mybir.ActivationFunctionType
ALU = mybir.AluOpType
AX = mybir.AxisListType

D = 1024
SEQ = 2048
CTX = 256
NTOK = SEQ + CTX
NT = NTOK // 128
NL = SEQ // 128
EPS = 1e-6
NE = 32
DE = 768
N_DSEM = 40
CAP = 512
NJ = CAP // 128
_LVL = int(os.environ.get('KLVL', '9'))
_KEV = os.environ.get('KEV', 'act')


class Reg:
    __slots__ = ("w", "r", "name")

    def __init__(self, name=""):
        self.w = None
        self.r = {}
        self.name = name


class Sem:
    def __init__(self, handle):
        self.handle = handle
        self.count = 0


class EngW:
    def __init__(self, name, eng, sem):
        self.name = name
        self.eng = eng
        self.sem = sem
        self.waited = {}


class KB:
    def __init__(self, nc, es):
        self.nc = nc
        self._es = es
        self.pslots = []
        self.regs = []
        mk = lambda n: Sem(es.enter_context(nc.semaphore(n)))
        self.E = {
            "pe": EngW("pe", nc.tensor, mk("s_pe")),
            "act": EngW("act", nc.scalar, mk("s_act")),
            "dve": EngW("dve", nc.vector, mk("s_dve")),
            "pool": EngW("pool", nc.gpsimd, mk("s_pool")),
            "sp": EngW("sp", nc.sync, mk("s_sp")),
        }
        self.dsems = [mk(f"s_d{i}") for i in range(N_DSEM)]
        self.dnext = 0
        self.psems = [mk(f"s_q{i}") for i in range(24)]
        self.pnext = 0

    def reg(self, name=""):
        r = Reg(name)
        self.regs.append(r)
        return r

    def regs_n(self, n, name=""):
        return [self.reg(f"{name}{i}") for i in range(n)]

    def wait(self, ew, tok):
        sem, val = tok
        if ew.waited.get(id(sem), 0) >= val:
            return
        ew.eng.wait_ge(sem.handle, val)
        ew.waited[id(sem)] = val

    def _deps(self, ew, reads, writes):
        for r in reads:
            if r.w is not None:
                if ew.name == "pe" and r.w[0] is ew.sem:
                    continue
                self.wait(ew, r.w)
        for w in writes:
            toks = list(w.r.values())
            if w.w is not None:
                toks.append(w.w)
            for t in toks:
                if ew.name == "pe" and t[0] is ew.sem:
                    continue
                self.wait(ew, t)

    def _record(self, tok, reads, writes):
        for r in reads:
            r.r[id(tok[0])] = tok
        for w in writes:
            w.w = tok
            w.r = {}

    def op(self, en, fn, reads=(), writes=()):
        ew = self.E[en]
        self._deps(ew, reads, writes)
        ins = fn(ew.eng)
        ew.sem.count += 1
        ins.then_inc(ew.sem.handle, 1)
        tok = (ew.sem, ew.sem.count)
        self._record(tok, reads, writes)
        return tok

    def dma(self, qn, out, in_, reads=(), writes=(), fn=None):
        ew = self.E[qn]
        self._deps(ew, reads, writes)
        d = self.dsems[self.dnext % N_DSEM]
        self.dnext += 1
        if d.count:
            self.wait(ew, (d, d.count))
        if fn is None:
            ins = ew.eng.dma_start(out=out, in_=in_)
        else:
            ins = fn(ew.eng)
        d.count += 16
        ins.then_inc(d.handle, 16)
        tok = (d, d.count)
        self._record(tok, reads, writes)
        return tok

    def pslot(self, name):
        return None

    def pdma(self, slot, out, in_, reads=(), writes=(), fn=None):
        ew = self.E["pool"]
        self._deps(ew, reads, writes)
        d = self.psems[self.pnext % len(self.psems)]
        self.pnext += 1
        if d.count:
            self.wait(ew, (d, d.count))
        ins = ew.eng.dma_start(out=out, in_=in_) if fn is None else fn(ew.eng)
        d.count += 16
        ins.then_inc(d.handle, 16)
        tok = (d, d.count)
        self._record(tok, reads, writes)
        return tok

    def barrier(self):
        for ew in self.E.values():
            for other in self.E.values():
                if other.sem.count and not (other is ew and ew.name in ("pe", "sp")):
                    self.wait(ew, (other.sem, other.sem.count))
            for d in self.dsems:
                if d.count:
                    self.wait(ew, (d, d.count))
            for d in self.psems:
                if d.count:
                    self.wait(ew, (d, d.count))
        for r in self.regs:
            r.w = None
            r.r = {}


def build_program(debug=False, stop_after=None):
    nc = bass.Bass("TRN2", target_bir_lowering=False)

    def din(name, shape, dt=F32):
        return nc.dram_tensor(name, list(shape), dt, kind="ExternalInput").ap()

    xin = din("xin", [NTOK, D])
    cT_d = din("cT", [128, 16])
    w_mod_d = din("w_mod", [D, 6 * D])
    bm_d = din("bm_b", [128, 6 * D])
    g1_d = din("g1_b", [128, D])
    g2_d = din("g2_b", [128, D])
    fg_d = din("fg_b", [128, D])
    w_in_d = din("w_in", [D, 2304])
    qg_d = din("qg_b", [128, 64])
    kg_d = din("kg_b", [128, 64])
    cw_d = din("cwT", [128, 12])
    gall_d = din("gallT", [128, 8])
    w_out_d = din("w_out", [D, D])
    wr_d = din("w_r", [D, 36])
    wg_d = din("w_gate", [NE, D, DE])
    wu_d = din("w_up", [NE, D, DE])
    wd_d = din("w_down", [NE, DE, D])
    identf_d = din("ident_f", [128, 128])
    cos_d = din("cosT", [128, NL, 32])
    sin_d = din("sinT", [128, NL, 32])
    bd64_d = din("bd64", [128, 128])
    c65_d = din("c65", [65, 64])
    e0_d = din("e0", [128, 1])
    ustr_d = din("ustr", [128, 128])
    eC_d = din("eC", [128, NE])

    y_d = nc.dram_tensor("y", [SEQ, D], F32, kind="ExternalOutput").ap()
    xnew_d = nc.dram_tensor("xnew_scr", [SEQ, D], F32,
                            kind="ExternalOutput" if debug else "Internal").ap()
    xbuf_d = nc.dram_tensor("xbuf_scr", [NE * CAP + 1, D], BF16, kind="Internal").ap()
    ybuf_d = nc.dram_tensor("ybuf_scr", [NE * CAP + 1, D], F32, kind="Internal").ap()
    hx2_d = nc.dram_tensor("hx2_scr", [SEQ, D], BF16, kind="Internal").ap()
    dbg = {}

    def dbg_out(name, shape, dt=F32):
        if debug:
            dbg[name] = nc.dram_tensor(name, list(shape), dt, kind="ExternalOutput").ap()
        return dbg.get(name)

    d_smod = dbg_out("d_smod", [128, 32])
    d_modb = dbg_out("d_modb", [128, 6 * D])
    d_QT = dbg_out("d_QT", [128, 4, SEQ], BF16)
    d_KT = dbg_out("d_KT", [128, 2, NTOK], BF16)
    d_V = dbg_out("d_V", [128, NT, 2, 65], BF16)
    d_convn = dbg_out("d_convn", [128, 4, SEQ], BF16)
    d_attn = dbg_out("d_attn", [64, 8, SEQ], BF16)
    d_wgt = dbg_out("d_wgt", [128, NL, NE])
    d_acc = dbg_out("d_acc", [128, NL, D])

    with ExitStack() as es:
        K = KB(nc, es)
        op, dma = K.op, K.dma

        def sb(stk, name, shape, dt):
            return stk.enter_context(nc.sbuf_tensor("sb_" + name, list(shape), dt))

        PSUM = es.enter_context(nc.psum_tensor("psum", [128, 8 * 512], F32))
        PS = [PSUM[:, i * 512:(i + 1) * 512] for i in range(8)]
        PSR = K.regs_n(8, "ps")

        identf = sb(es, "identf", [128, 128], F32)
        identb = sb(es, "identb", [128, 128], BF16)
        fgb = sb(es, "fgb", [128, D], F32)
        gt2b = sb(es, "gt2b", [128, D], F32)
        wgt = sb(es, "wgt", [128, NL, NE], F32)
        dest = sb(es, "dest", [128, NL, 2], I32)
        wsel = sb(es, "wsel", [128, NL, 2], F32)
        R_dest = K.regs_n(NL, "dest")
        R_const = K.reg("const")
        R_gt2 = K.reg("gt2")
        R_wgt = K.regs_n(NL, "wgt")

        dma("sp", identf[:], identf_d, writes=[R_const])
        dma("sp", fgb[:], fg_d, writes=[R_const])
        op("dve", lambda e: e.tensor_copy(identb[:], identf[:]), reads=[R_const], writes=[R_const])

        with ExitStack() as sAB:
            modb = sb(sAB, "modb", [128, 6 * D], F32)
            smod = sb(sAB, "smod", [128, 32], F32)
            cosT = sb(sAB, "cosT", [128, NL, 32], F32)
            sinT = sb(sAB, "sinT", [128, NL, 32], F32)
            qgb = sb(sAB, "qgb", [128, 64], F32)
            kgb = sb(sAB, "kgb", [128, 64], F32)
            cw = sb(sAB, "cw", [128, 12], F32)
            bd64 = sb(sAB, "bd64", [128, 128], F32)
            c65 = sb(sAB, "c65", [65, 64], F32)
            e0 = sb(sAB, "e0", [128, 1], F32)
            wr = sb(sAB, "wr", [128, 8, 36], F32)
            R_modb = K.regs_n(6, "modb")
            R_smod = K.reg("smod")
            for t_, d_ in ((cosT, cos_d), (sinT, sin_d), (qgb, qg_d), (kgb, kg_d), (cw, cw_d),
                           (bd64, bd64_d), (c65, c65_d), (e0, e0_d)):
                dma("sp", t_[:], d_, writes=[R_const])
            dma("sp", wr[:], wr_d.rearrange("(k p) n -> p k n", p=128), writes=[R_const])

            with ExitStack() as s0:
                cTt = sb(s0, "cTt", [128, 16], F32)
                scT = sb(s0, "scT", [128, 16], F32)
                lhs_c = sb(s0, "lhs_c", [128, 8, 128], BF16)
                lhs_x = sb(s0, "lhs_x", [128, 8, 128], BF16)
                bm = sb(s0, "bm", [128, 6 * D], F32)
                g1b = sb(s0, "g1b", [128, D], F32)
                g2b = sb(s0, "g2b", [128, D], F32)
                modx = sb(s0, "modx", [128, 2 * D], F32)
                wm = [sb(s0, f"wm{i}", [128, 8, 512], BF16) for i in range(2)]
                R_c = K.reg("c")
                R_bm = K.reg("bm")
                R_g = K.reg("g12")
                R_modx = K.regs_n(2, "modx")
                R_wm = K.regs_n(2, "wm")
                P_wm = [K.pslot(f"wm{i}") for i in range(2)]
                dma("sp", cTt[:], cT_d, writes=[R_c])
                dma("sp", bm[:], bm_d, writes=[R_bm])
                dma("sp", g1b[:], g1_d, writes=[R_g])
                dma("sp", g2b[:], g2_d, writes=[R_g])
                op("act", lambda e: e.activation(scT[:], cTt[:], AF.Silu), reads=[R_c], writes=[R_c])
                op("dve", lambda e: e.tensor_copy(
                    lhs_c[:], scT[:, 0:8].unsqueeze(2).to_broadcast([128, 8, 128])), reads=[R_c], writes=[R_c])
                op("dve", lambda e: e.tensor_copy(
                    lhs_x[:], scT[:, 8:16].unsqueeze(2).to_broadcast([128, 8, 128])), reads=[R_c], writes=[R_c])
                wmod_v = w_mod_d.rearrange("(k p) n -> p k n", p=128)
                for blk in range(12):
                    b = blk % 2
                    cs = slice(blk * 512, (blk + 1) * 512)
                    K.pdma(P_wm[b], wm[b][:], wmod_v[:, :, cs], writes=[R_wm[b]])
                    for k in range(8):
                        op("pe", lambda e, k=k: e.matmul(PS[b], lhs_c[:, k, :], wm[b][:, k, :],
                                                       start=(k == 0), stop=(k == 7)),
                           reads=[R_c, R_wm[b]], writes=[PSR[b]])
                    op("dve", lambda e: e.tensor_tensor(modb[:, cs], PS[b], bm[:, cs], ALU.add),
                       reads=[PSR[b], R_bm], writes=[R_modb[blk // 2]])
                    if blk < 4:
                        for k in range(8):
                            op("pe", lambda e, k=k: e.matmul(PS[2 + b], lhs_x[:, k, :], wm[b][:, k, :],
                                                           start=(k == 0), stop=(k == 7)),
                               reads=[R_c, R_wm[b]], writes=[PSR[2 + b]])
                        op("dve", lambda e: e.tensor_tensor(modx[:, cs], PS[2 + b], bm[:, cs], ALU.add),
                           reads=[PSR[2 + b], R_bm], writes=[R_modx[blk // 2]])
                op("dve", lambda e: e.scalar_tensor_tensor(modb[:, D:2 * D], modb[:, D:2 * D], 1.0, g1b[:],
                                                          ALU.add, ALU.mult),
                   reads=[R_modb[1], R_g], writes=[R_modb[1]])
                op("dve", lambda e: e.scalar_tensor_tensor(modx[:, D:2 * D], modx[:, D:2 * D], 1.0, g1b[:],
                                                          ALU.add, ALU.mult),
                   reads=[R_modx[1], R_g], writes=[R_modx[1]])
                op("dve", lambda e: e.scalar_tensor_tensor(modb[:, 4 * D:5 * D], modb[:, 4 * D:5 * D], 1.0, g2b[:],
                                                          ALU.add, ALU.mult),
                   reads=[R_modb[4], R_g], writes=[R_modb[4]])
                op("act", lambda e: e.activation(gt2b[:], modb[:, 5 * D:6 * D], AF.Copy),
                   reads=[R_modb[5]], writes=[R_gt2])
                srcs = [(modb, D, R_modb[1]), (modb, 0, R_modb[0]), (modx, D, R_modx[1]), (modx, 0, R_modx[0])]
                for vi, (tt, off, rr) in enumerate(srcs):
                    for k in range(8):
                        c0 = vi * 8 + k
                        op("pe", lambda e, tt=tt, off=off, k=k, c0=c0: e.matmul(
                            PS[4][:, c0:c0 + 1], tt[:, off + k * 128: off + (k + 1) * 128], e0[:, 0:1],
                            start=True, stop=True), reads=[rr, R_const], writes=[PSR[4]])
                op("act", lambda e: e.activation(smod[:], PS[4][:, 0:32], AF.Copy), reads=[PSR[4]], writes=[R_smod])
                if debug:
                    dma("sp", d_smod, smod[:], reads=[R_smod])
                    dma("sp", d_modb, modb[:], reads=R_modb)
                K.barrier()
                if stop_after == "0":
                    return nc, list(dbg.keys())

            with ExitStack() as sB:
                QT = sb(sB, "QT", [128, 4, SEQ], BF16)
                KT = sb(sB, "KT", [128, 2, 2, NTOK], BF16)
                VE = sb(sB, "VE", [128, NT, 2, 128], BF16)
                convn = sb(sB, "convn", [128, 4, SEQ], BF16)
                R_QT = K.regs_n(NL, "QT")
                R_KT = K.regs_n(NT, "KT")
                R_VE = K.regs_n(NT, "VE")
                R_convn = K.regs_n(4, "convn")
                R_ve1 = K.reg("ve1")
                if _LVL >= 0:
                    op("dve", lambda e: e.memset(VE[:], 0.0), writes=[R_ve1])
                    op("dve", lambda e: e.memset(VE[:, :, :, 64:65], 1.0), writes=[R_ve1])
                    op("dve", lambda e: e.memset(KT[:], 0.0), writes=[R_ve1])

                with ExitStack() as sA:
                    hT = sb(sA, "hT", [128, 8, NTOK], BF16)
                    R_hTd = K.regs_n(NT, "hTd")
                    R_hTa = K.regs_n(NT, "hTa")
                    with ExitStack() as sA1:
                        winq = sb(sA1, "winq", [128, 8, 768], BF16)
                        R_winq = K.reg("winq")
                        if _LVL >= 1:
                            K.pdma(K.pslot("winq"), winq[:], w_in_d.rearrange("(k p) n -> p k n", p=128)[:, :, 0:768],
                                   writes=[R_winq])
                        xt = [sb(sA1, f"xt{i}", [128, D], F32) for i in range(2)]
                        junk = sb(sA1, "junk", [128, D], BF16)
                        xn = [sb(sA1, f"xn{i}", [128, D], BF16) for i in range(2)]
                        st1 = sb(sA1, "st1", [128, 3, NT], F32)
                        zq = [sb(sA1, f"zq{i}", [128, 512], F32) for i in range(2)]
                        zkv = [sb(sA1, f"zkv{i}", [128, 256], F32) for i in range(2)]
                        sq = sb(sA1, "sq", [128, 512], F32)
                        qn = sb(sA1, "qn", [128, 512], F32)
                        kn = sb(sA1, "kn", [128, 128], F32)
                        knb = sb(sA1, "knb", [128, 128], BF16)
                        ta = sb(sA1, "ta", [128, 256], F32)
                        tb = sb(sA1, "tb", [128, 256], F32)
                        qst = sb(sA1, "qst", [128, 3, 16], F32)
                        qkb = [sb(sA1, f"qkb{i}", [128, 768], BF16) for i in range(2)]
                        R_xt = K.regs_n(2, "xt")
                        R_xn = K.regs_n(2, "xn")
                        R_st1 = K.regs_n(NT, "st1")
                        R_zq = K.regs_n(2, "zq")
                        R_zkv = K.regs_n(2, "zkv")
                        R_tmp = K.reg("tmpq")
                        R_qst = K.reg("qst")
                        R_qkb = K.regs_n(2, "qkb")
                        R_knb = K.reg("knb")
                        def a1_front(T):
                            b = T % 2
                            lat = T >= 2
                            t = T - 2
                            tsl = slice(T * 128, (T + 1) * 128)
                            dma("sp", xt[b][:], xin[tsl, :], writes=[R_xt[b]])
                            if _LVL < -4:
                                return
                            op("act", lambda e: e.activation(junk[:], xt[b][:], AF.Square,
                                                             accum_out=st1[:, 0, T:T + 1]),
                               reads=[R_xt[b]], writes=[R_st1[T]])
                            if _LVL < -3:
                                return
                            op("act", lambda e: e.activation(st1[:, 1, T:T + 1], st1[:, 0, T:T + 1], AF.Ln,
                                                             scale=1.0 / D, bias=EPS),
                               reads=[R_st1[T]], writes=[R_st1[T]])
                            op("act", lambda e: e.activation(st1[:, 2, T:T + 1], st1[:, 1, T:T + 1], AF.Exp,
                                                             scale=-0.5),
                               reads=[R_st1[T]], writes=[R_st1[T]])
                            op("act", lambda e: e.activation(xn[b][:], xt[b][:], AF.Copy, scale=st1[:, 2, T:T + 1]),
                               reads=[R_xt[b], R_st1[T]], writes=[R_xn[b]])
                            if _LVL < -2:
                                return
                            psb = PS[b].bitcast(BF16)
                            for k in range(8):
                                op("pe", lambda e, k=k: e.transpose(psb[:, k * 128:(k + 1) * 128],
                                                                    xn[b][:, k * 128:(k + 1) * 128], identb[:]),
                                   reads=[R_xn[b], R_const], writes=[PSR[b]])
                            if _LVL < -1:
                                return
                            so = 0 if lat else 16
                            for k in range(8):
                                if (b == 0 and _KEV != 'act') or _KEV == 'dve':
                                    op("dve", lambda e, k=k: e.tensor_scalar(
                                        hT[:, k, tsl], psb[:, k * 128:(k + 1) * 128],
                                        smod[:, so + k:so + k + 1], smod[:, so + 8 + k:so + 9 + k],
                                        ALU.mult, ALU.add), reads=[PSR[b], R_smod], writes=[R_hTd[T]])
                                else:
                                    op("act", lambda e, k=k: e.activation(
                                        hT[:, k, tsl], psb[:, k * 128:(k + 1) * 128], AF.Identity,
                                        bias=smod[:, so + 8 + k:so + 9 + k], scale=smod[:, so + k:so + k + 1]),
                                       reads=[PSR[b], R_smod], writes=[R_hTa[T]])
                            if _LVL < 1:
                                return
                            if lat:
                                for k in range(8):
                                    op("pe", lambda e, k=k: e.matmul(PS[2], hT[:, k, tsl], winq[:, k, 0:512],
                                                                   start=(k == 0), stop=(k == 7)),
                                       reads=[R_hTd[T], R_hTa[T], R_winq], writes=[PSR[2]])
                            for k in range(8):
                                op("pe", lambda e, k=k: e.matmul(PS[3][:, 0:256], hT[:, k, tsl], winq[:, k, 512:768],
                                                               start=(k == 0), stop=(k == 7)),
                                   reads=[R_hTd[T], R_hTa[T], R_winq], writes=[PSR[3]])

                        def a1_front_b(T):
                            b = T % 2
                            lat = T >= 2
                            t = T - 2
                            tsl = slice(T * 128, (T + 1) * 128)
                            if lat:
                                op("act", lambda e: e.activation(zq[b][:], PS[2], AF.Copy),
                                   reads=[PSR[2]], writes=[R_zq[b]])
                            op("act", lambda e: e.activation(zkv[b][:], PS[3][:, 0:256], AF.Copy),
                               reads=[PSR[3]], writes=[R_zkv[b]])
                            op("act", lambda e: e.activation(
                                VE[:, T, :, 0:64], zkv[b][:, 128:256].rearrange("p (g d) -> p g d", g=2), AF.Copy),
                               reads=[R_zkv[b], R_ve1], writes=[R_VE[T]])

                        def a1_back(T, part):
                            b = T % 2
                            lat = T >= 2
                            t = T - 2
                            tsl = slice(T * 128, (T + 1) * 128)
                            nh_list = ([("q", zq[b], 8, qn, qgb, R_zq[b])] if lat else []) + \
                                      [("k", zkv[b], 2, kn, kgb, R_zkv[b])]
                            for nm, src, nh, dst, gb_, rsrc in nh_list:
                                W = nh * 64
                                c0 = 0 if nm == "q" else 8
                                op("dve", lambda e: e.tensor_tensor(sq[:, 0:W], src[:, 0:W], src[:, 0:W], ALU.mult),
                                   reads=[rsrc], writes=[R_tmp])
                                op("dve", lambda e: e.tensor_reduce(
                                    qst[:, 0, c0:c0 + nh], sq[:, 0:W].rearrange("p (h d) -> p h d", h=nh),
                                    AX.X, ALU.add), reads=[R_tmp], writes=[R_qst])
                            lo = 0 if lat else 8
                            op("act", lambda e: e.activation(qst[:, 1, lo:10], qst[:, 0, lo:10],
                                                             AF.Ln, scale=1.0 / 64, bias=EPS),
                               reads=[R_qst], writes=[R_qst])
                            op("act", lambda e: e.activation(qst[:, 2, lo:10], qst[:, 1, lo:10],
                                                             AF.Exp, scale=-0.5),
                               reads=[R_qst], writes=[R_qst])

                        def a1_back_rest(T):
                            b = T % 2
                            lat = T >= 2
                            t = T - 2
                            tsl = slice(T * 128, (T + 1) * 128)
                            nh_list = ([("q", zq[b], 8, qn, qgb, R_zq[b])] if lat else []) + \
                                      [("k", zkv[b], 2, kn, kgb, R_zkv[b])]
                            for nm, src, nh, dst, gb_, rsrc in nh_list:
                                W = nh * 64
                                c0 = 0 if nm == "q" else 8
                                d3 = dst[:, 0:W].rearrange("p (h d) -> p h d", h=nh)
                                op("dve", lambda e: e.tensor_tensor(
                                    d3, src[:, 0:W].rearrange("p (h d) -> p h d", h=nh),
                                    qst[:, 2, c0:c0 + nh].unsqueeze(2).to_broadcast([128, nh, 64]), ALU.mult),
                                   reads=[rsrc, R_qst], writes=[R_tmp])
                                op("dve", lambda e: e.tensor_tensor(
                                    d3, d3, gb_[:, :].unsqueeze(1).to_broadcast([128, nh, 64]), ALU.mult),
                                   reads=[R_tmp, R_const], writes=[R_tmp])
                                if nm == "q":
                                    outb = qkb[b][:, 0:512]
                                    R_ob = R_qkb[b]
                                else:
                                    outb = knb[:, 0:128]
                                    R_ob = R_knb
                                if lat:
                                    d5 = dst[:, 0:W].rearrange("p (h a f d) -> p h a f d", h=nh, a=2, f=2)
                                    o5 = outb.rearrange("p (h a f d) -> p h a f d", h=nh, a=2, f=2)
                                    x1 = d5[:, :, :, 0, :]
                                    x2 = d5[:, :, :, 1, :]
                                    cb = cosT[:, t, :].rearrange("p (a d) -> p a d", a=2).unsqueeze(1) \
                                        .to_broadcast([128, nh, 2, 16])
                                    sb_ = sinT[:, t, :].rearrange("p (a d) -> p a d", a=2).unsqueeze(1) \
                                        .to_broadcast([128, nh, 2, 16])
                                    hw = nh * 32
                                    ta4 = ta[:, 0:hw].rearrange("p (h a d) -> p h a d", h=nh, a=2)
                                    tb4 = tb[:, 0:hw].rearrange("p (h a d) -> p h a d", h=nh, a=2)
                                    op("dve", lambda e: e.tensor_tensor(ta4, x1, cb, ALU.mult),
                                       reads=[R_tmp, R_const], writes=[R_tmp])
                                    op("dve", lambda e: e.tensor_tensor(tb4, x2, sb_, ALU.mult),
                                       reads=[R_tmp, R_const], writes=[R_tmp])
                                    op("dve", lambda e: e.tensor_tensor(o5[:, :, :, 0, :], ta4, tb4, ALU.subtract),
                                       reads=[R_tmp], writes=[R_ob])
                                    op("dve", lambda e: e.tensor_tensor(ta4, x1, sb_, ALU.mult),
                                       reads=[R_tmp, R_const], writes=[R_tmp])
                                    op("dve", lambda e: e.tensor_tensor(tb4, x2, cb, ALU.mult),
                                       reads=[R_tmp, R_const], writes=[R_tmp])
                                    op("dve", lambda e: e.tensor_tensor(o5[:, :, :, 1, :], ta4, tb4, ALU.add),
                                       reads=[R_tmp], writes=[R_ob])
                                else:
                                    op("dve", lambda e: e.tensor_copy(outb, dst[:, 0:W]),
                                       reads=[R_tmp], writes=[R_ob])
                            if _LVL < 4:
                                return
                            op("dve", lambda e: e.tensor_copy(
                                qkb[b][:, 512:768].rearrange("p (g r d) -> p g r d", g=2, r=2),
                                knb[:, 0:128].rearrange("p (g d) -> p g d", g=2).unsqueeze(2)
                                .to_broadcast([128, 2, 2, 64])), reads=[R_knb], writes=[R_qkb[b]])
                            if _LVL < 5:
                                return
                            ps4 = PS[4].bitcast(BF16)
                            ps5 = PS[5].bitcast(BF16)
                            if lat:
                                for j in range(4):
                                    op("pe", lambda e, j=j: e.transpose(ps4[:, j * 128:(j + 1) * 128],
                                                                        qkb[b][:, j * 128:(j + 1) * 128], identb[:]),
                                       reads=[R_qkb[b], R_const], writes=[PSR[4]])
                                op("dve", lambda e: e.tensor_copy(
                                    QT[:, :, t * 128:(t + 1) * 128],
                                    ps4[:, 0:512].rearrange("p (j n) -> p j n", j=4)),
                                   reads=[PSR[4]], writes=[R_QT[t]])
                            for j in range(2):
                                op("pe", lambda e, j=j: e.transpose(ps5[:, j * 128:(j + 1) * 128],
                                                                    qkb[b][:, 512 + j * 128:512 + (j + 1) * 128],
                                                                    identb[:]),
                                   reads=[R_qkb[b], R_const], writes=[PSR[5]])
                            for u in range(2):
                                op("dve", lambda e, u=u: e.tensor_copy(
                                    KT[64 * u:64 * u + 64, :, u, tsl],
                                    ps5[64 * u:64 * u + 64, 0:256].rearrange("p (j n) -> p j n", j=2)),
                                   reads=[PSR[5], R_ve1], writes=[R_KT[T]])

                        a1_front(0)
                        a1_front_b(0)
                        for T in range(NT):
                            if T + 1 < NT:
                                a1_front(T + 1)
                            a1_back(T, "stats")
                            if T + 1 < NT:
                                a1_front_b(T + 1)
                            a1_back_rest(T)
                        K.barrier()
                        if stop_after == "A1":
                            if debug:
                                dma("sp", d_QT, QT[:]); dma("sp", d_KT[0:64], KT[0:64, :, 0, :]); dma("sp", d_KT[64:128], KT[64:128, :, 1, :]); dma("sp", d_V, VE[:, :, :, 0:65])
                            K.barrier()
                            return nc, list(dbg.keys())
                    with ExitStack() as sA2:
                        wc = [sb(sA2, f"wc{i}", [128, 8, 384], BF16) for i in range(2)]
                        vT = [sb(sA2, f"vT{i}", [128, SEQ + 2], F32) for i in range(2)]
                        gbT = [sb(sA2, f"gbT{i}", [128, SEQ], F32) for i in range(2)]
                        gct = [sb(sA2, f"gct{i}", [128, 512], F32) for i in range(2)]
                        yb = sb(sA2, "yb", [128, 1024], F32)
                        ysq = sb(sA2, "ysq", [128, 1024], F32)
                        rs = sb(sA2, "rs", [128, 1024], F32)
                        R_wc = K.regs_n(2, "wc")
                        P_wc = [[K.pslot(f"wc{i}_{j}") for j in range(3)] for i in range(2)]
                        R_vT = K.regs_n(2, "vT")
                        R_gbT = K.regs_n(2, "gbT")
                        R_gct = K.regs_n(2, "gct")
                        R_y = K.reg("y")
                        R_ysq = K.reg("ysq")
                        R_rs = K.reg("rs")
                        winv = w_in_d.rearrange("(k p) n -> p k n", p=128)
                        for i in range(2):
                            op("dve", lambda e, i=i: e.memset(vT[i][:, 0:1], 0.0), writes=[R_vT[i]])
                            op("dve", lambda e, i=i: e.memset(vT[i][:, SEQ + 1:SEQ + 2], 0.0), writes=[R_vT[i]])
                        cnt2 = {'gi': 0, 'pi': 0}
                        def a2_mm(c4):
                            b = c4 % 2
                            for s3 in range(3):
                                c0 = 768 + s3 * 512 + c4 * 128
                                K.pdma(P_wc[b][s3], wc[b][:, :, s3 * 128:(s3 + 1) * 128], winv[:, :, c0:c0 + 128],
                                       writes=[R_wc[b]])
                            for tb_ in range(4):
                                tok = slice(256 + tb_ * 512, 256 + (tb_ + 1) * 512)
                                osl = slice(tb_ * 512, (tb_ + 1) * 512)
                                g_ = cnt2['gi'] % 2
                                cnt2['gi'] += 1
                                for s3 in range(3):
                                    pb = 5 + cnt2['pi'] % 3
                                    cnt2['pi'] += 1
                                    for k in range(8):
                                        op("pe", lambda e, k=k, s3=s3, pb=pb: e.matmul(
                                            PS[pb], wc[b][:, k, s3 * 128:(s3 + 1) * 128], hT[:, k, tok],
                                            start=(k == 0), stop=(k == 7)),
                                           reads=[R_wc[b]], writes=[PSR[pb]])
                                    if s3 == 0:
                                        op("act", lambda e, pb=pb: e.activation(gbT[b][:, osl], PS[pb], AF.Copy),
                                           reads=[PSR[pb]], writes=[R_gbT[b]])
                                    elif s3 == 1:
                                        op("act", lambda e, pb=pb: e.activation(gct[g_][:], PS[pb], AF.Copy),
                                           reads=[PSR[pb]], writes=[R_gct[g_]])
                                    else:
                                        op("dve", lambda e, pb=pb: e.tensor_tensor(
                                            vT[b][:, 1 + tb_ * 512:1 + (tb_ + 1) * 512], PS[pb], gct[g_][:], ALU.mult),
                                           reads=[PSR[pb], R_gct[g_]], writes=[R_vT[b]])

                        def a2_fin(c4):
                            b = c4 % 2
                            for hf in range(2):
                                o = hf * 1024
                                op("dve", lambda e: e.tensor_scalar(yb[:], vT[b][:, o:o + 1024],
                                                                    cw[:, c4 * 3:c4 * 3 + 1], None, ALU.mult),
                                   reads=[R_vT[b], R_const], writes=[R_y])
                                op("dve", lambda e: e.scalar_tensor_tensor(
                                    yb[:], vT[b][:, o + 1:o + 1025], cw[:, c4 * 3 + 1:c4 * 3 + 2], yb[:],
                                    ALU.mult, ALU.add), reads=[R_vT[b], R_const, R_y], writes=[R_y])
                                op("dve", lambda e: e.scalar_tensor_tensor(
                                    yb[:], vT[b][:, o + 2:o + 1026], cw[:, c4 * 3 + 2:c4 * 3 + 3], yb[:],
                                    ALU.mult, ALU.add), reads=[R_vT[b], R_const, R_y], writes=[R_y])
                                op("dve", lambda e: e.tensor_tensor(yb[:], yb[:], gbT[b][:, o:o + 1024], ALU.mult),
                                   reads=[R_y, R_gbT[b]], writes=[R_y])
                                op("act", lambda e: e.activation(ysq[:], yb[:], AF.Square),
                                   reads=[R_y], writes=[R_ysq])
                                for q2 in range(2):
                                    op("pe", lambda e, q2=q2: e.matmul(PS[q2], bd64[:], ysq[:, q2 * 512:(q2 + 1) * 512],
                                                                     start=True, stop=True),
                                       reads=[R_ysq, R_const], writes=[PSR[q2]])
                                    op("act", lambda e, q2=q2: e.activation(rs[:, q2 * 512:(q2 + 1) * 512], PS[q2],
                                                                            AF.Ln, bias=EPS),
                                       reads=[PSR[q2]], writes=[R_rs])
                                op("act", lambda e: e.activation(rs[:], rs[:], AF.Exp, scale=-0.5),
                                   reads=[R_rs], writes=[R_rs])
                                op("dve", lambda e: e.tensor_tensor(convn[:, c4, o:o + 1024], yb[:], rs[:], ALU.mult),
                                   reads=[R_y, R_rs], writes=[R_convn[c4]])

                        a2_mm(0)
                        for c4 in range(4):
                            if c4 + 1 < 4:
                                a2_mm(c4 + 1)
                            a2_fin(c4)
                        K.barrier()
                if debug:
                    dma("sp", d_QT, QT[:], reads=R_QT)
                    dma("sp", d_KT[0:64], KT[0:64, :, 0, :], reads=R_KT)
                    dma("sp", d_KT[64:128], KT[64:128, :, 1, :], reads=R_KT)
                    dma("sp", d_V, VE[:, :, :, 0:65], reads=R_VE)
                    dma("sp", d_convn, convn[:], reads=R_convn)
                if stop_after == "A2":
                    K.barrier()
                    return nc, list(dbg.keys())

                with ExitStack() as sB2:
                    attnT = sb(sB2, "attnT", [128, 4, SEQ], BF16)
                    R_attn = K.regs_n(8, "attn")
                    woall = sb(sB2, "woall", [128, 8, D], BF16)
                    gall = sb(sB2, "gall", [128, 8], F32)
                    R_wst = K.reg("wst")
                    R_wo = K.reg("wo")
                    with ExitStack() as sBa:
                        PT = [sb(sBa, f"PT{i}", [128, 1024], BF16) for i in range(3)]
                        sqb = [sb(sBa, f"sqb{i}", [65, 512], F32) for i in range(2)]
                        lnb = [sb(sBa, f"lnb{i}", [64, 512], F32) for i in range(2)]
                        R_PT = K.regs_n(3, "PT")
                        R_sqb = K.regs_n(2, "sqb")
                        R_lnb = K.regs_n(2, "lnb")
                        wst = sb(sBa, "wst", [128, 4, D], F32)
                        dma("sp", gall[:], gall_d, writes=[R_wo])
                        wov = w_out_d.rearrange("(c p) n -> p c n", p=128)
                        for half in range(2):
                            dma("sp", wst[:], wov[:, half * 4:(half + 1) * 4, :], writes=[R_wst])
                            op("dve", lambda e: e.tensor_tensor(
                                woall[:, half * 4:(half + 1) * 4, :], wst[:],
                                gall[:, half * 4:(half + 1) * 4].unsqueeze(2).to_broadcast([128, 4, D]), ALU.mult),
                               reads=[R_wst, R_wo], writes=[R_wo])
                        zt = sb(sBa, "zt", [128, 8, D], BF16)
                        R_zt = K.reg("zt")
                        op("dve", lambda e: e.memset(zt[:], 0.0), writes=[R_zt])
                        for i in range(NE * CAP // 1024):
                            dma("sp", xbuf_d[i * 1024:(i + 1) * 1024, :].rearrange("(p j) d -> p j d", j=8), zt[:],
                                reads=[R_zt])
                        dma("sp", xbuf_d[NE * CAP:NE * CAP + 1, :], zt[0:1, 0, :], reads=[R_zt])
                        it = 0
                        pti = 0
                        spi = 0
                        for j in range(4):
                            g = j // 2
                            for qb in range(4):
                                qs = slice(qb * 512, (qb + 1) * 512)
                                obase = 4 + 2 * (it % 2)
                                it += 1
                                Oab = [PS[obase], PS[obase + 1]]

                                def s_step(kt, sp):
                                    for u in range(2):
                                        p0 = 64 * u
                                        bk = 2 * sp + u
                                        op("pe", lambda e: e.matmul(
                                            PS[bk], KT[:, g, u, kt * 128:(kt + 1) * 128],
                                            QT[:, j, qs], start=True, stop=True),
                                           writes=[PSR[2 * sp], PSR[2 * sp + 1]] if u == 0 else [PSR[bk]])
                                sp_of = {0: spi % 2}
                                spi += 1
                                s_step(0, sp_of[0])
                                for kt in range(NT):
                                    if kt + 1 < NT:
                                        sp_of[kt + 1] = spi % 2
                                        spi += 1
                                        s_step(kt + 1, sp_of[kt + 1])
                                    sp = sp_of[kt]
                                    pb_ = pti % 3
                                    pti += 1
                                    op("act", lambda e: e.activation(
                                        PT[pb_][:], PSUM[:, 2 * sp * 512:(2 * sp + 2) * 512], AF.Exp, scale=0.125),
                                       reads=[PSR[2 * sp], PSR[2 * sp + 1]], writes=[R_PT[pb_]])
                                    for u in range(2):
                                        op("pe", lambda e: e.matmul(Oab[u][:, :], VE[:, kt, g, :],
                                                                    PT[pb_][:, u * 512:(u + 1) * 512],
                                                                    start=(kt == 0), stop=(kt == NT - 1)),
                                           reads=[R_PT[pb_]], writes=[PSR[obase + u]])
                                sp = spi % 2
                                spi += 1
                                for u in range(2):
                                    op("act", lambda e: e.activation(sqb[u][:], Oab[u][0:65, :], AF.Square),
                                       reads=[PSR[obase + u]], writes=[R_sqb[u]])
                                    op("pe", lambda e: e.matmul(PS[2 * sp + u][0:64, :], c65[:, :], sqb[u][:],
                                                                start=True, stop=True),
                                       reads=[R_sqb[u], R_const], writes=[PSR[2 * sp + u]])
                                for u in range(2):
                                    op("act", lambda e: e.activation(lnb[u][:], PS[2 * sp + u][0:64, :], AF.Ln),
                                       reads=[PSR[2 * sp + u]], writes=[R_lnb[u]])
                                    op("act", lambda e: e.activation(lnb[u][:], lnb[u][:], AF.Exp, scale=-0.5),
                                       reads=[R_lnb[u]], writes=[R_lnb[u]])
                                    op("dve", lambda e: e.tensor_tensor(attnT[64 * u:64 * u + 64, j, qs], Oab[u][0:64, :],
                                                                        lnb[u][:], ALU.mult),
                                       reads=[PSR[obase + u], R_lnb[u]], writes=[R_attn[2 * j + u]])
                        K.barrier()
                    if debug:
                        for u in range(2):
                            dma("sp", d_attn.rearrange("d (j u) s -> d u j s", u=2)[:, u], attnT[64 * u:64 * u + 64, :, :],
                                reads=R_attn)
                    if stop_after == "B":
                        K.barrier()
                        return nc, list(dbg.keys())

                    if True:
                        with ExitStack() as sB3:
                            xr = [sb(sB3, "xr0", [128, D], F32)] * 2
                            xnw = [sb(sB3, "xnw0", [128, D], F32)] * 2
                            hx = [sb(sB3, f"hx{i}", [128, D], F32) for i in range(2)]
                            hxT = [sb(sB3, "hxT0", [128, 8, 128], F32)] * 2
                            st2 = sb(sB3, "st2", [128, 3, NL], F32)
                            lg = sb(sB3, "lg", [128, NL, 36], F32)
                            rt = sb(sB3, "rt", [128, 64], F32)
                            RB = sb(sB3, "RB", [128, 16, NL, 8], F32)
                            m8 = sb(sB3, "m8", [128, 8], F32)
                            ustr = sb(sB3, "ustr", [128, 128], BF16)
                            onesb = sb(sB3, "onesb", [128, 128], BF16)
                            ustf = sb(sB3, "ustf", [128, 128], F32)
                            eC = sb(sB3, "eC", [128, NE], F32)
                            maskb = sb(sB3, "maskb", [128, NL, NE], BF16)
                            mkk = sb(sB3, "mkk", [128, 2, NL, NE], F32)
                            rkk = sb(sB3, "rkk", [128, 2, NL, NE], F32)
                            t3 = sb(sB3, "t3", [128, NL, NE], F32)
                            dfa = sb(sB3, "dfa", [128, 3, 2, NL], F32)
                            hxb = [sb(sB3, f"hxb{i}", [128, D], BF16) for i in range(2)]
                            R_mask = K.regs_n(NL, "mask")
                            R_rk = K.reg("rk")
                            R_hxb = K.regs_n(2, "hxb")
                            hxs = [sb(sB3, f"hxs{i}", [128, D], BF16) for i in range(4)]
                            R_hxs = K.regs_n(4, "hxs")
                            R_xbuf = K.reg("xbuf")
                            dma("sp", ustf[:], ustr_d, writes=[R_wo])
                            dma("sp", eC[:], eC_d, writes=[R_wo])
                            op("dve", lambda e: e.tensor_copy(ustr[:], ustf[:]), reads=[R_wo], writes=[R_wo])
                            op("dve", lambda e: e.memset(onesb[:], 1.0), writes=[R_wo])
                            R_xr = [K.reg("xr")] * 2
                            R_xnw = [K.reg("xnw")] * 2
                            R_hx = K.regs_n(2, "hx")
                            R_hxT = [K.reg("hxT")] * 2
                            R_st2 = K.regs_n(NL, "st2")
                            R_lg = K.reg("lg")
                            R_rt = K.reg("rt")
                            def b2_front(t):
                                b = t % 2
                                tsl = slice(t * 128, (t + 1) * 128)
                                dma("sp", xr[b][:], xin[256 + t * 128:256 + (t + 1) * 128, :], writes=[R_xr[b]])
                                for hf in range(2):
                                    pb = hf + 6 * (t % 2)
                                    cs = slice(hf * 512, (hf + 1) * 512)
                                    for c in range(4):
                                        op("pe", lambda e, c=c: e.matmul(PS[pb], attnT[:, c, tsl], woall[:, c, cs],
                                                                       start=(c == 0), stop=False),
                                           reads=[R_wo], writes=[PSR[pb]])
                                    for c4 in range(4):
                                        op("pe", lambda e, c4=c4: e.matmul(PS[pb], convn[:, c4, tsl], woall[:, 4 + c4, cs],
                                                                         start=False, stop=(c4 == 3)),
                                           reads=[R_wo], writes=[PSR[pb]])
                                    op("dve", lambda e: e.tensor_tensor(xnw[b][:, cs], PS[pb], modb[:, 2 * D + hf * 512:
                                                                                                  2 * D + (hf + 1) * 512],
                                                                        ALU.mult),
                                       reads=[PSR[pb]], writes=[R_xnw[b]])
                                op("dve", lambda e: e.tensor_tensor(xnw[b][:], xnw[b][:], xr[b][:], ALU.add),
                                   reads=[R_xnw[b], R_xr[b]], writes=[R_xnw[b]])
                                dma("sp", xnew_d[tsl, :], xnw[b][:], reads=[R_xnw[b]])
                                op("act", lambda e: e.activation(hxb[b][:], xnw[b][:], AF.Square,
                                                                 accum_out=st2[:, 0, t:t + 1]),
                                   reads=[R_xnw[b]], writes=[R_st2[t], R_hxb[b]])
                                op("act", lambda e: e.activation(st2[:, 1, t:t + 1], st2[:, 0, t:t + 1], AF.Ln,
                                                                 scale=1.0 / D, bias=EPS),
                                   reads=[R_st2[t]], writes=[R_st2[t]])
                                op("act", lambda e: e.activation(st2[:, 2, t:t + 1], st2[:, 1, t:t + 1], AF.Exp,
                                                                 scale=-0.5),
                                   reads=[R_st2[t]], writes=[R_st2[t]])
                                op("dve", lambda e: e.scalar_tensor_tensor(
                                    hx[b][:], xnw[b][:], st2[:, 2, t:t + 1], modb[:, 4 * D:5 * D], ALU.mult, ALU.mult),
                                   reads=[R_xnw[b], R_st2[t]], writes=[R_hx[b]])
                                op("dve", lambda e: e.tensor_tensor(hx[b][:], hx[b][:], modb[:, 3 * D:4 * D], ALU.add),
                                   reads=[R_hx[b]], writes=[R_hx[b]])

                            def b2_back(t):
                                b = t % 2
                                tsl = slice(t * 128, (t + 1) * 128)
                                PP = PSUM[:, 2 * 512:4 * 512]
                                for k in range(8):
                                    op("pe", lambda e, k=k: e.transpose(PP[:, k * 128:(k + 1) * 128],
                                                                        hx[b][:, k * 128:(k + 1) * 128], identf[:]),
                                       reads=[R_hx[b], R_const], writes=[PSR[2], PSR[3]])
                                op("act", lambda e: e.activation(
                                    hxT[b][:], PP.rearrange("p (k n) -> p k n", k=8), AF.Copy),
                                   reads=[PSR[2], PSR[3]], writes=[R_hxT[b]])
                                op("act", lambda e: e.activation(hxb[b][:], hx[b][:], AF.Copy),
                                   reads=[R_hx[b]], writes=[R_hxb[b]])
                                for k in range(8):
                                    op("pe", lambda e, k=k: e.matmul(PS[4][:, 0:36], hxT[b][:, k, :], wr[:, k, :],
                                                                   start=(k == 0), stop=(k == 7)),
                                       reads=[R_hxT[b], R_const], writes=[PSR[4]])
                                op("act", lambda e: e.activation(lg[:, t, :], PS[4][:, 0:36], AF.Copy),
                                   reads=[PSR[4]], writes=[R_lg])
                                dma("sp", hx2_d[tsl, :], hxb[b][:], reads=[R_hxb[b]], writes=[R_xbuf])

                            b2_front(0)
                            for t in range(NL):
                                if t + 1 < NL:
                                    b2_front(t + 1)
                                b2_back(t)
                            def slab(i, w=8):
                                return RB[:, i, :, 0:w]

                            def col(i):
                                return RB[:, i, :, 0]

                            def bc(a2, w):
                                return a2.unsqueeze(2).to_broadcast([128, NL, w])
                            R_B = K.reg("RB")

                            def dv(fn, rd=(), wr_=()):
                                op("dve", fn, reads=[R_lg, R_B] + list(rd), writes=[R_B] + list(wr_))

                            def ac(fn):
                                op("act", fn, reads=[R_lg, R_B], writes=[R_B])
                            G4 = lg[:, :, 0:4]
                            dv(lambda e: e.tensor_reduce(col(0), G4, AX.X, ALU.max))
                            dv(lambda e: e.tensor_tensor(slab(1, 4), G4, bc(col(0), 4), ALU.is_equal))
                            dv(lambda e: e.tensor_tensor(slab(2, 4), G4, bc(col(0), 4), ALU.subtract))
                            ac(lambda e: e.activation(slab(2, 4), slab(2, 4), AF.Exp))
                            dv(lambda e: e.tensor_reduce(col(3), slab(2, 4), AX.X, ALU.add))
                            dv(lambda e: e.reciprocal(col(4), col(3)))
                            dv(lambda e: e.tensor_tensor(slab(5), lg[:, :, 4:12], bc(RB[:, 1, :, 0], 8), ALU.mult))
                            for gq in range(1, 4):
                                dv(lambda e, gq=gq: e.tensor_tensor(slab(6), lg[:, :, 4 + 8 * gq:12 + 8 * gq],
                                                                    bc(RB[:, 1, :, gq], 8), ALU.mult))
                                dv(lambda e: e.tensor_tensor(slab(5), slab(5), slab(6), ALU.add))
                            dv(lambda e: e.tensor_reduce(col(7), slab(5), AX.X, ALU.max))
                            dv(lambda e: e.tensor_tensor(slab(8), slab(5), bc(col(7), 8), ALU.is_equal))
                            dv(lambda e: e.scalar_tensor_tensor(slab(9), slab(8), -1.0e30, slab(5), ALU.mult, ALU.add))
                            dv(lambda e: e.tensor_reduce(col(10), slab(9), AX.X, ALU.max))
                            dv(lambda e: e.tensor_tensor(slab(11), slab(5), bc(col(10), 8), ALU.is_ge))
                            dv(lambda e: e.tensor_tensor(slab(12), slab(11), slab(8), ALU.subtract))
                            dv(lambda e: e.tensor_tensor(slab(13), slab(5), bc(col(7), 8), ALU.subtract))
                            ac(lambda e: e.activation(slab(13), slab(13), AF.Exp))
                            dv(lambda e: e.tensor_tensor(slab(13), slab(13), slab(11), ALU.mult))
                            dv(lambda e: e.tensor_reduce(col(14), slab(13), AX.X, ALU.add))
                            dv(lambda e: e.reciprocal(col(15), col(14)))
                            dv(lambda e: e.tensor_tensor(col(15), col(15), col(4), ALU.mult))
                            dv(lambda e: e.tensor_tensor(slab(13), slab(13), bc(col(15), 8), ALU.mult))
                            for gq in range(4):
                                ohg = bc(RB[:, 1, :, gq], 8)
                                dv(lambda e, gq=gq, ohg=ohg: e.tensor_tensor(wgt[:, :, gq * 8:(gq + 1) * 8], slab(13), ohg,
                                                                             ALU.mult), wr_=R_wgt)
                                dv(lambda e, gq=gq, ohg=ohg: e.tensor_tensor(mkk[:, 0, :, gq * 8:(gq + 1) * 8], slab(8), ohg,
                                                                             ALU.mult))
                                dv(lambda e, gq=gq, ohg=ohg: e.tensor_tensor(mkk[:, 1, :, gq * 8:(gq + 1) * 8], slab(12), ohg,
                                                                             ALU.mult))
                            dv(lambda e: e.tensor_tensor(maskb[:], mkk[:, 0], mkk[:, 1], ALU.add), wr_=[R_mask[0]])
                            for t in range(NL):
                                op("pe", lambda e, t=t: e.matmul(PS[5][:, t * NE:(t + 1) * NE], ustr[:], maskb[:, t, :],
                                                               start=True, stop=(t == 0)),
                                   reads=[R_mask[0], R_wo], writes=[PSR[5]])
                                for t2 in range(t):
                                    op("pe", lambda e, t=t, t2=t2: e.matmul(PS[5][:, t * NE:(t + 1) * NE], onesb[:],
                                                                           maskb[:, t2, :], start=False, stop=(t2 == t - 1)),
                                       reads=[R_mask[0], R_wo], writes=[PSR[5]])
                            op("act", lambda e: e.activation(rkk[:, 0].rearrange("p t e -> p (t e)"), PS[5], AF.Copy),
                               reads=[PSR[5]], writes=[R_rk])
                            op("dve", lambda e: e.tensor_tensor(rkk[:, 1], rkk[:, 0],
                                                                eC[:, :].unsqueeze(1).to_broadcast([128, NL, NE]), ALU.add),
                               reads=[R_rk, R_wo], writes=[R_rk])
                            for kk in range(2):
                                for src_i, di in ((1, 0), (0, 1)):
                                    op("dve", lambda e, kk=kk, src_i=src_i: e.tensor_tensor(
                                        t3[:], mkk[:, kk], rkk[:, src_i], ALU.mult), reads=[R_rk, R_B], writes=[R_B])
                                    op("dve", lambda e, kk=kk, di=di: e.tensor_reduce(
                                        dfa[:, di, kk, :], t3[:], AX.X, ALU.add), reads=[R_B], writes=[R_B])
                                op("dve", lambda e, kk=kk: e.tensor_tensor(t3[:], mkk[:, kk], wgt[:], ALU.mult),
                                   reads=[R_B] + R_wgt, writes=[R_B])
                                op("dve", lambda e, kk=kk: e.tensor_reduce(wsel[:, :, kk], t3[:], AX.X, ALU.add),
                                   reads=[R_B], writes=R_dest)
                            dv(lambda e: e.tensor_scalar(dfa[:, 2], dfa[:, 1], float(CAP), None, ALU.is_ge))
                            dv(lambda e: e.scalar_tensor_tensor(dfa[:, 0], dfa[:, 2], 1.0e6, dfa[:, 0], ALU.mult, ALU.add))
                            dv(lambda e: e.tensor_scalar(dfa[:, 0], dfa[:, 0], float(NE * CAP), None, ALU.min))
                            dv(lambda e: e.tensor_copy(dest[:].rearrange("p t k -> p k t"), dfa[:, 0]), wr_=R_dest)
                            for t in range(NL):
                                b = t % 4
                                dma("sp", hxs[b][:], hx2_d[t * 128:(t + 1) * 128, :], reads=[R_xbuf], writes=[R_hxs[b]])
                                for kk in range(2):
                                    K.pdma(None, None, None, reads=[R_hxs[b]] + R_dest, writes=[],
                                           fn=lambda g_, kk=kk, t=t: g_.indirect_dma_start(
                                               out=xbuf_d[:, :],
                                               out_offset=bass.IndirectOffsetOnAxis(ap=dest[:, t, kk:kk + 1], axis=0),
                                               in_=hxs[b][:, :], in_offset=None))
                            K.barrier()
                        if debug:
                            dma("sp", d_wgt, wgt[:], reads=R_wgt)
                        if stop_after == "B2":
                            K.barrier()
                            return nc, list(dbg.keys())

        with ExitStack() as sM:
            NWB = 3
            wgs = [sb(sM, f"wgs{i}", [128, 8, DE], BF16) for i in range(NWB)]
            wus = [sb(sM, f"wus{i}", [128, 8, DE], BF16) for i in range(NWB)]
            wds = [sb(sM, f"wds{i}", [128, 6, D], BF16) for i in range(NWB)]
            Xs = [sb(sM, f"Xs{i}", [128, NJ, D], BF16) for i in range(2)]
            XT = [sb(sM, f"XT{i}", [128, 8, CAP], BF16) for i in range(2)]
            HT = [sb(sM, f"HT{i}", [128, 6, CAP], BF16) for i in range(2)]
            sg = [sb(sM, f"sg{i}", [128, CAP], F32) for i in range(2)]
            Yst = [sb(sM, f"Yst{i}", [128, D], F32) for i in range(2)]
            R_wg = K.regs_n(NWB, "wg")
            R_wu = K.regs_n(NWB, "wu")
            R_wd = K.regs_n(NWB, "wd")
            R_Xs = K.regs_n(2, "Xs")
            R_XT = K.regs_n(2, "XT")
            R_HT = K.regs_n(2, "HT")
            R_sg = K.regs_n(2, "sg")
            R_Yst = K.regs_n(2, "Yst")
            cnt = {"si": 0, "yi": 0, "ti": 0}
            op("dve", lambda e: e.memset(Yst[0][0:1, :], 0.0), writes=[R_Yst[0]])
            dma("sp", ybuf_d[NE * CAP:NE * CAP + 1, :], Yst[0][0:1, :], reads=[R_Yst[0]])

            def load_w(ex):
                w3 = ex % NWB
                K.pdma(None, wgs[w3][:], wg_d[ex].rearrange("(k p) f -> p k f", p=128), writes=[R_wg[w3]])
                K.pdma(None, wus[w3][:], wu_d[ex].rearrange("(k p) f -> p k f", p=128), writes=[R_wu[w3]])
                K.pdma(None, wds[w3][:], wd_d[ex].rearrange("(c p) n -> p c n", p=128), writes=[R_wd[w3]])

            def load_x(ex):
                xb_ = ex % 2
                dma("sp", Xs[xb_][:], xbuf_d[ex * CAP:(ex + 1) * CAP, :].rearrange("(j p) d -> p j d", p=128),
                    writes=[R_Xs[xb_]])

            def transposes(ex):
                xb_ = ex % 2
                for j in range(NJ):
                    tbk = 6 + cnt["ti"] % 2
                    cnt["ti"] += 1
                    psb = PS[tbk].bitcast(BF16)
                    for k in range(8):
                        op("pe", lambda e, k=k: e.transpose(psb[:, k * 128:(k + 1) * 128],
                                                            Xs[xb_][:, j, k * 128:(k + 1) * 128], identb[:]),
                           reads=[R_Xs[xb_], R_const], writes=[PSR[tbk]])
                    if tbk == 6:
                        op("dve", lambda e: e.tensor_copy(XT[xb_][:, :, j * 128:(j + 1) * 128],
                                                          psb.rearrange("p (k n) -> p k n", k=8)),
                           reads=[PSR[tbk]], writes=[R_XT[xb_]])
                    else:
                        op("act", lambda e: e.activation(XT[xb_][:, :, j * 128:(j + 1) * 128],
                                                         psb.rearrange("p (k n) -> p k n", k=8), AF.Copy),
                           reads=[PSR[tbk]], writes=[R_XT[xb_]])

            load_w(0)
            load_w(1)
            load_x(0)
            transposes(0)
            for ex in range(NE):
                wb = ex % 2
                w3 = ex % NWB
                if ex + 2 < NE:
                    load_w(ex + 2)
                if ex + 1 < NE:
                    load_x(ex + 1)
                for fc in range(6):
                    fs = slice(fc * 128, (fc + 1) * 128)
                    gbk = fc % 2
                    ubk = 2 + fc % 2
                    for k in range(8):
                        op("pe", lambda e, k=k: e.matmul(PS[gbk][:, 0:CAP], wgs[w3][:, k, fs], XT[wb][:, k, :],
                                                       start=(k == 0), stop=(k == 7)),
                           reads=[R_wg[w3], R_XT[wb]], writes=[PSR[gbk]])
                    for k in range(8):
                        op("pe", lambda e, k=k: e.matmul(PS[ubk][:, 0:CAP], wus[w3][:, k, fs], XT[wb][:, k, :],
                                                       start=(k == 0), stop=(k == 7)),
                           reads=[R_wu[w3], R_XT[wb]], writes=[PSR[ubk]])
                    sb2 = cnt["si"] % 2
                    cnt["si"] += 1
                    op("act", lambda e: e.activation(sg[sb2][:], PS[gbk][:, 0:CAP], AF.Silu),
                       reads=[PSR[gbk]], writes=[R_sg[sb2]])
                    op("dve", lambda e: e.tensor_tensor(HT[wb][:, fc, :], PS[ubk][:, 0:CAP], sg[sb2][:], ALU.mult),
                       reads=[PSR[ubk], R_sg[sb2]], writes=[R_HT[wb]])
                if ex + 1 < NE:
                    transposes(ex + 1)
                for j in range(NJ):
                    ys = cnt["yi"] % 2
                    cnt["yi"] += 1
                    for hf in range(2):
                        yb_ = 4 + hf
                        cs = slice(hf * 512, (hf + 1) * 512)
                        for fc in range(6):
                            op("pe", lambda e, fc=fc: e.matmul(
                                PS[yb_], HT[wb][:, fc, j * 128:(j + 1) * 128], wds[w3][:, fc, cs],
                                start=(fc == 0), stop=(fc == 5)),
                               reads=[R_HT[wb], R_wd[w3]], writes=[PSR[yb_]])
                        op("dve", lambda e: e.tensor_copy(Yst[ys][:, cs], PS[yb_]),
                           reads=[PSR[yb_]], writes=[R_Yst[ys]])
                    dma("sp", ybuf_d[ex * CAP + j * 128:ex * CAP + (j + 1) * 128, :], Yst[ys][:],
                        reads=[R_Yst[ys]])
            K.barrier()
        with ExitStack() as sF:
            y12 = [[sb(sF, f"y{k}_{i}", [128, D], F32) for k in range(2)] for i in range(4)]
            xr2 = [sb(sF, f"xq{i}", [128, D], F32) for i in range(2)]
            ot = [sb(sF, f"ot{i}", [128, D], F32) for i in range(2)]
            junk3 = sb(sF, "junk3", [128, D], BF16)
            st3 = sb(sF, "st3", [128, 3, NL], F32)
            R_y12 = [K.regs_n(2, f"y12_{i}") for i in range(4)]
            R_xr2 = K.regs_n(2, "xr2")
            R_ot = K.regs_n(2, "ot")
            R_st3 = K.regs_n(NL, "st3")
            def fin_front(tg):
                b = tg % 2
                tsl = slice(tg * 128, (tg + 1) * 128)
                dma("sp", xr2[b][:], xnew_d[tsl, :], writes=[R_xr2[b]])
                for kk in range(2):
                    K.pdma(None, None, None, reads=[], writes=[R_y12[tg % 4][kk]],
                           fn=lambda g, kk=kk: g.indirect_dma_start(
                               out=y12[tg % 4][kk][:, :], out_offset=None, in_=ybuf_d[:, :],
                               in_offset=bass.IndirectOffsetOnAxis(ap=dest[:, tg, kk:kk + 1], axis=0)))
                op("dve", lambda e: e.tensor_scalar(ot[b][:], y12[tg % 4][0][:], wsel[:, tg, 0:1], None, ALU.mult),
                   reads=[R_y12[tg % 4][0]], writes=[R_ot[b]])
                op("dve", lambda e: e.scalar_tensor_tensor(ot[b][:], y12[tg % 4][1][:], wsel[:, tg, 1:2], ot[b][:],
                                                          ALU.mult, ALU.add),
                   reads=[R_y12[tg % 4][1], R_ot[b]], writes=[R_ot[b]])
                if debug:
                    dma("sp", d_acc[:, tg, :], ot[b][:], reads=[R_ot[b]])
                op("dve", lambda e: e.tensor_tensor(ot[b][:], ot[b][:], gt2b[:], ALU.mult),
                   reads=[R_ot[b], R_gt2], writes=[R_ot[b]])
                op("dve", lambda e: e.tensor_tensor(ot[b][:], ot[b][:], xr2[b][:], ALU.add),
                   reads=[R_ot[b], R_xr2[b]], writes=[R_ot[b]])

            def fin_back(tg):
                b = tg % 2
                tsl = slice(tg * 128, (tg + 1) * 128)
                op("act", lambda e: e.activation(junk3[:], ot[b][:], AF.Square, accum_out=st3[:, 0, tg:tg + 1]),
                   reads=[R_ot[b]], writes=[R_st3[tg]])
                op("act", lambda e: e.activation(st3[:, 1, tg:tg + 1], st3[:, 0, tg:tg + 1], AF.Ln,
                                                 scale=1.0 / D, bias=EPS),
                   reads=[R_st3[tg]], writes=[R_st3[tg]])
                op("act", lambda e: e.activation(st3[:, 2, tg:tg + 1], st3[:, 1, tg:tg + 1], AF.Exp, scale=-0.5),
                   reads=[R_st3[tg]], writes=[R_st3[tg]])
                op("dve", lambda e: e.scalar_tensor_tensor(
                    ot[b][:], ot[b][:], st3[:, 2, tg:tg + 1], fgb[:], ALU.mult, ALU.mult),
                   reads=[R_ot[b], R_st3[tg], R_const], writes=[R_ot[b]])
                dma("sp", y_d[tsl, :], ot[b][:], reads=[R_ot[b]])

            fin_front(0)
            for tg in range(NL):
                if tg + 1 < NL:
                    fin_front(tg + 1)
                fin_back(tg)
            K.barrier()
    return nc, list(dbg.keys())


def _host_consts():
    ident = np.eye(128, dtype=np.float32)
    rows = SEQ // 64
    row_idx = np.repeat(np.arange(rows, dtype=np.float32), 64)
    col_idx = np.tile(np.arange(64, dtype=np.float32), rows)
    inv_freq = (np.float32(10000.0) ** (-np.arange(0, 32, 2, dtype=np.float32) / np.float32(32))).astype(np.float32)
    ang = np.stack([row_idx[:, None] * inv_freq, col_idx[:, None] * inv_freq], axis=1).astype(np.float32)
    cos = np.cos(ang).astype(np.float32).reshape(SEQ, 32)
    sin = np.sin(ang).astype(np.float32).reshape(SEQ, 32)
    cosT = np.ascontiguousarray(cos.reshape(NL, 128, 32).transpose(1, 0, 2))
    sinT = np.ascontiguousarray(sin.reshape(NL, 128, 32).transpose(1, 0, 2))
    bd64 = np.zeros((128, 128), np.float32)
    bd64[:64, :64] = 1.0 / 64
    bd64[64:, 64:] = 1.0 / 64
    c65 = np.full((65, 64), 1.0 / 64, np.float32)
    c65[64, :] = EPS
    e0 = np.zeros((128, 1), np.float32)
    e0[0, 0] = 1.0
    ustr = np.triu(np.ones((128, 128), np.float32), k=1)
    eC = np.ascontiguousarray(np.broadcast_to((np.arange(NE, dtype=np.float32) * CAP)[None, :], (128, NE)))
    return dict(ident_f=ident, cosT=cosT, sinT=sinT, bd64=bd64, c65=c65, e0=e0, ustr=ustr, eC=eC)


def make_in_maps(inputs, cores=range(8)):
    f = lambda a: np.ascontiguousarray(np.asarray(a, dtype=np.float32))
    x = f(inputs["x"]); c = f(inputs["c"]); ctx = f(inputs["ctx"]); c_ctx = f(inputs["c_ctx"])
    consts = _host_consts()
    bc = lambda v: np.ascontiguousarray(np.broadcast_to(f(v).reshape(1, -1), (128, f(v).size)))
    shared = dict(
        w_mod=f(inputs["w_mod"])[0], bm_b=bc(inputs["b_mod"][0]), g1_b=bc(inputs["norm1_g"][0]),
        g2_b=bc(inputs["norm2_g"][0]), fg_b=bc(inputs["final_g"]), w_in=f(inputs["w_in"])[0],
        qg_b=bc(inputs["q_norm_g"][0]), kg_b=bc(inputs["k_norm_g"][0]),
        cwT=np.ascontiguousarray(f(inputs["conv_w"])[0].reshape(3, 4, 128).transpose(2, 1, 0).reshape(128, 12)),
        gallT=np.ascontiguousarray(np.concatenate([f(inputs["attn_out_g"])[0], f(inputs["conv_out_g"])[0]])
                                   .reshape(8, 128).T),
        w_out=f(inputs["w_out"])[0],
        w_r=np.ascontiguousarray(np.concatenate([f(inputs["w_group"])[0], f(inputs["w_router"])[0]], axis=1)),
        w_gate=f(inputs["w_gate"])[0], w_up=f(inputs["w_up"])[0], w_down=f(inputs["w_down"])[0],
        **consts,
    )
    maps = []
    for b in cores:
        cT = np.concatenate([c[b].reshape(8, 128).T, c_ctx.reshape(8, 128).T], axis=1)
        m = dict(shared)
        m["xin"] = np.ascontiguousarray(np.concatenate([ctx[b], x[b]], axis=0))
        m["cT"] = np.ascontiguousarray(cT.astype(np.float32))
        maps.append(m)
    return maps


_CACHE = {}


def kernel(**inputs):
    if "nc" not in _CACHE:
        _CACHE["nc"] = build_program(debug=False)[0]
    nc = _CACHE["nc"]
    maps = make_in_maps(inputs)
    res = run_bass_kernel_spmd(nc, maps, core_ids=list(range(8)))
    out = np.stack([np.asarray(r["y"], dtype=np.float32) for r in res.results], axis=0)
    return out
```

```python
import os
import numpy as np
from contextlib import ExitStack
import concourse.bass as bass
import concourse.mybir as mybir
from concourse.bass_utils import run_bass_kernel_spmd

F32 = mybir.dt.float32
BF16 = mybir.dt.bfloat16
I32 = mybir.dt.int32
AF = mybir.ActivationFunctionType
ALU = mybir.AluOpType
AX = mybir.AxisListType

D = 1024
SEQ = 2048
CTX = 256
NTOK = SEQ + CTX
NT = NTOK // 128
NL = SEQ // 128
EPS = 1e-6
NE = 32
DE = 768
N_DSEM = 40
CAP = 512
NJ = CAP // 128
_LVL = int(os.environ.get('KLVL', '9'))
_KEV = os.environ.get('KEV', 'act')
_KENG = os.environ.get('KENG', 'pool')


class Reg:
    __slots__ = ("w", "r", "name")

    def __init__(self, name=""):
        self.w = None
        self.r = {}
        self.name = name


class Sem:
    def __init__(self, handle):
        self.handle = handle
        self.count = 0


class EngW:
    def __init__(self, name, eng, sem):
        self.name = name
        self.eng = eng
        self.sem = sem
        self.waited = {}


class KB:
    def __init__(self, nc, es):
        self.nc = nc
        self._es = es
        self.pslots = []
        self.regs = []
        mk = lambda n: Sem(es.enter_context(nc.semaphore(n)))
        self.E = {
            "pe": EngW("pe", nc.tensor, mk("s_pe")),
            "act": EngW("act", nc.scalar, mk("s_act")),
            "dve": EngW("dve", nc.vector, mk("s_dve")),
            "pool": EngW("pool", nc.gpsimd, mk("s_pool")),
            "sp": EngW("sp", nc.sync, mk("s_sp")),
        }
        self.dsems = [mk(f"s_d{i}") for i in range(N_DSEM)]
        self.dnext = 0
        self.psems = [mk(f"s_q{i}") for i in range(24)]
        self.pnext = 0

    def reg(self, name=""):
        r = Reg(name)
        self.regs.append(r)
        return r

    def regs_n(self, n, name=""):
        return [self.reg(f"{name}{i}") for i in range(n)]

    def wait(self, ew, tok):
        sem, val = tok
        if ew.waited.get(id(sem), 0) >= val:
            return
        ew.eng.wait_ge(sem.handle, val)
        ew.waited[id(sem)] = val

    def _deps(self, ew, reads, writes):
        for r in reads:
            if r.w is not None:
                if ew.name == "pe" and r.w[0] is ew.sem:
                    continue
                self.wait(ew, r.w)
        for w in writes:
            toks = list(w.r.values())
            if w.w is not None:
                toks.append(w.w)
            for t in toks:
                if ew.name == "pe" and t[0] is ew.sem:
                    continue
                self.wait(ew, t)

    def _record(self, tok, reads, writes):
        for r in reads:
            r.r[id(tok[0])] = tok
        for w in writes:
            w.w = tok
            w.r = {}

    def op(self, en, fn, reads=(), writes=()):
        ew = self.E[en]
        self._deps(ew, reads, writes)
        ins = fn(ew.eng)
        ew.sem.count += 1
        ins.then_inc(ew.sem.handle, 1)
        tok = (ew.sem, ew.sem.count)
        self._record(tok, reads, writes)
        return tok

    def dma(self, qn, out, in_, reads=(), writes=(), fn=None):
        ew = self.E[qn]
        self._deps(ew, reads, writes)
        d = self.dsems[self.dnext % N_DSEM]
        self.dnext += 1
        if d.count:
            self.wait(ew, (d, d.count))
        if fn is None:
            ins = ew.eng.dma_start(out=out, in_=in_)
        else:
            ins = fn(ew.eng)
        d.count += 16
        ins.then_inc(d.handle, 16)
        tok = (d, d.count)
        self._record(tok, reads, writes)
        return tok

    def pslot(self, name):
        return None

    def pdma(self, slot, out, in_, reads=(), writes=(), fn=None):
        ew = self.E["pool"]
        self._deps(ew, reads, writes)
        d = self.psems[self.pnext % len(self.psems)]
        self.pnext += 1
        if d.count:
            self.wait(ew, (d, d.count))
        ins = ew.eng.dma_start(out=out, in_=in_) if fn is None else fn(ew.eng)
        d.count += 16
        ins.then_inc(d.handle, 16)
        tok = (d, d.count)
        self._record(tok, reads, writes)
        return tok

    def barrier(self):
        for ew in self.E.values():
            for other in self.E.values():
                if other.sem.count and not (other is ew and ew.name in ("pe", "sp")):
                    self.wait(ew, (other.sem, other.sem.count))
            for d in self.dsems:
                if d.count:
                    self.wait(ew, (d, d.count))
            for d in self.psems:
                if d.count:
                    self.wait(ew, (d, d.count))
        for r in self.regs:
            r.w = None
            r.r = {}


def build_program(debug=False, stop_after=None):
    nc = bass.Bass("TRN2", target_bir_lowering=False)

    def din(name, shape, dt=F32):
        return nc.dram_tensor(name, list(shape), dt, kind="ExternalInput").ap()

    xin = din("xin", [NTOK, D])
    cT_d = din("cT", [128, 16])
    w_mod_d = din("w_mod", [D, 6 * D])
    bm_d = din("bm_b", [128, 6 * D])
    g1_d = din("g1_b", [128, D])
    g2_d = din("g2_b", [128, D])
    fg_d = din("fg_b", [128, D])
    w_in_d = din("w_in", [D, 2304])
    qg_d = din("qg_b", [128, 64])
    kg_d = din("kg_b", [128, 64])
    cw_d = din("cwT", [128, 12])
    gall_d = din("gallT", [128, 8])
    w_out_d = din("w_out", [D, D])
    wr_d = din("w_r", [D, 36])
    wg_d = din("w_gate", [NE, D, DE])
    wu_d = din("w_up", [NE, D, DE])
    wd_d = din("w_down", [NE, DE, D])
    identf_d = din("ident_f", [128, 128])
    cos_d = din("cosT", [128, NL, 32])
    sin_d = din("sinT", [128, NL, 32])
    bd64_d = din("bd64", [128, 128])
    c65_d = din("c65", [65, 64])
    e0_d = din("e0", [128, 1])
    ustr_d = din("ustr", [128, 128])
    eC_d = din("eC", [128, NE])

    y_d = nc.dram_tensor("y", [SEQ, D], F32, kind="ExternalOutput").ap()
    xnew_d = nc.dram_tensor("xnew_scr", [SEQ, D], F32,
                            kind="ExternalOutput" if debug else "Internal").ap()
    xbuf_d = nc.dram_tensor("xbuf_scr", [NE * CAP + 1, D], BF16, kind="Internal").ap()
    ybuf_d = nc.dram_tensor("ybuf_scr", [NE * CAP + 1, D], F32, kind="Internal").ap()
    hx2_d = nc.dram_tensor("hx2_scr", [SEQ, D], BF16, kind="Internal").ap()
    dbg = {}

    def dbg_out(name, shape, dt=F32):
        if debug:
            dbg[name] = nc.dram_tensor(name, list(shape), dt, kind="ExternalOutput").ap()
        return dbg.get(name)

    d_smod = dbg_out("d_smod", [128, 32])
    d_modb = dbg_out("d_modb", [128, 6 * D])
    d_QT = dbg_out("d_QT", [128, 4, SEQ], BF16)
    d_KT = dbg_out("d_KT", [128, 2, NTOK], BF16)
    d_V = dbg_out("d_V", [128, NT, 2, 65], BF16)
    d_convn = dbg_out("d_convn", [128, 4, SEQ], BF16)
    d_attn = dbg_out("d_attn", [64, 8, SEQ], BF16)
    d_wgt = dbg_out("d_wgt", [128, NL, NE])
    d_acc = dbg_out("d_acc", [128, NL, D])

    with ExitStack() as es:
        K = KB(nc, es)
        op, dma = K.op, K.dma

        def sb(stk, name, shape, dt):
            return stk.enter_context(nc.sbuf_tensor("sb_" + name, list(shape), dt))

        PSUM = es.enter_context(nc.psum_tensor("psum", [128, 8 * 512], F32))
        PS = [PSUM[:, i * 512:(i + 1) * 512] for i in range(8)]
        PSR = K.regs_n(8, "ps")

        identf = sb(es, "identf", [128, 128], F32)
        identb = sb(es, "identb", [128, 128], BF16)
        fgb = sb(es, "fgb", [128, D], F32)
        gt2b = sb(es, "gt2b", [128, D], F32)
        wgt = sb(es, "wgt", [128, NL, NE], F32)
        dest = sb(es, "dest", [128, NL, 2], I32)
        wsel = sb(es, "wsel", [128, NL, 2], F32)
        R_dest = K.regs_n(NL, "dest")
        R_const = K.reg("const")
        R_gt2 = K.reg("gt2")
        R_wgt = K.regs_n(NL, "wgt")

        dma("sp", identf[:], identf_d, writes=[R_const])
        dma("sp", fgb[:], fg_d, writes=[R_const])
        op("dve", lambda e: e.tensor_copy(identb[:], identf[:]), reads=[R_const], writes=[R_const])

        with ExitStack() as sAB:
            modb = sb(sAB, "modb", [128, 6 * D], F32)
            smod = sb(sAB, "smod", [128, 32], F32)
            cosT = sb(sAB, "cosT", [128, NL, 32], F32)
            sinT = sb(sAB, "sinT", [128, NL, 32], F32)
            qgb = sb(sAB, "qgb", [128, 64], F32)
            kgb = sb(sAB, "kgb", [128, 64], F32)
            cw = sb(sAB, "cw", [128, 12], F32)
            bd64 = sb(sAB, "bd64", [128, 128], F32)
            c65 = sb(sAB, "c65", [65, 64], F32)
            e0 = sb(sAB, "e0", [128, 1], F32)
            wr = sb(sAB, "wr", [128, 8, 36], F32)
            R_modb = K.regs_n(6, "modb")
            R_smod = K.reg("smod")
            for t_, d_ in ((cosT, cos_d), (sinT, sin_d), (qgb, qg_d), (kgb, kg_d), (cw, cw_d),
                           (bd64, bd64_d), (c65, c65_d), (e0, e0_d)):
                dma("sp", t_[:], d_, writes=[R_const])
            dma("sp", wr[:], wr_d.rearrange("(k p) n -> p k n", p=128), writes=[R_const])

            with ExitStack() as s0:
                cTt = sb(s0, "cTt", [128, 16], F32)
                scT = sb(s0, "scT", [128, 16], F32)
                lhs_c = sb(s0, "lhs_c", [128, 8, 128], BF16)
                lhs_x = sb(s0, "lhs_x", [128, 8, 128], BF16)
                bm = sb(s0, "bm", [128, 6 * D], F32)
                g1b = sb(s0, "g1b", [128, D], F32)
                g2b = sb(s0, "g2b", [128, D], F32)
                modx = sb(s0, "modx", [128, 2 * D], F32)
                wm = [sb(s0, f"wm{i}", [128, 8, 512], BF16) for i in range(2)]
                R_c = K.reg("c")
                R_bm = K.reg("bm")
                R_g = K.reg("g12")
                R_modx = K.regs_n(2, "modx")
                R_wm = K.regs_n(2, "wm")
                P_wm = [K.pslot(f"wm{i}") for i in range(2)]
                dma("sp", cTt[:], cT_d, writes=[R_c])
                dma("sp", bm[:], bm_d, writes=[R_bm])
                dma("sp", g1b[:], g1_d, writes=[R_g])
                dma("sp", g2b[:], g2_d, writes=[R_g])
                op("act", lambda e: e.activation(scT[:], cTt[:], AF.Silu), reads=[R_c], writes=[R_c])
                op("dve", lambda e: e.tensor_copy(
                    lhs_c[:], scT[:, 0:8].unsqueeze(2).to_broadcast([128, 8, 128])), reads=[R_c], writes=[R_c])
                op("dve", lambda e: e.tensor_copy(
                    lhs_x[:], scT[:, 8:16].unsqueeze(2).to_broadcast([128, 8, 128])), reads=[R_c], writes=[R_c])
                wmod_v = w_mod_d.rearrange("(k p) n -> p k n", p=128)
                for blk in range(12):
                    b = blk % 2
                    cs = slice(blk * 512, (blk + 1) * 512)
                    K.pdma(P_wm[b], wm[b][:], wmod_v[:, :, cs], writes=[R_wm[b]])
                    for k in range(8):
                        op("pe", lambda e, k=k: e.matmul(PS[b], lhs_c[:, k, :], wm[b][:, k, :],
                                                       start=(k == 0), stop=(k == 7)),
                           reads=[R_c, R_wm[b]], writes=[PSR[b]])
                    op("dve", lambda e: e.tensor_tensor(modb[:, cs], PS[b], bm[:, cs], ALU.add),
                       reads=[PSR[b], R_bm], writes=[R_modb[blk // 2]])
                    if blk < 4:
                        for k in range(8):
                            op("pe", lambda e, k=k: e.matmul(PS[2 + b], lhs_x[:, k, :], wm[b][:, k, :],
                                                           start=(k == 0), stop=(k == 7)),
                               reads=[R_c, R_wm[b]], writes=[PSR[2 + b]])
                        op("dve", lambda e: e.tensor_tensor(modx[:, cs], PS[2 + b], bm[:, cs], ALU.add),
                           reads=[PSR[2 + b], R_bm], writes=[R_modx[blk // 2]])
                op("dve", lambda e: e.scalar_tensor_tensor(modb[:, D:2 * D], modb[:, D:2 * D], 1.0, g1b[:],
                                                          ALU.add, ALU.mult),
                   reads=[R_modb[1], R_g], writes=[R_modb[1]])
                op("dve", lambda e: e.scalar_tensor_tensor(modx[:, D:2 * D], modx[:, D:2 * D], 1.0, g1b[:],
                                                          ALU.add, ALU.mult),
                   reads=[R_modx[1], R_g], writes=[R_modx[1]])
                op("dve", lambda e: e.scalar_tensor_tensor(modb[:, 4 * D:5 * D], modb[:, 4 * D:5 * D], 1.0, g2b[:],
                                                          ALU.add, ALU.mult),
                   reads=[R_modb[4], R_g], writes=[R_modb[4]])
                op("act", lambda e: e.activation(gt2b[:], modb[:, 5 * D:6 * D], AF.Copy),
                   reads=[R_modb[5]], writes=[R_gt2])
                srcs = [(modb, D, R_modb[1]), (modb, 0, R_modb[0]), (modx, D, R_modx[1]), (modx, 0, R_modx[0])]
                for vi, (tt, off, rr) in enumerate(srcs):
                    for k in range(8):
                        c0 = vi * 8 + k
                        op("pe", lambda e, tt=tt, off=off, k=k, c0=c0: e.matmul(
                            PS[4][:, c0:c0 + 1], tt[:, off + k * 128: off + (k + 1) * 128], e0[:, 0:1],
                            start=True, stop=True), reads=[rr, R_const], writes=[PSR[4]])
                op("act", lambda e: e.activation(smod[:], PS[4][:, 0:32], AF.Copy), reads=[PSR[4]], writes=[R_smod])
                if debug:
                    dma("sp", d_smod, smod[:], reads=[R_smod])
                    dma("sp", d_modb, modb[:], reads=R_modb)
                K.barrier()
                if stop_after == "0":
                    return nc, list(dbg.keys())

            with ExitStack() as sB:
                QT = sb(sB, "QT", [128, 4, SEQ], BF16)
                KT = sb(sB, "KT", [128, 2, 2, NTOK], BF16)
                VE = sb(sB, "VE", [128, NT, 2, 128], BF16)
                convn = sb(sB, "convn", [128, 4, SEQ], BF16)
                R_QT = K.regs_n(NL, "QT")
                R_KT = K.regs_n(NT, "KT")
                R_VE = K.regs_n(NT, "VE")
                R_convn = K.regs_n(4, "convn")
                R_ve1 = K.reg("ve1")
                if _LVL >= 0:
                    op("dve", lambda e: e.memset(VE[:], 0.0), writes=[R_ve1])
                    op("dve", lambda e: e.memset(VE[:, :, :, 64:65], 1.0), writes=[R_ve1])
                    op("dve", lambda e: e.memset(KT[:], 0.0), writes=[R_ve1])

                with ExitStack() as sA:
                    hT = sb(sA, "hT", [128, 8, NTOK], BF16)
                    R_hTd = K.regs_n(NT, "hTd")
                    R_hTa = K.regs_n(NT, "hTa")
                    with ExitStack() as sA1:
                        winq = sb(sA1, "winq", [128, 8, 768], BF16)
                        R_winq = K.reg("winq")
                        if _LVL >= 1:
                            K.pdma(K.pslot("winq"), winq[:], w_in_d.rearrange("(k p) n -> p k n", p=128)[:, :, 0:768],
                                   writes=[R_winq])
                        xt = [sb(sA1, f"xt{i}", [128, D], F32) for i in range(2)]
                        junk = sb(sA1, "junk", [128, D], BF16)
                        xn = [sb(sA1, f"xn{i}", [128, D], BF16) for i in range(2)]
                        st1 = sb(sA1, "st1", [128, 3, NT], F32)
                        zq = [sb(sA1, f"zq{i}", [128, 512], F32) for i in range(2)]
                        zkv = [sb(sA1, f"zkv{i}", [128, 256], F32) for i in range(2)]
                        sq = sb(sA1, "sq", [128, 512], F32)
                        qn = sb(sA1, "qn", [128, 512], F32)
                        kn = sb(sA1, "kn", [128, 128], F32)
                        knb = sb(sA1, "knb", [128, 128], BF16)
                        ta = sb(sA1, "ta", [128, 256], F32)
                        tb = sb(sA1, "tb", [128, 256], F32)
                        qst = sb(sA1, "qst", [128, 3, 16], F32)
                        qkb = [sb(sA1, f"qkb{i}", [128, 768], BF16) for i in range(2)]
                        R_xt = K.regs_n(2, "xt")
                        R_xn = K.regs_n(2, "xn")
                        R_st1 = K.regs_n(NT, "st1")
                        R_zq = K.regs_n(2, "zq")
                        R_zkv = K.regs_n(2, "zkv")
                        R_tmp = K.reg("tmpq")
                        R_tmpk = K.reg("tmpk")
                        sqk = sb(sA1, "sqk", [128, 128], F32)
                        tak = sb(sA1, "tak", [128, 64], F32)
                        tbk = sb(sA1, "tbk", [128, 64], F32)
                        R_qst = K.reg("qst")
                        R_qkb = K.regs_n(2, "qkb")
                        R_knb = K.reg("knb")
                        def a1_front(T):
                            b = T % 2
                            lat = T >= 2
                            t = T - 2
                            tsl = slice(T * 128, (T + 1) * 128)
                            dma("sp", xt[b][:], xin[tsl, :], writes=[R_xt[b]])
                            if _LVL < -4:
                                return
                            op("act", lambda e: e.activation(junk[:], xt[b][:], AF.Square,
                                                             accum_out=st1[:, 0, T:T + 1]),
                               reads=[R_xt[b]], writes=[R_st1[T]])
                            if _LVL < -3:
                                return
                            op("act", lambda e: e.activation(st1[:, 1, T:T + 1], st1[:, 0, T:T + 1], AF.Ln,
                                                             scale=1.0 / D, bias=EPS),
                               reads=[R_st1[T]], writes=[R_st1[T]])
                            op("act", lambda e: e.activation(st1[:, 2, T:T + 1], st1[:, 1, T:T + 1], AF.Exp,
                                                             scale=-0.5),
                               reads=[R_st1[T]], writes=[R_st1[T]])
                            op("act", lambda e: e.activation(xn[b][:], xt[b][:], AF.Copy, scale=st1[:, 2, T:T + 1]),
                               reads=[R_xt[b], R_st1[T]], writes=[R_xn[b]])
                            if _LVL < -2:
                                return
                            psb = PS[b].bitcast(BF16)
                            for k in range(8):
                                op("pe", lambda e, k=k: e.transpose(psb[:, k * 128:(k + 1) * 128],
                                                                    xn[b][:, k * 128:(k + 1) * 128], identb[:]),
                                   reads=[R_xn[b], R_const], writes=[PSR[b]])
                            if _LVL < -1:
                                return
                            so = 0 if lat else 16
                            for k in range(8):
                                if (b == 0 and _KEV != 'act') or _KEV == 'dve':
                                    op("dve", lambda e, k=k: e.tensor_scalar(
                                        hT[:, k, tsl], psb[:, k * 128:(k + 1) * 128],
                                        smod[:, so + k:so + k + 1], smod[:, so + 8 + k:so + 9 + k],
                                        ALU.mult, ALU.add), reads=[PSR[b], R_smod], writes=[R_hTd[T]])
                                else:
                                    op("act", lambda e, k=k: e.activation(
                                        hT[:, k, tsl], psb[:, k * 128:(k + 1) * 128], AF.Identity,
                                        bias=smod[:, so + 8 + k:so + 9 + k], scale=smod[:, so + k:so + k + 1]),
                                       reads=[PSR[b], R_smod], writes=[R_hTa[T]])
                            if _LVL < 1:
                                return
                            if lat:
                                for k in range(8):
                                    op("pe", lambda e, k=k: e.matmul(PS[2], hT[:, k, tsl], winq[:, k, 0:512],
                                                                   start=(k == 0), stop=(k == 7)),
                                       reads=[R_hTd[T], R_hTa[T], R_winq], writes=[PSR[2]])
                            for k in range(8):
                                op("pe", lambda e, k=k: e.matmul(PS[3][:, 0:256], hT[:, k, tsl], winq[:, k, 512:768],
                                                               start=(k == 0), stop=(k == 7)),
                                   reads=[R_hTd[T], R_hTa[T], R_winq], writes=[PSR[3]])

                        def a1_front_b(T):
                            b = T % 2
                            lat = T >= 2
                            t = T - 2
                            tsl = slice(T * 128, (T + 1) * 128)
                            if lat:
                                op("act", lambda e: e.activation(zq[b][:], PS[2], AF.Copy),
                                   reads=[PSR[2]], writes=[R_zq[b]])
                            op("act", lambda e: e.activation(zkv[b][:], PS[3][:, 0:256], AF.Copy),
                               reads=[PSR[3]], writes=[R_zkv[b]])
                            op("act", lambda e: e.activation(
                                VE[:, T, :, 0:64], zkv[b][:, 128:256].rearrange("p (g d) -> p g d", g=2), AF.Copy),
                               reads=[R_zkv[b], R_ve1], writes=[R_VE[T]])

                        def a1_back(T, part):
                            b = T % 2
                            lat = T >= 2
                            t = T - 2
                            tsl = slice(T * 128, (T + 1) * 128)
                            nh_list = ([("q", zq[b], 8, qn, qgb, R_zq[b])] if lat else []) + \
                                      [("k", zkv[b], 2, kn, kgb, R_zkv[b])]
                            for nm, src, nh, dst, gb_, rsrc in nh_list:
                                W = nh * 64
                                c0 = 0 if nm == "q" else 8
                                eng_, sq_, ta_, tb_, Rt_ = ("dve", sq, ta, tb, R_tmp) if nm == "q" else \
                                    ("dve", sqk, tak, tbk, R_tmpk)
                                op(eng_, lambda e: e.tensor_tensor(sq_[:, 0:W], src[:, 0:W], src[:, 0:W], ALU.mult),
                                   reads=[rsrc], writes=[Rt_])
                                op(eng_, lambda e: e.tensor_reduce(
                                    qst[:, 0, c0:c0 + nh], sq_[:, 0:W].rearrange("p (h d) -> p h d", h=nh),
                                    AX.X, ALU.add), reads=[Rt_], writes=[R_qst])
                            lo = 0 if lat else 8
                            op("act", lambda e: e.activation(qst[:, 1, lo:10], qst[:, 0, lo:10],
                                                             AF.Ln, scale=1.0 / 64, bias=EPS),
                               reads=[R_qst], writes=[R_qst])
                            op("act", lambda e: e.activation(qst[:, 2, lo:10], qst[:, 1, lo:10],
                                                             AF.Exp, scale=-0.5),
                               reads=[R_qst], writes=[R_qst])

                        def a1_back_rest(T):
                            b = T % 2
                            lat = T >= 2
                            t = T - 2
                            tsl = slice(T * 128, (T + 1) * 128)
                            nh_list = ([("q", zq[b], 8, qn, qgb, R_zq[b])] if lat else []) + \
                                      [("k", zkv[b], 2, kn, kgb, R_zkv[b])]
                            for nm, src, nh, dst, gb_, rsrc in nh_list:
                                W = nh * 64
                                c0 = 0 if nm == "q" else 8
                                eng_, sq_, ta_, tb_, Rt_ = ("dve", sq, ta, tb, R_tmp) if nm == "q" else \
                                    (_KENG, sqk, tak, tbk, R_tmpk)
                                d3 = dst[:, 0:W].rearrange("p (h d) -> p h d", h=nh)
                                op(eng_, lambda e: e.tensor_tensor(
                                    d3, src[:, 0:W].rearrange("p (h d) -> p h d", h=nh),
                                    qst[:, 2, c0:c0 + nh].unsqueeze(2).to_broadcast([128, nh, 64]), ALU.mult),
                                   reads=[rsrc, R_qst], writes=[Rt_])
                                op(eng_, lambda e: e.tensor_tensor(
                                    d3, d3, gb_[:, :].unsqueeze(1).to_broadcast([128, nh, 64]), ALU.mult),
                                   reads=[Rt_, R_const], writes=[Rt_])
                                if nm == "q":
                                    outb = qkb[b][:, 0:512]
                                    R_ob = R_qkb[b]
                                else:
                                    outb = knb[:, 0:128]
                                    R_ob = R_knb
                                if lat:
                                    d5 = dst[:, 0:W].rearrange("p (h a f d) -> p h a f d", h=nh, a=2, f=2)
                                    o5 = outb.rearrange("p (h a f d) -> p h a f d", h=nh, a=2, f=2)
                                    x1 = d5[:, :, :, 0, :]
                                    x2 = d5[:, :, :, 1, :]
                                    cb = cosT[:, t, :].rearrange("p (a d) -> p a d", a=2).unsqueeze(1) \
                                        .to_broadcast([128, nh, 2, 16])
                                    sb_ = sinT[:, t, :].rearrange("p (a d) -> p a d", a=2).unsqueeze(1) \
                                        .to_broadcast([128, nh, 2, 16])
                                    hw = nh * 32
                                    ta4 = ta_[:, 0:hw].rearrange("p (h a d) -> p h a d", h=nh, a=2)
                                    tb4 = tb_[:, 0:hw].rearrange("p (h a d) -> p h a d", h=nh, a=2)
                                    op(eng_, lambda e: e.tensor_tensor(ta4, x1, cb, ALU.mult),
                                       reads=[Rt_, R_const], writes=[Rt_])
                                    op(eng_, lambda e: e.tensor_tensor(tb4, x2, sb_, ALU.mult),
                                       reads=[Rt_, R_const], writes=[Rt_])
                                    op(eng_, lambda e: e.tensor_tensor(o5[:, :, :, 0, :], ta4, tb4, ALU.subtract),
                                       reads=[Rt_], writes=[R_ob])
                                    op(eng_, lambda e: e.tensor_tensor(ta4, x1, sb_, ALU.mult),
                                       reads=[Rt_, R_const], writes=[Rt_])
                                    op(eng_, lambda e: e.tensor_tensor(tb4, x2, cb, ALU.mult),
                                       reads=[Rt_, R_const], writes=[Rt_])
                                    op(eng_, lambda e: e.tensor_tensor(o5[:, :, :, 1, :], ta4, tb4, ALU.add),
                                       reads=[Rt_], writes=[R_ob])
                                else:
                                    op(eng_, lambda e: e.tensor_copy(outb, dst[:, 0:W]),
                                       reads=[Rt_], writes=[R_ob])
                            if _LVL < 4:
                                return
                            op("dve", lambda e: e.tensor_copy(
                                qkb[b][:, 512:768].rearrange("p (g r d) -> p g r d", g=2, r=2),
                                knb[:, 0:128].rearrange("p (g d) -> p g d", g=2).unsqueeze(2)
                                .to_broadcast([128, 2, 2, 64])), reads=[R_knb], writes=[R_qkb[b]])
                            if _LVL < 5:
                                return
                            ps4 = PS[4].bitcast(BF16)
                            ps5 = PS[5].bitcast(BF16)
                            if lat:
                                for j in range(4):
                                    op("pe", lambda e, j=j: e.transpose(ps4[:, j * 128:(j + 1) * 128],
                                                                        qkb[b][:, j * 128:(j + 1) * 128], identb[:]),
                                       reads=[R_qkb[b], R_const], writes=[PSR[4]])
                                op("dve", lambda e: e.tensor_copy(
                                    QT[:, :, t * 128:(t + 1) * 128],
                                    ps4[:, 0:512].rearrange("p (j n) -> p j n", j=4)),
                                   reads=[PSR[4]], writes=[R_QT[t]])
                            for j in range(2):
                                op("pe", lambda e, j=j: e.transpose(ps5[:, j * 128:(j + 1) * 128],
                                                                    qkb[b][:, 512 + j * 128:512 + (j + 1) * 128],
                                                                    identb[:]),
                                   reads=[R_qkb[b], R_const], writes=[PSR[5]])
                            for u in range(2):
                                op("dve", lambda e, u=u: e.tensor_copy(
                                    KT[64 * u:64 * u + 64, :, u, tsl],
                                    ps5[64 * u:64 * u + 64, 0:256].rearrange("p (j n) -> p j n", j=2)),
                                   reads=[PSR[5], R_ve1], writes=[R_KT[T]])

                        a1_front(0)
                        a1_front_b(0)
                        for T in range(NT):
                            if T + 1 < NT:
                                a1_front(T + 1)
                            a1_back(T, "stats")
                            if T + 1 < NT:
                                a1_front_b(T + 1)
                            a1_back_rest(T)
                        K.barrier()
                        if stop_after == "A1":
                            if debug:
                                dma("sp", d_QT, QT[:]); dma("sp", d_KT[0:64], KT[0:64, :, 0, :]); dma("sp", d_KT[64:128], KT[64:128, :, 1, :]); dma("sp", d_V, VE[:, :, :, 0:65])
                            K.barrier()
                            return nc, list(dbg.keys())
                    with ExitStack() as sA2:
                        wc = [sb(sA2, f"wc{i}", [128, 8, 384], BF16) for i in range(2)]
                        vT = [sb(sA2, f"vT{i}", [128, SEQ + 2], F32) for i in range(2)]
                        gbT = [sb(sA2, f"gbT{i}", [128, SEQ], F32) for i in range(2)]
                        gct = [sb(sA2, f"gct{i}", [128, 512], F32) for i in range(2)]
                        yb = sb(sA2, "yb", [128, 1024], F32)
                        ysq = sb(sA2, "ysq", [128, 1024], F32)
                        rs = sb(sA2, "rs", [128, 1024], F32)
                        R_wc = K.regs_n(2, "wc")
                        P_wc = [[K.pslot(f"wc{i}_{j}") for j in range(3)] for i in range(2)]
                        R_vT = K.regs_n(2, "vT")
                        R_gbT = K.regs_n(2, "gbT")
                        R_gct = K.regs_n(2, "gct")
                        R_y = K.reg("y")
                        R_ysq = K.reg("ysq")
                        R_rs = K.reg("rs")
                        winv = w_in_d.rearrange("(k p) n -> p k n", p=128)
                        for i in range(2):
                            op("dve", lambda e, i=i: e.memset(vT[i][:, 0:1], 0.0), writes=[R_vT[i]])
                            op("dve", lambda e, i=i: e.memset(vT[i][:, SEQ + 1:SEQ + 2], 0.0), writes=[R_vT[i]])
                        cnt2 = {'gi': 0, 'pi': 0}
                        def a2_mm(c4):
                            b = c4 % 2
                            for s3 in range(3):
                                c0 = 768 + s3 * 512 + c4 * 128
                                K.pdma(P_wc[b][s3], wc[b][:, :, s3 * 128:(s3 + 1) * 128], winv[:, :, c0:c0 + 128],
                                       writes=[R_wc[b]])
                            for tb_ in range(4):
                                tok = slice(256 + tb_ * 512, 256 + (tb_ + 1) * 512)
                                osl = slice(tb_ * 512, (tb_ + 1) * 512)
                                g_ = cnt2['gi'] % 2
                                cnt2['gi'] += 1
                                for s3 in range(3):
                                    pb = 5 + cnt2['pi'] % 3
                                    cnt2['pi'] += 1
                                    for k in range(8):
                                        op("pe", lambda e, k=k, s3=s3, pb=pb: e.matmul(
                                            PS[pb], wc[b][:, k, s3 * 128:(s3 + 1) * 128], hT[:, k, tok],
                                            start=(k == 0), stop=(k == 7)),
                                           reads=[R_wc[b]], writes=[PSR[pb]])
                                    if s3 == 0:
                                        op("act", lambda e, pb=pb: e.activation(gbT[b][:, osl], PS[pb], AF.Copy),
                                           reads=[PSR[pb]], writes=[R_gbT[b]])
                                    elif s3 == 1:
                                        op("act", lambda e, pb=pb: e.activation(gct[g_][:], PS[pb], AF.Copy),
                                           reads=[PSR[pb]], writes=[R_gct[g_]])
                                    else:
                                        op("dve", lambda e, pb=pb: e.tensor_tensor(
                                            vT[b][:, 1 + tb_ * 512:1 + (tb_ + 1) * 512], PS[pb], gct[g_][:], ALU.mult),
                                           reads=[PSR[pb], R_gct[g_]], writes=[R_vT[b]])

                        def a2_fin(c4):
                            b = c4 % 2
                            for hf in range(2):
                                o = hf * 1024
                                op("dve", lambda e: e.tensor_scalar(yb[:], vT[b][:, o:o + 1024],
                                                                    cw[:, c4 * 3:c4 * 3 + 1], None, ALU.mult),
                                   reads=[R_vT[b], R_const], writes=[R_y])
                                op("dve", lambda e: e.scalar_tensor_tensor(
                                    yb[:], vT[b][:, o + 1:o + 1025], cw[:, c4 * 3 + 1:c4 * 3 + 2], yb[:],
                                    ALU.mult, ALU.add), reads=[R_vT[b], R_const, R_y], writes=[R_y])
                                op("dve", lambda e: e.scalar_tensor_tensor(
                                    yb[:], vT[b][:, o + 2:o + 1026], cw[:, c4 * 3 + 2:c4 * 3 + 3], yb[:],
                                    ALU.mult, ALU.add), reads=[R_vT[b], R_const, R_y], writes=[R_y])
                                op("dve", lambda e: e.tensor_tensor(yb[:], yb[:], gbT[b][:, o:o + 1024], ALU.mult),
                                   reads=[R_y, R_gbT[b]], writes=[R_y])
                                op("act", lambda e: e.activation(ysq[:], yb[:], AF.Square),
                                   reads=[R_y], writes=[R_ysq])
                                for q2 in range(2):
                                    op("pe", lambda e, q2=q2: e.matmul(PS[q2], bd64[:], ysq[:, q2 * 512:(q2 + 1) * 512],
                                                                     start=True, stop=True),
                                       reads=[R_ysq, R_const], writes=[PSR[q2]])
                                    op("act", lambda e, q2=q2: e.activation(rs[:, q2 * 512:(q2 + 1) * 512], PS[q2],
                                                                            AF.Ln, bias=EPS),
                                       reads=[PSR[q2]], writes=[R_rs])
                                op("act", lambda e: e.activation(rs[:], rs[:], AF.Exp, scale=-0.5),
                                   reads=[R_rs], writes=[R_rs])
                                op("dve", lambda e: e.tensor_tensor(convn[:, c4, o:o + 1024], yb[:], rs[:], ALU.mult),
                                   reads=[R_y, R_rs], writes=[R_convn[c4]])

                        a2_mm(0)
                        for c4 in range(4):
                            if c4 + 1 < 4:
                                a2_mm(c4 + 1)
                            a2_fin(c4)
                        K.barrier()
                if debug:
                    dma("sp", d_QT, QT[:], reads=R_QT)
                    dma("sp", d_KT[0:64], KT[0:64, :, 0, :], reads=R_KT)
                    dma("sp", d_KT[64:128], KT[64:128, :, 1, :], reads=R_KT)
                    dma("sp", d_V, VE[:, :, :, 0:65], reads=R_VE)
                    dma("sp", d_convn, convn[:], reads=R_convn)
                if stop_after == "A2":
                    K.barrier()
                    return nc, list(dbg.keys())

                with ExitStack() as sB2:
                    attnT = sb(sB2, "attnT", [128, 4, SEQ], BF16)
                    R_attn = K.regs_n(8, "attn")
                    woall = sb(sB2, "woall", [128, 8, D], BF16)
                    gall = sb(sB2, "gall", [128, 8], F32)
                    R_wst = K.reg("wst")
                    R_wo = K.reg("wo")
                    with ExitStack() as sBa:
                        PT = [sb(sBa, f"PT{i}", [128, 1024], BF16) for i in range(3)]
                        sqb = [sb(sBa, f"sqb{i}", [65, 512], F32) for i in range(2)]
                        lnb = [sb(sBa, f"lnb{i}", [64, 512], F32) for i in range(2)]
                        R_PT = K.regs_n(3, "PT")
                        R_sqb = K.regs_n(2, "sqb")
                        R_lnb = K.regs_n(2, "lnb")
                        wst = sb(sBa, "wst", [128, 4, D], F32)
                        dma("sp", gall[:], gall_d, writes=[R_wo])
                        wov = w_out_d.rearrange("(c p) n -> p c n", p=128)
                        for half in range(2):
                            dma("sp", wst[:], wov[:, half * 4:(half + 1) * 4, :], writes=[R_wst])
                            op("dve", lambda e: e.tensor_tensor(
                                woall[:, half * 4:(half + 1) * 4, :], wst[:],
                                gall[:, half * 4:(half + 1) * 4].unsqueeze(2).to_broadcast([128, 4, D]), ALU.mult),
                               reads=[R_wst, R_wo], writes=[R_wo])
                        zt = sb(sBa, "zt", [128, 8, D], BF16)
                        R_zt = K.reg("zt")
                        op("dve", lambda e: e.memset(zt[:], 0.0), writes=[R_zt])
                        for i in range(NE * CAP // 1024):
                            dma("sp", xbuf_d[i * 1024:(i + 1) * 1024, :].rearrange("(p j) d -> p j d", j=8), zt[:],
                                reads=[R_zt])
                        dma("sp", xbuf_d[NE * CAP:NE * CAP + 1, :], zt[0:1, 0, :], reads=[R_zt])
                        it = 0
                        pti = 0
                        spi = 0
                        for j in range(4):
                            g = j // 2
                            for qb in range(4):
                                qs = slice(qb * 512, (qb + 1) * 512)
                                obase = 4 + 2 * (it % 2)
                                it += 1
                                Oab = [PS[obase], PS[obase + 1]]

                                def s_step(kt, sp):
                                    for u in range(2):
                                        p0 = 64 * u
                                        bk = 2 * sp + u
                                        op("pe", lambda e: e.matmul(
                                            PS[bk], KT[:, g, u, kt * 128:(kt + 1) * 128],
                                            QT[:, j, qs], start=True, stop=True),
                                           writes=[PSR[2 * sp], PSR[2 * sp + 1]] if u == 0 else [PSR[bk]])
                                sp_of = {0: spi % 2}
                                spi += 1
                                s_step(0, sp_of[0])
                                for kt in range(NT):
                                    if kt + 1 < NT:
                                        sp_of[kt + 1] = spi % 2
                                        spi += 1
                                        s_step(kt + 1, sp_of[kt + 1])
                                    sp = sp_of[kt]
                                    pb_ = pti % 3
                                    pti += 1
                                    op("act", lambda e: e.activation(
                                        PT[pb_][:], PSUM[:, 2 * sp * 512:(2 * sp + 2) * 512], AF.Exp, scale=0.125),
                                       reads=[PSR[2 * sp], PSR[2 * sp + 1]], writes=[R_PT[pb_]])
                                    for u in range(2):
                                        op("pe", lambda e: e.matmul(Oab[u][:, :], VE[:, kt, g, :],
                                                                    PT[pb_][:, u * 512:(u + 1) * 512],
                                                                    start=(kt == 0), stop=(kt == NT - 1)),
                                           reads=[R_PT[pb_]], writes=[PSR[obase + u]])
                                sp = spi % 2
                                spi += 1
                                for u in range(2):
                                    op("act", lambda e: e.activation(sqb[u][:], Oab[u][0:65, :], AF.Square),
                                       reads=[PSR[obase + u]], writes=[R_sqb[u]])
                                    op("pe", lambda e: e.matmul(PS[2 * sp + u][0:64, :], c65[:, :], sqb[u][:],
                                                                start=True, stop=True),
                                       reads=[R_sqb[u], R_const], writes=[PSR[2 * sp + u]])
                                for u in range(2):
                                    op("act", lambda e: e.activation(lnb[u][:], PS[2 * sp + u][0:64, :], AF.Ln),
                                       reads=[PSR[2 * sp + u]], writes=[R_lnb[u]])
                                    op("act", lambda e: e.activation(lnb[u][:], lnb[u][:], AF.Exp, scale=-0.5),
                                       reads=[R_lnb[u]], writes=[R_lnb[u]])
                                    op("dve", lambda e: e.tensor_tensor(attnT[64 * u:64 * u + 64, j, qs], Oab[u][0:64, :],
                                                                        lnb[u][:], ALU.mult),
                                       reads=[PSR[obase + u], R_lnb[u]], writes=[R_attn[2 * j + u]])
                        K.barrier()
                    if debug:
                        for u in range(2):
                            dma("sp", d_attn.rearrange("d (j u) s -> d u j s", u=2)[:, u], attnT[64 * u:64 * u + 64, :, :],
                                reads=R_attn)
                    if stop_after == "B":
                        K.barrier()
                        return nc, list(dbg.keys())

                    if True:
                        with ExitStack() as sB3:
                            xr = [sb(sB3, "xr0", [128, D], F32)] * 2
                            xnw = [sb(sB3, "xnw0", [128, D], F32)] * 2
                            hx = [sb(sB3, f"hx{i}", [128, D], F32) for i in range(2)]
                            hxT = [sb(sB3, "hxT0", [128, 8, 128], F32)] * 2
                            st2 = sb(sB3, "st2", [128, 3, NL], F32)
                            lg = sb(sB3, "lg", [128, NL, 36], F32)
                            rt = sb(sB3, "rt", [128, 64], F32)
                            RB = sb(sB3, "RB", [128, 16, NL, 8], F32)
                            m8 = sb(sB3, "m8", [128, 8], F32)
                            ustr = sb(sB3, "ustr", [128, 128], BF16)
                            onesb = sb(sB3, "onesb", [128, 128], BF16)
                            ustf = sb(sB3, "ustf", [128, 128], F32)
                            eC = sb(sB3, "eC", [128, NE], F32)
                            maskb = sb(sB3, "maskb", [128, NL, NE], BF16)
                            mkk = sb(sB3, "mkk", [128, 2, NL, NE], F32)
                            rkk = sb(sB3, "rkk", [128, 2, NL, NE], F32)
                            t3 = sb(sB3, "t3", [128, NL, NE], F32)
                            dfa = sb(sB3, "dfa", [128, 3, 2, NL], F32)
                            hxb = [sb(sB3, f"hxb{i}", [128, D], BF16) for i in range(2)]
                            R_mask = K.regs_n(NL, "mask")
                            R_rk = K.reg("rk")
                            R_hxb = K.regs_n(2, "hxb")
                            hxs = [sb(sB3, f"hxs{i}", [128, D], BF16) for i in range(4)]
                            R_hxs = K.regs_n(4, "hxs")
                            R_xbuf = K.reg("xbuf")
                            dma("sp", ustf[:], ustr_d, writes=[R_wo])
                            dma("sp", eC[:], eC_d, writes=[R_wo])
                            op("dve", lambda e: e.tensor_copy(ustr[:], ustf[:]), reads=[R_wo], writes=[R_wo])
                            op("dve", lambda e: e.memset(onesb[:], 1.0), writes=[R_wo])
                            R_xr = [K.reg("xr")] * 2
                            R_xnw = [K.reg("xnw")] * 2
                            R_hx = K.regs_n(2, "hx")
                            R_hxT = [K.reg("hxT")] * 2
                            R_st2 = K.regs_n(NL, "st2")
                            R_lg = K.reg("lg")
                            R_rt = K.reg("rt")
                            def b2_front(t):
                                b = t % 2
                                tsl = slice(t * 128, (t + 1) * 128)
                                dma("sp", xr[b][:], xin[256 + t * 128:256 + (t + 1) * 128, :], writes=[R_xr[b]])
                                for hf in range(2):
                                    pb = hf + 6 * (t % 2)
                                    cs = slice(hf * 512, (hf + 1) * 512)
                                    for c in range(4):
                                        op("pe", lambda e, c=c: e.matmul(PS[pb], attnT[:, c, tsl], woall[:, c, cs],
                                                                       start=(c == 0), stop=False),
                                           reads=[R_wo], writes=[PSR[pb]])
                                    for c4 in range(4):
                                        op("pe", lambda e, c4=c4: e.matmul(PS[pb], convn[:, c4, tsl], woall[:, 4 + c4, cs],
                                                                         start=False, stop=(c4 == 3)),
                                           reads=[R_wo], writes=[PSR[pb]])
                                    op("dve", lambda e: e.tensor_tensor(xnw[b][:, cs], PS[pb], modb[:, 2 * D + hf * 512:
                                                                                                  2 * D + (hf + 1) * 512],
                                                                        ALU.mult),
                                       reads=[PSR[pb]], writes=[R_xnw[b]])
                                op("dve", lambda e: e.tensor_tensor(xnw[b][:], xnw[b][:], xr[b][:], ALU.add),
                                   reads=[R_xnw[b], R_xr[b]], writes=[R_xnw[b]])
                                dma("sp", xnew_d[tsl, :], xnw[b][:], reads=[R_xnw[b]])
                                op("act", lambda e: e.activation(hxb[b][:], xnw[b][:], AF.Square,
                                                                 accum_out=st2[:, 0, t:t + 1]),
                                   reads=[R_xnw[b]], writes=[R_st2[t], R_hxb[b]])
                                op("act", lambda e: e.activation(st2[:, 1, t:t + 1], st2[:, 0, t:t + 1], AF.Ln,
                                                                 scale=1.0 / D, bias=EPS),
                                   reads=[R_st2[t]], writes=[R_st2[t]])
                                op("act", lambda e: e.activation(st2[:, 2, t:t + 1], st2[:, 1, t:t + 1], AF.Exp,
                                                                 scale=-0.5),
                                   reads=[R_st2[t]], writes=[R_st2[t]])
                                op("dve", lambda e: e.scalar_tensor_tensor(
                                    hx[b][:], xnw[b][:], st2[:, 2, t:t + 1], modb[:, 4 * D:5 * D], ALU.mult, ALU.mult),
                                   reads=[R_xnw[b], R_st2[t]], writes=[R_hx[b]])
                                op("dve", lambda e: e.tensor_tensor(hx[b][:], hx[b][:], modb[:, 3 * D:4 * D], ALU.add),
                                   reads=[R_hx[b]], writes=[R_hx[b]])

                            def b2_back(t):
                                b = t % 2
                                tsl = slice(t * 128, (t + 1) * 128)
                                PP = PSUM[:, 2 * 512:4 * 512]
                                for k in range(8):
                                    op("pe", lambda e, k=k: e.transpose(PP[:, k * 128:(k + 1) * 128],
                                                                        hx[b][:, k * 128:(k + 1) * 128], identf[:]),
                                       reads=[R_hx[b], R_const], writes=[PSR[2], PSR[3]])
                                op("act", lambda e: e.activation(
                                    hxT[b][:], PP.rearrange("p (k n) -> p k n", k=8), AF.Copy),
                                   reads=[PSR[2], PSR[3]], writes=[R_hxT[b]])
                                op("act", lambda e: e.activation(hxb[b][:], hx[b][:], AF.Copy),
                                   reads=[R_hx[b]], writes=[R_hxb[b]])
                                for k in range(8):
                                    op("pe", lambda e, k=k: e.matmul(PS[4][:, 0:36], hxT[b][:, k, :], wr[:, k, :],
                                                                   start=(k == 0), stop=(k == 7)),
                                       reads=[R_hxT[b], R_const], writes=[PSR[4]])
                                op("act", lambda e: e.activation(lg[:, t, :], PS[4][:, 0:36], AF.Copy),
                                   reads=[PSR[4]], writes=[R_lg])
                                dma("sp", hx2_d[tsl, :], hxb[b][:], reads=[R_hxb[b]], writes=[R_xbuf])

                            b2_front(0)
                            for t in range(NL):
                                if t + 1 < NL:
                                    b2_front(t + 1)
                                b2_back(t)
                            def slab(i, w=8):
                                return RB[:, i, :, 0:w]

                            def col(i):
                                return RB[:, i, :, 0]

                            def bc(a2, w):
                                return a2.unsqueeze(2).to_broadcast([128, NL, w])
                            R_B = K.reg("RB")

                            def dv(fn, rd=(), wr_=()):
                                op("dve", fn, reads=[R_lg, R_B] + list(rd), writes=[R_B] + list(wr_))

                            def ac(fn):
                                op("act", fn, reads=[R_lg, R_B], writes=[R_B])
                            G4 = lg[:, :, 0:4]
                            dv(lambda e: e.tensor_reduce(col(0), G4, AX.X, ALU.max))
                            dv(lambda e: e.tensor_tensor(slab(1, 4), G4, bc(col(0), 4), ALU.is_equal))
                            dv(lambda e: e.tensor_tensor(slab(2, 4), G4, bc(col(0), 4), ALU.subtract))
                            ac(lambda e: e.activation(slab(2, 4), slab(2, 4), AF.Exp))
                            dv(lambda e: e.tensor_reduce(col(3), slab(2, 4), AX.X, ALU.add))
                            dv(lambda e: e.reciprocal(col(4), col(3)))
                            dv(lambda e: e.tensor_tensor(slab(5), lg[:, :, 4:12], bc(RB[:, 1, :, 0], 8), ALU.mult))
                            for gq in range(1, 4):
                                dv(lambda e, gq=gq: e.tensor_tensor(slab(6), lg[:, :, 4 + 8 * gq:12 + 8 * gq],
                                                                    bc(RB[:, 1, :, gq], 8), ALU.mult))
                                dv(lambda e: e.tensor_tensor(slab(5), slab(5), slab(6), ALU.add))
                            dv(lambda e: e.tensor_reduce(col(7), slab(5), AX.X, ALU.max))
                            dv(lambda e: e.tensor_tensor(slab(8), slab(5), bc(col(7), 8), ALU.is_equal))
                            dv(lambda e: e.scalar_tensor_tensor(slab(9), slab(8), -1.0e30, slab(5), ALU.mult, ALU.add))
                            dv(lambda e: e.tensor_reduce(col(10), slab(9), AX.X, ALU.max))
                            dv(lambda e: e.tensor_tensor(slab(11), slab(5), bc(col(10), 8), ALU.is_ge))
                            dv(lambda e: e.tensor_tensor(slab(12), slab(11), slab(8), ALU.subtract))
                            dv(lambda e: e.tensor_tensor(slab(13), slab(5), bc(col(7), 8), ALU.subtract))
                            ac(lambda e: e.activation(slab(13), slab(13), AF.Exp))
                            dv(lambda e: e.tensor_tensor(slab(13), slab(13), slab(11), ALU.mult))
                            dv(lambda e: e.tensor_reduce(col(14), slab(13), AX.X, ALU.add))
                            dv(lambda e: e.reciprocal(col(15), col(14)))
                            dv(lambda e: e.tensor_tensor(col(15), col(15), col(4), ALU.mult))
                            dv(lambda e: e.tensor_tensor(slab(13), slab(13), bc(col(15), 8), ALU.mult))
                            for gq in range(4):
                                ohg = bc(RB[:, 1, :, gq], 8)
                                dv(lambda e, gq=gq, ohg=ohg: e.tensor_tensor(wgt[:, :, gq * 8:(gq + 1) * 8], slab(13), ohg,
                                                                             ALU.mult), wr_=R_wgt)
                                dv(lambda e, gq=gq, ohg=ohg: e.tensor_tensor(mkk[:, 0, :, gq * 8:(gq + 1) * 8], slab(8), ohg,
                                                                             ALU.mult))
                                dv(lambda e, gq=gq, ohg=ohg: e.tensor_tensor(mkk[:, 1, :, gq * 8:(gq + 1) * 8], slab(12), ohg,
                                                                             ALU.mult))
                            dv(lambda e: e.tensor_tensor(maskb[:], mkk[:, 0], mkk[:, 1], ALU.add), wr_=[R_mask[0]])
                            for t in range(NL):
                                op("pe", lambda e, t=t: e.matmul(PS[5][:, t * NE:(t + 1) * NE], ustr[:], maskb[:, t, :],
                                                               start=True, stop=(t == 0)),
                                   reads=[R_mask[0], R_wo], writes=[PSR[5]])
                                for t2 in range(t):
                                    op("pe", lambda e, t=t, t2=t2: e.matmul(PS[5][:, t * NE:(t + 1) * NE], onesb[:],
                                                                           maskb[:, t2, :], start=False, stop=(t2 == t - 1)),
                                       reads=[R_mask[0], R_wo], writes=[PSR[5]])
                            op("act", lambda e: e.activation(rkk[:, 0].rearrange("p t e -> p (t e)"), PS[5], AF.Copy),
                               reads=[PSR[5]], writes=[R_rk])
                            op("dve", lambda e: e.tensor_tensor(rkk[:, 1], rkk[:, 0],
                                                                eC[:, :].unsqueeze(1).to_broadcast([128, NL, NE]), ALU.add),
                               reads=[R_rk, R_wo], writes=[R_rk])
                            for kk in range(2):
                                for src_i, di in ((1, 0), (0, 1)):
                                    op("dve", lambda e, kk=kk, src_i=src_i: e.tensor_tensor(
                                        t3[:], mkk[:, kk], rkk[:, src_i], ALU.mult), reads=[R_rk, R_B], writes=[R_B])
                                    op("dve", lambda e, kk=kk, di=di: e.tensor_reduce(
                                        dfa[:, di, kk, :], t3[:], AX.X, ALU.add), reads=[R_B], writes=[R_B])
                                op("dve", lambda e, kk=kk: e.tensor_tensor(t3[:], mkk[:, kk], wgt[:], ALU.mult),
                                   reads=[R_B] + R_wgt, writes=[R_B])
                                op("dve", lambda e, kk=kk: e.tensor_reduce(wsel[:, :, kk], t3[:], AX.X, ALU.add),
                                   reads=[R_B], writes=R_dest)
                            dv(lambda e: e.tensor_scalar(dfa[:, 2], dfa[:, 1], float(CAP), None, ALU.is_ge))
                            dv(lambda e: e.scalar_tensor_tensor(dfa[:, 0], dfa[:, 2], 1.0e6, dfa[:, 0], ALU.mult, ALU.add))
                            dv(lambda e: e.tensor_scalar(dfa[:, 0], dfa[:, 0], float(NE * CAP), None, ALU.min))
                            dv(lambda e: e.tensor_copy(dest[:].rearrange("p t k -> p k t"), dfa[:, 0]), wr_=R_dest)
                            for t in range(NL):
                                b = t % 4
                                dma("sp", hxs[b][:], hx2_d[t * 128:(t + 1) * 128, :], reads=[R_xbuf], writes=[R_hxs[b]])
                                for kk in range(2):
                                    K.pdma(None, None, None, reads=[R_hxs[b]] + R_dest, writes=[],
                                           fn=lambda g_, kk=kk, t=t: g_.indirect_dma_start(
                                               out=xbuf_d[:, :],
                                               out_offset=bass.IndirectOffsetOnAxis(ap=dest[:, t, kk:kk + 1], axis=0),
                                               in_=hxs[b][:, :], in_offset=None))
                            K.barrier()
                        if debug:
                            dma("sp", d_wgt, wgt[:], reads=R_wgt)
                        if stop_after == "B2":
                            K.barrier()
                            return nc, list(dbg.keys())

        with ExitStack() as sM:
            NWB = 3
            wgs = [sb(sM, f"wgs{i}", [128, 8, DE], BF16) for i in range(NWB)]
            wus = [sb(sM, f"wus{i}", [128, 8, DE], BF16) for i in range(NWB)]
            wds = [sb(sM, f"wds{i}", [128, 6, D], BF16) for i in range(NWB)]
            Xs = [sb(sM, f"Xs{i}", [128, NJ, D], BF16) for i in range(2)]
            XT = [sb(sM, f"XT{i}", [128, 8, CAP], BF16) for i in range(2)]
            HT = [sb(sM, f"HT{i}", [128, 6, CAP], BF16) for i in range(2)]
            sg = [sb(sM, f"sg{i}", [128, CAP], F32) for i in range(2)]
            Yst = [sb(sM, f"Yst{i}", [128, D], F32) for i in range(2)]
            R_wg = K.regs_n(NWB, "wg")
            R_wu = K.regs_n(NWB, "wu")
            R_wd = K.regs_n(NWB, "wd")
            R_Xs = K.regs_n(2, "Xs")
            R_XT = K.regs_n(2, "XT")
            R_HT = K.regs_n(2, "HT")
            R_sg = K.regs_n(2, "sg")
            R_Yst = K.regs_n(2, "Yst")
            cnt = {"si": 0, "yi": 0, "ti": 0}
            op("dve", lambda e: e.memset(Yst[0][0:1, :], 0.0), writes=[R_Yst[0]])
            dma("sp", ybuf_d[NE * CAP:NE * CAP + 1, :], Yst[0][0:1, :], reads=[R_Yst[0]])

            def load_w(ex):
                w3 = ex % NWB
                K.pdma(None, wgs[w3][:], wg_d[ex].rearrange("(k p) f -> p k f", p=128), writes=[R_wg[w3]])
                K.pdma(None, wus[w3][:], wu_d[ex].rearrange("(k p) f -> p k f", p=128), writes=[R_wu[w3]])
                K.pdma(None, wds[w3][:], wd_d[ex].rearrange("(c p) n -> p c n", p=128), writes=[R_wd[w3]])

            def load_x(ex):
                xb_ = ex % 2
                dma("sp", Xs[xb_][:], xbuf_d[ex * CAP:(ex + 1) * CAP, :].rearrange("(j p) d -> p j d", p=128),
                    writes=[R_Xs[xb_]])

            def transposes(ex):
                xb_ = ex % 2
                for j in range(NJ):
                    tbk = 6 + cnt["ti"] % 2
                    cnt["ti"] += 1
                    psb = PS[tbk].bitcast(BF16)
                    for k in range(8):
                        op("pe", lambda e, k=k: e.transpose(psb[:, k * 128:(k + 1) * 128],
                                                            Xs[xb_][:, j, k * 128:(k + 1) * 128], identb[:]),
                           reads=[R_Xs[xb_], R_const], writes=[PSR[tbk]])
                    if tbk == 6:
                        op("dve", lambda e: e.tensor_copy(XT[xb_][:, :, j * 128:(j + 1) * 128],
                                                          psb.rearrange("p (k n) -> p k n", k=8)),
                           reads=[PSR[tbk]], writes=[R_XT[xb_]])
                    else:
                        op("act", lambda e: e.activation(XT[xb_][:, :, j * 128:(j + 1) * 128],
                                                         psb.rearrange("p (k n) -> p k n", k=8), AF.Copy),
                           reads=[PSR[tbk]], writes=[R_XT[xb_]])

            load_w(0)
            load_w(1)
            load_x(0)
            transposes(0)
            for ex in range(NE):
                wb = ex % 2
                w3 = ex % NWB
                if ex + 2 < NE:
                    load_w(ex + 2)
                if ex + 1 < NE:
                    load_x(ex + 1)
                for fc in range(6):
                    fs = slice(fc * 128, (fc + 1) * 128)
                    gbk = fc % 2
                    ubk = 2 + fc % 2
                    for k in range(8):
                        op("pe", lambda e, k=k: e.matmul(PS[gbk][:, 0:CAP], wgs[w3][:, k, fs], XT[wb][:, k, :],
                                                       start=(k == 0), stop=(k == 7)),
                           reads=[R_wg[w3], R_XT[wb]], writes=[PSR[gbk]])
                    for k in range(8):
                        op("pe", lambda e, k=k: e.matmul(PS[ubk][:, 0:CAP], wus[w3][:, k, fs], XT[wb][:, k, :],
                                                       start=(k == 0), stop=(k == 7)),
                           reads=[R_wu[w3], R_XT[wb]], writes=[PSR[ubk]])
                    sb2 = cnt["si"] % 2
                    cnt["si"] += 1
                    op("act", lambda e: e.activation(sg[sb2][:], PS[gbk][:, 0:CAP], AF.Silu),
                       reads=[PSR[gbk]], writes=[R_sg[sb2]])
                    op("dve", lambda e: e.tensor_tensor(HT[wb][:, fc, :], PS[ubk][:, 0:CAP], sg[sb2][:], ALU.mult),
                       reads=[PSR[ubk], R_sg[sb2]], writes=[R_HT[wb]])
                if ex + 1 < NE:
                    transposes(ex + 1)
                for j in range(NJ):
                    ys = cnt["yi"] % 2
                    cnt["yi"] += 1
                    for hf in range(2):
                        yb_ = 4 + hf
                        cs = slice(hf * 512, (hf + 1) * 512)
                        for fc in range(6):
                            op("pe", lambda e, fc=fc: e.matmul(
                                PS[yb_], HT[wb][:, fc, j * 128:(j + 1) * 128], wds[w3][:, fc, cs],
                                start=(fc == 0), stop=(fc == 5)),
                               reads=[R_HT[wb], R_wd[w3]], writes=[PSR[yb_]])
                        op("dve", lambda e: e.tensor_copy(Yst[ys][:, cs], PS[yb_]),
                           reads=[PSR[yb_]], writes=[R_Yst[ys]])
                    dma("sp", ybuf_d[ex * CAP + j * 128:ex * CAP + (j + 1) * 128, :], Yst[ys][:],
                        reads=[R_Yst[ys]])
            K.barrier()
        with ExitStack() as sF:
            y12 = [[sb(sF, f"y{k}_{i}", [128, D], F32) for k in range(2)] for i in range(4)]
            xr2 = [sb(sF, f"xq{i}", [128, D], F32) for i in range(2)]
            ot = [sb(sF, f"ot{i}", [128, D], F32) for i in range(2)]
            junk3 = sb(sF, "junk3", [128, D], BF16)
            st3 = sb(sF, "st3", [128, 3, NL], F32)
            R_y12 = [K.regs_n(2, f"y12_{i}") for i in range(4)]
            R_xr2 = K.regs_n(2, "xr2")
            R_ot = K.regs_n(2, "ot")
            R_st3 = K.regs_n(NL, "st3")
            def fin_front(tg):
                b = tg % 2
                tsl = slice(tg * 128, (tg + 1) * 128)
                dma("sp", xr2[b][:], xnew_d[tsl, :], writes=[R_xr2[b]])
                for kk in range(2):
                    K.pdma(None, None, None, reads=[], writes=[R_y12[tg % 4][kk]],
                           fn=lambda g, kk=kk: g.indirect_dma_start(
                               out=y12[tg % 4][kk][:, :], out_offset=None, in_=ybuf_d[:, :],
                               in_offset=bass.IndirectOffsetOnAxis(ap=dest[:, tg, kk:kk + 1], axis=0)))
                op("dve", lambda e: e.tensor_scalar(ot[b][:], y12[tg % 4][0][:], wsel[:, tg, 0:1], None, ALU.mult),
                   reads=[R_y12[tg % 4][0]], writes=[R_ot[b]])
                op("dve", lambda e: e.scalar_tensor_tensor(ot[b][:], y12[tg % 4][1][:], wsel[:, tg, 1:2], ot[b][:],
                                                          ALU.mult, ALU.add),
                   reads=[R_y12[tg % 4][1], R_ot[b]], writes=[R_ot[b]])
                if debug:
                    dma("sp", d_acc[:, tg, :], ot[b][:], reads=[R_ot[b]])
                op("dve", lambda e: e.tensor_tensor(ot[b][:], ot[b][:], gt2b[:], ALU.mult),
                   reads=[R_ot[b], R_gt2], writes=[R_ot[b]])
                op("dve", lambda e: e.tensor_tensor(ot[b][:], ot[b][:], xr2[b][:], ALU.add),
                   reads=[R_ot[b], R_xr2[b]], writes=[R_ot[b]])

            def fin_back(tg):
                b = tg % 2
                tsl = slice(tg * 128, (tg + 1) * 128)
                op("act", lambda e: e.activation(junk3[:], ot[b][:], AF.Square, accum_out=st3[:, 0, tg:tg + 1]),
                   reads=[R_ot[b]], writes=[R_st3[tg]])
                op("act", lambda e: e.activation(st3[:, 1, tg:tg + 1], st3[:, 0, tg:tg + 1], AF.Ln,
                                                 scale=1.0 / D, bias=EPS),
                   reads=[R_st3[tg]], writes=[R_st3[tg]])
                op("act", lambda e: e.activation(st3[:, 2, tg:tg + 1], st3[:, 1, tg:tg + 1], AF.Exp, scale=-0.5),
                   reads=[R_st3[tg]], writes=[R_st3[tg]])
                op("dve", lambda e: e.scalar_tensor_tensor(
                    ot[b][:], ot[b][:], st3[:, 2, tg:tg + 1], fgb[:], ALU.mult, ALU.mult),
                   reads=[R_ot[b], R_st3[tg], R_const], writes=[R_ot[b]])
                dma("sp", y_d[tsl, :], ot[b][:], reads=[R_ot[b]])

            fin_front(0)
            for tg in range(NL):
                if tg + 1 < NL:
                    fin_front(tg + 1)
                fin_back(tg)
            K.barrier()
    return nc, list(dbg.keys())


def _host_consts():
    ident = np.eye(128, dtype=np.float32)
    rows = SEQ // 64
    row_idx = np.repeat(np.arange(rows, dtype=np.float32), 64)
    col_idx = np.tile(np.arange(64, dtype=np.float32), rows)
    inv_freq = (np.float32(10000.0) ** (-np.arange(0, 32, 2, dtype=np.float32) / np.float32(32))).astype(np.float32)
    ang = np.stack([row_idx[:, None] * inv_freq, col_idx[:, None] * inv_freq], axis=1).astype(np.float32)
    cos = np.cos(ang).astype(np.float32).reshape(SEQ, 32)
    sin = np.sin(ang).astype(np.float32).reshape(SEQ, 32)
    cosT = np.ascontiguousarray(cos.reshape(NL, 128, 32).transpose(1, 0, 2))
    sinT = np.ascontiguousarray(sin.reshape(NL, 128, 32).transpose(1, 0, 2))
    bd64 = np.zeros((128, 128), np.float32)
    bd64[:64, :64] = 1.0 / 64
    bd64[64:, 64:] = 1.0 / 64
    c65 = np.full((65, 64), 1.0 / 64, np.float32)
    c65[64, :] = EPS
    e0 = np.zeros((128, 1), np.float32)
    e0[0, 0] = 1.0
    ustr = np.triu(np.ones((128, 128), np.float32), k=1)
    eC = np.ascontiguousarray(np.broadcast_to((np.arange(NE, dtype=np.float32) * CAP)[None, :], (128, NE)))
    return dict(ident_f=ident, cosT=cosT, sinT=sinT, bd64=bd64, c65=c65, e0=e0, ustr=ustr, eC=eC)


def make_in_maps(inputs, cores=range(8)):
    f = lambda a: np.ascontiguousarray(np.asarray(a, dtype=np.float32))
    x = f(inputs["x"]); c = f(inputs["c"]); ctx = f(inputs["ctx"]); c_ctx = f(inputs["c_ctx"])
    consts = _host_consts()
    bc = lambda v: np.ascontiguousarray(np.broadcast_to(f(v).reshape(1, -1), (128, f(v).size)))
    shared = dict(
        w_mod=f(inputs["w_mod"])[0], bm_b=bc(inputs["b_mod"][0]), g1_b=bc(inputs["norm1_g"][0]),
        g2_b=bc(inputs["norm2_g"][0]), fg_b=bc(inputs["final_g"]), w_in=f(inputs["w_in"])[0],
        qg_b=bc(inputs["q_norm_g"][0]), kg_b=bc(inputs["k_norm_g"][0]),
        cwT=np.ascontiguousarray(f(inputs["conv_w"])[0].reshape(3, 4, 128).transpose(2, 1, 0).reshape(128, 12)),
        gallT=np.ascontiguousarray(np.concatenate([f(inputs["attn_out_g"])[0], f(inputs["conv_out_g"])[0]])
                                   .reshape(8, 128).T),
        w_out=f(inputs["w_out"])[0],
        w_r=np.ascontiguousarray(np.concatenate([f(inputs["w_group"])[0], f(inputs["w_router"])[0]], axis=1)),
        w_gate=f(inputs["w_gate"])[0], w_up=f(inputs["w_up"])[0], w_down=f(inputs["w_down"])[0],
        **consts,
    )
    maps = []
    for b in cores:
        cT = np.concatenate([c[b].reshape(8, 128).T, c_ctx.reshape(8, 128).T], axis=1)
        m = dict(shared)
        m["xin"] = np.ascontiguousarray(np.concatenate([ctx[b], x[b]], axis=0))
        m["cT"] = np.ascontiguousarray(cT.astype(np.float32))
        maps.append(m)
    return maps


_CACHE = {}


def kernel(**inputs):
    if "nc" not in _CACHE:
        _CACHE["nc"] = build_program(debug=False)[0]
    nc = _CACHE["nc"]
    maps = make_in_maps(inputs)
    res = run_bass_kernel_spmd(nc, maps, core_ids=list(range(8)))
    out = np.stack([np.asarray(r["y"], dtype=np.float32) for r in res.results], axis=0)
    return out
```

```python
import os
import numpy as np
from contextlib import ExitStack
import concourse.bass as bass
import concourse.mybir as mybir
from concourse.bass_utils import run_bass_kernel_spmd

F32 = mybir.dt.float32
BF16 = mybir.dt.bfloat16
I32 = mybir.dt.int32
AF = mybir.ActivationFunctionType
ALU = mybir.AluOpType
AX = mybir.AxisListType

D = 1024
SEQ = 2048
CTX = 256
NTOK = SEQ + CTX
NT = NTOK // 128
NL = SEQ // 128
EPS = 1e-6
NE = 32
DE = 768
N_DSEM = 40
CAP = 512
NJ = CAP // 128
_LVL = int(os.environ.get('KLVL', '9'))
_KEV = os.environ.get('KEV', 'act')


class Reg:
    __slots__ = ("w", "r", "name")

    def __init__(self, name=""):
        self.w = None
        self.r = {}
        self.name = name


class Sem:
    def __init__(self, handle):
        self.handle = handle
        self.count = 0


class EngW:
    def __init__(self, name, eng, sem):
        self.name = name
        self.eng = eng
        self.sem = sem
        self.waited = {}


class KB:
    def __init__(self, nc, es):
        self.nc = nc
        self._es = es
        self.pslots = []
        self.regs = []
        mk = lambda n: Sem(es.enter_context(nc.semaphore(n)))
        self.E = {
            "pe": EngW("pe", nc.tensor, mk("s_pe")),
            "act": EngW("act", nc.scalar, mk("s_act")),
            "dve": EngW("dve", nc.vector, mk("s_dve")),
            "pool": EngW("pool", nc.gpsimd, mk("s_pool")),
            "sp": EngW("sp", nc.sync, mk("s_sp")),
        }
        self.dsems = [mk(f"s_d{i}") for i in range(N_DSEM)]
        self.dnext = 0
        self.psems = [mk(f"s_q{i}") for i in range(24)]
        self.pnext = 0

    def reg(self, name=""):
        r = Reg(name)
        self.regs.append(r)
        return r

    def regs_n(self, n, name=""):
        return [self.reg(f"{name}{i}") for i in range(n)]

    def wait(self, ew, tok):
        sem, val = tok
        if ew.waited.get(id(sem), 0) >= val:
            return
        ew.eng.wait_ge(sem.handle, val)
        ew.waited[id(sem)] = val

    def _deps(self, ew, reads, writes):
        for r in reads:
            if r.w is not None:
                if ew.name == "pe" and r.w[0] is ew.sem:
                    continue
                self.wait(ew, r.w)
        for w in writes:
            toks = list(w.r.values())
            if w.w is not None:
                toks.append(w.w)
            for t in toks:
                if ew.name == "pe" and t[0] is ew.sem:
                    continue
                self.wait(ew, t)

    def _record(self, tok, reads, writes):
        for r in reads:
            r.r[id(tok[0])] = tok
        for w in writes:
            w.w = tok
            w.r = {}

    def op(self, en, fn, reads=(), writes=()):
        ew = self.E[en]
        self._deps(ew, reads, writes)
        ins = fn(ew.eng)
        ew.sem.count += 1
        ins.then_inc(ew.sem.handle, 1)
        tok = (ew.sem, ew.sem.count)
        self._record(tok, reads, writes)
        return tok

    def dma(self, qn, out, in_, reads=(), writes=(), fn=None):
        ew = self.E[qn]
        self._deps(ew, reads, writes)
        d = self.dsems[self.dnext % N_DSEM]
        self.dnext += 1
        if d.count:
            self.wait(ew, (d, d.count))
        if fn is None:
            ins = ew.eng.dma_start(out=out, in_=in_)
        else:
            ins = fn(ew.eng)
        d.count += 16
        ins.then_inc(d.handle, 16)
        tok = (d, d.count)
        self._record(tok, reads, writes)
        return tok

    def pslot(self, name):
        return None

    def pdma(self, slot, out, in_, reads=(), writes=(), fn=None):
        ew = self.E["pool"]
        self._deps(ew, reads, writes)
        d = self.psems[self.pnext % len(self.psems)]
        self.pnext += 1
        if d.count:
            self.wait(ew, (d, d.count))
        ins = ew.eng.dma_start(out=out, in_=in_) if fn is None else fn(ew.eng)
        d.count += 16
        ins.then_inc(d.handle, 16)
        tok = (d, d.count)
        self._record(tok, reads, writes)
        return tok

    def barrier(self):
        for ew in self.E.values():
            for other in self.E.values():
                if other.sem.count and not (other is ew and ew.name in ("pe", "sp")):
                    self.wait(ew, (other.sem, other.sem.count))
            for d in self.dsems:
                if d.count:
                    self.wait(ew, (d, d.count))
            for d in self.psems:
                if d.count:
                    self.wait(ew, (d, d.count))
        for r in self.regs:
            r.w = None
            r.r = {}


def build_program(debug=False, stop_after=None):
    nc = bass.Bass("TRN2", target_bir_lowering=False)

    def din(name, shape, dt=F32):
        return nc.dram_tensor(name, list(shape), dt, kind="ExternalInput").ap()

    xin = din("xin", [NTOK, D])
    cT_d = din("cT", [128, 16])
    w_mod_d = din("w_mod", [D, 6 * D])
    bm_d = din("bm_b", [128, 6 * D])
    g1_d = din("g1_b", [128, D])
    g2_d = din("g2_b", [128, D])
    fg_d = din("fg_b", [128, D])
    w_in_d = din("w_in", [D, 2304])
    qg_d = din("qg_b", [128, 64])
    kg_d = din("kg_b", [128, 64])
    cw_d = din("cwT", [128, 12])
    gall_d = din("gallT", [128, 8])
    w_out_d = din("w_out", [D, D])
    wr_d = din("w_r", [D, 36])
    wg_d = din("w_gate", [NE, D, DE])
    wu_d = din("w_up", [NE, D, DE])
    wd_d = din("w_down", [NE, DE, D])
    identf_d = din("ident_f", [128, 128])
    cos_d = din("cosT", [128, NL, 32])
    sin_d = din("sinT", [128, NL, 32])
    bd64_d = din("bd64", [128, 128])
    c65_d = din("c65", [65, 64])
    e0_d = din("e0", [128, 1])
    ustr_d = din("ustr", [128, 128])
    eC_d = din("eC", [128, NE])

    y_d = nc.dram_tensor("y", [SEQ, D], F32, kind="ExternalOutput").ap()
    xnew_d = nc.dram_tensor("xnew_scr", [SEQ, D], F32,
                            kind="ExternalOutput" if debug else "Internal").ap()
    xbuf_d = nc.dram_tensor("xbuf_scr", [NE * CAP + 1, D], BF16, kind="Internal").ap()
    ybuf_d = nc.dram_tensor("ybuf_scr", [NE * CAP + 1, D], F32, kind="Internal").ap()
    hx2_d = nc.dram_tensor("hx2_scr", [SEQ, D], BF16, kind="Internal").ap()
    dbg = {}

    def dbg_out(name, shape, dt=F32):
        if debug:
            dbg[name] = nc.dram_tensor(name, list(shape), dt, kind="ExternalOutput").ap()
        return dbg.get(name)

    d_smod = dbg_out("d_smod", [128, 32])
    d_modb = dbg_out("d_modb", [128, 6 * D])
    d_QT = dbg_out("d_QT", [128, 4, SEQ], BF16)
    d_KT = dbg_out("d_KT", [128, 2, NTOK], BF16)
    d_V = dbg_out("d_V", [128, NT, 2, 65], BF16)
    d_convn = dbg_out("d_convn", [128, 4, SEQ], BF16)
    d_attn = dbg_out("d_attn", [64, 8, SEQ], BF16)
    d_wgt = dbg_out("d_wgt", [128, NL, NE])
    d_acc = dbg_out("d_acc", [128, NL, D])

    with ExitStack() as es:
        K = KB(nc, es)
        op, dma = K.op, K.dma

        def sb(stk, name, shape, dt):
            return stk.enter_context(nc.sbuf_tensor("sb_" + name, list(shape), dt))

        PSUM = es.enter_context(nc.psum_tensor("psum", [128, 8 * 512], F32))
        PS = [PSUM[:, i * 512:(i + 1) * 512] for i in range(8)]
        PSR = K.regs_n(8, "ps")

        identf = sb(es, "identf", [128, 128], F32)
        identb = sb(es, "identb", [128, 128], BF16)
        fgb = sb(es, "fgb", [128, D], F32)
        gt2b = sb(es, "gt2b", [128, D], F32)
        wgt = sb(es, "wgt", [128, NL, NE], F32)
        dest = sb(es, "dest", [128, NL, 2], I32)
        wsel = sb(es, "wsel", [128, NL, 2], F32)
        R_dest = K.regs_n(NL, "dest")
        R_const = K.reg("const")
        R_gt2 = K.reg("gt2")
        R_wgt = K.regs_n(NL, "wgt")

        dma("sp", identf[:], identf_d, writes=[R_const])
        dma("sp", fgb[:], fg_d, writes=[R_const])
        op("dve", lambda e: e.tensor_copy(identb[:], identf[:]), reads=[R_const], writes=[R_const])

        with ExitStack() as sAB:
            modb = sb(sAB, "modb", [128, 6 * D], F32)
            smod = sb(sAB, "smod", [128, 32], F32)
            cosT = sb(sAB, "cosT", [128, NL, 32], F32)
            sinT = sb(sAB, "sinT", [128, NL, 32], F32)
            qgb = sb(sAB, "qgb", [128, 64], F32)
            kgb = sb(sAB, "kgb", [128, 64], F32)
            cw = sb(sAB, "cw", [128, 12], F32)
            bd64 = sb(sAB, "bd64", [128, 128], F32)
            c65 = sb(sAB, "c65", [65, 64], F32)
            e0 = sb(sAB, "e0", [128, 1], F32)
            wr = sb(sAB, "wr", [128, 8, 36], F32)
            R_modb = K.regs_n(6, "modb")
            R_smod = K.reg("smod")
            for t_, d_ in ((cosT, cos_d), (sinT, sin_d), (qgb, qg_d), (kgb, kg_d), (cw, cw_d),
                           (bd64, bd64_d), (c65, c65_d), (e0, e0_d)):
                dma("sp", t_[:], d_, writes=[R_const])
            dma("sp", wr[:], wr_d.rearrange("(k p) n -> p k n", p=128), writes=[R_const])

            with ExitStack() as s0:
                cTt = sb(s0, "cTt", [128, 16], F32)
                scT = sb(s0, "scT", [128, 16], F32)
                lhs_c = sb(s0, "lhs_c", [128, 8, 128], BF16)
                lhs_x = sb(s0, "lhs_x", [128, 8, 128], BF16)
                bm = sb(s0, "bm", [128, 6 * D], F32)
                g1b = sb(s0, "g1b", [128, D], F32)
                g2b = sb(s0, "g2b", [128, D], F32)
                modx = sb(s0, "modx", [128, 2 * D], F32)
                wm = [sb(s0, f"wm{i}", [128, 8, 512], BF16) for i in range(2)]
                R_c = K.reg("c")
                R_bm = K.reg("bm")
                R_g = K.reg("g12")
                R_modx = K.regs_n(2, "modx")
                R_wm = K.regs_n(2, "wm")
                P_wm = [K.pslot(f"wm{i}") for i in range(2)]
                dma("sp", cTt[:], cT_d, writes=[R_c])
                dma("sp", bm[:], bm_d, writes=[R_bm])
                dma("sp", g1b[:], g1_d, writes=[R_g])
                dma("sp", g2b[:], g2_d, writes=[R_g])
                op("act", lambda e: e.activation(scT[:], cTt[:], AF.Silu), reads=[R_c], writes=[R_c])
                op("dve", lambda e: e.tensor_copy(
                    lhs_c[:], scT[:, 0:8].unsqueeze(2).to_broadcast([128, 8, 128])), reads=[R_c], writes=[R_c])
                op("dve", lambda e: e.tensor_copy(
                    lhs_x[:], scT[:, 8:16].unsqueeze(2).to_broadcast([128, 8, 128])), reads=[R_c], writes=[R_c])
                wmod_v = w_mod_d.rearrange("(k p) n -> p k n", p=128)
                for blk in range(12):
                    b = blk % 2
                    cs = slice(blk * 512, (blk + 1) * 512)
                    K.pdma(P_wm[b], wm[b][:], wmod_v[:, :, cs], writes=[R_wm[b]])
                    for k in range(8):
                        op("pe", lambda e, k=k: e.matmul(PS[b], lhs_c[:, k, :], wm[b][:, k, :],
                                                       start=(k == 0), stop=(k == 7)),
                           reads=[R_c, R_wm[b]], writes=[PSR[b]])
                    op("dve", lambda e: e.tensor_tensor(modb[:, cs], PS[b], bm[:, cs], ALU.add),
                       reads=[PSR[b], R_bm], writes=[R_modb[blk // 2]])
                    if blk < 4:
                        for k in range(8):
                            op("pe", lambda e, k=k: e.matmul(PS[2 + b], lhs_x[:, k, :], wm[b][:, k, :],
                                                           start=(k == 0), stop=(k == 7)),
                               reads=[R_c, R_wm[b]], writes=[PSR[2 + b]])
                        op("dve", lambda e: e.tensor_tensor(modx[:, cs], PS[2 + b], bm[:, cs], ALU.add),
                           reads=[PSR[2 + b], R_bm], writes=[R_modx[blk // 2]])
                op("dve", lambda e: e.scalar_tensor_tensor(modb[:, D:2 * D], modb[:, D:2 * D], 1.0, g1b[:],
                                                          ALU.add, ALU.mult),
                   reads=[R_modb[1], R_g], writes=[R_modb[1]])
                op("dve", lambda e: e.scalar_tensor_tensor(modx[:, D:2 * D], modx[:, D:2 * D], 1.0, g1b[:],
                                                          ALU.add, ALU.mult),
                   reads=[R_modx[1], R_g], writes=[R_modx[1]])
                op("dve", lambda e: e.scalar_tensor_tensor(modb[:, 4 * D:5 * D], modb[:, 4 * D:5 * D], 1.0, g2b[:],
                                                          ALU.add, ALU.mult),
                   reads=[R_modb[4], R_g], writes=[R_modb[4]])
                op("act", lambda e: e.activation(gt2b[:], modb[:, 5 * D:6 * D], AF.Copy),
                   reads=[R_modb[5]], writes=[R_gt2])
                srcs = [(modb, D, R_modb[1]), (modb, 0, R_modb[0]), (modx, D, R_modx[1]), (modx, 0, R_modx[0])]
                for vi, (tt, off, rr) in enumerate(srcs):
                    for k in range(8):
                        c0 = vi * 8 + k
                        op("pe", lambda e, tt=tt, off=off, k=k, c0=c0: e.matmul(
                            PS[4][:, c0:c0 + 1], tt[:, off + k * 128: off + (k + 1) * 128], e0[:, 0:1],
                            start=True, stop=True), reads=[rr, R_const], writes=[PSR[4]])
                op("act", lambda e: e.activation(smod[:], PS[4][:, 0:32], AF.Copy), reads=[PSR[4]], writes=[R_smod])
                if debug:
                    dma("sp", d_smod, smod[:], reads=[R_smod])
                    dma("sp", d_modb, modb[:], reads=R_modb)
                K.barrier()
                if stop_after == "0":
                    return nc, list(dbg.keys())

            with ExitStack() as sB:
                QT = sb(sB, "QT", [128, 4, SEQ], BF16)
                KT = sb(sB, "KT", [128, 2, 2, NTOK], BF16)
                VE = sb(sB, "VE", [128, NT, 2, 128], BF16)
                convn = sb(sB, "convn", [128, 4, SEQ], BF16)
                R_QT = K.regs_n(NL, "QT")
                R_KT = K.regs_n(NT, "KT")
                R_VE = K.regs_n(NT, "VE")
                R_convn = K.regs_n(4, "convn")
                R_ve1 = K.reg("ve1")
                if _LVL >= 0:
                    op("dve", lambda e: e.memset(VE[:], 0.0), writes=[R_ve1])
                    op("dve", lambda e: e.memset(VE[:, :, :, 64:65], 1.0), writes=[R_ve1])
                    op("dve", lambda e: e.memset(KT[:], 0.0), writes=[R_ve1])

                with ExitStack() as sA:
                    hT = sb(sA, "hT", [128, 8, NTOK], BF16)
                    R_hTd = K.regs_n(NT, "hTd")
                    R_hTa = K.regs_n(NT, "hTa")
                    with ExitStack() as sA1:
                        winq = sb(sA1, "winq", [128, 8, 768], BF16)
                        R_winq = K.reg("winq")
                        if _LVL >= 1:
                            K.pdma(K.pslot("winq"), winq[:], w_in_d.rearrange("(k p) n -> p k n", p=128)[:, :, 0:768],
                                   writes=[R_winq])
                        xt = [sb(sA1, f"xt{i}", [128, D], F32) for i in range(2)]
                        junk = sb(sA1, "junk", [128, D], BF16)
                        xn = [sb(sA1, f"xn{i}", [128, D], BF16) for i in range(2)]
                        st1 = sb(sA1, "st1", [128, 3, NT], F32)
                        zq = [sb(sA1, f"zq{i}", [128, 512], F32) for i in range(2)]
                        zkv = [sb(sA1, f"zkv{i}", [128, 256], F32) for i in range(2)]
                        sq = sb(sA1, "sq", [128, 512], F32)
                        qn = sb(sA1, "qn", [128, 512], F32)
                        kn = sb(sA1, "kn", [128, 128], F32)
                        knb = sb(sA1, "knb", [128, 128], BF16)
                        ta = sb(sA1, "ta", [128, 256], F32)
                        tb = sb(sA1, "tb", [128, 256], F32)
                        qst = sb(sA1, "qst", [128, 3, 16], F32)
                        qkb = [sb(sA1, f"qkb{i}", [128, 768], BF16) for i in range(2)]
                        R_xt = K.regs_n(2, "xt")
                        R_xn = K.regs_n(2, "xn")
                        R_st1 = K.regs_n(NT, "st1")
                        R_zq = K.regs_n(2, "zq")
                        R_zkv = K.regs_n(2, "zkv")
                        R_tmp = K.reg("tmpq")
                        R_qst = K.reg("qst")
                        R_qkb = K.regs_n(2, "qkb")
                        R_knb = K.reg("knb")
                        def a1_front(T):
                            b = T % 2
                            lat = T >= 2
                            t = T - 2
                            tsl = slice(T * 128, (T + 1) * 128)
                            dma("sp", xt[b][:], xin[tsl, :], writes=[R_xt[b]])
                            if _LVL < -4:
                                return
                            op("act", lambda e: e.activation(junk[:], xt[b][:], AF.Square,
                                                             accum_out=st1[:, 0, T:T + 1]),
                               reads=[R_xt[b]], writes=[R_st1[T]])
                            if _LVL < -3:
                                return
                            op("act", lambda e: e.activation(st1[:, 1, T:T + 1], st1[:, 0, T:T + 1], AF.Ln,
                                                             scale=1.0 / D, bias=EPS),
                               reads=[R_st1[T]], writes=[R_st1[T]])
                            op("act", lambda e: e.activation(st1[:, 2, T:T + 1], st1[:, 1, T:T + 1], AF.Exp,
                                                             scale=-0.5),
                               reads=[R_st1[T]], writes=[R_st1[T]])
                            op("act", lambda e: e.activation(xn[b][:], xt[b][:], AF.Copy, scale=st1[:, 2, T:T + 1]),
                               reads=[R_xt[b], R_st1[T]], writes=[R_xn[b]])
                            if _LVL < -2:
                                return
                            psb = PS[b].bitcast(BF16)
                            for k in range(8):
                                op("pe", lambda e, k=k: e.transpose(psb[:, k * 128:(k + 1) * 128],
                                                                    xn[b][:, k * 128:(k + 1) * 128], identb[:]),
                                   reads=[R_xn[b], R_const], writes=[PSR[b]])
                            if _LVL < -1:
                                return
                            so = 0 if lat else 16
                            for k in range(8):
                                if (b == 0 and _KEV != 'act') or _KEV == 'dve':
                                    op("dve", lambda e, k=k: e.tensor_scalar(
                                        hT[:, k, tsl], psb[:, k * 128:(k + 1) * 128],
                                        smod[:, so + k:so + k + 1], smod[:, so + 8 + k:so + 9 + k],
                                        ALU.mult, ALU.add), reads=[PSR[b], R_smod], writes=[R_hTd[T]])
                                else:
                                    op("act", lambda e, k=k: e.activation(
                                        hT[:, k, tsl], psb[:, k * 128:(k + 1) * 128], AF.Identity,
                                        bias=smod[:, so + 8 + k:so + 9 + k], scale=smod[:, so + k:so + k + 1]),
                                       reads=[PSR[b], R_smod], writes=[R_hTa[T]])
                            if _LVL < 1:
                                return
                            if lat:
                                for k in range(8):
                                    op("pe", lambda e, k=k: e.matmul(PS[2], hT[:, k, tsl], winq[:, k, 0:512],
                                                                   start=(k == 0), stop=(k == 7)),
                                       reads=[R_hTd[T], R_hTa[T], R_winq], writes=[PSR[2]])
                            for k in range(8):
                                op("pe", lambda e, k=k: e.matmul(PS[3][:, 0:256], hT[:, k, tsl], winq[:, k, 512:768],
                                                               start=(k == 0), stop=(k == 7)),
                                   reads=[R_hTd[T], R_hTa[T], R_winq], writes=[PSR[3]])

                        def a1_front_b(T):
                            b = T % 2
                            lat = T >= 2
                            t = T - 2
                            tsl = slice(T * 128, (T + 1) * 128)
                            if lat:
                                op("act", lambda e: e.activation(zq[b][:], PS[2], AF.Copy),
                                   reads=[PSR[2]], writes=[R_zq[b]])
                            op("act", lambda e: e.activation(zkv[b][:], PS[3][:, 0:256], AF.Copy),
                               reads=[PSR[3]], writes=[R_zkv[b]])
                            op("act", lambda e: e.activation(
                                VE[:, T, :, 0:64], zkv[b][:, 128:256].rearrange("p (g d) -> p g d", g=2), AF.Copy),
                               reads=[R_zkv[b], R_ve1], writes=[R_VE[T]])

                        def a1_back(T, part):
                            b = T % 2
                            lat = T >= 2
                            t = T - 2
                            tsl = slice(T * 128, (T + 1) * 128)
                            nh_list = ([("q", zq[b], 8, qn, qgb, R_zq[b])] if lat else []) + \
                                      [("k", zkv[b], 2, kn, kgb, R_zkv[b])]
                            for nm, src, nh, dst, gb_, rsrc in nh_list:
                                W = nh * 64
                                c0 = 0 if nm == "q" else 8
                                op("dve", lambda e: e.tensor_tensor(sq[:, 0:W], src[:, 0:W], src[:, 0:W], ALU.mult),
                                   reads=[rsrc], writes=[R_tmp])
                                op("dve", lambda e: e.tensor_reduce(
                                    qst[:, 0, c0:c0 + nh], sq[:, 0:W].rearrange("p (h d) -> p h d", h=nh),
                                    AX.X, ALU.add), reads=[R_tmp], writes=[R_qst])
                            lo = 0 if lat else 8
                            op("act", lambda e: e.activation(qst[:, 1, lo:10], qst[:, 0, lo:10],
                                                             AF.Ln, scale=1.0 / 64, bias=EPS),
                               reads=[R_qst], writes=[R_qst])
                            op("act", lambda e: e.activation(qst[:, 2, lo:10], qst[:, 1, lo:10],
                                                             AF.Exp, scale=-0.5),
                               reads=[R_qst], writes=[R_qst])

                        def a1_back_rest(T):
                            b = T % 2
                            lat = T >= 2
                            t = T - 2
                            tsl = slice(T * 128, (T + 1) * 128)
                            nh_list = ([("q", zq[b], 8, qn, qgb, R_zq[b])] if lat else []) + \
                                      [("k", zkv[b], 2, kn, kgb, R_zkv[b])]
                            for nm, src, nh, dst, gb_, rsrc in nh_list:
                                W = nh * 64
                                c0 = 0 if nm == "q" else 8
                                d3 = dst[:, 0:W].rearrange("p (h d) -> p h d", h=nh)
                                op("dve", lambda e: e.tensor_tensor(
                                    d3, src[:, 0:W].rearrange("p (h d) -> p h d", h=nh),
                                    qst[:, 2, c0:c0 + nh].unsqueeze(2).to_broadcast([128, nh, 64]), ALU.mult),
                                   reads=[rsrc, R_qst], writes=[R_tmp])
                                op("dve", lambda e: e.tensor_tensor(
                                    d3, d3, gb_[:, :].unsqueeze(1).to_broadcast([128, nh, 64]), ALU.mult),
                                   reads=[R_tmp, R_const], writes=[R_tmp])
                                if nm == "q":
                                    outb = qkb[b][:, 0:512]
                                    R_ob = R_qkb[b]
                                else:
                                    outb = knb[:, 0:128]
                                    R_ob = R_knb
                                if lat:
                                    d5 = dst[:, 0:W].rearrange("p (h a f d) -> p h a f d", h=nh, a=2, f=2)
                                    o5 = outb.rearrange("p (h a f d) -> p h a f d", h=nh, a=2, f=2)
                                    x1 = d5[:, :, :, 0, :]
                                    x2 = d5[:, :, :, 1, :]
                                    cb = cosT[:, t, :].rearrange("p (a d) -> p a d", a=2).unsqueeze(1) \
                                        .to_broadcast([128, nh, 2, 16])
                                    sb_ = sinT[:, t, :].rearrange("p (a d) -> p a d", a=2).unsqueeze(1) \
                                        .to_broadcast([128, nh, 2, 16])
                                    hw = nh * 32
                                    ta4 = ta[:, 0:hw].rearrange("p (h a d) -> p h a d", h=nh, a=2)
                                    tb4 = tb[:, 0:hw].rearrange("p (h a d) -> p h a d", h=nh, a=2)
                                    op("dve", lambda e: e.tensor_tensor(ta4, x1, cb, ALU.mult),
                                       reads=[R_tmp, R_const], writes=[R_tmp])
                                    op("dve", lambda e: e.tensor_tensor(tb4, x2, sb_, ALU.mult),
                                       reads=[R_tmp, R_const], writes=[R_tmp])
                                    op("dve", lambda e: e.tensor_tensor(o5[:, :, :, 0, :], ta4, tb4, ALU.subtract),
                                       reads=[R_tmp], writes=[R_ob])
                                    op("dve", lambda e: e.tensor_tensor(ta4, x1, sb_, ALU.mult),
                                       reads=[R_tmp, R_const], writes=[R_tmp])
                                    op("dve", lambda e: e.tensor_tensor(tb4, x2, cb, ALU.mult),
                                       reads=[R_tmp, R_const], writes=[R_tmp])
                                    op("dve", lambda e: e.tensor_tensor(o5[:, :, :, 1, :], ta4, tb4, ALU.add),
                                       reads=[R_tmp], writes=[R_ob])
                                else:
                                    op("dve", lambda e: e.tensor_copy(outb, dst[:, 0:W]),
                                       reads=[R_tmp], writes=[R_ob])
                            if _LVL < 4:
                                return
                            op("dve", lambda e: e.tensor_copy(
                                qkb[b][:, 512:768].rearrange("p (g r d) -> p g r d", g=2, r=2),
                                knb[:, 0:128].rearrange("p (g d) -> p g d", g=2).unsqueeze(2)
                                .to_broadcast([128, 2, 2, 64])), reads=[R_knb], writes=[R_qkb[b]])
                            if _LVL < 5:
                                return
                            ps4 = PS[4].bitcast(BF16)
                            ps5 = PS[5].bitcast(BF16)
                            if lat:
                                for j in range(4):
                                    op("pe", lambda e, j=j: e.transpose(ps4[:, j * 128:(j + 1) * 128],
                                                                        qkb[b][:, j * 128:(j + 1) * 128], identb[:]),
                                       reads=[R_qkb[b], R_const], writes=[PSR[4]])
                                op("dve", lambda e: e.tensor_copy(
                                    QT[:, :, t * 128:(t + 1) * 128],
                                    ps4[:, 0:512].rearrange("p (j n) -> p j n", j=4)),
                                   reads=[PSR[4]], writes=[R_QT[t]])
                            for j in range(2):
                                op("pe", lambda e, j=j: e.transpose(ps5[:, j * 128:(j + 1) * 128],
                                                                    qkb[b][:, 512 + j * 128:512 + (j + 1) * 128],
                                                                    identb[:]),
                                   reads=[R_qkb[b], R_const], writes=[PSR[5]])
                            for u in range(2):
                                op("dve", lambda e, u=u: e.tensor_copy(
                                    KT[64 * u:64 * u + 64, :, u, tsl],
                                    ps5[64 * u:64 * u + 64, 0:256].rearrange("p (j n) -> p j n", j=2)),
                                   reads=[PSR[5], R_ve1], writes=[R_KT[T]])

                        a1_front(0)
                        a1_front_b(0)
                        for T in range(NT):
                            if T + 1 < NT:
                                a1_front(T + 1)
                            a1_back(T, "stats")
                            if T + 1 < NT:
                                a1_front_b(T + 1)
                            a1_back_rest(T)
                        K.barrier()
                        if stop_after == "A1":
                            if debug:
                                dma("sp", d_QT, QT[:]); dma("sp", d_KT[0:64], KT[0:64, :, 0, :]); dma("sp", d_KT[64:128], KT[64:128, :, 1, :]); dma("sp", d_V, VE[:, :, :, 0:65])
                            K.barrier()
                            return nc, list(dbg.keys())
                    with ExitStack() as sA2:
                        wc = [sb(sA2, f"wc{i}", [128, 8, 384], BF16) for i in range(2)]
                        vT = [sb(sA2, f"vT{i}", [128, SEQ + 2], F32) for i in range(2)]
                        gbT = [sb(sA2, f"gbT{i}", [128, SEQ], F32) for i in range(2)]
                        gct = [sb(sA2, f"gct{i}", [128, 512], F32) for i in range(2)]
                        yb = sb(sA2, "yb", [128, 1024], F32)
                        ysq = sb(sA2, "ysq", [128, 1024], F32)
                        rs = sb(sA2, "rs", [128, 1024], F32)
                        R_wc = K.regs_n(2, "wc")
                        P_wc = [[K.pslot(f"wc{i}_{j}") for j in range(3)] for i in range(2)]
                        R_vT = K.regs_n(2, "vT")
                        R_gbT = K.regs_n(2, "gbT")
                        R_gct = K.regs_n(2, "gct")
                        R_y = K.reg("y")
                        R_ysq = K.reg("ysq")
                        R_rs = K.reg("rs")
                        winv = w_in_d.rearrange("(k p) n -> p k n", p=128)
                        for i in range(2):
                            op("dve", lambda e, i=i: e.memset(vT[i][:, 0:1], 0.0), writes=[R_vT[i]])
                            op("dve", lambda e, i=i: e.memset(vT[i][:, SEQ + 1:SEQ + 2], 0.0), writes=[R_vT[i]])
                        cnt2 = {'gi': 0, 'pi': 0}
                        def a2_mm(c4):
                            b = c4 % 2
                            for s3 in range(3):
                                c0 = 768 + s3 * 512 + c4 * 128
                                K.pdma(P_wc[b][s3], wc[b][:, :, s3 * 128:(s3 + 1) * 128], winv[:, :, c0:c0 + 128],
                                       writes=[R_wc[b]])
                            for tb_ in range(4):
                                tok = slice(256 + tb_ * 512, 256 + (tb_ + 1) * 512)
                                osl = slice(tb_ * 512, (tb_ + 1) * 512)
                                g_ = cnt2['gi'] % 2
                                cnt2['gi'] += 1
                                for s3 in range(3):
                                    pb = 5 + cnt2['pi'] % 3
                                    cnt2['pi'] += 1
                                    for k in range(8):
                                        op("pe", lambda e, k=k, s3=s3, pb=pb: e.matmul(
                                            PS[pb], wc[b][:, k, s3 * 128:(s3 + 1) * 128], hT[:, k, tok],
                                            start=(k == 0), stop=(k == 7)),
                                           reads=[R_wc[b]], writes=[PSR[pb]])
                                    if s3 == 0:
                                        op("act", lambda e, pb=pb: e.activation(gbT[b][:, osl], PS[pb], AF.Copy),
                                           reads=[PSR[pb]], writes=[R_gbT[b]])
                                    elif s3 == 1:
                                        op("act", lambda e, pb=pb: e.activation(gct[g_][:], PS[pb], AF.Copy),
                                           reads=[PSR[pb]], writes=[R_gct[g_]])
                                    else:
                                        op("dve", lambda e, pb=pb: e.tensor_tensor(
                                            vT[b][:, 1 + tb_ * 512:1 + (tb_ + 1) * 512], PS[pb], gct[g_][:], ALU.mult),
                                           reads=[PSR[pb], R_gct[g_]], writes=[R_vT[b]])

                        def a2_fin(c4):
                            b = c4 % 2
                            for hf in range(2):
                                o = hf * 1024
                                op("dve", lambda e: e.tensor_scalar(yb[:], vT[b][:, o:o + 1024],
                                                                    cw[:, c4 * 3:c4 * 3 + 1], None, ALU.mult),
                                   reads=[R_vT[b], R_const], writes=[R_y])
                                op("dve", lambda e: e.scalar_tensor_tensor(
                                    yb[:], vT[b][:, o + 1:o + 1025], cw[:, c4 * 3 + 1:c4 * 3 + 2], yb[:],
                                    ALU.mult, ALU.add), reads=[R_vT[b], R_const, R_y], writes=[R_y])
                                op("dve", lambda e: e.scalar_tensor_tensor(
                                    yb[:], vT[b][:, o + 2:o + 1026], cw[:, c4 * 3 + 2:c4 * 3 + 3], yb[:],
                                    ALU.mult, ALU.add), reads=[R_vT[b], R_const, R_y], writes=[R_y])
                                op("dve", lambda e: e.tensor_tensor(yb[:], yb[:], gbT[b][:, o:o + 1024], ALU.mult),
                                   reads=[R_y, R_gbT[b]], writes=[R_y])
                                op("act", lambda e: e.activation(ysq[:], yb[:], AF.Square),
                                   reads=[R_y], writes=[R_ysq])
                                for q2 in range(2):
                                    op("pe", lambda e, q2=q2: e.matmul(PS[q2], bd64[:], ysq[:, q2 * 512:(q2 + 1) * 512],
                                                                     start=True, stop=True),
                                       reads=[R_ysq, R_const], writes=[PSR[q2]])
                                    op("act", lambda e, q2=q2: e.activation(rs[:, q2 * 512:(q2 + 1) * 512], PS[q2],
                                                                            AF.Ln, bias=EPS),
                                       reads=[PSR[q2]], writes=[R_rs])
                                op("act", lambda e: e.activation(rs[:], rs[:], AF.Exp, scale=-0.5),
                                   reads=[R_rs], writes=[R_rs])
                                op("dve", lambda e: e.tensor_tensor(convn[:, c4, o:o + 1024], yb[:], rs[:], ALU.mult),
                                   reads=[R_y, R_rs], writes=[R_convn[c4]])

                        a2_mm(0)
                        for c4 in range(4):
                            if c4 + 1 < 4:
                                a2_mm(c4 + 1)
                            a2_fin(c4)
                        K.barrier()
                if debug:
                    dma("sp", d_QT, QT[:], reads=R_QT)
                    dma("sp", d_KT[0:64], KT[0:64, :, 0, :], reads=R_KT)
                    dma("sp", d_KT[64:128], KT[64:128, :, 1, :], reads=R_KT)
                    dma("sp", d_V, VE[:, :, :, 0:65], reads=R_VE)
                    dma("sp", d_convn, convn[:], reads=R_convn)
                if stop_after == "A2":
                    K.barrier()
                    return nc, list(dbg.keys())

                with ExitStack() as sB2:
                    attnT = sb(sB2, "attnT", [128, 4, SEQ], BF16)
                    R_attn = K.regs_n(8, "attn")
                    woall = sb(sB2, "woall", [128, 8, D], BF16)
                    gall = sb(sB2, "gall", [128, 8], F32)
                    R_wst = K.reg("wst")
                    R_wo = K.reg("wo")
                    with ExitStack() as sBa:
                        PT = [sb(sBa, f"PT{i}", [128, 1024], BF16) for i in range(3)]
                        sqb = [sb(sBa, f"sqb{i}", [65, 512], F32) for i in range(2)]
                        lnb = [sb(sBa, f"lnb{i}", [64, 512], F32) for i in range(2)]
                        R_PT = K.regs_n(3, "PT")
                        R_sqb = K.regs_n(2, "sqb")
                        R_lnb = K.regs_n(2, "lnb")
                        wst = sb(sBa, "wst", [128, 4, D], F32)
                        dma("sp", gall[:], gall_d, writes=[R_wo])
                        wov = w_out_d.rearrange("(c p) n -> p c n", p=128)
                        for half in range(2):
                            dma("sp", wst[:], wov[:, half * 4:(half + 1) * 4, :], writes=[R_wst])
                            op("dve", lambda e: e.tensor_tensor(
                                woall[:, half * 4:(half + 1) * 4, :], wst[:],
                                gall[:, half * 4:(half + 1) * 4].unsqueeze(2).to_broadcast([128, 4, D]), ALU.mult),
                               reads=[R_wst, R_wo], writes=[R_wo])
                        zt = sb(sBa, "zt", [128, 8, D], BF16)
                        R_zt = K.reg("zt")
                        op("dve", lambda e: e.memset(zt[:], 0.0), writes=[R_zt])
                        for i in range(NE * CAP // 1024):
                            dma("sp", xbuf_d[i * 1024:(i + 1) * 1024, :].rearrange("(p j) d -> p j d", j=8), zt[:],
                                reads=[R_zt])
                        dma("sp", xbuf_d[NE * CAP:NE * CAP + 1, :], zt[0:1, 0, :], reads=[R_zt])
                        it = 0
                        pti = 0
                        spi = 0
                        for j in range(4):
                            g = j // 2
                            for qb in range(4):
                                qs = slice(qb * 512, (qb + 1) * 512)
                                obase = 4 + 2 * (it % 2)
                                it += 1
                                Oab = [PS[obase], PS[obase + 1]]

                                def s_step(kt, sp):
                                    for u in range(2):
                                        p0 = 64 * u
                                        bk = 2 * sp + u
                                        op("pe", lambda e: e.matmul(
                                            PS[bk], KT[:, g, u, kt * 128:(kt + 1) * 128],
                                            QT[:, j, qs], start=True, stop=True),
                                           writes=[PSR[2 * sp], PSR[2 * sp + 1]] if u == 0 else [PSR[bk]])
                                sp_of = {0: spi % 2}
                                spi += 1
                                s_step(0, sp_of[0])
                                for kt in range(NT):
                                    if kt + 1 < NT:
                                        sp_of[kt + 1] = spi % 2
                                        spi += 1
                                        s_step(kt + 1, sp_of[kt + 1])
                                    sp = sp_of[kt]
                                    pb_ = pti % 3
                                    pti += 1
                                    op("act", lambda e: e.activation(
                                        PT[pb_][:], PSUM[:, 2 * sp * 512:(2 * sp + 2) * 512], AF.Exp, scale=0.125),
                                       reads=[PSR[2 * sp], PSR[2 * sp + 1]], writes=[R_PT[pb_]])
                                    for u in range(2):
                                        op("pe", lambda e: e.matmul(Oab[u][:, :], VE[:, kt, g, :],
                                                                    PT[pb_][:, u * 512:(u + 1) * 512],
                                                                    start=(kt == 0), stop=(kt == NT - 1)),
                                           reads=[R_PT[pb_]], writes=[PSR[obase + u]])
                                sp = spi % 2
                                spi += 1
                                for u in range(2):
                                    op("act", lambda e: e.activation(sqb[u][:], Oab[u][0:65, :], AF.Square),
                                       reads=[PSR[obase + u]], writes=[R_sqb[u]])
                                    op("pe", lambda e: e.matmul(PS[2 * sp + u][0:64, :], c65[:, :], sqb[u][:],
                                                                start=True, stop=True),
                                       reads=[R_sqb[u], R_const], writes=[PSR[2 * sp + u]])
                                for u in range(2):
                                    op("act", lambda e: e.activation(lnb[u][:], PS[2 * sp + u][0:64, :], AF.Ln),
                                       reads=[PSR[2 * sp + u]], writes=[R_lnb[u]])
                                    op("act", lambda e: e.activation(lnb[u][:], lnb[u][:], AF.Exp, scale=-0.5),
                                       reads=[R_lnb[u]], writes=[R_lnb[u]])
                                    op("dve", lambda e: e.tensor_tensor(attnT[64 * u:64 * u + 64, j, qs], Oab[u][0:64, :],
                                                                        lnb[u][:], ALU.mult),
                                       reads=[PSR[obase + u], R_lnb[u]], writes=[R_attn[2 * j + u]])
                        K.barrier()
                    if debug:
                        for u in range(2):
                            dma("sp", d_attn.rearrange("d (j u) s -> d u j s", u=2)[:, u], attnT[64 * u:64 * u + 64, :, :],
                                reads=R_attn)
                    if stop_after == "B":
                        K.barrier()
                        return nc, list(dbg.keys())

                    if True:
                        with ExitStack() as sB3:
                            xr = [sb(sB3, "xr0", [128, D], F32)] * 2
                            xnw = [sb(sB3, "xnw0", [128, D], F32)] * 2
                            hx = [sb(sB3, f"hx{i}", [128, D], F32) for i in range(2)]
                            hxT = [sb(sB3, "hxT0", [128, 8, 128], F32)] * 2
                            st2 = sb(sB3, "st2", [128, 3, NL], F32)
                            lg = sb(sB3, "lg", [128, NL, 36], F32)
                            rt = sb(sB3, "rt", [128, 64], F32)
                            RB = sb(sB3, "RB", [128, 16, NL, 8], F32)
                            m8 = sb(sB3, "m8", [128, 8], F32)
                            ustr = sb(sB3, "ustr", [128, 128], BF16)
                            onesb = sb(sB3, "onesb", [128, 128], BF16)
                            ustf = sb(sB3, "ustf", [128, 128], F32)
                            eC = sb(sB3, "eC", [128, NE], F32)
                            maskb = sb(sB3, "maskb", [128, NL, NE], BF16)
                            mkk = sb(sB3, "mkk", [128, 2, NL, NE], F32)
                            rkk = sb(sB3, "rkk", [128, 2, NL, NE], F32)
                            t3 = sb(sB3, "t3", [128, NL, NE], F32)
                            dfa = sb(sB3, "dfa", [128, 3, 2, NL], F32)
                            hxb = [sb(sB3, f"hxb{i}", [128, D], BF16) for i in range(2)]
                            R_mask = K.regs_n(NL, "mask")
                            R_rk = K.reg("rk")
                            R_hxb = K.regs_n(2, "hxb")
                            hxs = [sb(sB3, f"hxs{i}", [128, D], BF16) for i in range(4)]
                            R_hxs = K.regs_n(4, "hxs")
                            R_xbuf = K.reg("xbuf")
                            dma("sp", ustf[:], ustr_d, writes=[R_wo])
                            dma("sp", eC[:], eC_d, writes=[R_wo])
                            op("dve", lambda e: e.tensor_copy(ustr[:], ustf[:]), reads=[R_wo], writes=[R_wo])
                            op("dve", lambda e: e.memset(onesb[:], 1.0), writes=[R_wo])
                            R_xr = [K.reg("xr")] * 2
                            R_xnw = [K.reg("xnw")] * 2
                            R_hx = K.regs_n(2, "hx")
                            R_hxT = [K.reg("hxT")] * 2
                            R_st2 = K.regs_n(NL, "st2")
                            R_lg = K.reg("lg")
                            R_rt = K.reg("rt")
                            def b2_front(t):
                                b = t % 2
                                tsl = slice(t * 128, (t + 1) * 128)
                                dma("sp", xr[b][:], xin[256 + t * 128:256 + (t + 1) * 128, :], writes=[R_xr[b]])
                                for hf in range(2):
                                    pb = hf + 6 * (t % 2)
                                    cs = slice(hf * 512, (hf + 1) * 512)
                                    for c in range(4):
                                        op("pe", lambda e, c=c: e.matmul(PS[pb], attnT[:, c, tsl], woall[:, c, cs],
                                                                       start=(c == 0), stop=False),
                                           reads=[R_wo], writes=[PSR[pb]])
                                    for c4 in range(4):
                                        op("pe", lambda e, c4=c4: e.matmul(PS[pb], convn[:, c4, tsl], woall[:, 4 + c4, cs],
                                                                         start=False, stop=(c4 == 3)),
                                           reads=[R_wo], writes=[PSR[pb]])
                                    op("dve", lambda e: e.tensor_tensor(xnw[b][:, cs], PS[pb], modb[:, 2 * D + hf * 512:
                                                                                                  2 * D + (hf + 1) * 512],
                                                                        ALU.mult),
                                       reads=[PSR[pb]], writes=[R_xnw[b]])
                                op("dve", lambda e: e.tensor_tensor(xnw[b][:], xnw[b][:], xr[b][:], ALU.add),
                                   reads=[R_xnw[b], R_xr[b]], writes=[R_xnw[b]])
                                dma("sp", xnew_d[tsl, :], xnw[b][:], reads=[R_xnw[b]])
                                op("act", lambda e: e.activation(hxb[b][:], xnw[b][:], AF.Square,
                                                                 accum_out=st2[:, 0, t:t + 1]),
                                   reads=[R_xnw[b]], writes=[R_st2[t], R_hxb[b]])
                                op("act", lambda e: e.activation(st2[:, 1, t:t + 1], st2[:, 0, t:t + 1], AF.Ln,
                                                                 scale=1.0 / D, bias=EPS),
                                   reads=[R_st2[t]], writes=[R_st2[t]])
                                op("act", lambda e: e.activation(st2[:, 2, t:t + 1], st2[:, 1, t:t + 1], AF.Exp,
                                                                 scale=-0.5),
                                   reads=[R_st2[t]], writes=[R_st2[t]])
                                op("dve", lambda e: e.scalar_tensor_tensor(
                                    hx[b][:], xnw[b][:], st2[:, 2, t:t + 1], modb[:, 4 * D:5 * D], ALU.mult, ALU.mult),
                                   reads=[R_xnw[b], R_st2[t]], writes=[R_hx[b]])
                                op("dve", lambda e: e.tensor_tensor(hx[b][:], hx[b][:], modb[:, 3 * D:4 * D], ALU.add),
                                   reads=[R_hx[b]], writes=[R_hx[b]])

                            def b2_back(t):
                                b = t % 2
                                tsl = slice(t * 128, (t + 1) * 128)
                                PP = PSUM[:, 2 * 512:4 * 512]
                                for k in range(8):
                                    op("pe", lambda e, k=k: e.transpose(PP[:, k * 128:(k + 1) * 128],
                                                                        hx[b][:, k * 128:(k + 1) * 128], identf[:]),
                                       reads=[R_hx[b], R_const], writes=[PSR[2], PSR[3]])
                                op("act", lambda e: e.activation(
                                    hxT[b][:], PP.rearrange("p (k n) -> p k n", k=8), AF.Copy),
                                   reads=[PSR[2], PSR[3]], writes=[R_hxT[b]])
                                op("act", lambda e: e.activation(hxb[b][:], hx[b][:], AF.Copy),
                                   reads=[R_hx[b]], writes=[R_hxb[b]])
                                for k in range(8):
                                    op("pe", lambda e, k=k: e.matmul(PS[4][:, 0:36], hxT[b][:, k, :], wr[:, k, :],
                                                                   start=(k == 0), stop=(k == 7)),
                                       reads=[R_hxT[b], R_const], writes=[PSR[4]])
                                op("act", lambda e: e.activation(lg[:, t, :], PS[4][:, 0:36], AF.Copy),
                                   reads=[PSR[4]], writes=[R_lg])
                                dma("sp", hx2_d[tsl, :], hxb[b][:], reads=[R_hxb[b]], writes=[R_xbuf])

                            b2_front(0)
                            for t in range(NL):
                                if t + 1 < NL:
                                    b2_front(t + 1)
                                b2_back(t)
                            def slab(i, w=8):
                                return RB[:, i, :, 0:w]

                            def col(i):
                                return RB[:, i, :, 0]

                            def bc(a2, w):
                                return a2.unsqueeze(2).to_broadcast([128, NL, w])
                            R_B = K.reg("RB")

                            def dv(fn, rd=(), wr_=()):
                                op("dve", fn, reads=[R_lg, R_B] + list(rd), writes=[R_B] + list(wr_))

                            def ac(fn):
                                op("act", fn, reads=[R_lg, R_B], writes=[R_B])
                            G4 = lg[:, :, 0:4]
                            dv(lambda e: e.tensor_reduce(col(0), G4, AX.X, ALU.max))
                            dv(lambda e: e.tensor_tensor(slab(1, 4), G4, bc(col(0), 4), ALU.is_equal))
                            dv(lambda e: e.tensor_tensor(slab(2, 4), G4, bc(col(0), 4), ALU.subtract))
                            ac(lambda e: e.activation(slab(2, 4), slab(2, 4), AF.Exp))
                            dv(lambda e: e.tensor_reduce(col(3), slab(2, 4), AX.X, ALU.add))
                            dv(lambda e: e.reciprocal(col(4), col(3)))
                            dv(lambda e: e.tensor_tensor(slab(5), lg[:, :, 4:12], bc(RB[:, 1, :, 0], 8), ALU.mult))
                            for gq in range(1, 4):
                                dv(lambda e, gq=gq: e.tensor_tensor(slab(6), lg[:, :, 4 + 8 * gq:12 + 8 * gq],
                                                                    bc(RB[:, 1, :, gq], 8), ALU.mult))
                                dv(lambda e: e.tensor_tensor(slab(5), slab(5), slab(6), ALU.add))
                            dv(lambda e: e.tensor_reduce(col(7), slab(5), AX.X, ALU.max))
                            dv(lambda e: e.tensor_tensor(slab(8), slab(5), bc(col(7), 8), ALU.is_equal))
                            dv(lambda e: e.scalar_tensor_tensor(slab(9), slab(8), -1.0e30, slab(5), ALU.mult, ALU.add))
                            dv(lambda e: e.tensor_reduce(col(10), slab(9), AX.X, ALU.max))
                            dv(lambda e: e.tensor_tensor(slab(11), slab(5), bc(col(10), 8), ALU.is_ge))
                            dv(lambda e: e.tensor_tensor(slab(12), slab(11), slab(8), ALU.subtract))
                            dv(lambda e: e.tensor_tensor(slab(13), slab(5), bc(col(7), 8), ALU.subtract))
                            ac(lambda e: e.activation(slab(13), slab(13), AF.Exp))
                            dv(lambda e: e.tensor_tensor(slab(13), slab(13), slab(11), ALU.mult))
                            dv(lambda e: e.tensor_reduce(col(14), slab(13), AX.X, ALU.add))
                            dv(lambda e: e.reciprocal(col(15), col(14)))
                            dv(lambda e: e.tensor_tensor(col(15), col(15), col(4), ALU.mult))
                            dv(lambda e: e.tensor_tensor(slab(13), slab(13), bc(col(15), 8), ALU.mult))
                            for gq in range(4):
                                ohg = bc(RB[:, 1, :, gq], 8)
                                dv(lambda e, gq=gq, ohg=ohg: e.tensor_tensor(wgt[:, :, gq * 8:(gq + 1) * 8], slab(13), ohg,
                                                                             ALU.mult), wr_=R_wgt)
                                dv(lambda e, gq=gq, ohg=ohg: e.tensor_tensor(mkk[:, 0, :, gq * 8:(gq + 1) * 8], slab(8), ohg,
                                                                             ALU.mult))
                                dv(lambda e, gq=gq, ohg=ohg: e.tensor_tensor(mkk[:, 1, :, gq * 8:(gq + 1) * 8], slab(12), ohg,
                                                                             ALU.mult))
                            dv(lambda e: e.tensor_tensor(maskb[:], mkk[:, 0], mkk[:, 1], ALU.add), wr_=[R_mask[0]])
                            for t in range(NL):
                                op("pe", lambda e, t=t: e.matmul(PS[5][:, t * NE:(t + 1) * NE], ustr[:], maskb[:, t, :],
                                                               start=True, stop=(t == 0)),
                                   reads=[R_mask[0], R_wo], writes=[PSR[5]])
                                for t2 in range(t):
                                    op("pe", lambda e, t=t, t2=t2: e.matmul(PS[5][:, t * NE:(t + 1) * NE], onesb[:],
                                                                           maskb[:, t2, :], start=False, stop=(t2 == t - 1)),
                                       reads=[R_mask[0], R_wo], writes=[PSR[5]])
                            op("act", lambda e: e.activation(rkk[:, 0].rearrange("p t e -> p (t e)"), PS[5], AF.Copy),
                               reads=[PSR[5]], writes=[R_rk])
                            op("dve", lambda e: e.tensor_tensor(rkk[:, 1], rkk[:, 0],
                                                                eC[:, :].unsqueeze(1).to_broadcast([128, NL, NE]), ALU.add),
                               reads=[R_rk, R_wo], writes=[R_rk])
                            for kk in range(2):
                                for src_i, di in ((1, 0), (0, 1)):
                                    op("dve", lambda e, kk=kk, src_i=src_i: e.tensor_tensor(
                                        t3[:], mkk[:, kk], rkk[:, src_i], ALU.mult), reads=[R_rk, R_B], writes=[R_B])
                                    op("dve", lambda e, kk=kk, di=di: e.tensor_reduce(
                                        dfa[:, di, kk, :], t3[:], AX.X, ALU.add), reads=[R_B], writes=[R_B])
                                op("dve", lambda e, kk=kk: e.tensor_tensor(t3[:], mkk[:, kk], wgt[:], ALU.mult),
                                   reads=[R_B] + R_wgt, writes=[R_B])
                                op("dve", lambda e, kk=kk: e.tensor_reduce(wsel[:, :, kk], t3[:], AX.X, ALU.add),
                                   reads=[R_B], writes=R_dest)
                            dv(lambda e: e.tensor_scalar(dfa[:, 2], dfa[:, 1], float(CAP), None, ALU.is_ge))
                            dv(lambda e: e.scalar_tensor_tensor(dfa[:, 0], dfa[:, 2], 1.0e6, dfa[:, 0], ALU.mult, ALU.add))
                            dv(lambda e: e.tensor_scalar(dfa[:, 0], dfa[:, 0], float(NE * CAP), None, ALU.min))
                            dv(lambda e: e.tensor_copy(dest[:].rearrange("p t k -> p k t"), dfa[:, 0]), wr_=R_dest)
                            for t in range(NL):
                                b = t % 4
                                dma("sp", hxs[b][:], hx2_d[t * 128:(t + 1) * 128, :], reads=[R_xbuf], writes=[R_hxs[b]])
                                for kk in range(2):
                                    K.pdma(None, None, None, reads=[R_hxs[b]] + R_dest, writes=[],
                                           fn=lambda g_, kk=kk, t=t: g_.indirect_dma_start(
                                               out=xbuf_d[:, :],
                                               out_offset=bass.IndirectOffsetOnAxis(ap=dest[:, t, kk:kk + 1], axis=0),
                                               in_=hxs[b][:, :], in_offset=None))
                            K.barrier()
                        if debug:
                            dma("sp", d_wgt, wgt[:], reads=R_wgt)
                        if stop_after == "B2":
                            K.barrier()
                            return nc, list(dbg.keys())

        with ExitStack() as sM:
            NWB = 3
            wgs = [sb(sM, f"wgs{i}", [128, 8, DE], BF16) for i in range(NWB)]
            wus = [sb(sM, f"wus{i}", [128, 8, DE], BF16) for i in range(NWB)]
            wds = [sb(sM, f"wds{i}", [128, 6, D], BF16) for i in range(NWB)]
            Xs = [sb(sM, f"Xs{i}", [128, NJ, D], BF16) for i in range(2)]
            XT = [sb(sM, f"XT{i}", [128, 8, CAP], BF16) for i in range(2)]
            HT = [sb(sM, f"HT{i}", [128, 6, CAP], BF16) for i in range(2)]
            sg = [sb(sM, f"sg{i}", [128, CAP], F32) for i in range(2)]
            Yst = [sb(sM, f"Yst{i}", [128, D], F32) for i in range(2)]
            R_wg = K.regs_n(NWB, "wg")
            R_wu = K.regs_n(NWB, "wu")
            R_wd = K.regs_n(NWB, "wd")
            R_Xs = K.regs_n(2, "Xs")
            R_XT = K.regs_n(2, "XT")
            R_HT = K.regs_n(2, "HT")
            R_sg = K.regs_n(2, "sg")
            R_Yst = K.regs_n(2, "Yst")
            cnt = {"si": 0, "yi": 0, "ti": 0}
            op("dve", lambda e: e.memset(Yst[0][0:1, :], 0.0), writes=[R_Yst[0]])
            dma("sp", ybuf_d[NE * CAP:NE * CAP + 1, :], Yst[0][0:1, :], reads=[R_Yst[0]])

            def load_w(ex):
                w3 = ex % NWB
                K.pdma(None, wgs[w3][:], wg_d[ex].rearrange("(k p) f -> p k f", p=128), writes=[R_wg[w3]])
                K.pdma(None, wus[w3][:], wu_d[ex].rearrange("(k p) f -> p k f", p=128), writes=[R_wu[w3]])
                K.pdma(None, wds[w3][:], wd_d[ex].rearrange("(c p) n -> p c n", p=128), writes=[R_wd[w3]])

            def load_x(ex):
                xb_ = ex % 2
                dma("sp", Xs[xb_][:], xbuf_d[ex * CAP:(ex + 1) * CAP, :].rearrange("(j p) d -> p j d", p=128),
                    writes=[R_Xs[xb_]])

            def transposes(ex):
                xb_ = ex % 2
                for j in range(NJ):
                    tbk = 6 + cnt["ti"] % 2
                    cnt["ti"] += 1
                    psb = PS[tbk].bitcast(BF16)
                    for k in range(8):
                        op("pe", lambda e, k=k: e.transpose(psb[:, k * 128:(k + 1) * 128],
                                                            Xs[xb_][:, j, k * 128:(k + 1) * 128], identb[:]),
                           reads=[R_Xs[xb_], R_const], writes=[PSR[tbk]])
                    if tbk == 6:
                        op("dve", lambda e: e.tensor_copy(XT[xb_][:, :, j * 128:(j + 1) * 128],
                                                          psb.rearrange("p (k n) -> p k n", k=8)),
                           reads=[PSR[tbk]], writes=[R_XT[xb_]])
                    else:
                        op("act", lambda e: e.activation(XT[xb_][:, :, j * 128:(j + 1) * 128],
                                                         psb.rearrange("p (k n) -> p k n", k=8), AF.Copy),
                           reads=[PSR[tbk]], writes=[R_XT[xb_]])

            load_w(0)
            load_w(1)
            load_x(0)
            transposes(0)
            for ex in range(NE):
                wb = ex % 2
                w3 = ex % NWB
                if ex + 2 < NE:
                    load_w(ex + 2)
                if ex + 1 < NE:
                    load_x(ex + 1)
                for fc in range(6):
                    fs = slice(fc * 128, (fc + 1) * 128)
                    gbk = fc % 2
                    ubk = 2 + fc % 2
                    for k in range(8):
                        op("pe", lambda e, k=k: e.matmul(PS[gbk][:, 0:CAP], wgs[w3][:, k, fs], XT[wb][:, k, :],
                                                       start=(k == 0), stop=(k == 7)),
                           reads=[R_wg[w3], R_XT[wb]], writes=[PSR[gbk]])
                    for k in range(8):
                        op("pe", lambda e, k=k: e.matmul(PS[ubk][:, 0:CAP], wus[w3][:, k, fs], XT[wb][:, k, :],
                                                       start=(k == 0), stop=(k == 7)),
                           reads=[R_wu[w3], R_XT[wb]], writes=[PSR[ubk]])
                    sb2 = cnt["si"] % 2
                    cnt["si"] += 1
                    op("act", lambda e: e.activation(sg[sb2][:], PS[gbk][:, 0:CAP], AF.Silu),
                       reads=[PSR[gbk]], writes=[R_sg[sb2]])
                    op("dve", lambda e: e.tensor_tensor(HT[wb][:, fc, :], PS[ubk][:, 0:CAP], sg[sb2][:], ALU.mult),
                       reads=[PSR[ubk], R_sg[sb2]], writes=[R_HT[wb]])
                if ex + 1 < NE:
                    transposes(ex + 1)
                for j in range(NJ):
                    ys = cnt["yi"] % 2
                    cnt["yi"] += 1
                    for hf in range(2):
                        yb_ = 4 + hf
                        cs = slice(hf * 512, (hf + 1) * 512)
                        for fc in range(6):
                            op("pe", lambda e, fc=fc: e.matmul(
                                PS[yb_], HT[wb][:, fc, j * 128:(j + 1) * 128], wds[w3][:, fc, cs],
                                start=(fc == 0), stop=(fc == 5)),
                               reads=[R_HT[wb], R_wd[w3]], writes=[PSR[yb_]])
                        op("dve", lambda e: e.tensor_copy(Yst[ys][:, cs], PS[yb_]),
                           reads=[PSR[yb_]], writes=[R_Yst[ys]])
                    dma("sp", ybuf_d[ex * CAP + j * 128:ex * CAP + (j + 1) * 128, :], Yst[ys][:],
                        reads=[R_Yst[ys]])
            K.barrier()
        with ExitStack() as sF:
            y12 = [[sb(sF, f"y{k}_{i}", [128, D], F32) for k in range(2)] for i in range(4)]
            xr2 = [sb(sF, f"xq{i}", [128, D], F32) for i in range(4)]
            ot = [sb(sF, f"ot{i}", [128, D], F32) for i in range(4)]
            junk3 = sb(sF, "junk3", [128, D], BF16)
            st3 = sb(sF, "st3", [128, 3, NL], F32)
            R_y12 = [K.regs_n(2, f"y12_{i}") for i in range(4)]
            R_xr2 = K.regs_n(4, "xr2")
            R_ot = K.regs_n(4, "ot")
            R_st3 = K.regs_n(NL, "st3")
            def fin_load(tg):
                b = tg % 4
                tsl = slice(tg * 128, (tg + 1) * 128)
                dma("sp", xr2[b][:], xnew_d[tsl, :], writes=[R_xr2[b]])
                for kk in range(2):
                    K.pdma(None, None, None, reads=[], writes=[R_y12[tg % 4][kk]],
                           fn=lambda g, kk=kk: g.indirect_dma_start(
                               out=y12[tg % 4][kk][:, :], out_offset=None, in_=ybuf_d[:, :],
                               in_offset=bass.IndirectOffsetOnAxis(ap=dest[:, tg, kk:kk + 1], axis=0)))

            def fin_front(tg):
                b = tg % 4
                op("dve", lambda e: e.tensor_scalar(ot[b][:], y12[tg % 4][0][:], wsel[:, tg, 0:1], None, ALU.mult),
                   reads=[R_y12[tg % 4][0]], writes=[R_ot[b]])
                op("dve", lambda e: e.scalar_tensor_tensor(ot[b][:], y12[tg % 4][1][:], wsel[:, tg, 1:2], ot[b][:],
                                                          ALU.mult, ALU.add),
                   reads=[R_y12[tg % 4][1], R_ot[b]], writes=[R_ot[b]])
                if debug:
                    dma("sp", d_acc[:, tg, :], ot[b][:], reads=[R_ot[b]])
                op("dve", lambda e: e.tensor_tensor(ot[b][:], ot[b][:], gt2b[:], ALU.mult),
                   reads=[R_ot[b], R_gt2], writes=[R_ot[b]])
                op("dve", lambda e: e.tensor_tensor(ot[b][:], ot[b][:], xr2[b][:], ALU.add),
                   reads=[R_ot[b], R_xr2[b]], writes=[R_ot[b]])

            def fin_back(tg):
                b = tg % 4
                tsl = slice(tg * 128, (tg + 1) * 128)
                op("act", lambda e: e.activation(junk3[:], ot[b][:], AF.Square, accum_out=st3[:, 0, tg:tg + 1]),
                   reads=[R_ot[b]], writes=[R_st3[tg]])
                op("act", lambda e: e.activation(st3[:, 1, tg:tg + 1], st3[:, 0, tg:tg + 1], AF.Ln,
                                                 scale=1.0 / D, bias=EPS),
                   reads=[R_st3[tg]], writes=[R_st3[tg]])
                op("act", lambda e: e.activation(st3[:, 2, tg:tg + 1], st3[:, 1, tg:tg + 1], AF.Exp, scale=-0.5),
                   reads=[R_st3[tg]], writes=[R_st3[tg]])
                op("dve", lambda e: e.scalar_tensor_tensor(
                    ot[b][:], ot[b][:], st3[:, 2, tg:tg + 1], fgb[:], ALU.mult, ALU.mult),
                   reads=[R_ot[b], R_st3[tg], R_const], writes=[R_ot[b]])
                dma("sp", y_d[tsl, :], ot[b][:], reads=[R_ot[b]])

            fin_load(0)
            fin_load(1)
            fin_front(0)
            for tg in range(NL):
                if tg + 2 < NL:
                    fin_load(tg + 2)
                if tg + 1 < NL:
                    fin_front(tg + 1)
                fin_back(tg)
            K.barrier()
    return nc, list(dbg.keys())


def _host_consts():
    ident = np.eye(128, dtype=np.float32)
    rows = SEQ // 64
    row_idx = np.repeat(np.arange(rows, dtype=np.float32), 64)
    col_idx = np.tile(np.arange(64, dtype=np.float32), rows)
    inv_freq = (np.float32(10000.0) ** (-np.arange(0, 32, 2, dtype=np.float32) / np.float32(32))).astype(np.float32)
    ang = np.stack([row_idx[:, None] * inv_freq, col_idx[:, None] * inv_freq], axis=1).astype(np.float32)
    cos = np.cos(ang).astype(np.float32).reshape(SEQ, 32)
    sin = np.sin(ang).astype(np.float32).reshape(SEQ, 32)
    cosT = np.ascontiguousarray(cos.reshape(NL, 128, 32).transpose(1, 0, 2))
    sinT = np.ascontiguousarray(sin.reshape(NL, 128, 32).transpose(1, 0, 2))
    bd64 = np.zeros((128, 128), np.float32)
    bd64[:64, :64] = 1.0 / 64
    bd64[64:, 64:] = 1.0 / 64
    c65 = np.full((65, 64), 1.0 / 64, np.float32)
    c65[64, :] = EPS
    e0 = np.zeros((128, 1), np.float32)
    e0[0, 0] = 1.0
    ustr = np.triu(np.ones((128, 128), np.float32), k=1)
    eC = np.ascontiguousarray(np.broadcast_to((np.arange(NE, dtype=np.float32) * CAP)[None, :], (128, NE)))
    return dict(ident_f=ident, cosT=cosT, sinT=sinT, bd64=bd64, c65=c65, e0=e0, ustr=ustr, eC=eC)


def make_in_maps(inputs, cores=range(8)):
    f = lambda a: np.ascontiguousarray(np.asarray(a, dtype=np.float32))
    x = f(inputs["x"]); c = f(inputs["c"]); ctx = f(inputs["ctx"]); c_ctx = f(inputs["c_ctx"])
    consts = _host_consts()
    bc = lambda v: np.ascontiguousarray(np.broadcast_to(f(v).reshape(1, -1), (128, f(v).size)))
    shared = dict(
        w_mod=f(inputs["w_mod"])[0], bm_b=bc(inputs["b_mod"][0]), g1_b=bc(inputs["norm1_g"][0]),
        g2_b=bc(inputs["norm2_g"][0]), fg_b=bc(inputs["final_g"]), w_in=f(inputs["w_in"])[0],
        qg_b=bc(inputs["q_norm_g"][0]), kg_b=bc(inputs["k_norm_g"][0]),
        cwT=np.ascontiguousarray(f(inputs["conv_w"])[0].reshape(3, 4, 128).transpose(2, 1, 0).reshape(128, 12)),
        gallT=np.ascontiguousarray(np.concatenate([f(inputs["attn_out_g"])[0], f(inputs["conv_out_g"])[0]])
                                   .reshape(8, 128).T),
        w_out=f(inputs["w_out"])[0],
        w_r=np.ascontiguousarray(np.concatenate([f(inputs["w_group"])[0], f(inputs["w_router"])[0]], axis=1)),
        w_gate=f(inputs["w_gate"])[0], w_up=f(inputs["w_up"])[0], w_down=f(inputs["w_down"])[0],
        **consts,
    )
    maps = []
    for b in cores:
        cT = np.concatenate([c[b].reshape(8, 128).T, c_ctx.reshape(8, 128).T], axis=1)
        m = dict(shared)
        m["xin"] = np.ascontiguousarray(np.concatenate([ctx[b], x[b]], axis=0))
        m["cT"] = np.ascontiguousarray(cT.astype(np.float32))
        maps.append(m)
    return maps


_CACHE = {}


def kernel(**inputs):
    if "nc" not in _CACHE:
        _CACHE["nc"] = build_program(debug=False)[0]
    nc = _CACHE["nc"]
    maps = make_in_maps(inputs)
    res = run_bass_kernel_spmd(nc, maps, core_ids=list(range(8)))
    out = np.stack([np.asarray(r["y"], dtype=np.float32) for r in res.results], axis=0)
    return out
```

```python
import os
import numpy as np
from contextlib import ExitStack
import concourse.bass as bass
import concourse.mybir as mybir
from concourse.bass_utils import run_bass_kernel_spmd

F32 = mybir.dt.float32
BF16 = mybir.dt.bfloat16
I32 = mybir.dt.int32
AF = mybir.ActivationFunctionType
ALU = mybir.AluOpType
AX = mybir.AxisListType

D = 1024
SEQ = 2048
CTX = 256
NTOK = SEQ + CTX
NT = NTOK // 128
NL = SEQ // 128
EPS = 1e-6
NE = 32
DE = 768
N_DSEM = 40
CAP = 512
NJ = CAP // 128
_LVL = int(os.environ.get('KLVL', '9'))
_KEV = os.environ.get('KEV', 'act')


class Reg:
    __slots__ = ("w", "r", "name")

    def __init__(self, name=""):
        self.w = None
        self.r = {}
        self.name = name


class Sem:
    def __init__(self, handle):
        self.handle = handle
        self.count = 0


class EngW:
    def __init__(self, name, eng, sem):
        self.name = name
        self.eng = eng
        self.sem = sem
        self.waited = {}


class KB:
    def __init__(self, nc, es):
        self.nc = nc
        self._es = es
        self.pslots = []
        self.regs = []
        mk = lambda n: Sem(es.enter_context(nc.semaphore(n)))
        self.E = {
            "pe": EngW("pe", nc.tensor, mk("s_pe")),
            "act": EngW("act", nc.scalar, mk("s_act")),
            "dve": EngW("dve", nc.vector, mk("s_dve")),
            "pool": EngW("pool", nc.gpsimd, mk("s_pool")),
            "sp": EngW("sp", nc.sync, mk("s_sp")),
        }
        self.dsems = [mk(f"s_d{i}") for i in range(N_DSEM)]
        self.dnext = 0
        self.psems = [mk(f"s_q{i}") for i in range(24)]
        self.pnext = 0

    def reg(self, name=""):
        r = Reg(name)
        self.regs.append(r)
        return r

    def regs_n(self, n, name=""):
        return [self.reg(f"{name}{i}") for i in range(n)]

    def wait(self, ew, tok):
        sem, val = tok
        if ew.waited.get(id(sem), 0) >= val:
            return
        ew.eng.wait_ge(sem.handle, val)
        ew.waited[id(sem)] = val

    def _deps(self, ew, reads, writes):
        for r in reads:
            if r.w is not None:
                if ew.name == "pe" and r.w[0] is ew.sem:
                    continue
                self.wait(ew, r.w)
        for w in writes:
            toks = list(w.r.values())
            if w.w is not None:
                toks.append(w.w)
            for t in toks:
                if ew.name == "pe" and t[0] is ew.sem:
                    continue
                self.wait(ew, t)

    def _record(self, tok, reads, writes):
        for r in reads:
            r.r[id(tok[0])] = tok
        for w in writes:
            w.w = tok
            w.r = {}

    def op(self, en, fn, reads=(), writes=()):
        ew = self.E[en]
        self._deps(ew, reads, writes)
        ins = fn(ew.eng)
        ew.sem.count += 1
        ins.then_inc(ew.sem.handle, 1)
        tok = (ew.sem, ew.sem.count)
        self._record(tok, reads, writes)
        return tok

    def dma(self, qn, out, in_, reads=(), writes=(), fn=None):
        ew = self.E[qn]
        self._deps(ew, reads, writes)
        d = self.dsems[self.dnext % N_DSEM]
        self.dnext += 1
        if d.count:
            self.wait(ew, (d, d.count))
        if fn is None:
            ins = ew.eng.dma_start(out=out, in_=in_)
        else:
            ins = fn(ew.eng)
        d.count += 16
        ins.then_inc(d.handle, 16)
        tok = (d, d.count)
        self._record(tok, reads, writes)
        return tok

    def pslot(self, name):
        return None

    def pdma(self, slot, out, in_, reads=(), writes=(), fn=None):
        ew = self.E["pool"]
        self._deps(ew, reads, writes)
        d = self.psems[self.pnext % len(self.psems)]
        self.pnext += 1
        if d.count:
            self.wait(ew, (d, d.count))
        ins = ew.eng.dma_start(out=out, in_=in_) if fn is None else fn(ew.eng)
        d.count += 16
        ins.then_inc(d.handle, 16)
        tok = (d, d.count)
        self._record(tok, reads, writes)
        return tok

    def barrier(self):
        for ew in self.E.values():
            for other in self.E.values():
                if other.sem.count and not (other is ew and ew.name in ("pe", "sp")):
                    self.wait(ew, (other.sem, other.sem.count))
            for d in self.dsems:
                if d.count:
                    self.wait(ew, (d, d.count))
            for d in self.psems:
                if d.count:
                    self.wait(ew, (d, d.count))
        for r in self.regs:
            r.w = None
            r.r = {}


def build_program(debug=False, stop_after=None):
    nc = bass.Bass("TRN2", target_bir_lowering=False)

    def din(name, shape, dt=F32):
        return nc.dram_tensor(name, list(shape), dt, kind="ExternalInput").ap()

    xin = din("xin", [NTOK, D])
    cT_d = din("cT", [128, 16])
    w_mod_d = din("w_mod", [D, 6 * D])
    bm_d = din("bm_b", [128, 6 * D])
    g1_d = din("g1_b", [128, D])
    g2_d = din("g2_b", [128, D])
    fg_d = din("fg_b", [128, D])
    w_in_d = din("w_in", [D, 2304])
    qg_d = din("qg_b", [128, 64])
    kg_d = din("kg_b", [128, 64])
    cw_d = din("cwT", [128, 12])
    gall_d = din("gallT", [128, 8])
    w_out_d = din("w_out", [D, D])
    wr_d = din("w_r", [D, 36])
    wg_d = din("w_gate", [NE, D, DE])
    wu_d = din("w_up", [NE, D, DE])
    wd_d = din("w_down", [NE, DE, D])
    identf_d = din("ident_f", [128, 128])
    cos_d = din("cosT", [128, NL, 32])
    sin_d = din("sinT", [128, NL, 32])
    bd64_d = din("bd64", [128, 128])
    c65_d = din("c65", [65, 64])
    e0_d = din("e0", [128, 1])
    ustr_d = din("ustr", [128, 128])
    eC_d = din("eC", [128, NE])

    y_d = nc.dram_tensor("y", [SEQ, D], F32, kind="ExternalOutput").ap()
    xnew_d = nc.dram_tensor("xnew_scr", [SEQ, D], F32,
                            kind="ExternalOutput" if debug else "Internal").ap()
    xbuf_d = nc.dram_tensor("xbuf_scr", [NE * CAP + 1, D], BF16, kind="Internal").ap()
    ybuf_d = nc.dram_tensor("ybuf_scr", [NE * CAP + 1, D], F32, kind="Internal").ap()
    hx2_d = nc.dram_tensor("hx2_scr", [SEQ, D], BF16, kind="Internal").ap()
    dbg = {}

    def dbg_out(name, shape, dt=F32):
        if debug:
            dbg[name] = nc.dram_tensor(name, list(shape), dt, kind="ExternalOutput").ap()
        return dbg.get(name)

    d_smod = dbg_out("d_smod", [128, 32])
    d_modb = dbg_out("d_modb", [128, 6 * D])
    d_QT = dbg_out("d_QT", [128, 4, SEQ], BF16)
    d_KT = dbg_out("d_KT", [128, 2, NTOK], BF16)
    d_V = dbg_out("d_V", [128, NT, 2, 65], BF16)
    d_convn = dbg_out("d_convn", [128, 4, SEQ], BF16)
    d_attn = dbg_out("d_attn", [64, 8, SEQ], BF16)
    d_wgt = dbg_out("d_wgt", [128, NL, NE])
    d_acc = dbg_out("d_acc", [128, NL, D])

    with ExitStack() as es:
        K = KB(nc, es)
        op, dma = K.op, K.dma

        def sb(stk, name, shape, dt):
            return stk.enter_context(nc.sbuf_tensor("sb_" + name, list(shape), dt))

        PSUM = es.enter_context(nc.psum_tensor("psum", [128, 8 * 512], F32))
        PS = [PSUM[:, i * 512:(i + 1) * 512] for i in range(8)]
        PSR = K.regs_n(8, "ps")

        identf = sb(es, "identf", [128, 128], F32)
        identb = sb(es, "identb", [128, 128], BF16)
        fgb = sb(es, "fgb", [128, D], F32)
        gt2b = sb(es, "gt2b", [128, D], F32)
        wgt = sb(es, "wgt", [128, NL, NE], F32)
        dest = sb(es, "dest", [128, NL, 2], I32)
        wsel = sb(es, "wsel", [128, NL, 2], F32)
        R_dest = K.regs_n(NL, "dest")
        R_const = K.reg("const")
        R_gt2 = K.reg("gt2")
        R_wgt = K.regs_n(NL, "wgt")

        dma("sp", identf[:], identf_d, writes=[R_const])
        dma("sp", fgb[:], fg_d, writes=[R_const])
        op("dve", lambda e: e.tensor_copy(identb[:], identf[:]), reads=[R_const], writes=[R_const])

        with ExitStack() as sAB:
            modb = sb(sAB, "modb", [128, 6 * D], F32)
            smod = sb(sAB, "smod", [128, 32], F32)
            cosT = sb(sAB, "cosT", [128, NL, 32], F32)
            sinT = sb(sAB, "sinT", [128, NL, 32], F32)
            qgb = sb(sAB, "qgb", [128, 64], F32)
            kgb = sb(sAB, "kgb", [128, 64], F32)
            cw = sb(sAB, "cw", [128, 12], F32)
            bd64 = sb(sAB, "bd64", [128, 128], F32)
            c65 = sb(sAB, "c65", [65, 64], F32)
            e0 = sb(sAB, "e0", [128, 1], F32)
            wr = sb(sAB, "wr", [128, 8, 36], F32)
            R_modb = K.regs_n(6, "modb")
            R_smod = K.reg("smod")
            for t_, d_ in ((cosT, cos_d), (sinT, sin_d), (qgb, qg_d), (kgb, kg_d), (cw, cw_d),
                           (bd64, bd64_d), (c65, c65_d), (e0, e0_d)):
                dma("sp", t_[:], d_, writes=[R_const])
            dma("sp", wr[:], wr_d.rearrange("(k p) n -> p k n", p=128), writes=[R_const])

            with ExitStack() as s0:
                cTt = sb(s0, "cTt", [128, 16], F32)
                scT = sb(s0, "scT", [128, 16], F32)
                lhs_c = sb(s0, "lhs_c", [128, 8, 128], BF16)
                lhs_x = sb(s0, "lhs_x", [128, 8, 128], BF16)
                bm = sb(s0, "bm", [128, 6 * D], F32)
                g1b = sb(s0, "g1b", [128, D], F32)
                g2b = sb(s0, "g2b", [128, D], F32)
                modx = sb(s0, "modx", [128, 2 * D], F32)
                wm = [sb(s0, f"wm{i}", [128, 8, 512], BF16) for i in range(2)]
                R_c = K.reg("c")
                R_bm = K.reg("bm")
                R_g = K.reg("g12")
                R_modx = K.regs_n(2, "modx")
                R_wm = K.regs_n(2, "wm")
                P_wm = [K.pslot(f"wm{i}") for i in range(2)]
                dma("sp", cTt[:], cT_d, writes=[R_c])
                dma("sp", bm[:], bm_d, writes=[R_bm])
                dma("sp", g1b[:], g1_d, writes=[R_g])
                dma("sp", g2b[:], g2_d, writes=[R_g])
                op("act", lambda e: e.activation(scT[:], cTt[:], AF.Silu), reads=[R_c], writes=[R_c])
                op("dve", lambda e: e.tensor_copy(
                    lhs_c[:], scT[:, 0:8].unsqueeze(2).to_broadcast([128, 8, 128])), reads=[R_c], writes=[R_c])
                op("dve", lambda e: e.tensor_copy(
                    lhs_x[:], scT[:, 8:16].unsqueeze(2).to_broadcast([128, 8, 128])), reads=[R_c], writes=[R_c])
                wmod_v = w_mod_d.rearrange("(k p) n -> p k n", p=128)
                for blk in range(12):
                    b = blk % 2
                    cs = slice(blk * 512, (blk + 1) * 512)
                    K.pdma(P_wm[b], wm[b][:], wmod_v[:, :, cs], writes=[R_wm[b]])
                    for k in range(8):
                        op("pe", lambda e, k=k: e.matmul(PS[b], lhs_c[:, k, :], wm[b][:, k, :],
                                                       start=(k == 0), stop=(k == 7)),
                           reads=[R_c, R_wm[b]], writes=[PSR[b]])
                    op("dve", lambda e: e.tensor_tensor(modb[:, cs], PS[b], bm[:, cs], ALU.add),
                       reads=[PSR[b], R_bm], writes=[R_modb[blk // 2]])
                    if blk < 4:
                        for k in range(8):
                            op("pe", lambda e, k=k: e.matmul(PS[2 + b], lhs_x[:, k, :], wm[b][:, k, :],
                                                           start=(k == 0), stop=(k == 7)),
                               reads=[R_c, R_wm[b]], writes=[PSR[2 + b]])
                        op("dve", lambda e: e.tensor_tensor(modx[:, cs], PS[2 + b], bm[:, cs], ALU.add),
                           reads=[PSR[2 + b], R_bm], writes=[R_modx[blk // 2]])
                op("dve", lambda e: e.scalar_tensor_tensor(modb[:, D:2 * D], modb[:, D:2 * D], 1.0, g1b[:],
                                                          ALU.add, ALU.mult),
                   reads=[R_modb[1], R_g], writes=[R_modb[1]])
                op("dve", lambda e: e.scalar_tensor_tensor(modx[:, D:2 * D], modx[:, D:2 * D], 1.0, g1b[:],
                                                          ALU.add, ALU.mult),
                   reads=[R_modx[1], R_g], writes=[R_modx[1]])
                op("dve", lambda e: e.scalar_tensor_tensor(modb[:, 4 * D:5 * D], modb[:, 4 * D:5 * D], 1.0, g2b[:],
                                                          ALU.add, ALU.mult),
                   reads=[R_modb[4], R_g], writes=[R_modb[4]])
                op("act", lambda e: e.activation(gt2b[:], modb[:, 5 * D:6 * D], AF.Copy),
                   reads=[R_modb[5]], writes=[R_gt2])
                srcs = [(modb, D, R_modb[1]), (modb, 0, R_modb[0]), (modx, D, R_modx[1]), (modx, 0, R_modx[0])]
                for vi, (tt, off, rr) in enumerate(srcs):
                    for k in range(8):
                        c0 = vi * 8 + k
                        op("pe", lambda e, tt=tt, off=off, k=k, c0=c0: e.matmul(
                            PS[4][:, c0:c0 + 1], tt[:, off + k * 128: off + (k + 1) * 128], e0[:, 0:1],
                            start=True, stop=True), reads=[rr, R_const], writes=[PSR[4]])
                op("act", lambda e: e.activation(smod[:], PS[4][:, 0:32], AF.Copy), reads=[PSR[4]], writes=[R_smod])
                if debug:
                    dma("sp", d_smod, smod[:], reads=[R_smod])
                    dma("sp", d_modb, modb[:], reads=R_modb)
                K.barrier()
                if stop_after == "0":
                    return nc, list(dbg.keys())

            with ExitStack() as sB:
                QT = sb(sB, "QT", [128, 4, SEQ], BF16)
                KT = sb(sB, "KT", [128, 2, 2, NTOK], BF16)
                VE = sb(sB, "VE", [128, NT, 2, 128], BF16)
                convn = sb(sB, "convn", [128, 4, SEQ], BF16)
                R_QT = K.regs_n(NL, "QT")
                R_KT = K.regs_n(NT, "KT")
                R_VE = K.regs_n(NT, "VE")
                R_convn = K.regs_n(4, "convn")
                R_ve1 = K.reg("ve1")
                if _LVL >= 0:
                    op("dve", lambda e: e.memset(VE[:], 0.0), writes=[R_ve1])
                    op("dve", lambda e: e.memset(VE[:, :, :, 64:65], 1.0), writes=[R_ve1])
                    op("dve", lambda e: e.memset(KT[:], 0.0), writes=[R_ve1])

                with ExitStack() as sA:
                    hT = sb(sA, "hT", [128, 8, NTOK], BF16)
                    R_hTd = K.regs_n(NT, "hTd")
                    R_hTa = K.regs_n(NT, "hTa")
                    with ExitStack() as sA1:
                        winq = sb(sA1, "winq", [128, 8, 768], BF16)
                        R_winq = K.reg("winq")
                        if _LVL >= 1:
                            K.pdma(K.pslot("winq"), winq[:], w_in_d.rearrange("(k p) n -> p k n", p=128)[:, :, 0:768],
                                   writes=[R_winq])
                        xt = [sb(sA1, f"xt{i}", [128, D], F32) for i in range(2)]
                        junk = sb(sA1, "junk", [128, D], BF16)
                        xn = [sb(sA1, f"xn{i}", [128, D], BF16) for i in range(2)]
                        st1 = sb(sA1, "st1", [128, 3, NT], F32)
                        zq = [sb(sA1, f"zq{i}", [128, 512], F32) for i in range(2)]
                        zkv = [sb(sA1, f"zkv{i}", [128, 256], F32) for i in range(2)]
                        sq = sb(sA1, "sq", [128, 512], F32)
                        qn = sb(sA1, "qn", [128, 512], F32)
                        kn = sb(sA1, "kn", [128, 128], F32)
                        knb = sb(sA1, "knb", [128, 128], BF16)
                        ta = sb(sA1, "ta", [128, 256], F32)
                        tb = sb(sA1, "tb", [128, 256], F32)
                        qst = sb(sA1, "qst", [128, 3, 16], F32)
                        qkb = [sb(sA1, f"qkb{i}", [128, 768], BF16) for i in range(2)]
                        R_xt = K.regs_n(2, "xt")
                        R_xn = K.regs_n(2, "xn")
                        R_st1 = K.regs_n(NT, "st1")
                        R_zq = K.regs_n(2, "zq")
                        R_zkv = K.regs_n(2, "zkv")
                        R_tmp = K.reg("tmpq")
                        R_qst = K.reg("qst")
                        R_qkb = K.regs_n(2, "qkb")
                        R_knb = K.reg("knb")
                        def a1_front(T):
                            b = T % 2
                            lat = T >= 2
                            t = T - 2
                            tsl = slice(T * 128, (T + 1) * 128)
                            dma("sp", xt[b][:], xin[tsl, :], writes=[R_xt[b]])
                            if _LVL < -4:
                                return
                            op("act", lambda e: e.activation(junk[:], xt[b][:], AF.Square,
                                                             accum_out=st1[:, 0, T:T + 1]),
                               reads=[R_xt[b]], writes=[R_st1[T]])
                            if _LVL < -3:
                                return
                            op("act", lambda e: e.activation(st1[:, 1, T:T + 1], st1[:, 0, T:T + 1], AF.Ln,
                                                             scale=1.0 / D, bias=EPS),
                               reads=[R_st1[T]], writes=[R_st1[T]])
                            op("act", lambda e: e.activation(st1[:, 2, T:T + 1], st1[:, 1, T:T + 1], AF.Exp,
                                                             scale=-0.5),
                               reads=[R_st1[T]], writes=[R_st1[T]])
                            op("act", lambda e: e.activation(xn[b][:], xt[b][:], AF.Copy, scale=st1[:, 2, T:T + 1]),
                               reads=[R_xt[b], R_st1[T]], writes=[R_xn[b]])
                            if _LVL < -2:
                                return
                            psb = PS[b].bitcast(BF16)
                            for k in range(8):
                                op("pe", lambda e, k=k: e.transpose(psb[:, k * 128:(k + 1) * 128],
                                                                    xn[b][:, k * 128:(k + 1) * 128], identb[:]),
                                   reads=[R_xn[b], R_const], writes=[PSR[b]])
                            if _LVL < -1:
                                return
                            so = 0 if lat else 16
                            for k in range(8):
                                if (b == 0 and _KEV != 'act') or _KEV == 'dve':
                                    op("dve", lambda e, k=k: e.tensor_scalar(
                                        hT[:, k, tsl], psb[:, k * 128:(k + 1) * 128],
                                        smod[:, so + k:so + k + 1], smod[:, so + 8 + k:so + 9 + k],
                                        ALU.mult, ALU.add), reads=[PSR[b], R_smod], writes=[R_hTd[T]])
                                else:
                                    op("act", lambda e, k=k: e.activation(
                                        hT[:, k, tsl], psb[:, k * 128:(k + 1) * 128], AF.Identity,
                                        bias=smod[:, so + 8 + k:so + 9 + k], scale=smod[:, so + k:so + k + 1]),
                                       reads=[PSR[b], R_smod], writes=[R_hTa[T]])
                            if _LVL < 1:
                                return
                            if lat:
                                for k in range(8):
                                    op("pe", lambda e, k=k: e.matmul(PS[2], hT[:, k, tsl], winq[:, k, 0:512],
                                                                   start=(k == 0), stop=(k == 7)),
                                       reads=[R_hTd[T], R_hTa[T], R_winq], writes=[PSR[2]])
                            for k in range(8):
                                op("pe", lambda e, k=k: e.matmul(PS[3][:, 0:256], hT[:, k, tsl], winq[:, k, 512:768],
                                                               start=(k == 0), stop=(k == 7)),
                                   reads=[R_hTd[T], R_hTa[T], R_winq], writes=[PSR[3]])

                        def a1_front_b(T):
                            b = T % 2
                            lat = T >= 2
                            t = T - 2
                            tsl = slice(T * 128, (T + 1) * 128)
                            if lat:
                                op("act", lambda e: e.activation(zq[b][:], PS[2], AF.Copy),
                                   reads=[PSR[2]], writes=[R_zq[b]])
                            op("act", lambda e: e.activation(zkv[b][:], PS[3][:, 0:256], AF.Copy),
                               reads=[PSR[3]], writes=[R_zkv[b]])
                            op("act", lambda e: e.activation(
                                VE[:, T, :, 0:64], zkv[b][:, 128:256].rearrange("p (g d) -> p g d", g=2), AF.Copy),
                               reads=[R_zkv[b], R_ve1], writes=[R_VE[T]])

                        def a1_back(T, part):
                            b = T % 2
                            lat = T >= 2
                            t = T - 2
                            tsl = slice(T * 128, (T + 1) * 128)
                            nh_list = ([("q", zq[b], 8, qn, qgb, R_zq[b])] if lat else []) + \
                                      [("k", zkv[b], 2, kn, kgb, R_zkv[b])]
                            for nm, src, nh, dst, gb_, rsrc in nh_list:
                                W = nh * 64
                                c0 = 0 if nm == "q" else 8
                                op("dve", lambda e: e.tensor_tensor(sq[:, 0:W], src[:, 0:W], src[:, 0:W], ALU.mult),
                                   reads=[rsrc], writes=[R_tmp])
                                op("dve", lambda e: e.tensor_reduce(
                                    qst[:, 0, c0:c0 + nh], sq[:, 0:W].rearrange("p (h d) -> p h d", h=nh),
                                    AX.X, ALU.add), reads=[R_tmp], writes=[R_qst])
                            lo = 0 if lat else 8
                            op("act", lambda e: e.activation(qst[:, 1, lo:10], qst[:, 0, lo:10],
                                                             AF.Ln, scale=1.0 / 64, bias=EPS),
                               reads=[R_qst], writes=[R_qst])
                            op("act", lambda e: e.activation(qst[:, 2, lo:10], qst[:, 1, lo:10],
                                                             AF.Exp, scale=-0.5),
                               reads=[R_qst], writes=[R_qst])

                        def a1_back_rest(T):
                            b = T % 2
                            lat = T >= 2
                            t = T - 2
                            tsl = slice(T * 128, (T + 1) * 128)
                            nh_list = ([("q", zq[b], 8, qn, qgb, R_zq[b])] if lat else []) + \
                                      [("k", zkv[b], 2, kn, kgb, R_zkv[b])]
                            for nm, src, nh, dst, gb_, rsrc in nh_list:
                                W = nh * 64
                                c0 = 0 if nm == "q" else 8
                                d3 = dst[:, 0:W].rearrange("p (h d) -> p h d", h=nh)
                                op("dve", lambda e: e.tensor_tensor(
                                    d3, src[:, 0:W].rearrange("p (h d) -> p h d", h=nh),
                                    qst[:, 2, c0:c0 + nh].unsqueeze(2).to_broadcast([128, nh, 64]), ALU.mult),
                                   reads=[rsrc, R_qst], writes=[R_tmp])
                                op("dve", lambda e: e.tensor_tensor(
                                    d3, d3, gb_[:, :].unsqueeze(1).to_broadcast([128, nh, 64]), ALU.mult),
                                   reads=[R_tmp, R_const], writes=[R_tmp])
                                if nm == "q":
                                    outb = qkb[b][:, 0:512]
                                    R_ob = R_qkb[b]
                                else:
                                    outb = knb[:, 0:128]
                                    R_ob = R_knb
                                if lat:
                                    d5 = dst[:, 0:W].rearrange("p (h a f d) -> p h a f d", h=nh, a=2, f=2)
                                    o5 = outb.rearrange("p (h a f d) -> p h a f d", h=nh, a=2, f=2)
                                    x1 = d5[:, :, :, 0, :]
                                    x2 = d5[:, :, :, 1, :]
                                    cb = cosT[:, t, :].rearrange("p (a d) -> p a d", a=2).unsqueeze(1) \
                                        .to_broadcast([128, nh, 2, 16])
                                    sb_ = sinT[:, t, :].rearrange("p (a d) -> p a d", a=2).unsqueeze(1) \
                                        .to_broadcast([128, nh, 2, 16])
                                    hw = nh * 32
                                    ta4 = ta[:, 0:hw].rearrange("p (h a d) -> p h a d", h=nh, a=2)
                                    tb4 = tb[:, 0:hw].rearrange("p (h a d) -> p h a d", h=nh, a=2)
                                    op("dve", lambda e: e.tensor_tensor(ta4, x1, cb, ALU.mult),
                                       reads=[R_tmp, R_const], writes=[R_tmp])
                                    op("dve", lambda e: e.tensor_tensor(tb4, x2, sb_, ALU.mult),
                                       reads=[R_tmp, R_const], writes=[R_tmp])
                                    op("dve", lambda e: e.tensor_tensor(o5[:, :, :, 0, :], ta4, tb4, ALU.subtract),
                                       reads=[R_tmp], writes=[R_ob])
                                    op("dve", lambda e: e.tensor_tensor(ta4, x1, sb_, ALU.mult),
                                       reads=[R_tmp, R_const], writes=[R_tmp])
                                    op("dve", lambda e: e.tensor_tensor(tb4, x2, cb, ALU.mult),
                                       reads=[R_tmp, R_const], writes=[R_tmp])
                                    op("dve", lambda e: e.tensor_tensor(o5[:, :, :, 1, :], ta4, tb4, ALU.add),
                                       reads=[R_tmp], writes=[R_ob])
                                else:
                                    op("dve", lambda e: e.tensor_copy(outb, dst[:, 0:W]),
                                       reads=[R_tmp], writes=[R_ob])
                            if _LVL < 4:
                                return
                            op("dve", lambda e: e.tensor_copy(
                                qkb[b][:, 512:768].rearrange("p (g r d) -> p g r d", g=2, r=2),
                                knb[:, 0:128].rearrange("p (g d) -> p g d", g=2).unsqueeze(2)
                                .to_broadcast([128, 2, 2, 64])), reads=[R_knb], writes=[R_qkb[b]])
                            if _LVL < 5:
                                return
                            ps4 = PS[4].bitcast(BF16)
                            ps5 = PS[5].bitcast(BF16)
                            if lat:
                                for j in range(4):
                                    op("pe", lambda e, j=j: e.transpose(ps4[:, j * 128:(j + 1) * 128],
                                                                        qkb[b][:, j * 128:(j + 1) * 128], identb[:]),
                                       reads=[R_qkb[b], R_const], writes=[PSR[4]])
                                op("dve", lambda e: e.tensor_copy(
                                    QT[:, :, t * 128:(t + 1) * 128],
                                    ps4[:, 0:512].rearrange("p (j n) -> p j n", j=4)),
                                   reads=[PSR[4]], writes=[R_QT[t]])
                            for j in range(2):
                                op("pe", lambda e, j=j: e.transpose(ps5[:, j * 128:(j + 1) * 128],
                                                                    qkb[b][:, 512 + j * 128:512 + (j + 1) * 128],
                                                                    identb[:]),
                                   reads=[R_qkb[b], R_const], writes=[PSR[5]])
                            for u in range(2):
                                op("dve", lambda e, u=u: e.tensor_copy(
                                    KT[64 * u:64 * u + 64, :, u, tsl],
                                    ps5[64 * u:64 * u + 64, 0:256].rearrange("p (j n) -> p j n", j=2)),
                                   reads=[PSR[5], R_ve1], writes=[R_KT[T]])

                        a1_front(0)
                        a1_front_b(0)
                        for T in range(NT):
                            if T + 1 < NT:
                                a1_front(T + 1)
                            a1_back(T, "stats")
                            if T + 1 < NT:
                                a1_front_b(T + 1)
                            a1_back_rest(T)
                        K.barrier()
                        if stop_after == "A1":
                            if debug:
                                dma("sp", d_QT, QT[:]); dma("sp", d_KT[0:64], KT[0:64, :, 0, :]); dma("sp", d_KT[64:128], KT[64:128, :, 1, :]); dma("sp", d_V, VE[:, :, :, 0:65])
                            K.barrier()
                            return nc, list(dbg.keys())
                    with ExitStack() as sA2:
                        wc = [sb(sA2, f"wc{i}", [128, 8, 384], BF16) for i in range(2)]
                        vT = [sb(sA2, f"vT{i}", [128, SEQ + 2], F32) for i in range(2)]
                        gbT = [sb(sA2, f"gbT{i}", [128, SEQ], F32) for i in range(2)]
                        gct = [sb(sA2, f"gct{i}", [128, 512], F32) for i in range(2)]
                        yb = sb(sA2, "yb", [128, 1024], F32)
                        ysq = sb(sA2, "ysq", [128, 1024], F32)
                        rs = sb(sA2, "rs", [128, 1024], F32)
                        R_wc = K.regs_n(2, "wc")
                        P_wc = [[K.pslot(f"wc{i}_{j}") for j in range(3)] for i in range(2)]
                        R_vT = K.regs_n(2, "vT")
                        R_gbT = K.regs_n(2, "gbT")
                        R_gct = K.regs_n(2, "gct")
                        R_y = K.reg("y")
                        R_ysq = K.reg("ysq")
                        R_rs = K.reg("rs")
                        winv = w_in_d.rearrange("(k p) n -> p k n", p=128)
                        for i in range(2):
                            op("dve", lambda e, i=i: e.memset(vT[i][:, 0:1], 0.0), writes=[R_vT[i]])
                            op("dve", lambda e, i=i: e.memset(vT[i][:, SEQ + 1:SEQ + 2], 0.0), writes=[R_vT[i]])
                        cnt2 = {'gi': 0, 'pi': 0}
                        def a2_mm(c4):
                            b = c4 % 2
                            for s3 in range(3):
                                c0 = 768 + s3 * 512 + c4 * 128
                                K.pdma(P_wc[b][s3], wc[b][:, :, s3 * 128:(s3 + 1) * 128], winv[:, :, c0:c0 + 128],
                                       writes=[R_wc[b]])
                            for tb_ in range(4):
                                tok = slice(256 + tb_ * 512, 256 + (tb_ + 1) * 512)
                                osl = slice(tb_ * 512, (tb_ + 1) * 512)
                                g_ = cnt2['gi'] % 2
                                cnt2['gi'] += 1
                                for s3 in range(3):
                                    pb = 5 + cnt2['pi'] % 3
                                    cnt2['pi'] += 1
                                    for k in range(8):
                                        op("pe", lambda e, k=k, s3=s3, pb=pb: e.matmul(
                                            PS[pb], wc[b][:, k, s3 * 128:(s3 + 1) * 128], hT[:, k, tok],
                                            start=(k == 0), stop=(k == 7)),
                                           reads=[R_wc[b]], writes=[PSR[pb]])
                                    if s3 == 0:
                                        op("act", lambda e, pb=pb: e.activation(gbT[b][:, osl], PS[pb], AF.Copy),
                                           reads=[PSR[pb]], writes=[R_gbT[b]])
                                    elif s3 == 1:
                                        op("act", lambda e, pb=pb: e.activation(gct[g_][:], PS[pb], AF.Copy),
                                           reads=[PSR[pb]], writes=[R_gct[g_]])
                                    else:
                                        op("dve", lambda e, pb=pb: e.tensor_tensor(
                                            vT[b][:, 1 + tb_ * 512:1 + (tb_ + 1) * 512], PS[pb], gct[g_][:], ALU.mult),
                                           reads=[PSR[pb], R_gct[g_]], writes=[R_vT[b]])

                        def a2_fin(c4):
                            b = c4 % 2
                            for hf in range(2):
                                o = hf * 1024
                                op("dve", lambda e: e.tensor_scalar(yb[:], vT[b][:, o:o + 1024],
                                                                    cw[:, c4 * 3:c4 * 3 + 1], None, ALU.mult),
                                   reads=[R_vT[b], R_const], writes=[R_y])
                                op("dve", lambda e: e.scalar_tensor_tensor(
                                    yb[:], vT[b][:, o + 1:o + 1025], cw[:, c4 * 3 + 1:c4 * 3 + 2], yb[:],
                                    ALU.mult, ALU.add), reads=[R_vT[b], R_const, R_y], writes=[R_y])
                                op("dve", lambda e: e.scalar_tensor_tensor(
                                    yb[:], vT[b][:, o + 2:o + 1026], cw[:, c4 * 3 + 2:c4 * 3 + 3], yb[:],
                                    ALU.mult, ALU.add), reads=[R_vT[b], R_const, R_y], writes=[R_y])
                                op("dve", lambda e: e.tensor_tensor(yb[:], yb[:], gbT[b][:, o:o + 1024], ALU.mult),
                                   reads=[R_y, R_gbT[b]], writes=[R_y])
                                op("act", lambda e: e.activation(ysq[:], yb[:], AF.Square),
                                   reads=[R_y], writes=[R_ysq])
                                for q2 in range(2):
                                    op("pe", lambda e, q2=q2: e.matmul(PS[q2], bd64[:], ysq[:, q2 * 512:(q2 + 1) * 512],
                                                                     start=True, stop=True),
                                       reads=[R_ysq, R_const], writes=[PSR[q2]])
                                    op("act", lambda e, q2=q2: e.activation(rs[:, q2 * 512:(q2 + 1) * 512], PS[q2],
                                                                            AF.Ln, bias=EPS),
                                       reads=[PSR[q2]], writes=[R_rs])
                                op("act", lambda e: e.activation(rs[:], rs[:], AF.Exp, scale=-0.5),
                                   reads=[R_rs], writes=[R_rs])
                                op("dve", lambda e: e.tensor_tensor(convn[:, c4, o:o + 1024], yb[:], rs[:], ALU.mult),
                                   reads=[R_y, R_rs], writes=[R_convn[c4]])

                        a2_mm(0)
                        for c4 in range(4):
                            if c4 + 1 < 4:
                                a2_mm(c4 + 1)
                            a2_fin(c4)
                        K.barrier()
                if debug:
                    dma("sp", d_QT, QT[:], reads=R_QT)
                    dma("sp", d_KT[0:64], KT[0:64, :, 0, :], reads=R_KT)
                    dma("sp", d_KT[64:128], KT[64:128, :, 1, :], reads=R_KT)
                    dma("sp", d_V, VE[:, :, :, 0:65], reads=R_VE)
                    dma("sp", d_convn, convn[:], reads=R_convn)
                if stop_after == "A2":
                    K.barrier()
                    return nc, list(dbg.keys())

                with ExitStack() as sB2:
                    attnT = sb(sB2, "attnT", [128, 4, SEQ], BF16)
                    R_attn = K.regs_n(8, "attn")
                    woall = sb(sB2, "woall", [128, 8, D], BF16)
                    gall = sb(sB2, "gall", [128, 8], F32)
                    R_wst = K.reg("wst")
                    R_wo = K.reg("wo")
                    with ExitStack() as sBa:
                        PT = [sb(sBa, f"PT{i}", [128, 1024], BF16) for i in range(3)]
                        sqb = [sb(sBa, f"sqb{i}", [65, 512], F32) for i in range(2)]
                        lnb = [sb(sBa, f"lnb{i}", [64, 512], F32) for i in range(2)]
                        R_PT = K.regs_n(3, "PT")
                        R_sqb = K.regs_n(2, "sqb")
                        R_lnb = K.regs_n(2, "lnb")
                        wst = sb(sBa, "wst", [128, 4, D], F32)
                        dma("sp", gall[:], gall_d, writes=[R_wo])
                        wov = w_out_d.rearrange("(c p) n -> p c n", p=128)
                        for half in range(2):
                            dma("sp", wst[:], wov[:, half * 4:(half + 1) * 4, :], writes=[R_wst])
                            op("dve", lambda e: e.tensor_tensor(
                                woall[:, half * 4:(half + 1) * 4, :], wst[:],
                                gall[:, half * 4:(half + 1) * 4].unsqueeze(2).to_broadcast([128, 4, D]), ALU.mult),
                               reads=[R_wst, R_wo], writes=[R_wo])
                        zt = sb(sBa, "zt", [128, 8, D], BF16)
                        R_zt = K.reg("zt")
                        op("dve", lambda e: e.memset(zt[:], 0.0), writes=[R_zt])
                        for i in range(NE * CAP // 1024):
                            dma("sp", xbuf_d[i * 1024:(i + 1) * 1024, :].rearrange("(p j) d -> p j d", j=8), zt[:],
                                reads=[R_zt])
                        dma("sp", xbuf_d[NE * CAP:NE * CAP + 1, :], zt[0:1, 0, :], reads=[R_zt])
                        it = 0
                        pti = 0
                        spi = 0
                        for j in range(4):
                            g = j // 2
                            for qb in range(4):
                                qs = slice(qb * 512, (qb + 1) * 512)
                                obase = 4 + 2 * (it % 2)
                                it += 1
                                Oab = [PS[obase], PS[obase + 1]]

                                def s_step(kt, sp):
                                    for u in range(2):
                                        p0 = 64 * u
                                        bk = 2 * sp + u
                                        op("pe", lambda e: e.matmul(
                                            PS[bk], KT[:, g, u, kt * 128:(kt + 1) * 128],
                                            QT[:, j, qs], start=True, stop=True),
                                           writes=[PSR[2 * sp], PSR[2 * sp + 1]] if u == 0 else [PSR[bk]])
                                sp_of = {0: spi % 2}
                                spi += 1
                                s_step(0, sp_of[0])
                                for kt in range(NT):
                                    if kt + 1 < NT:
                                        sp_of[kt + 1] = spi % 2
                                        spi += 1
                                        s_step(kt + 1, sp_of[kt + 1])
                                    sp = sp_of[kt]
                                    pb_ = pti % 3
                                    pti += 1
                                    op("act", lambda e: e.activation(
                                        PT[pb_][:], PSUM[:, 2 * sp * 512:(2 * sp + 2) * 512], AF.Exp, scale=0.125),
                                       reads=[PSR[2 * sp], PSR[2 * sp + 1]], writes=[R_PT[pb_]])
                                    for u in range(2):
                                        op("pe", lambda e: e.matmul(Oab[u][:, :], VE[:, kt, g, :],
                                                                    PT[pb_][:, u * 512:(u + 1) * 512],
                                                                    start=(kt == 0), stop=(kt == NT - 1)),
                                           reads=[R_PT[pb_]], writes=[PSR[obase + u]])
                                sp = spi % 2
                                spi += 1
                                for u in range(2):
                                    op("act", lambda e: e.activation(sqb[u][:], Oab[u][0:65, :], AF.Square),
                                       reads=[PSR[obase + u]], writes=[R_sqb[u]])
                                    op("pe", lambda e: e.matmul(PS[2 * sp + u][0:64, :], c65[:, :], sqb[u][:],
                                                                start=True, stop=True),
                                       reads=[R_sqb[u], R_const], writes=[PSR[2 * sp + u]])
                                for u in range(2):
                                    op("act", lambda e: e.activation(lnb[u][:], PS[2 * sp + u][0:64, :], AF.Ln),
                                       reads=[PSR[2 * sp + u]], writes=[R_lnb[u]])
                                    op("act", lambda e: e.activation(lnb[u][:], lnb[u][:], AF.Exp, scale=-0.5),
                                       reads=[R_lnb[u]], writes=[R_lnb[u]])
                                    op("dve", lambda e: e.tensor_tensor(attnT[64 * u:64 * u + 64, j, qs], Oab[u][0:64, :],
                                                                        lnb[u][:], ALU.mult),
                                       reads=[PSR[obase + u], R_lnb[u]], writes=[R_attn[2 * j + u]])
                        K.barrier()
                    if debug:
                        for u in range(2):
                            dma("sp", d_attn.rearrange("d (j u) s -> d u j s", u=2)[:, u], attnT[64 * u:64 * u + 64, :, :],
                                reads=R_attn)
                    if stop_after == "B":
                        K.barrier()
                        return nc, list(dbg.keys())

                    if True:
                        with ExitStack() as sB3:
                            xr = [sb(sB3, "xr0", [128, D], F32)] * 2
                            xnw = [sb(sB3, "xnw0", [128, D], F32)] * 2
                            hx = [sb(sB3, f"hx{i}", [128, D], F32) for i in range(2)]
                            hxT = [sb(sB3, "hxT0", [128, 8, 128], F32)] * 2
                            st2 = sb(sB3, "st2", [128, 3, NL], F32)
                            lg = sb(sB3, "lg", [128, NL, 36], F32)
                            rt = sb(sB3, "rt", [128, 64], F32)
                            RB = sb(sB3, "RB", [128, 16, NL, 8], F32)
                            m8 = sb(sB3, "m8", [128, 8], F32)
                            ustr = sb(sB3, "ustr", [128, 128], BF16)
                            onesb = sb(sB3, "onesb", [128, 128], BF16)
                            ustf = sb(sB3, "ustf", [128, 128], F32)
                            eC = sb(sB3, "eC", [128, NE], F32)
                            maskb = sb(sB3, "maskb", [128, NL, NE], BF16)
                            mkk = sb(sB3, "mkk", [128, 2, NL, NE], F32)
                            rkk = sb(sB3, "rkk", [128, 2, NL, NE], F32)
                            t3 = sb(sB3, "t3", [128, NL, NE], F32)
                            dfa = sb(sB3, "dfa", [128, 3, 2, NL], F32)
                            hxb = [sb(sB3, f"hxb{i}", [128, D], BF16) for i in range(2)]
                            R_mask = K.regs_n(NL, "mask")
                            R_rk = K.reg("rk")
                            R_hxb = K.regs_n(2, "hxb")
                            hxs = [sb(sB3, f"hxs{i}", [128, D], BF16) for i in range(4)]
                            R_hxs = K.regs_n(4, "hxs")
                            R_xbuf = K.reg("xbuf")
                            dma("sp", ustf[:], ustr_d, writes=[R_wo])
                            dma("sp", eC[:], eC_d, writes=[R_wo])
                            op("dve", lambda e: e.tensor_copy(ustr[:], ustf[:]), reads=[R_wo], writes=[R_wo])
                            op("dve", lambda e: e.memset(onesb[:], 1.0), writes=[R_wo])
                            R_xr = [K.reg("xr")] * 2
                            R_xnw = [K.reg("xnw")] * 2
                            R_hx = K.regs_n(2, "hx")
                            R_hxT = [K.reg("hxT")] * 2
                            R_st2 = K.regs_n(NL, "st2")
                            R_lg = K.reg("lg")
                            R_rt = K.reg("rt")
                            def b2_front(t):
                                b = t % 2
                                tsl = slice(t * 128, (t + 1) * 128)
                                dma("sp", xr[b][:], xin[256 + t * 128:256 + (t + 1) * 128, :], writes=[R_xr[b]])
                                for hf in range(2):
                                    pb = hf + 6 * (t % 2)
                                    cs = slice(hf * 512, (hf + 1) * 512)
                                    for c in range(4):
                                        op("pe", lambda e, c=c: e.matmul(PS[pb], attnT[:, c, tsl], woall[:, c, cs],
                                                                       start=(c == 0), stop=False),
                                           reads=[R_wo], writes=[PSR[pb]])
                                    for c4 in range(4):
                                        op("pe", lambda e, c4=c4: e.matmul(PS[pb], convn[:, c4, tsl], woall[:, 4 + c4, cs],
                                                                         start=False, stop=(c4 == 3)),
                                           reads=[R_wo], writes=[PSR[pb]])
                                    op("dve", lambda e: e.tensor_tensor(xnw[b][:, cs], PS[pb], modb[:, 2 * D + hf * 512:
                                                                                                  2 * D + (hf + 1) * 512],
                                                                        ALU.mult),
                                       reads=[PSR[pb]], writes=[R_xnw[b]])
                                op("dve", lambda e: e.tensor_tensor(xnw[b][:], xnw[b][:], xr[b][:], ALU.add),
                                   reads=[R_xnw[b], R_xr[b]], writes=[R_xnw[b]])
                                dma("sp", xnew_d[tsl, :], xnw[b][:], reads=[R_xnw[b]])
                                op("act", lambda e: e.activation(hxb[b][:], xnw[b][:], AF.Square,
                                                                 accum_out=st2[:, 0, t:t + 1]),
                                   reads=[R_xnw[b]], writes=[R_st2[t], R_hxb[b]])
                                op("act", lambda e: e.activation(st2[:, 1, t:t + 1], st2[:, 0, t:t + 1], AF.Ln,
                                                                 scale=1.0 / D, bias=EPS),
                                   reads=[R_st2[t]], writes=[R_st2[t]])
                                op("act", lambda e: e.activation(st2[:, 2, t:t + 1], st2[:, 1, t:t + 1], AF.Exp,
                                                                 scale=-0.5),
                                   reads=[R_st2[t]], writes=[R_st2[t]])
                                op("dve", lambda e: e.scalar_tensor_tensor(
                                    hx[b][:], xnw[b][:], st2[:, 2, t:t + 1], modb[:, 4 * D:5 * D], ALU.mult, ALU.mult),
                                   reads=[R_xnw[b], R_st2[t]], writes=[R_hx[b]])
                                op("dve", lambda e: e.tensor_tensor(hx[b][:], hx[b][:], modb[:, 3 * D:4 * D], ALU.add),
                                   reads=[R_hx[b]], writes=[R_hx[b]])

                            def b2_back(t):
                                b = t % 2
                                tsl = slice(t * 128, (t + 1) * 128)
                                PP = PSUM[:, 2 * 512:4 * 512]
                                for k in range(8):
                                    op("pe", lambda e, k=k: e.transpose(PP[:, k * 128:(k + 1) * 128],
                                                                        hx[b][:, k * 128:(k + 1) * 128], identf[:]),
                                       reads=[R_hx[b], R_const], writes=[PSR[2], PSR[3]])
                                op("act", lambda e: e.activation(
                                    hxT[b][:], PP.rearrange("p (k n) -> p k n", k=8), AF.Copy),
                                   reads=[PSR[2], PSR[3]], writes=[R_hxT[b]])
                                op("act", lambda e: e.activation(hxb[b][:], hx[b][:], AF.Copy),
                                   reads=[R_hx[b]], writes=[R_hxb[b]])
                                for k in range(8):
                                    op("pe", lambda e, k=k: e.matmul(PS[4][:, 0:36], hxT[b][:, k, :], wr[:, k, :],
                                                                   start=(k == 0), stop=(k == 7)),
                                       reads=[R_hxT[b], R_const], writes=[PSR[4]])
                                op("act", lambda e: e.activation(lg[:, t, :], PS[4][:, 0:36], AF.Copy),
                                   reads=[PSR[4]], writes=[R_lg])
                                dma("sp", hx2_d[tsl, :], hxb[b][:], reads=[R_hxb[b]], writes=[R_xbuf])

                            b2_front(0)
                            for t in range(NL):
                                if t + 1 < NL:
                                    b2_front(t + 1)
                                b2_back(t)
                            def slab(i, w=8):
                                return RB[:, i, :, 0:w]

                            def col(i):
                                return RB[:, i, :, 0]

                            def bc(a2, w):
                                return a2.unsqueeze(2).to_broadcast([128, NL, w])
                            R_B = K.reg("RB")

                            def dv(fn, rd=(), wr_=()):
                                op("dve", fn, reads=[R_lg, R_B] + list(rd), writes=[R_B] + list(wr_))

                            def ac(fn):
                                op("act", fn, reads=[R_lg, R_B], writes=[R_B])
                            G4 = lg[:, :, 0:4]
                            dv(lambda e: e.tensor_reduce(col(0), G4, AX.X, ALU.max))
                            dv(lambda e: e.tensor_tensor(slab(1, 4), G4, bc(col(0), 4), ALU.is_equal))
                            dv(lambda e: e.tensor_tensor(slab(2, 4), G4, bc(col(0), 4), ALU.subtract))
                            ac(lambda e: e.activation(slab(2, 4), slab(2, 4), AF.Exp))
                            dv(lambda e: e.tensor_reduce(col(3), slab(2, 4), AX.X, ALU.add))
                            dv(lambda e: e.reciprocal(col(4), col(3)))
                            dv(lambda e: e.tensor_tensor(slab(5), lg[:, :, 4:12], bc(RB[:, 1, :, 0], 8), ALU.mult))
                            for gq in range(1, 4):
                                dv(lambda e, gq=gq: e.tensor_tensor(slab(6), lg[:, :, 4 + 8 * gq:12 + 8 * gq],
                                                                    bc(RB[:, 1, :, gq], 8), ALU.mult))
                                dv(lambda e: e.tensor_tensor(slab(5), slab(5), slab(6), ALU.add))
                            dv(lambda e: e.tensor_reduce(col(7), slab(5), AX.X, ALU.max))
                            dv(lambda e: e.tensor_tensor(slab(8), slab(5), bc(col(7), 8), ALU.is_equal))
                            dv(lambda e: e.scalar_tensor_tensor(slab(9), slab(8), -1.0e30, slab(5), ALU.mult, ALU.add))
                            dv(lambda e: e.tensor_reduce(col(10), slab(9), AX.X, ALU.max))
                            dv(lambda e: e.tensor_tensor(slab(11), slab(5), bc(col(10), 8), ALU.is_ge))
                            dv(lambda e: e.tensor_tensor(slab(12), slab(11), slab(8), ALU.subtract))
                            dv(lambda e: e.tensor_tensor(slab(13), slab(5), bc(col(7), 8), ALU.subtract))
                            ac(lambda e: e.activation(slab(13), slab(13), AF.Exp))
                            dv(lambda e: e.tensor_tensor(slab(13), slab(13), slab(11), ALU.mult))
                            dv(lambda e: e.tensor_reduce(col(14), slab(13), AX.X, ALU.add))
                            dv(lambda e: e.reciprocal(col(15), col(14)))
                            dv(lambda e: e.tensor_tensor(col(15), col(15), col(4), ALU.mult))
                            dv(lambda e: e.tensor_tensor(slab(13), slab(13), bc(col(15), 8), ALU.mult))
                            for gq in range(4):
                                ohg = bc(RB[:, 1, :, gq], 8)
                                dv(lambda e, gq=gq, ohg=ohg: e.tensor_tensor(wgt[:, :, gq * 8:(gq + 1) * 8], slab(13), ohg,
                                                                             ALU.mult), wr_=R_wgt)
                                dv(lambda e, gq=gq, ohg=ohg: e.tensor_tensor(mkk[:, 0, :, gq * 8:(gq + 1) * 8], slab(8), ohg,
                                                                             ALU.mult))
                                dv(lambda e, gq=gq, ohg=ohg: e.tensor_tensor(mkk[:, 1, :, gq * 8:(gq + 1) * 8], slab(12), ohg,
                                                                             ALU.mult))
                            dv(lambda e: e.tensor_tensor(maskb[:], mkk[:, 0], mkk[:, 1], ALU.add), wr_=[R_mask[0]])
                            for t in range(NL):
                                op("pe", lambda e, t=t: e.matmul(PS[5][:, t * NE:(t + 1) * NE], ustr[:], maskb[:, t, :],
                                                               start=True, stop=(t == 0)),
                                   reads=[R_mask[0], R_wo], writes=[PSR[5]])
                                for t2 in range(t):
                                    op("pe", lambda e, t=t, t2=t2: e.matmul(PS[5][:, t * NE:(t + 1) * NE], onesb[:],
                                                                           maskb[:, t2, :], start=False, stop=(t2 == t - 1)),
                                       reads=[R_mask[0], R_wo], writes=[PSR[5]])
                            op("act", lambda e: e.activation(rkk[:, 0].rearrange("p t e -> p (t e)"), PS[5], AF.Copy),
                               reads=[PSR[5]], writes=[R_rk])
                            op("dve", lambda e: e.tensor_tensor(rkk[:, 1], rkk[:, 0],
                                                                eC[:, :].unsqueeze(1).to_broadcast([128, NL, NE]), ALU.add),
                               reads=[R_rk, R_wo], writes=[R_rk])
                            for kk in range(2):
                                for src_i, di in ((1, 0), (0, 1)):
                                    op("dve", lambda e, kk=kk, src_i=src_i: e.tensor_tensor(
                                        t3[:], mkk[:, kk], rkk[:, src_i], ALU.mult), reads=[R_rk, R_B], writes=[R_B])
                                    op("dve", lambda e, kk=kk, di=di: e.tensor_reduce(
                                        dfa[:, di, kk, :], t3[:], AX.X, ALU.add), reads=[R_B], writes=[R_B])
                                op("dve", lambda e, kk=kk: e.tensor_tensor(t3[:], mkk[:, kk], wgt[:], ALU.mult),
                                   reads=[R_B] + R_wgt, writes=[R_B])
                                op("dve", lambda e, kk=kk: e.tensor_reduce(wsel[:, :, kk], t3[:], AX.X, ALU.add),
                                   reads=[R_B], writes=R_dest)
                            dv(lambda e: e.tensor_scalar(dfa[:, 2], dfa[:, 1], float(CAP), None, ALU.is_ge))
                            dv(lambda e: e.scalar_tensor_tensor(dfa[:, 0], dfa[:, 2], 1.0e6, dfa[:, 0], ALU.mult, ALU.add))
                            dv(lambda e: e.tensor_scalar(dfa[:, 0], dfa[:, 0], float(NE * CAP), None, ALU.min))
                            dv(lambda e: e.tensor_copy(dest[:].rearrange("p t k -> p k t"), dfa[:, 0]), wr_=R_dest)
                            for t in range(NL):
                                b = t % 4
                                dma("sp", hxs[b][:], hx2_d[t * 128:(t + 1) * 128, :], reads=[R_xbuf], writes=[R_hxs[b]])
                                for kk in range(2):
                                    K.pdma(None, None, None, reads=[R_hxs[b]] + R_dest, writes=[],
                                           fn=lambda g_, kk=kk, t=t: g_.indirect_dma_start(
                                               out=xbuf_d[:, :],
                                               out_offset=bass.IndirectOffsetOnAxis(ap=dest[:, t, kk:kk + 1], axis=0),
                                               in_=hxs[b][:, :], in_offset=None))
                            K.barrier()
                        if debug:
                            dma("sp", d_wgt, wgt[:], reads=R_wgt)
                        if stop_after == "B2":
                            K.barrier()
                            return nc, list(dbg.keys())

        with ExitStack() as sM:
            NWB = 3
            wgs = [sb(sM, f"wgs{i}", [128, 8, DE], BF16) for i in range(NWB)]
            wus = [sb(sM, f"wus{i}", [128, 8, DE], BF16) for i in range(NWB)]
            wds = [sb(sM, f"wds{i}", [128, 6, D], BF16) for i in range(NWB)]
            Xs = [sb(sM, f"Xs{i}", [128, NJ, D], BF16) for i in range(2)]
            XT = [sb(sM, f"XT{i}", [128, 8, CAP], BF16) for i in range(2)]
            HT = [sb(sM, f"HT{i}", [128, 6, CAP], BF16) for i in range(2)]
            sg = [sb(sM, f"sg{i}", [128, CAP], F32) for i in range(2)]
            Yst = [sb(sM, f"Yst{i}", [128, D], F32) for i in range(2)]
            R_wg = K.regs_n(NWB, "wg")
            R_wu = K.regs_n(NWB, "wu")
            R_wd = K.regs_n(NWB, "wd")
            R_Xs = K.regs_n(2, "Xs")
            R_XT = K.regs_n(2, "XT")
            R_HT = K.regs_n(2, "HT")
            R_sg = K.regs_n(2, "sg")
            R_Yst = K.regs_n(2, "Yst")
            cnt = {"si": 0, "yi": 0, "ti": 0}
            op("dve", lambda e: e.memset(Yst[0][0:1, :], 0.0), writes=[R_Yst[0]])
            dma("sp", ybuf_d[NE * CAP:NE * CAP + 1, :], Yst[0][0:1, :], reads=[R_Yst[0]])

            def load_w(ex):
                w3 = ex % NWB
                K.pdma(None, wgs[w3][:], wg_d[ex].rearrange("(k p) f -> p k f", p=128), writes=[R_wg[w3]])
                K.pdma(None, wus[w3][:], wu_d[ex].rearrange("(k p) f -> p k f", p=128), writes=[R_wu[w3]])
                K.pdma(None, wds[w3][:], wd_d[ex].rearrange("(c p) n -> p c n", p=128), writes=[R_wd[w3]])

            def load_x(ex):
                xb_ = ex % 2
                dma("sp", Xs[xb_][:], xbuf_d[ex * CAP:(ex + 1) * CAP, :].rearrange("(j p) d -> p j d", p=128),
                    writes=[R_Xs[xb_]])

            def transposes(ex):
                xb_ = ex % 2
                for j in range(NJ):
                    tbk = 6 + cnt["ti"] % 2
                    cnt["ti"] += 1
                    psb = PS[tbk].bitcast(BF16)
                    for k in range(8):
                        op("pe", lambda e, k=k: e.transpose(psb[:, k * 128:(k + 1) * 128],
                                                            Xs[xb_][:, j, k * 128:(k + 1) * 128], identb[:]),
                           reads=[R_Xs[xb_], R_const], writes=[PSR[tbk]])
                    if tbk == 6:
                        op("dve", lambda e: e.tensor_copy(XT[xb_][:, :, j * 128:(j + 1) * 128],
                                                          psb.rearrange("p (k n) -> p k n", k=8)),
                           reads=[PSR[tbk]], writes=[R_XT[xb_]])
                    else:
                        op("act", lambda e: e.activation(XT[xb_][:, :, j * 128:(j + 1) * 128],
                                                         psb.rearrange("p (k n) -> p k n", k=8), AF.Copy),
                           reads=[PSR[tbk]], writes=[R_XT[xb_]])

            load_w(0)
            load_w(1)
            load_x(0)
            transposes(0)
            for ex in range(NE):
                wb = ex % 2
                w3 = ex % NWB
                if ex + 2 < NE:
                    load_w(ex + 2)
                if ex + 1 < NE:
                    load_x(ex + 1)
                for fc in range(6):
                    fs = slice(fc * 128, (fc + 1) * 128)
                    gbk = fc % 2
                    ubk = 2 + fc % 2
                    for k in range(8):
                        op("pe", lambda e, k=k: e.matmul(PS[gbk][:, 0:CAP], wgs[w3][:, k, fs], XT[wb][:, k, :],
                                                       start=(k == 0), stop=(k == 7)),
                           reads=[R_wg[w3], R_XT[wb]], writes=[PSR[gbk]])
                    for k in range(8):
                        op("pe", lambda e, k=k: e.matmul(PS[ubk][:, 0:CAP], wus[w3][:, k, fs], XT[wb][:, k, :],
                                                       start=(k == 0), stop=(k == 7)),
                           reads=[R_wu[w3], R_XT[wb]], writes=[PSR[ubk]])
                    sb2 = cnt["si"] % 2
                    cnt["si"] += 1
                    op("act", lambda e: e.activation(sg[sb2][:], PS[gbk][:, 0:CAP], AF.Silu),
                       reads=[PSR[gbk]], writes=[R_sg[sb2]])
                    op("dve", lambda e: e.tensor_tensor(HT[wb][:, fc, :], PS[ubk][:, 0:CAP], sg[sb2][:], ALU.mult),
                       reads=[PSR[ubk], R_sg[sb2]], writes=[R_HT[wb]])
                if ex + 1 < NE:
                    transposes(ex + 1)
                for j in range(NJ):
                    ys = cnt["yi"] % 2
                    cnt["yi"] += 1
                    for hf in range(2):
                        yb_ = 4 + hf
                        cs = slice(hf * 512, (hf + 1) * 512)
                        for fc in range(6):
                            op("pe", lambda e, fc=fc: e.matmul(
                                PS[yb_], HT[wb][:, fc, j * 128:(j + 1) * 128], wds[w3][:, fc, cs],
                                start=(fc == 0), stop=(fc == 5)),
                               reads=[R_HT[wb], R_wd[w3]], writes=[PSR[yb_]])
                        op("dve", lambda e: e.tensor_copy(Yst[ys][:, cs], PS[yb_]),
                           reads=[PSR[yb_]], writes=[R_Yst[ys]])
                    dma("sp", ybuf_d[ex * CAP + j * 128:ex * CAP + (j + 1) * 128, :], Yst[ys][:],
                        reads=[R_Yst[ys]])
            K.barrier()
        with ExitStack() as sF:
            y12 = [[sb(sF, f"y{k}_{i}", [128, D], F32) for k in range(2)] for i in range(4)]
            xr2 = [sb(sF, f"xq{i}", [128, D], F32) for i in range(4)]
            ot = [sb(sF, f"ot{i}", [128, D], F32) for i in range(4)]
            junk3 = sb(sF, "junk3", [128, D], BF16)
            st3 = sb(sF, "st3", [128, 3, NL], F32)
            R_y12 = [K.regs_n(2, f"y12_{i}") for i in range(4)]
            R_xr2 = K.regs_n(4, "xr2")
            R_ot = K.regs_n(4, "ot")
            R_st3 = K.regs_n(NL, "st3")
            def fin_load(tg):
                b = tg % 4
                tsl = slice(tg * 128, (tg + 1) * 128)
                dma("sp", xr2[b][:], xnew_d[tsl, :], writes=[R_xr2[b]])
                for kk in range(2):
                    K.pdma(None, None, None, reads=[], writes=[R_y12[tg % 4][kk]],
                           fn=lambda g, kk=kk: g.indirect_dma_start(
                               out=y12[tg % 4][kk][:, :], out_offset=None, in_=ybuf_d[:, :],
                               in_offset=bass.IndirectOffsetOnAxis(ap=dest[:, tg, kk:kk + 1], axis=0)))

            def fin_front(tg):
                b = tg % 4
                op("act", lambda e: e.activation(ot[b][:], y12[tg % 4][0][:], AF.Copy, scale=wsel[:, tg, 0:1]),
                   reads=[R_y12[tg % 4][0]], writes=[R_ot[b]])
                op("dve", lambda e: e.scalar_tensor_tensor(ot[b][:], y12[tg % 4][1][:], wsel[:, tg, 1:2], ot[b][:],
                                                          ALU.mult, ALU.add),
                   reads=[R_y12[tg % 4][1], R_ot[b]], writes=[R_ot[b]])
                if debug:
                    dma("sp", d_acc[:, tg, :], ot[b][:], reads=[R_ot[b]])
                op("dve", lambda e: e.tensor_tensor(ot[b][:], ot[b][:], gt2b[:], ALU.mult),
                   reads=[R_ot[b], R_gt2], writes=[R_ot[b]])
                op("dve", lambda e: e.tensor_tensor(ot[b][:], ot[b][:], xr2[b][:], ALU.add),
                   reads=[R_ot[b], R_xr2[b]], writes=[R_ot[b]])

            def fin_back(tg):
                b = tg % 4
                tsl = slice(tg * 128, (tg + 1) * 128)
                op("act", lambda e: e.activation(junk3[:], ot[b][:], AF.Square, accum_out=st3[:, 0, tg:tg + 1]),
                   reads=[R_ot[b]], writes=[R_st3[tg]])
                op("act", lambda e: e.activation(st3[:, 1, tg:tg + 1], st3[:, 0, tg:tg + 1], AF.Ln,
                                                 scale=1.0 / D, bias=EPS),
                   reads=[R_st3[tg]], writes=[R_st3[tg]])
                op("act", lambda e: e.activation(st3[:, 2, tg:tg + 1], st3[:, 1, tg:tg + 1], AF.Exp, scale=-0.5),
                   reads=[R_st3[tg]], writes=[R_st3[tg]])
                op("dve", lambda e: e.scalar_tensor_tensor(
                    ot[b][:], ot[b][:], st3[:, 2, tg:tg + 1], fgb[:], ALU.mult, ALU.mult),
                   reads=[R_ot[b], R_st3[tg], R_const], writes=[R_ot[b]])
                dma("sp", y_d[tsl, :], ot[b][:], reads=[R_ot[b]])

            fin_load(0)
            fin_load(1)
            fin_front(0)
            for tg in range(NL):
                if tg + 2 < NL:
                    fin_load(tg + 2)
                if tg + 1 < NL:
                    fin_front(tg + 1)
                fin_back(tg)
            K.barrier()
    return nc, list(dbg.keys())


def _host_consts():
    ident = np.eye(128, dtype=np.float32)
    rows = SEQ // 64
    row_idx = np.repeat(np.arange(rows, dtype=np.float32), 64)
    col_idx = np.tile(np.arange(64, dtype=np.float32), rows)
    inv_freq = (np.float32(10000.0) ** (-np.arange(0, 32, 2, dtype=np.float32) / np.float32(32))).astype(np.float32)
    ang = np.stack([row_idx[:, None] * inv_freq, col_idx[:, None] * inv_freq], axis=1).astype(np.float32)
    cos = np.cos(ang).astype(np.float32).reshape(SEQ, 32)
    sin = np.sin(ang).astype(np.float32).reshape(SEQ, 32)
    cosT = np.ascontiguousarray(cos.reshape(NL, 128, 32).transpose(1, 0, 2))
    sinT = np.ascontiguousarray(sin.reshape(NL, 128, 32).transpose(1, 0, 2))
    bd64 = np.zeros((128, 128), np.float32)
    bd64[:64, :64] = 1.0 / 64
    bd64[64:, 64:] = 1.0 / 64
    c65 = np.full((65, 64), 1.0 / 64, np.float32)
    c65[64, :] = EPS
    e0 = np.zeros((128, 1), np.float32)
    e0[0, 0] = 1.0
    ustr = np.triu(np.ones((128, 128), np.float32), k=1)
    eC = np.ascontiguousarray(np.broadcast_to((np.arange(NE, dtype=np.float32) * CAP)[None, :], (128, NE)))
    return dict(ident_f=ident, cosT=cosT, sinT=sinT, bd64=bd64, c65=c65, e0=e0, ustr=ustr, eC=eC)


def make_in_maps(inputs, cores=range(8)):
    f = lambda a: np.ascontiguousarray(np.asarray(a, dtype=np.float32))
    x = f(inputs["x"]); c = f(inputs["c"]); ctx = f(inputs["ctx"]); c_ctx = f(inputs["c_ctx"])
    consts = _host_consts()
    bc = lambda v: np.ascontiguousarray(np.broadcast_to(f(v).reshape(1, -1), (128, f(v).size)))
    shared = dict(
        w_mod=f(inputs["w_mod"])[0], bm_b=bc(inputs["b_mod"][0]), g1_b=bc(inputs["norm1_g"][0]),
        g2_b=bc(inputs["norm2_g"][0]), fg_b=bc(inputs["final_g"]), w_in=f(inputs["w_in"])[0],
        qg_b=bc(inputs["q_norm_g"][0]), kg_b=bc(inputs["k_norm_g"][0]),
        cwT=np.ascontiguousarray(f(inputs["conv_w"])[0].reshape(3, 4, 128).transpose(2, 1, 0).reshape(128, 12)),
        gallT=np.ascontiguousarray(np.concatenate([f(inputs["attn_out_g"])[0], f(inputs["conv_out_g"])[0]])
                                   .reshape(8, 128).T),
        w_out=f(inputs["w_out"])[0],
        w_r=np.ascontiguousarray(np.concatenate([f(inputs["w_group"])[0], f(inputs["w_router"])[0]], axis=1)),
        w_gate=f(inputs["w_gate"])[0], w_up=f(inputs["w_up"])[0], w_down=f(inputs["w_down"])[0],
        **consts,
    )
    maps = []
    for b in cores:
        cT = np.concatenate([c[b].reshape(8, 128).T, c_ctx.reshape(8, 128).T], axis=1)
        m = dict(shared)
        m["xin"] = np.ascontiguousarray(np.concatenate([ctx[b], x[b]], axis=0))
        m["cT"] = np.ascontiguousarray(cT.astype(np.float32))
        maps.append(m)
    return maps


_CACHE = {}


def kernel(**inputs):
    if "nc" not in _CACHE:
        _CACHE["nc"] = build_program(debug=False)[0]
    nc = _CACHE["nc"]
    maps = make_in_maps(inputs)
    res = run_bass_kernel_spmd(nc, maps, core_ids=list(range(8)))
    out = np.stack([np.asarray(r["y"], dtype=np.float32) for r in res.results], axis=0)
    return out
```
